# Optimizing a Trainium2 kernel written in Bass

```python
import math
import jax
import jax.numpy as jnp
from jax import lax
import numpy as np

D_MODEL = 1024
BATCH = 2
SEQ = 8192
DEPTH = 2

GRID_W = 64
CTX_LEN = 256
N_MIXERS = 4
GROUP_W = D_MODEL // N_MIXERS
HEAD_DIM = 64
N_HEADS = GROUP_W // HEAD_DIM
CONV_K = 3
W_LORA = 16
A_LORA = 16
G_LORA = 32
RWKV_DECAY_SCALE = math.exp(-0.5)
KV_HEADS = 2
Q_PER_KV = N_HEADS // KV_HEADS
KV_W = KV_HEADS * HEAD_DIM
WINDOW = 128
ATT_BLOCK = 128
ATT_SCALE = HEAD_DIM ** -0.5
ROPE_BASE = 10000.0
MLSTM_CHUNK = 128
N_GATE_COLS = 2 * 2 * N_HEADS
PEER_HEADS = 8
N_KEYS = 128
N_EXPERTS = N_KEYS * N_KEYS
PEER_TOPK = 16
PEER_QDIM = 256
PEER_HALF = PEER_QDIM // 2
PEER_BLOCK = 128
EPS = 1e-6
F32 = jnp.float32
IN_SIZES = (GROUP_W, GROUP_W, GROUP_W,
            GROUP_W, GROUP_W, GROUP_W, W_LORA, A_LORA, G_LORA,
            GROUP_W, KV_W, KV_W,
            GROUP_W, GROUP_W, GROUP_W, GROUP_W, N_GATE_COLS)
D_IN = sum(IN_SIZES)
IN_OFFSETS = tuple(int(o) for o in np.cumsum(IN_SIZES)[:-1])

kernel_name = 'hybrid_parallel_group_peer_dit_block'


def rms_norm(x, g):
    xf = x.astype(F32)
    y = xf * lax.rsqrt(jnp.mean(xf * xf, axis=-1, keepdims=True) + EPS)
    return (y * g.astype(F32)).astype(x.dtype)


def heads(t):
    return t.reshape(t.shape[:-1] + (N_HEADS, HEAD_DIM))


def head_norm_merge(y, g):
    return rms_norm(y, g).reshape(y.shape[:-2] + (GROUP_W,))


def rope_2d(x, row, col):
    quarter = HEAD_DIM // 4
    inv = ROPE_BASE ** (-jnp.arange(quarter, dtype=F32) / quarter)
    xf = x.astype(F32)
    extra = (1,) * (x.ndim - 3)

    def rot(xa, pos):
        ang = pos.astype(F32)[:, None] * inv[None, :]
        ang = ang.reshape((1, ang.shape[0]) + extra + (quarter,))
        cos, sin = jnp.cos(ang), jnp.sin(ang)
        x1, x2 = xa[..., :quarter], xa[..., quarter:]
        return jnp.concatenate([x1 * cos - x2 * sin, x2 * cos + x1 * sin], axis=-1)

    half = HEAD_DIM // 2
    return jnp.concatenate([rot(xf[..., :half], row), rot(xf[..., half:], col)], axis=-1).astype(x.dtype)


def conv_mixer(hx, b_gate, c_gate, w, g):
    u = c_gate * hx
    up = jnp.pad(u, ((0, 0), (1, 1), (0, 0)))
    y = b_gate * (w[0] * up[:, :-2] + w[1] * up[:, 1:-1] + w[2] * up[:, 2:])
    return head_norm_merge(heads(y), g)


def rwkv_stream(r, k, v, xw, xa, xg, w0, w2, a0, a2, g2, k_k, k_a):
    kk = heads((k * k_k).astype(F32))
    kk = kk * lax.rsqrt(jnp.sum(kk * kk, axis=-1, keepdims=True) + EPS)
    g = jax.nn.sigmoid(xg) @ g2
    per_dir = []
    for d in range(2):
        decay = jnp.exp(-RWKV_DECAY_SCALE * jax.nn.sigmoid(w0[d] + jnp.tanh(xw) @ w2[d]))
        a = jax.nn.sigmoid(a0[d] + xa @ a2[d])
        kd = k * (1 + (a - 1) * k_a)
        per_dir.append((heads(decay.astype(F32)), heads(a.astype(F32)), heads(kd.astype(F32))))
    return heads(r.astype(F32)), heads(v.astype(F32)), kk, g, per_dir


def rwkv_scan(S0, r, decay, k, v, kk, a, reverse):
    xs = tuple(jnp.moveaxis(t, 1, 0) for t in (r, decay, k, v, kk, a))

    def step(S, inp):
        r_t, w_t, k_t, v_t, kk_t, a_t = inp
        sa = jnp.einsum('bhvk,bhk->bhv', S, kk_t)
        S = (S * w_t[:, :, None, :] - sa[..., None] * (kk_t * a_t)[:, :, None, :]
             + v_t[..., None] * k_t[:, :, None, :])
        return S, jnp.einsum('bhvk,bhk->bhv', S, r_t)

    S, y = lax.scan(step, S0, xs, reverse=reverse)
    return S, jnp.moveaxis(y, 0, 1)


def rwkv_mixer(lat, ctx, w0, w2, a0, a2, g2, k_k, k_a, r_k, ln_g, need_ctx):
    sc = rwkv_stream(*ctx, w0, w2, a0, a2, g2, k_k, k_a)
    sl = rwkv_stream(*lat, w0, w2, a0, a2, g2, k_k, k_a)
    B = sl[0].shape[0]
    zero = jnp.zeros((B, N_HEADS, HEAD_DIM, HEAD_DIM), F32)
    ys_c, ys_l = [], []
    for d in range(2):
        dec_c, a_c, k_c = sc[4][d]
        S_c, y_c = rwkv_scan(zero, sc[0], dec_c, k_c, sc[1], sc[2], a_c, reverse=(d == 1))
        dec_l, a_l, k_l = sl[4][d]
        _, y_l = rwkv_scan(S_c, sl[0], dec_l, k_l, sl[1], sl[2], a_l, reverse=(d == 1))
        ys_c.append(y_c)
        ys_l.append(y_l)

    def finish(s, ys):
        r, v, _, g, per_dir = s
        y = rms_norm(ys[0] + ys[1], ln_g)
        bonus = (jnp.sum(r * per_dir[0][2] * r_k, axis=-1, keepdims=True)
                 + jnp.sum(r * per_dir[1][2] * r_k, axis=-1, keepdims=True)) * v
        return (y + bonus).reshape(y.shape[:-2] + (GROUP_W,)) * g

    y_lat = finish(sl, ys_l)
    y_ctx = finish(sc, ys_c) if need_ctx else None
    return y_lat, y_ctx


def attn_project(q, k, v, q_g, k_g):
    B, T, _ = q.shape
    q = rms_norm(q.reshape(B, T, KV_HEADS, Q_PER_KV, HEAD_DIM), q_g)
    k = rms_norm(k.reshape(B, T, KV_HEADS, HEAD_DIM), k_g)
    v = v.reshape(B, T, KV_HEADS, HEAD_DIM)
    return q, k, v


def latent_attention(q, k, v, kc, vc, sink):
    B, T = q.shape[:2]
    nb = T // ATT_BLOCK
    qb = q.reshape(B, nb, ATT_BLOCK, KV_HEADS, Q_PER_KV, HEAD_DIM)

    def band(t):
        tp = jnp.pad(t, ((0, 0), (ATT_BLOCK, ATT_BLOCK), (0, 0), (0, 0)))
        tp = tp.reshape(B, nb + 2, ATT_BLOCK, KV_HEADS, HEAD_DIM)
        return jnp.concatenate([tp[:, :-2], tp[:, 1:-1], tp[:, 2:]], axis=2)

    kw, vw = band(k), band(v)
    start = jnp.arange(nb)[:, None] * ATT_BLOCK
    qpos = start + jnp.arange(ATT_BLOCK)[None, :]
    kpos = start - ATT_BLOCK + jnp.arange(3 * ATT_BLOCK)[None, :]
    mask = ((jnp.abs(qpos[:, :, None] - kpos[:, None, :]) <= WINDOW)
            & (kpos[:, None, :] >= 0) & (kpos[:, None, :] < T))
    s_loc = jnp.einsum('bnqhgd,bnkhd->bhgnqk', qb, kw).astype(F32) * ATT_SCALE
    s_loc = jnp.where(mask, s_loc, -jnp.inf)
    s_ctx = jnp.einsum('bnqhgd,bchd->bhgnqc', qb, kc).astype(F32) * ATT_SCALE
    sk = jnp.broadcast_to(sink.astype(F32).reshape(KV_HEADS, Q_PER_KV, 1, 1, 1), s_loc.shape[:-1] + (1,))
    p = jax.nn.softmax(jnp.concatenate([s_loc, s_ctx, sk], axis=-1), axis=-1).astype(v.dtype)
    nw = 3 * ATT_BLOCK
    o = (jnp.einsum('bhgnqk,bnkhd->bnqhgd', p[..., :nw], vw)
         + jnp.einsum('bhgnqc,bchd->bnqhgd', p[..., nw:-1], vc))
    return o.reshape(B, T, N_HEADS, HEAD_DIM)


def ctx_attention(qc, kc, vc, sink):
    s = jnp.einsum('bqhgd,bchd->bhgqc', qc, kc).astype(F32) * ATT_SCALE
    sk = jnp.broadcast_to(sink.astype(F32).reshape(KV_HEADS, Q_PER_KV, 1, 1), s.shape[:-1] + (1,))
    p = jax.nn.softmax(jnp.concatenate([s, sk], axis=-1), axis=-1)[..., :-1].astype(vc.dtype)
    o = jnp.einsum('bhgqc,bchd->bqhgd', p, vc)
    return o.reshape(qc.shape[0], qc.shape[1], N_HEADS, HEAD_DIM)


def mlstm_chunk_scan(state, q, k, v, logi, logf):
    B, H, T, Dh = q.shape
    L = MLSTM_CHUNK
    nc = T // L

    def chunks(t):
        return jnp.moveaxis(t.reshape((B, H, nc, L) + t.shape[3:]), 2, 0)

    xs = (chunks(q), chunks(k), chunks(v), chunks(logi), chunks(logf))
    causal = jnp.tril(jnp.ones((L, L), dtype=bool))

    def step(carry, inp):
        C, n, m = carry
        qc, kc, vc, li, lf = inp
        b = jnp.cumsum(lf, axis=-1)
        d_intra = jnp.where(causal, b[..., :, None] - b[..., None, :] + li[..., None, :], -jnp.inf)
        d_inter = b + m[..., None]
        m_t = jnp.maximum(jnp.max(d_intra, axis=-1), d_inter)
        w_intra = jnp.exp(d_intra - m_t[..., None])
        w_inter = jnp.exp(d_inter - m_t)
        s = jnp.einsum('bhtd,bhsd->bhts', qc, kc) * w_intra
        num = (jnp.einsum('bhts,bhsd->bhtd', s, vc)
               + w_inter[..., None] * jnp.einsum('bhvk,bhtk->bhtv', C, qc))
        den = jnp.sum(s, axis=-1) + w_inter * jnp.einsum('bhk,bhtk->bht', n, qc)
        h = num / jnp.maximum(jnp.abs(den), jnp.exp(-m_t))[..., None]
        b_end = b[..., -1]
        d_end = b_end[..., None] - b + li
        m_new = jnp.maximum(b_end + m, jnp.max(d_end, axis=-1))
        w_end = jnp.exp(d_end - m_new[..., None])
        carry_decay = jnp.exp(b_end + m - m_new)
        C = carry_decay[..., None, None] * C + jnp.einsum('bhs,bhsv,bhsk->bhvk', w_end, vc, kc)
        n = carry_decay[..., None] * n + jnp.einsum('bhs,bhsk->bhk', w_end, kc)
        return (C, n, m_new), h

    state, h = lax.scan(step, state, xs)
    return state, jnp.moveaxis(h, 0, 2).reshape(B, H, T, Dh)


def mlstm_stream(q, k, v, o, gates, i_b, f_b):
    B, T, _ = q.shape

    def th(t):
        return jnp.moveaxis(heads(t.astype(F32)), 2, 1)

    gates = gates.astype(F32).reshape(B, T, 2, 2, N_HEADS) + jnp.stack([i_b, f_b], axis=1).astype(F32)
    gates = jnp.moveaxis(gates, 1, -1)
    logi = gates[:, :, 0]
    logf = jax.nn.log_sigmoid(gates[:, :, 1])
    return th(q), th(k) * (HEAD_DIM ** -0.5), th(v), jax.nn.sigmoid(o), logi, logf


def mlstm_mixer(lat, ctx, i_b, f_b, out_g, need_ctx):
    sc = mlstm_stream(*ctx, i_b, f_b)
    sl = mlstm_stream(*lat, i_b, f_b)
    B = sl[0].shape[0]
    zero = (jnp.zeros((B, N_HEADS, HEAD_DIM, HEAD_DIM), F32),
            jnp.zeros((B, N_HEADS, HEAD_DIM), F32),
            jnp.zeros((B, N_HEADS), F32))

    def flip(t):
        return jnp.flip(t, axis=2)

    def run(state, s, d):
        q, k, v, _, logi, logf = s
        li, lf = logi[:, d], logf[:, d]
        if d == 0:
            return mlstm_chunk_scan(state, q, k, v, li, lf)
        st, h = mlstm_chunk_scan(state, flip(q), flip(k), flip(v), flip(li), flip(lf))
        return st, flip(h)

    h_c, h_l = [], []
    for d in range(2):
        st, hc = run(zero, sc, d)
        _, hl = run(st, sl, d)
        h_c.append(hc)
        h_l.append(hl)

    def finish(s, hs):
        h = jnp.moveaxis(hs[0] + hs[1], 1, 2)
        return s[3] * head_norm_merge(h, out_g)

    y_lat = finish(sl, h_l)
    y_ctx = finish(sc, h_c) if need_ctx else None
    return y_lat, y_ctx


def peer_ffn(h, w_q, sub_keys, u, v):
    B, T, D = h.shape
    M = B * T
    hf = h.reshape(M, D)
    q = (hf @ w_q).astype(F32).reshape(M, PEER_HEADS, 2, PEER_HALF)
    s = jnp.einsum('mhpd,hpkd->mhpk', q, sub_keys.astype(F32))
    sv, si = lax.top_k(s, PEER_TOPK)
    n_cand = PEER_TOPK * PEER_TOPK
    cand = (sv[:, :, 0, :, None] + sv[:, :, 1, None, :]).reshape(M, PEER_HEADS, n_cand)
    cidx = (si[:, :, 0, :, None] * N_KEYS + si[:, :, 1, None, :]).reshape(M, PEER_HEADS, n_cand)
    tv, ti = lax.top_k(cand, PEER_TOPK)
    experts = jnp.take_along_axis(cidx, ti, axis=-1)
    gates = jax.nn.softmax(tv, axis=-1)
    nblk = M // PEER_BLOCK

    def block(args):
        hb, eb, gb = args
        act = jax.nn.gelu(jnp.einsum('mhkd,md->mhk', u[eb], hb).astype(F32))
        return jnp.einsum('mhk,mhkd->md', (gb * act).astype(v.dtype), v[eb])

    out = lax.map(block, (hf.reshape(nblk, PEER_BLOCK, D),
                          experts.reshape(nblk, PEER_BLOCK, PEER_HEADS, PEER_TOPK),
                          gates.reshape(nblk, PEER_BLOCK, PEER_HEADS, PEER_TOPK)))
    return out.reshape(B, T, D).astype(h.dtype)


def setup_inputs(seed: int = 0) -> dict:
    key = jax.random.key(seed)
    ks = jax.random.split(key, 40)
    L, D = DEPTH, D_MODEL

    def nrm(i, shape, scale):
        return scale * jax.random.normal(ks[i], shape, F32)

    return {
        'x': nrm(0, (BATCH, SEQ, D), 1.0),
        'c': nrm(1, (BATCH, D), 1.0),
        'ctx': nrm(2, (BATCH, CTX_LEN, D), 1.0),
        'c_ctx': nrm(3, (D,), 1.0),
        'ada_w': nrm(4, (L, D, 6 * D), 0.5 * D ** -0.5),
        'ada_b': nrm(5, (L, 6 * D), 0.02),
        'norm1_g': 1.0 + nrm(6, (L, D), 0.02),
        'norm2_g': 1.0 + nrm(7, (L, D), 0.02),
        'w_in': nrm(8, (L, D, D_IN), D ** -0.5),
        'w_out': nrm(9, (L, D, D), D ** -0.5),
        'conv_w': nrm(10, (L, CONV_K, GROUP_W), CONV_K ** -0.5),
        'conv_g': 1.0 + nrm(11, (L, N_HEADS, HEAD_DIM), 0.02),
        'rwkv_w0': nrm(12, (L, 2, GROUP_W), 0.5),
        'rwkv_w2': nrm(13, (L, 2, W_LORA, GROUP_W), W_LORA ** -0.5),
        'rwkv_a0': nrm(14, (L, 2, GROUP_W), 0.5),
        'rwkv_a2': nrm(15, (L, 2, A_LORA, GROUP_W), A_LORA ** -0.5),
        'rwkv_g2': nrm(16, (L, G_LORA, GROUP_W), G_LORA ** -0.5),
        'rwkv_kk': 0.85 + nrm(17, (L, GROUP_W), 0.05),
        'rwkv_ka': 1.0 + nrm(18, (L, GROUP_W), 0.05),
        'rwkv_rk': nrm(19, (L, N_HEADS, HEAD_DIM), 0.1),
        'rwkv_ln_g': 1.0 + nrm(20, (L, N_HEADS, HEAD_DIM), 0.02),
        'att_q_g': 1.0 + nrm(21, (L, HEAD_DIM), 0.02),
        'att_k_g': 1.0 + nrm(22, (L, HEAD_DIM), 0.02),
        'att_sink': nrm(23, (L, N_HEADS), 0.5),
        'att_out_g': 1.0 + nrm(24, (L, N_HEADS, HEAD_DIM), 0.02),
        'ml_i_b': nrm(25, (L, 2, N_HEADS), 0.1),
        'ml_f_b': 3.0 + nrm(26, (L, 2, N_HEADS), 0.5),
        'ml_out_g': 1.0 + nrm(27, (L, N_HEADS, HEAD_DIM), 0.02),
        'peer_wq': nrm(28, (L, D, PEER_HEADS * PEER_QDIM), D ** -0.5),
        'peer_keys': nrm(29, (L, PEER_HEADS, 2, N_KEYS, PEER_HALF), PEER_HALF ** -0.5),
        'peer_u': nrm(30, (L, N_EXPERTS, D), D ** -0.5),
        'peer_v': nrm(31, (L, N_EXPERTS, D), PEER_HEADS ** -0.5),
    }


def reference(x, c, ctx, c_ctx, ada_w, ada_b, norm1_g, norm2_g, w_in, w_out, conv_w, conv_g,
              rwkv_w0, rwkv_w2, rwkv_a0, rwkv_a2, rwkv_g2, rwkv_kk, rwkv_ka, rwkv_rk, rwkv_ln_g,
              att_q_g, att_k_g, att_sink, att_out_g, ml_i_b, ml_f_b, ml_out_g,
              peer_wq, peer_keys, peer_u, peer_v):
    B, T, D = x.shape
    ROWS = T // GRID_W
    row = jnp.repeat(jnp.arange(ROWS), GRID_W)
    col = jnp.arange(ROWS * GRID_W) % GRID_W
    for l in range(DEPTH):
        need_ctx = l < DEPTH - 1
        mod = jax.nn.silu(c) @ ada_w[l] + ada_b[l]
        mod_c = jax.nn.silu(c_ctx) @ ada_w[l] + ada_b[l]
        sh1, sc1, gt1, sh2, sc2, gt2 = jnp.split(mod[:, None, :], 6, axis=-1)
        csh1, csc1, cgt1, csh2, csc2, cgt2 = jnp.split(mod_c, 6, axis=-1)

        h = rms_norm(x, norm1_g[l]) * (1 + sc1) + sh1
        hc = rms_norm(ctx, norm1_g[l]) * (1 + csc1) + csh1
        P = jnp.split(h @ w_in[l], IN_OFFSETS, axis=-1)
        Pc = jnp.split(hc @ w_in[l], IN_OFFSETS, axis=-1)

        y_a = conv_mixer(P[0], P[1], P[2], conv_w[l], conv_g[l])
        y_b, yc_b = rwkv_mixer(P[3:9], Pc[3:9], rwkv_w0[l], rwkv_w2[l], rwkv_a0[l], rwkv_a2[l],
                               rwkv_g2[l], rwkv_kk[l], rwkv_ka[l], rwkv_rk[l], rwkv_ln_g[l], need_ctx)
        q, k, v = attn_project(P[9], P[10], P[11], att_q_g[l], att_k_g[l])
        q, k = rope_2d(q, row, col), rope_2d(k, row, col)
        qc, kc, vc = attn_project(Pc[9], Pc[10], Pc[11], att_q_g[l], att_k_g[l])
        y_c = head_norm_merge(latent_attention(q, k, v, kc, vc, att_sink[l]), att_out_g[l])
        y_d, yc_d = mlstm_mixer(P[12:17], Pc[12:17], ml_i_b[l], ml_f_b[l], ml_out_g[l], need_ctx)

        y = jnp.concatenate([t.astype(x.dtype) for t in (y_a, y_b, y_c, y_d)], axis=-1) @ w_out[l]
        x = x + gt1 * y
        h2 = rms_norm(x, norm2_g[l]) * (1 + sc2) + sh2
        x = x + gt2 * peer_ffn(h2, peer_wq[l], peer_keys[l], peer_u[l], peer_v[l])

        if need_ctx:
            yc_a = conv_mixer(Pc[0], Pc[1], Pc[2], conv_w[l], conv_g[l])
            yc_c = head_norm_merge(ctx_attention(qc, kc, vc, att_sink[l]), att_out_g[l])
            yc = jnp.concatenate([t.astype(ctx.dtype) for t in (yc_a, yc_b, yc_c, yc_d)], axis=-1) @ w_out[l]
            ctx = ctx + cgt1 * yc
            hc2 = rms_norm(ctx, norm2_g[l]) * (1 + csc2) + csh2
            ctx = ctx + cgt2 * peer_ffn(hc2, peer_wq[l], peer_keys[l], peer_u[l], peer_v[l])
    return x
```

```python
from concourse.bass_utils import run_bass_kernel_spmd
import contextlib
import numpy as np
import concourse.bass as bass
import concourse.mybir as mybir

F32 = mybir.dt.float32
BF16 = mybir.dt.bfloat16
U32 = mybir.dt.uint32
AF = mybir.ActivationFunctionType
ALU = mybir.AluOpType
AX = mybir.AxisListType


class Tile:
    def __init__(self, ap, name=""):
        self.ap = ap
        self.name = name
        self.writer = None
        self.readers = {}
        self.exclusive = False

    def __getitem__(self, key):
        return Ref(self, self.ap[key])

    @property
    def r(self):
        return Ref(self, self.ap)


class Ref:
    def __init__(self, tile, ap):
        self.tile = tile
        self.ap = ap

    def __getitem__(self, key):
        return Ref(self.tile, self.ap[key])

    def re(self, s, **kw):
        return Ref(self.tile, self.ap.rearrange(s, **kw))

    def bc(self, shape):
        return Ref(self.tile, self.ap.to_broadcast(shape))

    def with_ap(self, ap):
        return Ref(self.tile, ap)


def _ap(x):
    return x.ap if isinstance(x, Ref) else x


class Sched:
    COMPUTE_ROT = 30000
    DMA_ROT = 1900

    def __init__(self, nc):
        self.nc = nc
        self.es = contextlib.ExitStack()
        self.eng = {"pe": nc.tensor, "dve": nc.vector, "act": nc.scalar,
                    "pool": nc.gpsimd, "sp": nc.sync}
        self.sem = {}
        self.cnt = {}
        self.epoch = {}
        self.waited = {}
        self.nsem = 0
        self.n_ins = 0
        self.allsems = {}
        self.dram_cache = {}
        self.dma_keys = set()
        self.scopes = []

    def shared_dram(self, name, shape, dt=F32):
        if name not in self.dram_cache:
            t = self.nc.dram_tensor(name, list(shape), dt, kind="ExternalInput")
            self.dram_cache[name] = Tile(t.ap(), name)
        return self.dram_cache[name]

    def push_scope(self):
        self.scopes.append(contextlib.ExitStack())

    def pop_scope(self):
        print("scope end: sbuf left", self.nc.sbuf_bytes_remaining)
        self.barrier()
        self.scopes.pop().close()

    def barrier(self):
        for e in ("pe", "dve", "act", "pool", "sp"):
            self.wait_all(e)

    def sbuf(self, name, shape, dtype=F32):
        es = self.scopes[-1] if getattr(self, "scopes", None) else self.es
        self.nalloc = getattr(self, "nalloc", 0) + 1
        return es.enter_context(self.nc.sbuf_tensor(f"sb{self.nalloc}_" + name, list(shape), dtype))

    def psum(self, name, shape, dtype=F32):
        return self.es.enter_context(self.nc.psum_tensor("ps_" + name, list(shape), dtype))

    def tile(self, name, shape, dtype=F32):
        t = self.sbuf(name, shape, dtype)
        return Tile(t[tuple(slice(None) for _ in shape)], name)

    def ptile(self, name, shape, dtype=F32):
        t = self.psum(name, shape, dtype)
        tl = Tile(t[tuple(slice(None) for _ in shape)], name)
        tl.exclusive = True
        return tl

    def _stream(self, stream, is_dma):
        if stream not in self.cnt:
            self.epoch[stream] = 0
            self._newsem(stream)
        key, c = self.cnt[stream]
        lim = self.DMA_ROT if is_dma else self.COMPUTE_ROT
        if c >= lim:
            self.epoch[stream] += 1
            self._newsem(stream)
        return self.cnt[stream]

    def _newsem(self, stream):
        key = f"{stream}_{self.epoch[stream]}"
        h = self.es.enter_context(self.nc.semaphore(f"s_{key}"))
        self.sem[key] = h
        self.cnt[stream] = (key, 0)
        self.nsem += 1

    def emit(self, engine, fn, outs=(), ins=(), dma_group=None, inc_override=None):
        is_dma = dma_group is not None
        stream = dma_group if is_dma else engine
        key, c = self._stream(stream, is_dma)
        deps = {}

        def add(d):
            if d is None:
                return
            k, v = d
            if deps.get(k, 0) < v:
                deps[k] = v

        outs = list(outs) + [r for r in ins if isinstance(r, Ref) and r.tile.exclusive]
        for r in ins:
            if isinstance(r, Ref):
                add(r.tile.writer)
        for o in outs:
            if isinstance(o, Ref):
                add(o.tile.writer)
                for k, v in o.tile.readers.items():
                    add((k, v))
        e = self.eng[engine]
        for k, v in list(deps.items()):
            if k in self.dma_keys:
                v = max(v, self.allsems.get(k, v))
                deps[k] = v
        for k, v in deps.items():
            if engine == "pe" and not is_dma and k.rsplit("_", 1)[0] == "pe":
                continue
            if self.waited.get((engine, k), 0) >= v:
                continue
            e.wait_ge(self.sem[k], v)
            self.waited[(engine, k)] = v
        ins_obj = fn()
        inc = 16 if is_dma else 1
        if inc_override is not None:
            inc = inc_override
        c += inc
        self.cnt[stream] = (key, c)
        self.allsems[key] = c
        if is_dma:
            self.dma_keys.add(key)
        ins_obj.then_inc(self.sem[key], inc)
        self.n_ins += 1
        me = (key, c)
        for o in outs:
            if isinstance(o, Ref):
                o.tile.writer = me
                o.tile.readers = {}
        for r in ins:
            if isinstance(r, Ref):
                if not any(r.tile is o.tile for o in outs if isinstance(o, Ref)):
                    if r.tile.readers.get(key, 0) < c:
                        r.tile.readers[key] = c
        return ins_obj

    def wait_all(self, engine="sp"):
        e = self.eng[engine]
        for key, c in list(self.allsems.items()):
            if c > 0 and self.waited.get((engine, key), 0) < c:
                e.wait_ge(self.sem[key], c)
                self.waited[(engine, key)] = c

    def close(self):
        self.es.close()

    def all_gather(self, out, in_, groups):
        return self.emit("pool", lambda: self.nc.gpsimd.collective_compute(
            "AllGather", ALU.bypass, replica_groups=groups, ins=[_ap(in_).opt()], outs=[_ap(out).opt()]),
            outs=[out], ins=[in_], dma_group=f"cc{self._ncc()}", inc_override=1)

    def _ncc(self):
        self.ncc = getattr(self, "ncc", 0) + 1
        return self.ncc

    def dma(self, out, in_, group="ld0", q="sp"):
        return self.emit(q, lambda: self.eng[q].dma_start(out=_ap(out), in_=_ap(in_)),
                         outs=[out], ins=[in_], dma_group=group)

    def mm(self, out, lhsT, rhs, start=True, stop=True, skip=False):
        return self.emit("pe", lambda: self.nc.tensor.matmul(_ap(out), lhsT=_ap(lhsT), rhs=_ap(rhs),
                                                              start=start, stop=stop, skip_group_check=skip),
                         outs=[out], ins=[lhsT, rhs] + ([] if start else [out]))

    def tr(self, out, in_, ident):
        return self.emit("pe", lambda: self.nc.tensor.transpose(_ap(out), _ap(in_), _ap(ident)),
                         outs=[out], ins=[in_, ident])

    def act(self, out, in_, func, bias=None, scale=None, accum_out=None):
        kw = {}
        ins = [in_]
        outs = [out]
        if bias is not None:
            kw["bias"] = _ap(bias)
            ins.append(bias)
        if scale is not None:
            kw["scale"] = _ap(scale)
            ins.append(scale)
        if accum_out is not None:
            kw["accum_out"] = _ap(accum_out)
            outs.append(accum_out)
        return self.emit("act", lambda: self.nc.scalar.activation(out=_ap(out), in_=_ap(in_), func=func, **kw),
                         outs=outs, ins=ins)

    def tt(self, eng, out, in0, in1, op):
        return self.emit(eng, lambda: self.eng[eng].tensor_tensor(out=_ap(out), in0=_ap(in0), in1=_ap(in1), op=op),
                         outs=[out], ins=[in0, in1])

    def ts(self, eng, out, in0, s1, s2=None, op0=ALU.mult, op1=None, accum_out=None):
        kw = {}
        outs = [out]
        if op1 is not None:
            kw["op1"] = op1
        if accum_out is not None:
            kw["accum_out"] = _ap(accum_out)
            outs.append(accum_out)
        return self.emit(eng, lambda: self.eng[eng].tensor_scalar(out=_ap(out), in0=_ap(in0), scalar1=_ap(s1),
                                                                  scalar2=_ap(s2), op0=op0, **kw),
                         outs=outs, ins=[in0, s1, s2])

    def stt(self, eng, out, in0, scalar, in1, op0, op1):
        return self.emit(eng, lambda: self.eng[eng].scalar_tensor_tensor(out=_ap(out), in0=_ap(in0), scalar=_ap(scalar),
                                                                         in1=_ap(in1), op0=op0, op1=op1),
                         outs=[out], ins=[in0, scalar, in1])

    def copy(self, eng, out, in_):
        if eng == "act":
            return self.emit("act", lambda: self.nc.scalar.copy(out=_ap(out), in_=_ap(in_)), outs=[out], ins=[in_])
        return self.emit(eng, lambda: self.eng[eng].tensor_copy(out=_ap(out), in_=_ap(in_)), outs=[out], ins=[in_])

    def memset(self, eng, out, val):
        return self.emit(eng, lambda: self.eng[eng].memset(_ap(out), val), outs=[out], ins=[])

    def reduce(self, eng, out, in_, op, axis=AX.X):
        return self.emit(eng, lambda: self.eng[eng].tensor_reduce(out=_ap(out), in_=_ap(in_), axis=axis, op=op),
                         outs=[out], ins=[in_])

    def recip(self, out, in_):
        return self.emit("dve", lambda: self.nc.vector.reciprocal(out=_ap(out), in_=_ap(in_)), outs=[out], ins=[in_])

    def vmax(self, out, in_):
        return self.emit("dve", lambda: self.nc.vector.max(out=_ap(out), in_=_ap(in_)), outs=[out], ins=[in_])

    def vmax_index(self, out, in_max, in_values):
        return self.emit("dve", lambda: self.nc.vector.max_index(out=_ap(out), in_max=_ap(in_max), in_values=_ap(in_values)),
                         outs=[out], ins=[in_max, in_values])

    def vmatch_replace(self, out, in_to_replace, in_values, imm):
        return self.emit("dve", lambda: self.nc.vector.match_replace(out=_ap(out), in_to_replace=_ap(in_to_replace),
                                                                    in_values=_ap(in_values), imm_value=imm),
                         outs=[out], ins=[in_to_replace, in_values])

    def scan(self, out, data0, data1, initial, op0, op1):
        return self.emit("dve", lambda: self.nc.vector.tensor_tensor_scan(out=_ap(out), data0=_ap(data0), data1=_ap(data1),
                                                                          initial=_ap(initial), op0=op0, op1=op1),
                         outs=[out], ins=[data0, data1, initial])
import os

EPS = 1e-6
import math
RWKV_DECAY_SCALE = math.exp(-0.5)
TT = 8448
NTILE = 33
N = 256


def emit_A(S, nc, pb, sfx="", load_x=None, store_y=None, stage=None, dbg_d=None,
           mixers=("conv", "attn", "mlstm", "rwkv"), tile_limit=None):
    def D(name, shape, kind="ExternalInput", dt=F32):
        t = nc.dram_tensor(name + sfx, list(shape), dt, kind=kind)
        return Tile(t.ap(), name + sfx)

    cv_d = S.shared_dram("cv", [128, 8, 2])
    adaw_d = S.shared_dram("adaw" + sfx, [6, 2, 128, 8, 512])
    adab_d = S.shared_dram("adab" + sfx, [128, 48])
    n1g_d = D("n1g", [128, 8])
    Wc_d = D("Wc", [128, 8, 192])
    Wr_d = D("Wr", [128, 8, 256])
    Wa_d = D("Wa", [128, 8, 192])
    Wm_d = D("Wm", [128, 8, 260])
    pp_d = D("pp", [64, 16])
    w2_d = D("w2p", [16, 2, 64])
    a2_d = D("a2p", [16, 2, 64])
    g2_d = D("g2p", [32, 64])
    rowbc_d = D("rowbc", [128, 4, 64])
    scal_d = D("scal", [128, 8])
    ident_d = S.shared_dram("ident", [128, 128])
    trile_d = S.shared_dram("trile", [128, 128])
    trige_d = S.shared_dram("trige", [128, 128])
    rmask_d = S.shared_dram("rmask", [2, 64, 192])
    cmask_d = S.shared_dram("cmask", [64, 256])
    rope_d = S.shared_dram("rope", [64, 128, 64])

    class Done(Exception):
        pass

    dbg_n = [0]

    def chk(name, *refs):
        if stage != name:
            return
        o = 0
        for r in refs:
            n = 1
            for s_ in r.ap.shape[1:]:
                n *= s_
            P = r.ap.shape[0]
            t = S.tile(f"dbgt{dbg_n[0]}", [128, n])
            dbg_n[0] += 1
            tv = t[0:P, :]
            shp = r.ap.shape
            if len(shp) == 3:
                tv = tv.re("p (a b) -> p a b", a=shp[1])
            elif len(shp) == 4:
                tv = tv.re("p (a b c) -> p a b c", a=shp[1], b=shp[2])
            S.copy("dve", tv, r)
            S.dma(dbg_d[0:P, o:o + n], t[0:P, :], "st")
            o += n
        raise Done()

    S.push_scope()
    ident = S.tile("ident", [128, 128])
    ones = S.tile("ones", [128, 128])
    cv = S.tile("cv", [128, 8, 2])
    adab = S.tile("adab", [128, 48])
    n1g = S.tile("n1g", [128, 8])
    mod0 = S.tile("mod0", [128, 8, 2])
    mod1 = S.tile("mod1", [128, 8, 2])
    gm1 = S.tile("gm1", [128, 8, 2])
    pp = S.tile("pp", [64, 16])
    omka = S.tile("omka", [64, 1])
    rowbc = S.tile("rowbc", [128, 4, 64])
    scal = S.tile("scal", [128, 8])
    xs = S.tile("xs", [128, 8, N])
    hT = S.tile("hT", [128, 8, N])
    rstd = S.tile("rstd", [128, N])

    def body():
        S.dma(ident.r, ident_d.r, "ldc")
        S.dma(cv.r, cv_d.r, "ldc")
        S.dma(adab.r, adab_d.r, "ldc")
        S.dma(n1g.r, n1g_d.r, "ldc")
        S.dma(pp.r, pp_d.r, "ldc")
        S.dma(rowbc.r, rowbc_d.r, "ldc")
        S.dma(scal.r, scal_d.r, "ldc")
        S.memset("dve", ones.r, 1.0)
        S.act(cv.r, cv.r, AF.Silu)
        S.ts("dve", omka.r, pp[:, 9:10], -1.0, 1.0, op0=ALU.mult, op1=ALU.add)
        scr = S.tile("scr", [128, 4096])
        adaw_t = scr.r.re("p (c f) -> p c f", c=8)
        for seg, dst in ((0, mod0), (1, mod1)):
            for half in range(2):
                S.dma(adaw_t, adaw_d[seg, half], "ldc")
                for f4 in range(4):
                    fc = half * 4 + f4
                    for dc in range(8):
                        S.mm(pb[0][:, fc * 2:fc * 2 + 2], adaw_t[:, dc, f4 * 128:(f4 + 1) * 128], cv[:, dc, :],
                             start=(dc == 0), stop=(dc == 7))
            S.tt("dve", dst.r, pb[0][:, 0:16].re("p (c t) -> p c t", t=2),
                 adab.r.with_ap(adab.ap[:, seg * 8:(seg + 1) * 8].unsqueeze(2).to_broadcast([128, 8, 2])), ALU.add)
        S.ts("dve", gm1.r, mod1.r, 1.0, None, op0=ALU.add)
        S.tt("dve", gm1.r, gm1.r, n1g.r.with_ap(n1g.ap.unsqueeze(2).to_broadcast([128, 8, 2])), ALU.mult)
        sh1 = mod0
        chk("mod", mod0.r, gm1.r)

        def load_h(ti):
            t0 = ti * N
            col = 1 if ti == 0 else 0
            load_x(ti, xs)
            S.act(hT.r, xs.r, AF.Square)
            for kc in range(8):
                S.mm(pb[0][:, 0:N], ones.r, hT[:, kc, :], start=(kc == 0), stop=(kc == 7))
            S.ts("dve", rstd.r, pb[0][:, 0:N], 1.0 / 1024.0, EPS, op0=ALU.mult, op1=ALU.add)
            S.act(rstd.r, rstd.r, AF.Sqrt)
            S.recip(rstd.r, rstd.r)
            S.tt("dve", hT.r, xs.r, rstd.r.with_ap(rstd.ap.unsqueeze(1).to_broadcast([128, 8, N])), ALU.mult)
            for kc in range(8):
                S.ts("pool" if kc % 2 else "dve", hT[:, kc, :], hT[:, kc, :], gm1[:, kc, col:col + 1], sh1[:, kc, col:col + 1],
                     op0=ALU.mult, op1=ALU.add)

        tiles = list(range(NTILE)) if tile_limit is None else list(range(tile_limit))

        def head_norm_fm(y, sq, out, g_col, psb, n=N):
            S.act(sq, y, AF.Square)
            S.mm(psb[0:64, 0:n], ones[0:64, 0:64], sq)
            S.ts("dve", sq, psb[0:64, 0:n], 1.0 / 64.0, EPS, op0=ALU.mult, op1=ALU.add)
            S.act(sq, sq, AF.Sqrt)
            S.recip(sq, sq)
            S.stt("dve", out, y, g_col, sq, ALU.mult, ALU.mult)

        if "conv" in mixers:
            S.push_scope()
            Wc = S.tile("Wc", [128, 8, 192])
            S.dma(Wc.r, Wc_d.r, "ldc")
            U = S.tile("convU", [64, TT + 4])
            Bg = S.tile("convB", [64, TT])
            S.memset("dve", U.r, 0.0)

            def ucol(t):
                return t + 1 if t < 256 else t + 3

            for ti in tiles:
                load_h(ti)
                t0 = ti * N
                for g in range(3):
                    for kc in range(8):
                        S.mm(pb[1 + g][0:64, 0:N], Wc[:, kc, g * 64:(g + 1) * 64], hT[:, kc, :], start=(kc == 0), stop=(kc == 7))
                S.copy("act", Bg[:, t0:t0 + N], pb[2][0:64, 0:N])
                S.copy("act", U[:, ucol(t0):ucol(t0) + N], pb[1][0:64, 0:N])
                S.tt("dve", U[:, ucol(t0):ucol(t0) + N], U[:, ucol(t0):ucol(t0) + N], pb[3][0:64, 0:N], ALU.mult)
            cy = S.tile("convy", [64, N])
            csq = S.tile("convsq", [64, N])
            for ti in tiles:
                t0 = ti * N
                u0 = ucol(t0)
                S.ts("dve", cy.r, U[:, u0 - 1:u0 - 1 + N], pp[:, 0:1], None, op0=ALU.mult)
                S.stt("dve", cy.r, U[:, u0:u0 + N], pp[:, 1:2], cy.r, ALU.mult, ALU.add)
                S.stt("dve", cy.r, U[:, u0 + 1:u0 + 1 + N], pp[:, 2:3], cy.r, ALU.mult, ALU.add)
                S.tt("dve", cy.r, cy.r, Bg[:, t0:t0 + N], ALU.mult)
                head_norm_fm(cy.r, csq.r, cy.r, pp[:, 3:4], pb[1])
                store_y(0, t0, N, cy.r)
            chk("conv", cy.r)
            S.pop_scope()

        if "attn" in mixers:
            S.push_scope()
            Wa = S.tile("Wa", [128, 8, 192])
            S.dma(Wa.r, Wa_d.r, "ldc")
            trile = S.tile("trile", [128, 128])
            trige = S.tile("trige", [128, 128])
            S.dma(trile.r, trile_d.r, "ldc")
            S.dma(trige.r, trige_d.r, "ldc")
            QT = S.tile("QT", [64, TT])
            KT = S.tile("KT", [64, TT])
            V1 = S.tile("V1", [128, 66, 65])
            S.memset("dve", V1.r, 1.0)
            rope = S.tile("ropet", [128, 64])
            qk = S.tile("qk", [128, 2, 64])
            qr = S.tile("qr", [128, 2, 64])
            tmpa = S.tile("tmpa", [128, 2, 2, 16])
            ssq = S.tile("ssq", [128, 2])
            junk = S.tile("junk", [128, 64])
            junk2 = S.tile("junk2", [128, 128])
            for ti in tiles:
                load_h(ti)
                for sub in range(2):
                    bi = ti * 2 + sub
                    t0 = bi * 128
                    for kc in range(8):
                        S.mm(pb[1][:, 0:192], hT[:, kc, sub * 128:(sub + 1) * 128], Wa[:, kc, :], start=(kc == 0), stop=(kc == 7))
                    S.copy("act", V1[:, bi, 0:64], pb[1][:, 128:192])
                    S.act(junk2.r, pb[1][:, 0:128], AF.Square)
                    S.reduce("dve", ssq.r, junk2.r.re("p (w f) -> p w f", w=2), ALU.add)
                    S.ts("dve", ssq.r, ssq.r, 1.0 / 64.0, EPS, op0=ALU.mult, op1=ALU.add)
                    S.act(ssq.r, ssq.r, AF.Sqrt)
                    S.recip(ssq.r, ssq.r)
                    for w in range(2):
                        S.stt("dve", qk[:, w, :], pb[1][:, w * 64:(w + 1) * 64], ssq[:, w:w + 1], rowbc[:, w, :], ALU.mult, ALU.mult)
                    src = qk
                    if bi >= 2:
                        S.dma(rope.r, rope_d[bi - 2], "ldr")
                        cosv = rope.r.with_ap(rope.ap.rearrange("p (h cs f) -> p h cs f", h=2, cs=2)[:, :, 0, :])
                        sinv = rope.r.with_ap(rope.ap.rearrange("p (h cs f) -> p h cs f", h=2, cs=2)[:, :, 1, :])
                        for w in range(2):
                            q4 = qk[:, w, :].re("p (h x f) -> p h x f", h=2, x=2)
                            o4 = qr[:, w, :].re("p (h x f) -> p h x f", h=2, x=2)
                            S.tt("dve", o4, q4, cosv.with_ap(cosv.ap.unsqueeze(2).to_broadcast([128, 2, 2, 16])), ALU.mult)
                            S.tt("pool", tmpa[:, :, 0, :], q4[:, :, 1, :], sinv, ALU.mult)
                            S.tt("pool", tmpa[:, :, 1, :], q4[:, :, 0, :], sinv, ALU.mult)
                            S.tt("dve", o4[:, :, 0, :], o4[:, :, 0, :], tmpa[:, :, 0, :], ALU.subtract)
                            S.tt("dve", o4[:, :, 1, :], o4[:, :, 1, :], tmpa[:, :, 1, :], ALU.add)
                        src = qr
                    S.tr(pb[2][0:64, 0:128], src[:, 0, :], ident.r)
                    S.copy("act", QT[:, t0:t0 + 128], pb[2][0:64, 0:128])
                    S.tr(pb[3][0:64, 0:128], src[:, 1, :], ident.r)
                    S.copy("act", KT[:, t0:t0 + 128], pb[3][0:64, 0:128])
            chk("attn_qk", QT[:, 0:512], KT[:, 0:512])
            nblk = len(tiles) * 2
            E = [S.tile(f"attE{i}", [128, 128]) for i in range(5)]
            esink = S.tile("esink", [128, 1])
            S.act(esink.r, scal[:, 0:1], AF.Exp)
            den = S.tile("attden", [128, 1])
            ao = S.tile("atto", [128, 64])
            for bi in range(nblk):
                t0 = bi * 128
                if bi < 2:
                    kbs = [(0, None), (1, None)]
                else:
                    kbs = [(0, None), (1, None)]
                    if bi - 1 >= 2:
                        kbs.append((bi - 1, trige))
                    kbs.append((bi, None))
                    if bi + 1 < nblk:
                        kbs.append((bi + 1, trile))
                for i, (kb, mask) in enumerate(kbs):
                    ps = pb[1 + (i % 2)]
                    S.mm(ps[:, 0:128], KT[:, kb * 128:(kb + 1) * 128], QT[:, t0:t0 + 128])
                    S.act(E[i].r, ps[:, 0:128], AF.Exp, scale=0.125)
                    if mask is not None:
                        S.tt("dve", E[i].r, E[i].r, mask.r, ALU.mult)
                chk("attn_E", E[0].r, E[1].r)
                for i, (kb, mask) in enumerate(kbs):
                    S.mm(pb[3][:, 0:65], E[i].r, V1[:, kb, :], start=(i == 0), stop=(i == len(kbs) - 1))
                chk("attn_pv", pb[3][:, 0:65])
                S.tt("dve", den.r, pb[3][:, 64:65], esink.r, ALU.add)
                S.recip(den.r, den.r)
                S.ts("dve", ao.r, pb[3][:, 0:64], den.r, None, op0=ALU.mult)
                S.act(junk.r, ao.r, AF.Square)
                S.reduce("dve", ssq[:, 0:1], junk.r, ALU.add)
                S.ts("dve", ssq[:, 0:1], ssq[:, 0:1], 1.0 / 64.0, EPS, op0=ALU.mult, op1=ALU.add)
                S.act(ssq[:, 0:1], ssq[:, 0:1], AF.Sqrt)
                S.recip(ssq[:, 0:1], ssq[:, 0:1])
                S.stt("dve", ao.r, ao.r, ssq[:, 0:1], rowbc[:, 2, :], ALU.mult, ALU.mult)
                chk("attn_ao", ao.r)
                S.tr(pb[4][0:64, 0:128], ao.r, ident.r)
                S.copy("act", qk[0:64, :, :].re("p a b -> p (a b)"), pb[4][0:64, 0:128])
                store_y(2, t0, 128, qk[0:64, :, :].re("p a b -> p (a b)"))
                if bi == int(os.environ.get("BLIM", "99")):
                    chk("attn_blk", ao.r)
            chk("attn", ao.r)
            S.pop_scope()

        if "mlstm" in mixers:
            S.push_scope()
            Wm = S.tile("Wm", [128, 8, 260])
            S.dma(Wm.r, Wm_d.r, "ldc")
            trile = S.tile("trile", [128, 128])
            trige = S.tile("trige", [128, 128])
            S.dma(trile.r, trile_d.r, "ldc")
            S.dma(trige.r, trige_d.r, "ldc")
            nblk = len(tiles) * 2
            Qm = S.tile("Qm", [128, 66, 64])
            Km = S.tile("Km", [128, 66, 64])
            Vm1 = S.tile("Vm1", [128, 66, 65])
            Om = S.tile("Om", [128, 66, 64])
            Hs = S.tile("Hs", [128, 66, 64])
            G = S.tile("G", [128, 66, 4])
            nfb = S.tile("nfb", [128, 2])
            S.memset("dve", Vm1.r, 1.0)
            S.ts("dve", nfb[:, 0:1], scal[:, 2:3], -1.0, None, op0=ALU.mult)
            S.ts("dve", nfb[:, 1:2], scal[:, 4:5], -1.0, None, op0=ALU.mult)
            for ti in tiles:
                load_h(ti)
                for sub in range(2):
                    bi = ti * 2 + sub
                    for kc in range(8):
                        S.mm(pb[1][:, 0:260], hT[:, kc, sub * 128:(sub + 1) * 128], Wm[:, kc, :], start=(kc == 0), stop=(kc == 7))
                    S.copy("act", Qm[:, bi, :], pb[1][:, 0:64])
                    chk("ml_a", Qm[:, 0, :])
                    S.ts("dve", Km[:, bi, :], pb[1][:, 64:128], 0.125, None, op0=ALU.mult)
                    S.copy("dve", Vm1[:, bi, 0:64], pb[1][:, 128:192])
                    chk("ml_b", Km[:, 0, :])
                    S.act(Om[:, bi, :], pb[1][:, 192:256], AF.Sigmoid)
                    chk("ml_c", Om[:, 0, :])
                    for d in range(2):
                        S.ts("dve", G[:, bi, 2 * d:2 * d + 1], pb[1][:, 256 + 2 * d:257 + 2 * d], scal[:, 1 + 2 * d:2 + 2 * d], None, op0=ALU.add)
                        chk("ml_d", G[:, 0, :])
                        S.act(G[:, bi, 2 * d + 1:2 * d + 2], pb[1][:, 257 + 2 * d:258 + 2 * d], AF.Exp, bias=nfb[:, d:d + 1], scale=-1.0)
                        chk("ml_e", G[:, 0, :])
                        S.act(G[:, bi, 2 * d + 1:2 * d + 2], G[:, bi, 2 * d + 1:2 * d + 2], AF.Ln, bias=1.0)
                        chk("ml_f", G[:, 0, :])
                        S.ts("dve", G[:, bi, 2 * d + 1:2 * d + 2], G[:, bi, 2 * d + 1:2 * d + 2], -1.0, None, op0=ALU.mult)
            chk("ml_p1", Qm[:, 0, :], Km[:, 0, :], G[:, 0, :])
            C1T = S.tile("C1T", [64, 65])
            eb = S.tile("mleb", [128, 1])
            ek = S.tile("mlek", [128, 1])
            eL = S.tile("mleL", [64, 1])
            qt = S.tile("mlqt", [128, 64])
            kt = S.tile("mlkt", [128, 64])
            qtT = S.tile("mlqtT", [64, 128])
            ktT = S.tile("mlktT", [64, 128])
            STs = S.tile("mlST", [128, 128])
            rden = S.tile("mlrden", [128, 1])
            for d in range(2):
                tri = trile if d == 0 else trige
                order = list(range(nblk)) if d == 0 else [1, 0] + list(range(nblk - 1, 1, -1))
                S.memset("dve", C1T.r, 0.0)
                for bi in order:
                    lf = G[:, bi, 2 * d + 1:2 * d + 2]
                    S.mm(pb[2][:, 0:1], tri.r, lf)
                    S.mm(pb[2][0:64, 1:2], ones[:, 0:64], lf)
                    S.act(eb.r, pb[2][:, 0:1], AF.Exp)
                    S.tt("dve", ek.r, G[:, bi, 2 * d:2 * d + 1], pb[2][:, 0:1], ALU.subtract)
                    S.act(ek.r, ek.r, AF.Exp)
                    S.act(eL.r, pb[2][0:64, 1:2], AF.Exp)
                    S.ts("dve", qt.r, Qm[:, bi, :], eb.r, None, op0=ALU.mult)
                    S.ts("pool", kt.r, Km[:, bi, :], ek.r, None, op0=ALU.mult)
                    S.tr(pb[3][0:64, 0:128], qt.r, ident.r)
                    S.copy("act", qtT.r, pb[3][0:64, 0:128])
                    S.tr(pb[4][0:64, 0:128], kt.r, ident.r)
                    S.copy("dve", ktT.r, pb[4][0:64, 0:128])
                    S.mm(pb[5][:, 0:128], ktT.r, qtT.r)
                    S.tt("dve", STs.r, pb[5][:, 0:128], tri.r, ALU.mult)
                    S.mm(pb[6][:, 0:65], STs.r, Vm1[:, bi, :], start=True, stop=False)
                    S.mm(pb[6][:, 0:65], qtT.r, C1T.r, start=False, stop=True)
                    S.ts("dve", rden.r, pb[6][:, 64:65], -1.0, None, op0=ALU.mult)
                    S.tt("dve", rden.r, rden.r, pb[6][:, 64:65], ALU.max)
                    S.ts("dve", rden.r, rden.r, 1.0, None, op0=ALU.max)
                    S.recip(rden.r, rden.r)
                    if d == 0:
                        S.ts("dve", Hs[:, bi, :], pb[6][:, 0:64], rden.r, None, op0=ALU.mult)
                    else:
                        S.stt("dve", Hs[:, bi, :], pb[6][:, 0:64], rden.r, Hs[:, bi, :], ALU.mult, ALU.add)
                    S.mm(pb[7][0:64, 0:65], ident[0:64, 0:64], C1T.r, start=True, stop=False)
                    S.mm(pb[7][0:64, 0:65], kt.r, Vm1[:, bi, :], start=False, stop=True)
                    S.ts("dve", C1T.r, pb[7][0:64, 0:65], eL.r, None, op0=ALU.mult)
                if d == 0:
                    chk("ml_fwd", Hs[:, 0, :], Hs[:, 1, :], Hs[:, 2, :])
            mlsq = S.tile("mlsq", [128, 64])
            mlss = S.tile("mlss", [128, 1])
            mly = S.tile("mly", [128, 64])
            mlyT = S.tile("mlyT", [64, 128])
            for bi in range(nblk):
                S.act(mlsq.r, Hs[:, bi, :], AF.Square)
                S.reduce("dve", mlss.r, mlsq.r, ALU.add)
                S.ts("dve", mlss.r, mlss.r, 1.0 / 64.0, EPS, op0=ALU.mult, op1=ALU.add)
                S.act(mlss.r, mlss.r, AF.Sqrt)
                S.recip(mlss.r, mlss.r)
                S.stt("dve", mly.r, Hs[:, bi, :], mlss.r, rowbc[:, 3, :], ALU.mult, ALU.mult)
                S.tt("dve", mly.r, mly.r, Om[:, bi, :], ALU.mult)
                S.tr(pb[3][0:64, 0:128], mly.r, ident.r)
                S.copy("act", mlyT.r, pb[3][0:64, 0:128])
                store_y(3, bi * 128, 128, mlyT.r)
            chk("mlstm", mly.r)
            S.pop_scope()
        if "rwkv" in mixers:
            S.push_scope()
            Wr = S.tile("Wr", [128, 8, 256])
            S.dma(Wr.r, Wr_d.r, "ldc")
            w2p = S.tile("w2p", [16, 2, 64])
            a2p = S.tile("a2p", [16, 2, 64])
            g2p = S.tile("g2p", [32, 64])
            rmask = [S.tile(f"rmask{d}", [64, 192]) for d in range(2)]
            cmask = S.tile("cmask", [64, 256])
            S.dma(w2p.r, w2_d.r, "ldc")
            S.dma(a2p.r, a2_d.r, "ldc")
            S.dma(g2p.r, g2_d.r, "ldc")
            for d in range(2):
                S.dma(rmask[d].r, rmask_d[d], "ldc")
            S.dma(cmask.r, cmask_d.r, "ldc")
            RKbc = S.tile("RKbc", [64, 64])
            S.ts("dve", RKbc.r, ones[0:64, 0:64], pp[:, 11:12], None, op0=ALU.mult)
            Yst = S.tile("Yst", [64, TT])
            rT = S.tile("rw_r", [64, N])
            kT = S.tile("rw_k", [64, N])
            vT = S.tile("rw_v", [64, N])
            gT = S.tile("rw_g", [64, N])
            kkT = S.tile("rw_kk", [64, N])
            tw = S.tile("rw_tw", [16, N])
            xa = S.tile("rw_xa", [16, N])
            sg = S.tile("rw_sg", [32, N])
            lw = S.tile("rw_lw", [64, N])
            aT = [S.tile(f"rw_a{d}", [64, N]) for d in range(2)]
            kd = [S.tile(f"rw_kd{d}", [64, N]) for d in range(2)]
            cum = S.tile("rw_cum", [64, N])
            tmp = S.tile("rw_tmp", [64, N])
            Pin = S.tile("rw_Pin", [64, N])
            Pinv = S.tile("rw_Pinv", [64, N])
            Pex = S.tile("rw_Pex", [64, N])
            AR = S.tile("rw_AR", [64, 4, 2, 64])
            BK = S.tile("rw_BK", [64, 4, 2, 64])
            Vtok = S.tile("rw_Vtok", [64, 4, 64])
            NM = [S.tile(f"rw_NM{i}", [64, 128]) for i in range(2)]
            Pw = [S.tile(f"rw_P{i}", [64, 64]) for i in range(2)]
            PwT = [S.tile(f"rw_PT{i}", [64, 64]) for i in range(2)]
            X = [S.tile(f"rw_X{i}", [64, 64]) for i in range(2)]
            Btok = S.tile("rw_Btok", [64, 64])
            Ktok = S.tile("rw_Ktok", [64, 64])
            ZT = S.tile("rw_ZT", [64, 64])
            UT = S.tile("rw_UT", [64, 64])
            S0T = S.tile("rw_S0T", [64, 64])
            sq = S.tile("rw_sq", [64, N])
            yo = S.tile("rw_yo", [64, N])
            i64 = ident[0:64, 0:64]
            o64 = ones[0:64, 0:64]

            def prep_tile(ti, d, both):
                load_h(ti)
                for g, dst in ((0, rT), (1, kT), (2, vT)):
                    for kc in range(8):
                        S.mm(pb[1][0:64, 0:N], Wr[:, kc, g * 64:(g + 1) * 64], hT[:, kc, :], start=(kc == 0), stop=(kc == 7))
                    S.copy("act", dst.r, pb[1][0:64, 0:N])
                for kc in range(8):
                    S.mm(pb[2][0:16, 0:N], Wr[:, kc, 192:208], hT[:, kc, :], start=(kc == 0), stop=(kc == 7))
                S.act(tw.r, pb[2][0:16, 0:N], AF.Tanh)
                for kc in range(8):
                    S.mm(pb[2][0:16, 0:N], Wr[:, kc, 208:224], hT[:, kc, :], start=(kc == 0), stop=(kc == 7))
                S.copy("act", xa.r, pb[2][0:16, 0:N])
                for kc in range(8):
                    S.mm(pb[2][0:32, 0:N], Wr[:, kc, 224:256], hT[:, kc, :], start=(kc == 0), stop=(kc == 7))
                S.act(sg.r, pb[2][0:32, 0:N], AF.Sigmoid)
                for c in range(4):
                    for kc in range(8):
                        S.mm(pb[3][0:64, c * 64:(c + 1) * 64], hT[:, kc, c * 64:(c + 1) * 64], Wr[:, kc, 128:192],
                             start=(kc == 0), stop=(kc == 7))
                S.copy("act", Vtok.r.re("p c v -> p (c v)"), pb[3][0:64, 0:256])
                dirs = (0, 1) if both else (d,)
                for dd in dirs:
                    S.mm(pb[1][0:64, 0:N], a2p[:, dd, :], xa.r)
                    S.act(aT[dd].r, pb[1][0:64, 0:N], AF.Sigmoid, bias=pp[:, 6 + dd:7 + dd])
                    S.ts("dve", kd[dd].r, aT[dd].r, pp[:, 9:10], omka.r, op0=ALU.mult, op1=ALU.add)
                    S.tt("dve", kd[dd].r, kd[dd].r, kT.r, ALU.mult)
                S.mm(pb[1][0:64, 0:N], w2p[:, d, :], tw.r)
                S.act(lw.r, pb[1][0:64, 0:N], AF.Sigmoid, bias=pp[:, 4 + d:5 + d])
                S.ts("dve", lw.r, lw.r, -RWKV_DECAY_SCALE, None, op0=ALU.mult)
                if both:
                    S.mm(pb[1][0:64, 0:N], g2p.r, sg.r)
                    S.copy("act", gT.r, pb[1][0:64, 0:N])
                S.ts("dve", kkT.r, kT.r, pp[:, 8:9], None, op0=ALU.mult)
                S.act(sq.r, kkT.r, AF.Square)
                S.mm(pb[1][0:64, 0:N], o64, sq.r)
                S.ts("dve", sq.r, pb[1][0:64, 0:N], EPS, None, op0=ALU.add)
                S.act(sq.r, sq.r, AF.Sqrt)
                S.recip(sq.r, sq.r)
                S.tt("dve", kkT.r, kkT.r, sq.r, ALU.mult)
                S.scan(cum.r, cmask.r, lw.r, 0.0, ALU.mult, ALU.add)
                if d == 1:
                    c3 = cum.ap.rearrange("p (c t) -> p c t", c=4)
                    S.tt("dve", tmp.r, lw.r, cum.r, ALU.subtract)
                    S.tt("dve", cum.r.re("p (c t) -> p c t", c=4), tmp.r.re("p (c t) -> p c t", c=4),
                         cum.r.with_ap(c3[:, :, 63:64].to_broadcast([64, 4, 64])), ALU.add)
                S.act(Pin.r, cum.r, AF.Exp)
                S.act(Pinv.r, cum.r, AF.Exp, scale=-1.0)
                S.tt("dve", tmp.r, cum.r, lw.r, ALU.subtract)
                S.act(Pex.r, tmp.r, AF.Exp)
                A_v = AR.r.re("p c two t -> p c (two t)")
                S.stt("dve", AR[:, :, 0, :], kkT.r.re("p (c t) -> p c t", c=4), -1.0, Pex.r.re("p (c t) -> p c t", c=4), ALU.mult, ALU.mult)
                S.tt("dve", AR[:, :, 1, :], rT.r.re("p (c t) -> p c t", c=4), Pin.r.re("p (c t) -> p c t", c=4), ALU.mult)
                S.tt("dve", tmp.r, kkT.r, aT[d].r, ALU.mult)
                S.tt("dve", BK[:, :, 0, :], tmp.r.re("p (c t) -> p c t", c=4), Pinv.r.re("p (c t) -> p c t", c=4), ALU.mult)
                S.tt("dve", BK[:, :, 1, :], kd[d].r.re("p (c t) -> p c t", c=4), Pinv.r.re("p (c t) -> p c t", c=4), ALU.mult)

            def chunk(ti, c, d):
                m = rmask[d]
                ARc = AR[:, c, :, :].re("p two t -> p (two t)")
                A_c, R_c = AR[:, c, 0, :], AR[:, c, 1, :]
                B_c, K_c = BK[:, c, 0, :], BK[:, c, 1, :]
                V_c = Vtok[:, c, :]
                S.mm(pb[1][0:64, 0:128], B_c, ARc)
                S.tt("dve", NM[0].r, pb[1][0:64, 0:128], m[:, 0:128], ALU.mult)
                S.mm(pb[2][0:64, 0:128], K_c, ARc)
                S.tt("dve", NM[1].r, pb[2][0:64, 0:128], m[:, 0:128], ALU.mult)
                S.mm(pb[3][0:64, 0:64], A_c, B_c)
                S.tt("dve", PwT[0].r, pb[3][0:64, 0:64], m[:, 128:192], ALU.mult)
                S.copy("act", Pw[0].r, NM[0][:, 0:64])
                S.tt("dve", X[0].r, NM[0][:, 0:64], i64, ALU.add)
                cur = 0
                for lev in range(5):
                    nxt = 1 - cur
                    S.mm(pb[1][0:64, 0:64], Pw[cur].r, PwT[cur].r)
                    S.copy("act", PwT[nxt].r, pb[1][0:64, 0:64])
                    if lev < 4:
                        S.mm(pb[2][0:64, 0:64], PwT[cur].r, Pw[cur].r)
                        S.copy("dve", Pw[nxt].r, pb[2][0:64, 0:64])
                    S.mm(pb[3][0:64, 0:64], PwT[nxt].r, X[cur].r)
                    S.tt("dve", X[nxt].r, pb[3][0:64, 0:64], X[cur].r, ALU.add)
                    cur = nxt
                Xf = X[cur]
                S.tr(pb[1][0:64, 0:64], B_c, i64)
                S.copy("act", Btok.r, pb[1][0:64, 0:64])
                S.tr(pb[2][0:64, 0:64], K_c, i64)
                S.copy("dve", Ktok.r, pb[2][0:64, 0:64])
                S.mm(pb[4][0:64, 0:64], A_c, S0T.r, start=True, stop=False)
                S.mm(pb[4][0:64, 0:64], NM[1][:, 0:64], V_c, start=False, stop=True)
                S.copy("act", ZT.r, pb[4][0:64, 0:64])
                S.mm(pb[5][0:64, 0:64], Xf.r, ZT.r)
                S.copy("act", UT.r, pb[5][0:64, 0:64])
                S.mm(pb[6][0:64, 0:64], S0T.r, R_c, start=True, stop=False)
                S.mm(pb[6][0:64, 0:64], UT.r, NM[0][:, 64:128], start=False, stop=False)
                S.mm(pb[6][0:64, 0:64], V_c, NM[1][:, 64:128], start=False, stop=True)
                t0 = ti * N + c * 64
                if d == 0:
                    S.copy("dve", Yst[:, t0:t0 + 64], pb[6][0:64, 0:64])
                else:
                    S.tt("dve", Yst[:, t0:t0 + 64], Yst[:, t0:t0 + 64], pb[6][0:64, 0:64], ALU.add)
                S.mm(pb[7][0:64, 0:64], i64, S0T.r, start=True, stop=False)
                S.mm(pb[7][0:64, 0:64], Btok.r, UT.r, start=False, stop=False)
                S.mm(pb[7][0:64, 0:64], Ktok.r, V_c, start=False, stop=True)
                pl = c * 64 + (63 if d == 0 else 0)
                S.ts("dve", S0T.r, pb[7][0:64, 0:64], Pin[:, pl:pl + 1], None, op0=ALU.mult)

            def finish_tile(ti):
                t0 = ti * N
                S.tt("dve", tmp.r, kd[0].r, kd[1].r, ALU.add)
                S.tt("dve", tmp.r, tmp.r, rT.r, ALU.mult)
                S.mm(pb[1][0:64, 0:N], RKbc.r, tmp.r)
                S.tt("dve", tmp.r, pb[1][0:64, 0:N], vT.r, ALU.mult)
                head_norm_fm(Yst[:, t0:t0 + N], sq.r, yo.r, pp[:, 12:13], pb[2])
                S.tt("dve", yo.r, yo.r, tmp.r, ALU.add)
                S.tt("dve", yo.r, yo.r, gT.r, ALU.mult)
                store_y(1, t0, N, yo.r)

            for d in range(2):
                order = tiles if d == 0 else [0] + tiles[:0:-1]
                S.memset("dve", S0T.r, 0.0)
                for ti in order:
                    prep_tile(ti, d, both=(d == 1))
                    chk("rw_prep", AR[:, 0, :, :], BK[:, 0, :, :], cum[:, 0:64], lw[:, 0:64])
                    for c in (range(4) if d == 0 else range(3, -1, -1)):
                        chunk(ti, c, d)
                        chk("rw_c0", Yst[:, 0:64], S0T.r, X[1].r, ZT.r, UT.r)
                    if d == 1:
                        finish_tile(ti)
                if d == 0:
                    chk("rw_fwd", Yst[:, 0:256])
            S.pop_scope()

    try:
        body()
    except Done:
        while len(S.scopes) > 1:
            S.scopes.pop().close()
        raise
    S.pop_scope()


class StageDone(Exception):
    pass


def build_A(stage=None, mixers=("conv", "attn", "mlstm", "rwkv"), tile_limit=None):
    nc = bass.Bass("TRN2", target_bir_lowering=False)
    S = Sched(nc)
    xT_d = Tile(nc.dram_tensor("xT", [128, 8, TT], F32, kind="ExternalInput").ap(), "xT")
    yT_d = Tile(nc.dram_tensor("yT", [4, 64, TT], F32, kind="ExternalOutput").ap(), "yT")
    dbg_d = Tile(nc.dram_tensor("dbg", [128, 4096], F32, kind="ExternalOutput").ap(), "dbg")
    pb = [S.ptile(f"pb{i}", [128, 512]) for i in range(8)]

    def load_x(ti, xs):
        S.dma(xs.r, xT_d[:, :, ti * N:(ti + 1) * N], "ldx")

    def store_y(m, t0, n, src):
        S.dma(yT_d[m, :, t0:t0 + n], src, "st")

    try:
        emit_A(S, nc, pb, "", load_x, store_y, stage, dbg_d, mixers, tile_limit)
    except Exception as e:
        if type(e).__name__ != "Done":
            raise
    S.wait_all("sp")
    while getattr(S, "scopes", None):
        S.scopes.pop().close()
    print("build_A instructions", S.n_ins, "sems", S.nsem, "sbuf left", nc.sbuf_bytes_remaining)
    S.close()
    return nc

EPS = 1e-6
POOLENG = os.environ.get("POOLENG", "pool")
NEG = -1.0e30


def emit_B(S, nc, pb, sfx, groups, x_src, y_src, out_sink, stage=None, dbg_d=None):
    def D(name, shape, kind="ExternalInput", dt=F32):
        t = nc.dram_tensor(name + sfx, list(shape), dt, kind=kind)
        return Tile(t.ap(), name + sfx)

    cv_d = S.shared_dram("cv", [128, 8, 2])
    adaw_d = S.shared_dram("adaw" + sfx, [6, 2, 128, 8, 512])
    adab_d = S.shared_dram("adab" + sfx, [128, 48])
    n2g_d = D("n2g", [128, 8])
    wout_d = D("wout", [8, 128, 8, 128])
    wq_d = D("wq", [16, 128, 8, 128])
    keys_d = D("keysT", [128, 16, 128])
    UT_d = D("UT", [128, 128, 8, 128])
    VJ_d = D("VJ", [128, 128, 1024])
    ident_d = S.shared_dram("ident", [128, 128])
    iota_d = S.shared_dram("iota", [128, 128])

    GM = max(g[1] for g in groups)
    S.push_scope()

    class Done(Exception):
        pass

    def chk(name, *refs):
        if stage != name:
            return
        o = 0
        for r in refs:
            n = 1
            for s_ in r.ap.shape[1:]:
                n *= s_
            t = S.tile(f"dbgt{o}", [128, n])
            S.copy("dve", t.r, r if len(r.ap.shape) == 2 else r)
            S.dma(dbg_d[0:r.ap.shape[0], o:o + n], t[0:r.ap.shape[0], :], "st")
            o += n
        raise Done()
    ident = S.tile("ident", [128, 128])
    iota = S.tile("iota", [128, 128])
    ones = S.tile("ones", [128, 128])
    cv = S.tile("cv", [128, 8, 2])
    adab = S.tile("adab", [128, 48])
    n2g = S.tile("n2g", [128, 8])
    keysT = S.tile("keysT", [128, 16, 128])
    mod = [S.tile(f"mod{i}", [128, 8, 2]) for i in range(6)]
    gm2 = S.tile("gm2", [128, 8, 2])
    scr = S.tile("scr", [128, 4096])
    xs = S.tile("xs", [128, 8, GM])
    ys = S.tile("ys", [128, 8, GM])
    x1 = S.tile("x1", [128, 8, GM])
    h2 = S.tile("h2", [128, 8, GM])
    rstd = S.tile("rstd", [128, GM])
    wbuf = [S.tile(f"wbuf{i}", [128, 8, 128]) for i in range(2)]
    ubuf = [S.tile(f"ubuf{i}", [128, 8, 128]) for i in range(2)]
    vbuf = [S.tile(f"vbuf{i}", [128, 1024]) for i in range(2)]
    sc = S.tile("sc", [128, 16, 128])
    sc2 = S.tile("sc2", [128, 16, 128])
    sv = S.tile("sv", [128, 16, 16])
    si = S.tile("si", [128, 16, 16], U32)
    sif = S.tile("sif", [128, 16, 16])
    cand = S.tile("cand", [128, 8, 256])
    tv = S.tile("tv", [128, 8, 16])
    ti = S.tile("ti", [128, 8, 16], U32)
    tiu = S.tile("tiu", [128, 8, 16], U32)
    aq = S.tile("aq", [128, 8, 16])
    bq = S.tile("bq", [128, 8, 16])
    If = S.tile("If", [128, 128])
    Jf = S.tile("Jf", [128, 128])
    Wf = S.tile("Wf", [128, 128])
    mx = S.tile("mx", [128, 8])
    zs = S.tile("zs", [128, 8])
    IT = S.tile("IT", [128, GM])
    JT = S.tile("JT", [128, GM])
    WT = S.tile("WT", [128, GM])
    oiw = [S.tile(f"oiw{i}", [128, 128]) for i in range(2)]
    oj = [S.tile(f"oj{i}", [128, 128]) for i in range(2)]
    WW = S.tile("WW", [128, GM, 128], BF16)
    gj = [S.tile(f"gj{i}", [128, GM]) for i in range(2)]
    pj = [S.tile(f"pj{i}", [128, GM]) for i in range(2)]
    acc = pb[0:4]
    pa = pb[4:6]
    pw = pb[6:8]

    def build_body():
        S.dma(ident.r, ident_d.r, "ldc")
        S.dma(iota.r, iota_d.r, "ldc")
        S.dma(cv.r, cv_d.r, "ldc")
        S.dma(adab.r, adab_d.r, "ldc")
        S.dma(n2g.r, n2g_d.r, "ldc")
        S.dma(keysT.r, keys_d.r, "ldc")
        S.memset("dve", ones.r, 1.0)
        S.act(cv.r, cv.r, AF.Silu)
        adaw_t = scr[:, 0:4096].re("p (c f) -> p c f", c=8)
        for seg in (2, 3, 4, 5):
            for half in range(2):
                S.dma(adaw_t, adaw_d[seg, half], "ldc")
                for f4 in range(4):
                    fc = half * 4 + f4
                    for dc in range(8):
                        S.mm(pa[0][:, fc * 2:fc * 2 + 2], adaw_t[:, dc, f4 * 128:(f4 + 1) * 128], cv[:, dc, :],
                             start=(dc == 0), stop=(dc == 7))
            S.tt("dve", mod[seg].r, pa[0][:, 0:16].re("p (c t) -> p c t", t=2),
                 adab[:, seg * 8:(seg + 1) * 8].with_ap(adab.ap[:, seg * 8:(seg + 1) * 8].unsqueeze(2).to_broadcast([128, 8, 2])),
                 ALU.add)
        S.ts("dve", gm2.r, mod[4].r, 1.0, None, op0=ALU.add)
        S.tt("dve", gm2.r, gm2.r, n2g.r.with_ap(n2g.ap.unsqueeze(2).to_broadcast([128, 8, 2])), ALU.mult)
        gt1, sh2, gt2 = mod[2], mod[3], mod[5]
        chk("mod", mod[2].r, mod[3].r, mod[4].r, mod[5].r, gm2.r)

        wi = 0
        ui = 0
        for (g0, GN, col) in groups:
            NTL = GN // 128
            x_src(g0, GN, col, xs)
            y_src(g0, GN, col, ys, h2)
            for oc in range(8):
                wb = wbuf[wi % 2]
                S.dma(wb.r, wout_d[oc], f"ldw{wi % 2}")
                wi += 1
                p = pa[oc % 2]
                for kc in range(8):
                    S.mm(p[:, 0:GN], wb[:, kc, :], ys[:, kc, 0:GN], start=(kc == 0), stop=(kc == 7))
                S.stt("dve", x1[:, oc, 0:GN], p[:, 0:GN], gt1[:, oc, col:col + 1], xs[:, oc, 0:GN], ALU.mult, ALU.add)
            chk("x1", x1[:, :, 0:GN])
            S.act(ys[:, :, 0:GN], x1[:, :, 0:GN], AF.Square)
            for kc in range(8):
                S.mm(pa[0][:, 0:GN], ones.r, ys[:, kc, 0:GN], start=(kc == 0), stop=(kc == 7))
            S.ts("dve", rstd[:, 0:GN], pa[0][:, 0:GN], 1.0 / 1024.0, EPS, op0=ALU.mult, op1=ALU.add)
            S.act(rstd[:, 0:GN], rstd[:, 0:GN], AF.Sqrt)
            S.recip(rstd[:, 0:GN], rstd[:, 0:GN])
            for kc in range(8):
                S.tt("dve", h2[:, kc, 0:GN], x1[:, kc, 0:GN], rstd[:, 0:GN], ALU.mult)
                S.ts("dve", h2[:, kc, 0:GN], h2[:, kc, 0:GN], gm2[:, kc, col:col + 1], sh2[:, kc, col:col + 1],
                     op0=ALU.mult, op1=ALU.add)
            chk("h2", h2[:, :, 0:GN])
            qT = scr[:, 0:16 * GN].re("p (h t) -> p h t", h=16)
            for hp in range(16):
                wb = wbuf[wi % 2]
                S.dma(wb.r, wq_d[hp], f"ldw{wi % 2}")
                wi += 1
                p = pa[hp % 2]
                for kc in range(8):
                    S.mm(p[:, 0:GN], wb[:, kc, :], h2[:, kc, 0:GN], start=(kc == 0), stop=(kc == 7))
                S.copy("act", qT[:, hp, :], p[:, 0:GN])
            chk("qT", qT[:, :, 0:GN])
            for mt in range(NTL):
                ms = slice(mt * 128, (mt + 1) * 128)
                for hp in range(16):
                    S.mm(acc[hp // 4][:, (hp % 4) * 128:(hp % 4) * 128 + 128], qT[:, hp, ms], keysT[:, hp, :])
                for b4 in range(4):
                    S.copy("act" if b4 % 2 else "dve", sc[:, b4 * 4:(b4 + 1) * 4, :].re("p a k -> p (a k)"), acc[b4].r)
                chk("sc", sc.r)
                for hp in range(16):
                    S.vmax(sv[:, hp, 0:8], sc[:, hp, :])
                    S.vmax_index(si[:, hp, 0:8], sv[:, hp, 0:8], sc[:, hp, :])
                    S.vmatch_replace(sc2[:, hp, :], sv[:, hp, 0:8], sc[:, hp, :], NEG)
                    S.vmax(sv[:, hp, 8:16], sc2[:, hp, :])
                    S.vmax_index(si[:, hp, 8:16], sv[:, hp, 8:16], sc2[:, hp, :])
                S.copy("dve", sif.r, si.r)
                chk("top1", sv.r, sif.r)
                sv4 = sv.ap.rearrange("p (h two) a -> p h two a", two=2)
                sif4 = sif.ap.rearrange("p (h two) a -> p h two a", two=2)
                S.tt("dve", cand.r.re("p h (a b) -> p h a b", b=16),
                     sv.r.with_ap(sv4[:, :, 0, :].unsqueeze(3).to_broadcast([128, 8, 16, 16])),
                     sv.r.with_ap(sv4[:, :, 1, :].unsqueeze(2).to_broadcast([128, 8, 16, 16])), ALU.add)
                cand2 = sc2.r.re("p (h two) k -> p h (two k)", two=2)
                eq = sc.r.re("p (h two) (n a) -> p h (two n) a", two=2, a=16)
                for h in range(8):
                    S.vmax(tv[:, h, 0:8], cand[:, h, :])
                    S.vmax_index(ti[:, h, 0:8], tv[:, h, 0:8], cand[:, h, :])
                    S.vmatch_replace(cand2[:, h, :], tv[:, h, 0:8], cand[:, h, :], NEG)
                    S.vmax(tv[:, h, 8:16], cand2[:, h, :])
                    S.vmax_index(ti[:, h, 8:16], tv[:, h, 8:16], cand2[:, h, :])
                S.ts("dve", tiu.r, ti.r, 15, None, op0=ALU.bitwise_and)
                S.copy("dve", bq.r, tiu.r)
                S.ts("dve", tiu.r, ti.r, 4, None, op0=ALU.logical_shift_right)
                S.copy("dve", aq.r, tiu.r)
                iota16 = iota.r.with_ap(iota.ap[:, 0:16].unsqueeze(1).unsqueeze(1).to_broadcast([128, 8, 16, 16]))
                for (qv, half, dst) in ((aq, 0, If), (bq, 1, Jf)):
                    S.tt("dve", eq, qv.r.with_ap(qv.ap.unsqueeze(3).to_broadcast([128, 8, 16, 16])), iota16, ALU.is_equal)
                    S.tt("dve", eq, eq, sif.r.with_ap(sif4[:, :, half, :].unsqueeze(2).to_broadcast([128, 8, 16, 16])), ALU.mult)
                    S.reduce("dve", dst.r.re("p (h n) -> p h n", h=8), eq, ALU.add)
                chk("IJ", If.r, Jf.r, tv.r, aq.r, bq.r)
                S.reduce("dve", mx.r, tv.r, ALU.max)
                S.tt("dve", tv.r, tv.r, mx.r.with_ap(mx.ap.unsqueeze(2).to_broadcast([128, 8, 16])), ALU.subtract)
                S.act(tv.r, tv.r, AF.Exp)
                S.reduce("dve", zs.r, tv.r, ALU.add)
                S.recip(zs.r, zs.r)
                S.tt("dve", Wf.r.re("p (h n) -> p h n", h=8), tv.r,
                     zs.r.with_ap(zs.ap.unsqueeze(2).to_broadcast([128, 8, 16])), ALU.mult)
                for k, (src, dst) in enumerate(((If, IT), (Jf, JT), (Wf, WT))):
                    S.tr(pw[k % 2][:, 0:128], src.r, ident.r)
                    S.copy("act", dst[:, ms], pw[k % 2][:, 0:128])
            chk("ITW", IT[:, 0:GN], JT[:, 0:GN], WT[:, 0:GN])
            for m in range(int(os.environ.get('MLIM', GN))):
                a = oiw[m % 2]
                b = oj[m % 2]
                S.ts("dve", a.r, iota.r, IT[:, m:m + 1], WT[:, m:m + 1], op0=ALU.is_equal, op1=ALU.mult)
                S.ts(POOLENG, b.r, iota.r, JT[:, m:m + 1], None, op0=ALU.is_equal)
                p = pw[m % 2]
                S.mm(p[:, 0:128], a.r, b.r)
                S.copy("act", WW[:, m, :], p[:, 0:128])
            chk("WW", WW[:, 0:16, :])
            def load_j(j):
                nonlocal ui
                ub = ubuf[ui % 2]
                vb = vbuf[ui % 2]
                S.dma(ub.r, UT_d[j], f"ldu{ui % 2}")
                S.dma(vb.r, VJ_d[j], f"ldu{ui % 2}")
                ui += 1
                return ub, vb

            def a_mm(j, ub):
                p = pa[j % 2]
                for dc in range(8):
                    S.mm(p[:, 0:GN], ub[:, dc, :], h2[:, dc, 0:GN], start=(dc == 0), stop=(dc == 7))

            bufs = {}
            bufs[0] = load_j(0)
            bufs[1] = load_j(1)
            a_mm(0, bufs[0][0])
            for j in range(128):
                if j + 1 < 128:
                    a_mm(j + 1, bufs[j + 1][0])
                g = gj[j % 2]
                pp = pj[j % 2]
                S.act(g[:, 0:GN], pa[j % 2][:, 0:GN], AF.Gelu_apprx_tanh)
                S.tt("dve", pp[:, 0:GN], g[:, 0:GN], WW[:, 0:GN, j], ALU.mult)
                vb = bufs[j][1]
                for oc in range(8):
                    S.mm(acc[oc // 2][:, (oc % 2) * 256:(oc % 2) * 256 + GN], vb[:, oc * 128:(oc + 1) * 128], pp[:, 0:GN],
                         start=(j == 0 and oc % 2 == 0), stop=(j == 127), skip=True)
                if j + 2 < 128:
                    bufs[j + 2] = load_j(j + 2)
                del bufs[j]
            for oc in range(8):
                S.stt("dve", xs[:, oc, 0:GN], acc[oc // 2][:, (oc % 2) * 256:(oc % 2) * 256 + GN], gt2[:, oc, col:col + 1],
                      x1[:, oc, 0:GN], ALU.mult, ALU.add)
            out_sink(g0, GN, col, xs)

    try:
        build_body()
    except Done:
        while len(S.scopes) > 1:
            S.scopes.pop().close()
        raise
    S.pop_scope()


def build_B(NT, groups, stage=None):
    nc = bass.Bass("TRN2", target_bir_lowering=False)
    S = Sched(nc)
    xT_d = Tile(nc.dram_tensor("xT", [128, 8, NT], F32, kind="ExternalInput").ap(), "xT")
    yT_d = Tile(nc.dram_tensor("yT", [128, 8, NT], F32, kind="ExternalInput").ap(), "yT")
    out_d = Tile(nc.dram_tensor("outT", [128, 8, NT], F32, kind="ExternalOutput").ap(), "outT")
    dbg_d = Tile(nc.dram_tensor("dbg", [128, 4096], F32, kind="ExternalOutput").ap(), "dbg")
    pb = [S.ptile(f"pb{i}", [128, 512]) for i in range(8)]

    def x_src(g0, GN, col, xs):
        S.dma(xs[:, :, 0:GN], xT_d[:, :, g0:g0 + GN], "ldx")

    def y_src(g0, GN, col, ys, tmp):
        S.dma(ys[:, :, 0:GN], yT_d[:, :, g0:g0 + GN], "ldx")

    def out_sink(g0, GN, col, xs):
        S.dma(out_d[:, :, g0:g0 + GN], xs[:, :, 0:GN], "st")

    try:
        emit_B(S, nc, pb, "", groups, x_src, y_src, out_sink, stage, dbg_d)
    except Exception as e:
        if type(e).__name__ != "Done":
            raise
    S.wait_all("sp")
    while getattr(S, "scopes", None):
        S.scopes.pop().close()
    print("build_B instructions", S.n_ins, "sems", S.nsem)
    S.close()
    return nc

def fm(X):
    NT = X.shape[0]
    return np.ascontiguousarray(X.T.reshape(8, 128, NT).transpose(1, 0, 2))

def unfm(XT):
    NT = XT.shape[2]
    return np.ascontiguousarray(XT.transpose(1, 0, 2).reshape(1024, NT).T)

def vec_fm(v):
    return np.ascontiguousarray(v.reshape(-1, 128).T)

def consts():
    ident = np.eye(128, dtype=np.float32)
    iota = np.tile(np.arange(128, dtype=np.float32)[None, :], (128, 1))
    return ident, iota

def prep_B_weights(inp, l, permute_wout=False):
    d = {}
    aw = inp["ada_w"][l]
    d["adaw"] = np.ascontiguousarray(aw.reshape(8, 128, 6, 2, 512).transpose(2, 3, 1, 0, 4))
    d["adab"] = vec_fm(inp["ada_b"][l])
    d["n2g"] = vec_fm(inp["norm2_g"][l])
    wo = inp["w_out"][l]
    if permute_wout:
        g = np.arange(1024)
        r_, m_, ch_ = g // 256, (g % 256) // 64, g % 64
        wo = wo[m_ * 256 + r_ * 64 + ch_, :]
    d["wout"] = np.ascontiguousarray(wo.reshape(8, 128, 8, 128).transpose(2, 1, 0, 3))
    wq = inp["peer_wq"][l]
    d["wq"] = np.ascontiguousarray(wq.reshape(8, 128, 16, 128).transpose(2, 1, 0, 3))
    ks = inp["peer_keys"][l]
    d["keysT"] = np.ascontiguousarray(ks.reshape(16, 128, 128).transpose(2, 0, 1))
    u = inp["peer_u"][l]
    d["UT"] = np.ascontiguousarray(u.reshape(128, 128, 8, 128).transpose(1, 3, 2, 0))
    v = inp["peer_v"][l]
    d["VJ"] = np.ascontiguousarray(v.reshape(128, 128, 1024).transpose(1, 0, 2))
    d["ident"], d["iota"] = consts()
    return d

def cvec(inp, b):
    return np.ascontiguousarray(np.stack([vec_fm(inp["c"][b]), vec_fm(inp["c_ctx"])], axis=-1))

OFF = {"hx": 0, "cB": 256, "cC": 512, "r": 768, "k": 1024, "v": 1280, "xw": 1536, "xa": 1552, "xg": 1568,
       "aq": 1600, "ak": 1856, "av": 1984, "mq": 2112, "mk": 2368, "mv": 2624, "mo": 2880, "mg": 3136}

def packW(W, cols):
    Wc = W[:, cols]
    return np.ascontiguousarray(Wc.reshape(8, 128, len(cols)).transpose(1, 0, 2))

def rope_table():
    quarter = 16
    inv = (10000.0 ** (-np.arange(quarter, dtype=np.float32) / quarter)).astype(np.float32)
    t = np.arange(8192)
    row = (t // 64).astype(np.float32); col = (t % 64).astype(np.float32)
    ar = row[:, None] * inv[None, :]; ac = col[:, None] * inv[None, :]
    tab = np.concatenate([np.cos(ar), np.sin(ar), np.cos(ac), np.sin(ac)], -1).astype(np.float32)
    return np.ascontiguousarray(tab.reshape(64, 128, 64))

def prep_A_weights(inp, l, j):
    d = {}
    aw = inp["ada_w"][l]
    d["adaw"] = np.ascontiguousarray(aw.reshape(8, 128, 6, 2, 512).transpose(2, 3, 1, 0, 4))
    d["adab"] = vec_fm(inp["ada_b"][l])
    d["n1g"] = vec_fm(inp["norm1_g"][l])
    W = inp["w_in"][l]
    h64 = np.arange(64) + j * 64
    kv64 = np.arange(64) + (j // 2) * 64
    d["Wc"] = packW(W, np.concatenate([OFF["hx"] + h64, OFF["cB"] + h64, OFF["cC"] + h64]))
    d["Wr"] = packW(W, np.concatenate([OFF["r"] + h64, OFF["k"] + h64, OFF["v"] + h64, OFF["xw"] + np.arange(16),
                                       OFF["xa"] + np.arange(16), OFF["xg"] + np.arange(32)]))
    d["Wa"] = packW(W, np.concatenate([OFF["aq"] + h64, OFF["ak"] + kv64, OFF["av"] + kv64]))
    gcols = np.array([OFF["mg"] + dd * 8 + g * 4 + j for dd in range(2) for g in range(2)])
    d["Wm"] = packW(W, np.concatenate([OFF["mq"] + h64, OFF["mk"] + h64, OFF["mv"] + h64, OFF["mo"] + h64, gcols]))
    pp = np.zeros((64, 16), np.float32)
    pp[:, 0:3] = inp["conv_w"][l][:, h64].T
    pp[:, 3] = inp["conv_g"][l][j]
    pp[:, 4:6] = inp["rwkv_w0"][l][:, h64].T
    pp[:, 6:8] = inp["rwkv_a0"][l][:, h64].T
    pp[:, 8] = inp["rwkv_kk"][l][h64]
    pp[:, 9] = inp["rwkv_ka"][l][h64]
    pp[:, 11] = inp["rwkv_rk"][l][j]
    pp[:, 12] = inp["rwkv_ln_g"][l][j]
    d["pp"] = pp
    d["w2p"] = np.ascontiguousarray(inp["rwkv_w2"][l][:, :, h64].transpose(1, 0, 2))
    d["a2p"] = np.ascontiguousarray(inp["rwkv_a2"][l][:, :, h64].transpose(1, 0, 2))
    d["g2p"] = np.ascontiguousarray(inp["rwkv_g2"][l][:, h64])
    rb = np.stack([inp["att_q_g"][l], inp["att_k_g"][l], inp["att_out_g"][l][j], inp["ml_out_g"][l][j]], 0)
    d["rowbc"] = np.ascontiguousarray(np.tile(rb[None], (128, 1, 1)))
    sc = np.zeros((8,), np.float32)
    sc[0] = inp["att_sink"][l][j]
    sc[1] = inp["ml_i_b"][l][0, j]; sc[2] = inp["ml_f_b"][l][0, j]
    sc[3] = inp["ml_i_b"][l][1, j]; sc[4] = inp["ml_f_b"][l][1, j]
    d["scal"] = np.ascontiguousarray(np.tile(sc[None], (128, 1)))
    ident, _ = consts()
    d["ident"] = ident
    i = np.arange(128)
    d["trile"] = (i[:, None] <= i[None, :]).astype(np.float32)
    d["trige"] = (i[:, None] >= i[None, :]).astype(np.float32)
    i = np.arange(64)
    su = (i[:, None] < i[None, :]).astype(np.float32); iu = (i[:, None] <= i[None, :]).astype(np.float32)
    sl = (i[:, None] > i[None, :]).astype(np.float32); il = (i[:, None] >= i[None, :]).astype(np.float32)
    d["rmask"] = np.ascontiguousarray(np.stack([np.concatenate([su, iu, sl], 1), np.concatenate([sl, il, su], 1)], 0))
    cm = np.ones((64, 256), np.float32); cm[:, ::64] = 0.0
    d["cmask"] = cm
    d["rope"] = rope_table()
    return d

_NC_CACHE = {}


def _get_nc(kind, *args):
    key = (kind,) + args
    if key not in _NC_CACHE:
        if kind == "A":
            _NC_CACHE[key] = build_A()
        else:
            _NC_CACHE[key] = build_B(*args)
    return _NC_CACHE[key]


def kernel_unfused(**inputs):
    inp = {k: np.ascontiguousarray(np.asarray(v, dtype=np.float32)) for k, v in inputs.items()}
    x = inp["x"].copy()
    ctx = inp["ctx"].copy()
    cores = list(range(8))
    for l in range(2):
        ncA = _get_nc("A")
        maps = []
        wA = [prep_A_weights(inp, l, j) for j in range(4)]
        xfull = [fm(np.concatenate([ctx[b], x[b]], 0)) for b in range(2)]
        cvs = [cvec(inp, b) for b in range(2)]
        for c in cores:
            b, j = c // 4, c % 4
            d = dict(wA[j])
            d["xT"] = xfull[b]
            d["cv"] = cvs[b]
            maps.append(d)
        res = run_bass_kernel_spmd(ncA, maps, core_ids=cores)
        ycat = np.zeros((2, TT, 1024), np.float32)
        for c in cores:
            b, j = c // 4, c % 4
            yT = res.results[c]["yT"]
            for m in range(4):
                ycat[b, :, m * 256 + j * 64:m * 256 + (j + 1) * 64] = yT[m].T
        del res, maps, xfull
        with_ctx = (l == 0)
        if with_ctx:
            NT = 2048 + 128
            groups = tuple((g * 256, 256, 0) for g in range(8)) + ((2048, 128, 1),)
        else:
            NT = 2048
            groups = tuple((g * 256, 256, 0) for g in range(8))
        ncB = _get_nc("B", NT, groups)
        wB = prep_B_weights(inp, l)
        maps = []
        for c in cores:
            b, q = c // 4, c % 4
            xs_ = x[b, q * 2048:(q + 1) * 2048]
            ys_ = ycat[b, 256 + q * 2048:256 + (q + 1) * 2048]
            if with_ctx:
                pad = np.zeros((64, 1024), np.float32)
                xs_ = np.concatenate([xs_, ctx[b, q * 64:(q + 1) * 64], pad], 0)
                ys_ = np.concatenate([ys_, ycat[b, q * 64:(q + 1) * 64], pad], 0)
            d = dict(wB)
            d["xT"] = fm(xs_)
            d["yT"] = fm(ys_)
            d["cv"] = cvs[b]
            maps.append(d)
        res = run_bass_kernel_spmd(ncB, maps, core_ids=cores)
        for c in cores:
            b, q = c // 4, c % 4
            o = unfm(res.results[c]["outT"])
            x[b, q * 2048:(q + 1) * 2048] = o[:2048]
            if with_ctx:
                ctx[b, q * 64:(q + 1) * 64] = o[2048:2048 + 64]
        del res, maps
    return x


NTB = 2176
GROUPS4 = [[0, 1, 2, 3], [4, 5, 6, 7]]


def build_fused():
    nc = bass.Bass("TRN2", target_bir_lowering=False)
    S = Sched(nc)
    pb = [S.ptile(f"pb{i}", [128, 512]) for i in range(8)]
    xT_d = Tile(nc.dram_tensor("xT", [128, 8, TT], F32, kind="ExternalInput").ap(), "xT")
    xB_d = Tile(nc.dram_tensor("xB", [128, 8, NTB], F32, kind="ExternalInput").ap(), "xB")
    sel_d = Tile(nc.dram_tensor("sel", [128, 4], F32, kind="ExternalInput").ap(), "sel")
    out_d = Tile(nc.dram_tensor("outT", [128, 8, 2048], F32, kind="ExternalOutput").ap(), "outT")
    YC = 768
    NYC = TT // YC
    ybuf = [[Tile(nc.dram_tensor(f"ybuf{l}_{k}", [256, YC], F32).ap(), f"ybuf{l}_{k}") for k in range(NYC)] for l in range(2)]
    ygath = [[Tile(nc.dram_tensor(f"ygath{l}_{k}", [1024, YC], F32).ap(), f"ygath{l}_{k}") for k in range(NYC)] for l in range(2)]
    xw = [256] * 8 + [128]
    xown = [Tile(nc.dram_tensor(f"xown{k}", [128, 8 * xw[k]], F32).ap(), f"xown{k}") for k in range(9)]
    xg = [Tile(nc.dram_tensor(f"xg{k}", [512, 8 * xw[k]], F32).ap(), f"xg{k}") for k in range(9)]
    sel = S.tile("sel", [128, 4])
    S.dma(sel.r, sel_d.r, "ldc")

    def xown_v(k):
        return xown[k].r.re("p (c t) -> p c t", c=8)

    def xg_v(k):
        return xg[k].r.re("(r p) (c t) -> r p c t", r=4, c=8)

    def yv(l, t0, n):
        k, o = t0 // YC, t0 % YC
        assert o + n <= YC
        return ygath[l][k].r.re("(kc p) t -> p kc t", p=128)[:, :, o:o + n]

    for l in range(2):
        sfx = f"_l{l}"
        def load_x(ti, xs, l=l):
            if l == 0:
                S.dma(xs.r, xT_d[:, :, ti * N:(ti + 1) * N], "ldx")
            elif ti == 0:
                for r in range(4):
                    S.dma(xs[:, :, r * 64:(r + 1) * 64], xg_v(8)[r, :, :, 0:64], "ldx")
            else:
                r, k = (ti - 1) // 8, (ti - 1) % 8
                S.dma(xs.r, xg_v(k)[r], "ldx")

        def store_y(m, t0, n, src, l=l):
            k, o = t0 // YC, t0 % YC
            assert o + n <= YC
            S.dma(ybuf[l][k][m * 64:(m + 1) * 64, o:o + n], src, "st")

        emit_A(S, nc, pb, sfx, load_x, store_y, tile_limit=(int(os.environ["FUSED_TILES"]) if "FUSED_TILES" in os.environ else None))
        for k in range(NYC):
            S.all_gather(ygath[l][k].r, ybuf[l][k].r, GROUPS4)
        groups = [(g * 256, 256, 0) for g in range(8)]
        if l == 0:
            groups.append((2048, 128, 1))

        def x_src(g0, GN, col, xs, l=l):
            if l == 0:
                S.dma(xs[:, :, 0:GN], xB_d[:, :, g0:g0 + GN], "ldx")
            else:
                S.dma(xs[:, :, 0:GN], xown_v(g0 // 256), "ldx")

        def y_src(g0, GN, col, ys, tmp, l=l):
            if col == 0:
                n = GN
                srcs = [yv(l, 256 + q * 2048 + g0, GN) for q in range(4)]
            else:
                n = 64
                S.memset("dve", ys[:, :, 0:GN], 0.0)
                srcs = [yv(l, q * 64, 64) for q in range(4)]
            for q in range(4):
                S.dma(tmp[:, :, 0:n], srcs[q], "ldx")
                if q == 0:
                    S.ts("dve", ys[:, :, 0:n], tmp[:, :, 0:n], sel[:, 0:1], None, op0=ALU.mult)
                else:
                    S.stt("dve", ys[:, :, 0:n], tmp[:, :, 0:n], sel[:, q:q + 1], ys[:, :, 0:n], ALU.mult, ALU.add)

        def out_sink(g0, GN, col, xs, l=l):
            if l == 0:
                k = g0 // 256
                S.dma(xown_v(k), xs[:, :, 0:GN], "st")
                S.all_gather(xg[k].r, xown[k].r, GROUPS4)
            else:
                S.dma(out_d[:, :, g0:g0 + GN], xs[:, :, 0:GN], "st")

        if "FUSED_GROUPS" in os.environ:
            groups = groups[:int(os.environ["FUSED_GROUPS"])]
        emit_B(S, nc, pb, sfx, groups, x_src, y_src, out_sink)
    S.barrier()
    print("fused instructions", S.n_ins, "sems", S.nsem, "sbuf left", nc.sbuf_bytes_remaining)
    S.close()
    return nc


_FUSED = {}


def kernel_fused(**inputs):
    inp = {k: np.ascontiguousarray(np.asarray(v, dtype=np.float32)) for k, v in inputs.items()}
    x, ctx = inp["x"], inp["ctx"]
    if "nc" not in _FUSED:
        _FUSED["nc"] = build_fused()
    nc = _FUSED["nc"]
    shared_keys = ("ident", "iota", "trile", "trige", "rmask", "cmask", "rope")
    base = {}
    wA = {}
    for l in range(2):
        wb = prep_B_weights(inp, l, permute_wout=True)
        for k, v in wb.items():
            if k in shared_keys:
                base[k] = v
            else:
                base[k + f"_l{l}"] = v
        for j in range(4):
            wa = prep_A_weights(inp, l, j)
            d = {}
            for k, v in wa.items():
                if k in shared_keys:
                    base[k] = v
                elif k in ("adaw", "adab"):
                    pass
                else:
                    d[k + f"_l{l}"] = v
            wA[(l, j)] = d
    pad = np.zeros((64, 1024), np.float32)
    maps = []
    for c in range(8):
        b, r = c // 4, c % 4
        d = dict(base)
        d.update(wA[(0, r)])
        d.update(wA[(1, r)])
        d["cv"] = cvec(inp, b)
        d["xT"] = fm(np.concatenate([ctx[b], x[b]], 0))
        d["xB"] = fm(np.concatenate([x[b, r * 2048:(r + 1) * 2048], ctx[b, r * 64:(r + 1) * 64], pad], 0))
        s = np.zeros((128, 4), np.float32)
        s[:, r] = 1.0
        d["sel"] = s
        maps.append(d)
    res = run_bass_kernel_spmd(nc, maps, core_ids=list(range(8)))
    out = np.zeros_like(x)
    for c in range(8):
        b, r = c // 4, c % 4
        out[b, r * 2048:(r + 1) * 2048] = unfm(res.results[c]["outT"])
    return out


def kernel(**inputs):
    return kernel_fused(**inputs)
```

```python
from concourse.bass_utils import run_bass_kernel_spmd
import contextlib
import numpy as np
import concourse.bass as bass
import concourse.mybir as mybir

F32 = mybir.dt.float32
BF16 = mybir.dt.bfloat16
U32 = mybir.dt.uint32
AF = mybir.ActivationFunctionType
ALU = mybir.AluOpType
AX = mybir.AxisListType


class Tile:
    def __init__(self, ap, name=""):
        self.ap = ap
        self.name = name
        self.writer = None
        self.readers = {}
        self.exclusive = False

    def __getitem__(self, key):
        return Ref(self, self.ap[key])

    @property
    def r(self):
        return Ref(self, self.ap)


class Ref:
    def __init__(self, tile, ap):
        self.tile = tile
        self.ap = ap

    def __getitem__(self, key):
        return Ref(self.tile, self.ap[key])

    def re(self, s, **kw):
        return Ref(self.tile, self.ap.rearrange(s, **kw))

    def bc(self, shape):
        return Ref(self.tile, self.ap.to_broadcast(shape))

    def with_ap(self, ap):
        return Ref(self.tile, ap)


def _ap(x):
    return x.ap if isinstance(x, Ref) else x


class Sched:
    COMPUTE_ROT = 30000
    DMA_ROT = 1900

    def __init__(self, nc):
        self.nc = nc
        self.es = contextlib.ExitStack()
        self.eng = {"pe": nc.tensor, "dve": nc.vector, "act": nc.scalar,
                    "pool": nc.gpsimd, "sp": nc.sync}
        self.sem = {}
        self.cnt = {}
        self.epoch = {}
        self.waited = {}
        self.nsem = 0
        self.n_ins = 0
        self.allsems = {}
        self.dram_cache = {}
        self.dma_keys = set()
        self.scopes = []

    def shared_dram(self, name, shape, dt=F32):
        if name not in self.dram_cache:
            t = self.nc.dram_tensor(name, list(shape), dt, kind="ExternalInput")
            self.dram_cache[name] = Tile(t.ap(), name)
        return self.dram_cache[name]

    def push_scope(self):
        self.scopes.append(contextlib.ExitStack())

    def pop_scope(self):
        print("scope end: sbuf left", self.nc.sbuf_bytes_remaining)
        self.barrier()
        self.scopes.pop().close()

    def barrier(self):
        for e in ("pe", "dve", "act", "pool", "sp"):
            self.wait_all(e)

    def sbuf(self, name, shape, dtype=F32):
        es = self.scopes[-1] if getattr(self, "scopes", None) else self.es
        self.nalloc = getattr(self, "nalloc", 0) + 1
        return es.enter_context(self.nc.sbuf_tensor(f"sb{self.nalloc}_" + name, list(shape), dtype))

    def psum(self, name, shape, dtype=F32):
        return self.es.enter_context(self.nc.psum_tensor("ps_" + name, list(shape), dtype))

    def tile(self, name, shape, dtype=F32):
        t = self.sbuf(name, shape, dtype)
        return Tile(t[tuple(slice(None) for _ in shape)], name)

    def ptile(self, name, shape, dtype=F32):
        t = self.psum(name, shape, dtype)
        tl = Tile(t[tuple(slice(None) for _ in shape)], name)
        tl.exclusive = True
        return tl

    def _stream(self, stream, is_dma):
        if stream not in self.cnt:
            self.epoch[stream] = 0
            self._newsem(stream)
        key, c = self.cnt[stream]
        lim = self.DMA_ROT if is_dma else self.COMPUTE_ROT
        if c >= lim:
            self.epoch[stream] += 1
            self._newsem(stream)
        return self.cnt[stream]

    def _newsem(self, stream):
        key = f"{stream}_{self.epoch[stream]}"
        h = self.es.enter_context(self.nc.semaphore(f"s_{key}"))
        self.sem[key] = h
        self.cnt[stream] = (key, 0)
        self.nsem += 1

    def emit(self, engine, fn, outs=(), ins=(), dma_group=None, inc_override=None):
        is_dma = dma_group is not None
        stream = dma_group if is_dma else engine
        key, c = self._stream(stream, is_dma)
        deps = {}

        def add(d):
            if d is None:
                return
            k, v = d
            if deps.get(k, 0) < v:
                deps[k] = v

        outs = list(outs) + [r for r in ins if isinstance(r, Ref) and r.tile.exclusive]
        for r in ins:
            if isinstance(r, Ref):
                add(r.tile.writer)
        for o in outs:
            if isinstance(o, Ref):
                add(o.tile.writer)
                for k, v in o.tile.readers.items():
                    add((k, v))
        e = self.eng[engine]
        for k, v in list(deps.items()):
            if k in self.dma_keys:
                v = max(v, self.allsems.get(k, v))
                deps[k] = v
        for k, v in deps.items():
            if engine == "pe" and not is_dma and k.rsplit("_", 1)[0] == "pe":
                continue
            if self.waited.get((engine, k), 0) >= v:
                continue
            e.wait_ge(self.sem[k], v)
            self.waited[(engine, k)] = v
        ins_obj = fn()
        inc = 16 if is_dma else 1
        if inc_override is not None:
            inc = inc_override
        c += inc
        self.cnt[stream] = (key, c)
        self.allsems[key] = c
        if is_dma:
            self.dma_keys.add(key)
        ins_obj.then_inc(self.sem[key], inc)
        self.n_ins += 1
        me = (key, c)
        for o in outs:
            if isinstance(o, Ref):
                o.tile.writer = me
                o.tile.readers = {}
        for r in ins:
            if isinstance(r, Ref):
                if not any(r.tile is o.tile for o in outs if isinstance(o, Ref)):
                    if r.tile.readers.get(key, 0) < c:
                        r.tile.readers[key] = c
        return ins_obj

    def wait_all(self, engine="sp"):
        e = self.eng[engine]
        for key, c in list(self.allsems.items()):
            if c > 0 and self.waited.get((engine, key), 0) < c:
                e.wait_ge(self.sem[key], c)
                self.waited[(engine, key)] = c

    def close(self):
        self.es.close()

    def all_gather(self, out, in_, groups):
        return self.emit("pool", lambda: self.nc.gpsimd.collective_compute(
            "AllGather", ALU.bypass, replica_groups=groups, ins=[_ap(in_).opt()], outs=[_ap(out).opt()]),
            outs=[out], ins=[in_], dma_group=f"cc{self._ncc()}", inc_override=1)

    def _ncc(self):
        self.ncc = getattr(self, "ncc", 0) + 1
        return self.ncc

    def dma(self, out, in_, group="ld0", q="sp"):
        return self.emit(q, lambda: self.eng[q].dma_start(out=_ap(out), in_=_ap(in_)),
                         outs=[out], ins=[in_], dma_group=group)

    def mm(self, out, lhsT, rhs, start=True, stop=True, skip=False):
        return self.emit("pe", lambda: self.nc.tensor.matmul(_ap(out), lhsT=_ap(lhsT), rhs=_ap(rhs),
                                                              start=start, stop=stop, skip_group_check=skip),
                         outs=[out], ins=[lhsT, rhs] + ([] if start else [out]))

    def tr(self, out, in_, ident):
        return self.emit("pe", lambda: self.nc.tensor.transpose(_ap(out), _ap(in_), _ap(ident)),
                         outs=[out], ins=[in_, ident])

    def act(self, out, in_, func, bias=None, scale=None, accum_out=None):
        kw = {}
        ins = [in_]
        outs = [out]
        if bias is not None:
            kw["bias"] = _ap(bias)
            ins.append(bias)
        if scale is not None:
            kw["scale"] = _ap(scale)
            ins.append(scale)
        if accum_out is not None:
            kw["accum_out"] = _ap(accum_out)
            outs.append(accum_out)
        return self.emit("act", lambda: self.nc.scalar.activation(out=_ap(out), in_=_ap(in_), func=func, **kw),
                         outs=outs, ins=ins)

    def tt(self, eng, out, in0, in1, op):
        return self.emit(eng, lambda: self.eng[eng].tensor_tensor(out=_ap(out), in0=_ap(in0), in1=_ap(in1), op=op),
                         outs=[out], ins=[in0, in1])

    def ts(self, eng, out, in0, s1, s2=None, op0=ALU.mult, op1=None, accum_out=None):
        kw = {}
        outs = [out]
        if op1 is not None:
            kw["op1"] = op1
        if accum_out is not None:
            kw["accum_out"] = _ap(accum_out)
            outs.append(accum_out)
        return self.emit(eng, lambda: self.eng[eng].tensor_scalar(out=_ap(out), in0=_ap(in0), scalar1=_ap(s1),
                                                                  scalar2=_ap(s2), op0=op0, **kw),
                         outs=outs, ins=[in0, s1, s2])

    def stt(self, eng, out, in0, scalar, in1, op0, op1):
        return self.emit(eng, lambda: self.eng[eng].scalar_tensor_tensor(out=_ap(out), in0=_ap(in0), scalar=_ap(scalar),
                                                                         in1=_ap(in1), op0=op0, op1=op1),
                         outs=[out], ins=[in0, scalar, in1])

    def copy(self, eng, out, in_):
        if eng == "act":
            return self.emit("act", lambda: self.nc.scalar.copy(out=_ap(out), in_=_ap(in_)), outs=[out], ins=[in_])
        return self.emit(eng, lambda: self.eng[eng].tensor_copy(out=_ap(out), in_=_ap(in_)), outs=[out], ins=[in_])

    def memset(self, eng, out, val):
        return self.emit(eng, lambda: self.eng[eng].memset(_ap(out), val), outs=[out], ins=[])

    def reduce(self, eng, out, in_, op, axis=AX.X):
        return self.emit(eng, lambda: self.eng[eng].tensor_reduce(out=_ap(out), in_=_ap(in_), axis=axis, op=op),
                         outs=[out], ins=[in_])

    def recip(self, out, in_):
        return self.emit("dve", lambda: self.nc.vector.reciprocal(out=_ap(out), in_=_ap(in_)), outs=[out], ins=[in_])

    def vmax(self, out, in_):
        return self.emit("dve", lambda: self.nc.vector.max(out=_ap(out), in_=_ap(in_)), outs=[out], ins=[in_])

    def vmax_index(self, out, in_max, in_values):
        return self.emit("dve", lambda: self.nc.vector.max_index(out=_ap(out), in_max=_ap(in_max), in_values=_ap(in_values)),
                         outs=[out], ins=[in_max, in_values])

    def vmatch_replace(self, out, in_to_replace, in_values, imm):
        return self.emit("dve", lambda: self.nc.vector.match_replace(out=_ap(out), in_to_replace=_ap(in_to_replace),
                                                                    in_values=_ap(in_values), imm_value=imm),
                         outs=[out], ins=[in_to_replace, in_values])

    def scan(self, out, data0, data1, initial, op0, op1):
        return self.emit("dve", lambda: self.nc.vector.tensor_tensor_scan(out=_ap(out), data0=_ap(data0), data1=_ap(data1),
                                                                          initial=_ap(initial), op0=op0, op1=op1),
                         outs=[out], ins=[data0, data1, initial])
import os

EPS = 1e-6
import math
RWKV_DECAY_SCALE = math.exp(-0.5)
TT = 8448
NTILE = 33
N = 256


def emit_A(S, nc, pb, sfx="", load_x=None, store_y=None, stage=None, dbg_d=None,
           mixers=("conv", "attn", "mlstm", "rwkv"), tile_limit=None):
    def D(name, shape, kind="ExternalInput", dt=F32):
        t = nc.dram_tensor(name + sfx, list(shape), dt, kind=kind)
        return Tile(t.ap(), name + sfx)

    cv_d = S.shared_dram("cv", [128, 8, 2])
    adaw_d = S.shared_dram("adaw" + sfx, [6, 2, 128, 8, 512])
    adab_d = S.shared_dram("adab" + sfx, [128, 48])
    n1g_d = D("n1g", [128, 8])
    Wc_d = D("Wc", [128, 8, 192])
    Wr_d = D("Wr", [128, 8, 256])
    Wa_d = D("Wa", [128, 8, 192])
    Wm_d = D("Wm", [128, 8, 260])
    pp_d = D("pp", [64, 16])
    w2_d = D("w2p", [16, 2, 64])
    a2_d = D("a2p", [16, 2, 64])
    g2_d = D("g2p", [32, 64])
    rowbc_d = D("rowbc", [128, 4, 64])
    scal_d = D("scal", [128, 8])
    ident_d = S.shared_dram("ident", [128, 128])
    trile_d = S.shared_dram("trile", [128, 128])
    trige_d = S.shared_dram("trige", [128, 128])
    rmask_d = S.shared_dram("rmask", [2, 64, 192])
    cmask_d = S.shared_dram("cmask", [64, 256])
    rope_d = S.shared_dram("rope", [64, 128, 64])

    class Done(Exception):
        pass

    dbg_n = [0]

    def chk(name, *refs):
        if stage != name:
            return
        o = 0
        for r in refs:
            n = 1
            for s_ in r.ap.shape[1:]:
                n *= s_
            P = r.ap.shape[0]
            t = S.tile(f"dbgt{dbg_n[0]}", [128, n])
            dbg_n[0] += 1
            tv = t[0:P, :]
            shp = r.ap.shape
            if len(shp) == 3:
                tv = tv.re("p (a b) -> p a b", a=shp[1])
            elif len(shp) == 4:
                tv = tv.re("p (a b c) -> p a b c", a=shp[1], b=shp[2])
            S.copy("dve", tv, r)
            S.dma(dbg_d[0:P, o:o + n], t[0:P, :], "st")
            o += n
        raise Done()

    S.push_scope()
    ident = S.tile("ident", [128, 128])
    ones = S.tile("ones", [128, 128])
    cv = S.tile("cv", [128, 8, 2])
    adab = S.tile("adab", [128, 48])
    n1g = S.tile("n1g", [128, 8])
    mod0 = S.tile("mod0", [128, 8, 2])
    mod1 = S.tile("mod1", [128, 8, 2])
    gm1 = S.tile("gm1", [128, 8, 2])
    pp = S.tile("pp", [64, 16])
    omka = S.tile("omka", [64, 1])
    rowbc = S.tile("rowbc", [128, 4, 64])
    scal = S.tile("scal", [128, 8])
    xs = S.tile("xs", [128, 8, N])
    hT = S.tile("hT", [128, 8, N])
    rstd = S.tile("rstd", [128, N])

    def body():
        S.dma(ident.r, ident_d.r, "ldc")
        S.dma(cv.r, cv_d.r, "ldc")
        S.dma(adab.r, adab_d.r, "ldc")
        S.dma(n1g.r, n1g_d.r, "ldc")
        S.dma(pp.r, pp_d.r, "ldc")
        S.dma(rowbc.r, rowbc_d.r, "ldc")
        S.dma(scal.r, scal_d.r, "ldc")
        S.memset("dve", ones.r, 1.0)
        S.act(cv.r, cv.r, AF.Silu)
        S.ts("dve", omka.r, pp[:, 9:10], -1.0, 1.0, op0=ALU.mult, op1=ALU.add)
        scr = S.tile("scr", [128, 4096])
        adaw_t = scr.r.re("p (c f) -> p c f", c=8)
        for seg, dst in ((0, mod0), (1, mod1)):
            for half in range(2):
                S.dma(adaw_t, adaw_d[seg, half], "ldc")
                for f4 in range(4):
                    fc = half * 4 + f4
                    for dc in range(8):
                        S.mm(pb[0][:, fc * 2:fc * 2 + 2], adaw_t[:, dc, f4 * 128:(f4 + 1) * 128], cv[:, dc, :],
                             start=(dc == 0), stop=(dc == 7))
            S.tt("dve", dst.r, pb[0][:, 0:16].re("p (c t) -> p c t", t=2),
                 adab.r.with_ap(adab.ap[:, seg * 8:(seg + 1) * 8].unsqueeze(2).to_broadcast([128, 8, 2])), ALU.add)
        S.ts("dve", gm1.r, mod1.r, 1.0, None, op0=ALU.add)
        S.tt("dve", gm1.r, gm1.r, n1g.r.with_ap(n1g.ap.unsqueeze(2).to_broadcast([128, 8, 2])), ALU.mult)
        sh1 = mod0
        chk("mod", mod0.r, gm1.r)

        def load_h(ti):
            t0 = ti * N
            col = 1 if ti == 0 else 0
            load_x(ti, xs)
            S.act(hT.r, xs.r, AF.Square)
            for kc in range(8):
                S.mm(pb[0][:, 0:N], ones.r, hT[:, kc, :], start=(kc == 0), stop=(kc == 7))
            S.ts("dve", rstd.r, pb[0][:, 0:N], 1.0 / 1024.0, EPS, op0=ALU.mult, op1=ALU.add)
            S.act(rstd.r, rstd.r, AF.Sqrt)
            S.recip(rstd.r, rstd.r)
            S.tt("dve", hT.r, xs.r, rstd.r.with_ap(rstd.ap.unsqueeze(1).to_broadcast([128, 8, N])), ALU.mult)
            for kc in range(8):
                S.ts("pool" if kc % 2 else "dve", hT[:, kc, :], hT[:, kc, :], gm1[:, kc, col:col + 1], sh1[:, kc, col:col + 1],
                     op0=ALU.mult, op1=ALU.add)

        tiles = list(range(NTILE)) if tile_limit is None else list(range(tile_limit))

        def head_norm_fm(y, sq, out, g_col, psb, n=N):
            S.act(sq, y, AF.Square)
            S.mm(psb[0:64, 0:n], ones[0:64, 0:64], sq)
            S.ts("dve", sq, psb[0:64, 0:n], 1.0 / 64.0, EPS, op0=ALU.mult, op1=ALU.add)
            S.act(sq, sq, AF.Sqrt)
            S.recip(sq, sq)
            S.stt("dve", out, y, g_col, sq, ALU.mult, ALU.mult)

        if "conv" in mixers:
            S.push_scope()
            Wc = S.tile("Wc", [128, 8, 192])
            S.dma(Wc.r, Wc_d.r, "ldc")
            U = S.tile("convU", [64, TT + 4])
            Bg = S.tile("convB", [64, TT])
            S.memset("dve", U.r, 0.0)

            def ucol(t):
                return t + 1 if t < 256 else t + 3

            for ti in tiles:
                load_h(ti)
                t0 = ti * N
                for g in range(3):
                    for kc in range(8):
                        S.mm(pb[1 + g][0:64, 0:N], Wc[:, kc, g * 64:(g + 1) * 64], hT[:, kc, :], start=(kc == 0), stop=(kc == 7))
                S.copy("act", Bg[:, t0:t0 + N], pb[2][0:64, 0:N])
                S.copy("act", U[:, ucol(t0):ucol(t0) + N], pb[1][0:64, 0:N])
                S.tt("dve", U[:, ucol(t0):ucol(t0) + N], U[:, ucol(t0):ucol(t0) + N], pb[3][0:64, 0:N], ALU.mult)
            cy = S.tile("convy", [64, N])
            csq = S.tile("convsq", [64, N])
            for ti in tiles:
                t0 = ti * N
                u0 = ucol(t0)
                S.ts("dve", cy.r, U[:, u0 - 1:u0 - 1 + N], pp[:, 0:1], None, op0=ALU.mult)
                S.stt("dve", cy.r, U[:, u0:u0 + N], pp[:, 1:2], cy.r, ALU.mult, ALU.add)
                S.stt("dve", cy.r, U[:, u0 + 1:u0 + 1 + N], pp[:, 2:3], cy.r, ALU.mult, ALU.add)
                S.tt("dve", cy.r, cy.r, Bg[:, t0:t0 + N], ALU.mult)
                head_norm_fm(cy.r, csq.r, cy.r, pp[:, 3:4], pb[1])
                store_y(0, t0, N, cy.r)
            chk("conv", cy.r)
            S.pop_scope()

        if "attn" in mixers:
            S.push_scope()
            Wa = S.tile("Wa", [128, 8, 192])
            S.dma(Wa.r, Wa_d.r, "ldc")
            trile = S.tile("trile", [128, 128])
            trige = S.tile("trige", [128, 128])
            S.dma(trile.r, trile_d.r, "ldc")
            S.dma(trige.r, trige_d.r, "ldc")
            QT = S.tile("QT", [64, TT])
            KT = S.tile("KT", [64, TT])
            V1 = S.tile("V1", [128, 66, 65])
            S.memset("dve", V1.r, 1.0)
            rope = S.tile("ropet", [128, 64])
            qk = S.tile("qk", [128, 2, 64])
            qr = S.tile("qr", [128, 2, 64])
            tmpa = S.tile("tmpa", [128, 2, 2, 16])
            ssq = S.tile("ssq", [128, 2])
            junk = S.tile("junk", [128, 64])
            junk2 = S.tile("junk2", [128, 128])
            for ti in tiles:
                load_h(ti)
                for sub in range(2):
                    bi = ti * 2 + sub
                    t0 = bi * 128
                    for kc in range(8):
                        S.mm(pb[1][:, 0:192], hT[:, kc, sub * 128:(sub + 1) * 128], Wa[:, kc, :], start=(kc == 0), stop=(kc == 7))
                    S.copy("act", V1[:, bi, 0:64], pb[1][:, 128:192])
                    S.act(junk2.r, pb[1][:, 0:128], AF.Square)
                    S.reduce("dve", ssq.r, junk2.r.re("p (w f) -> p w f", w=2), ALU.add)
                    S.ts("dve", ssq.r, ssq.r, 1.0 / 64.0, EPS, op0=ALU.mult, op1=ALU.add)
                    S.act(ssq.r, ssq.r, AF.Sqrt)
                    S.recip(ssq.r, ssq.r)
                    for w in range(2):
                        S.stt("dve", qk[:, w, :], pb[1][:, w * 64:(w + 1) * 64], ssq[:, w:w + 1], rowbc[:, w, :], ALU.mult, ALU.mult)
                    src = qk
                    if bi >= 2:
                        S.dma(rope.r, rope_d[bi - 2], "ldr")
                        cosv = rope.r.with_ap(rope.ap.rearrange("p (h cs f) -> p h cs f", h=2, cs=2)[:, :, 0, :])
                        sinv = rope.r.with_ap(rope.ap.rearrange("p (h cs f) -> p h cs f", h=2, cs=2)[:, :, 1, :])
                        for w in range(2):
                            q4 = qk[:, w, :].re("p (h x f) -> p h x f", h=2, x=2)
                            o4 = qr[:, w, :].re("p (h x f) -> p h x f", h=2, x=2)
                            S.tt("dve", o4, q4, cosv.with_ap(cosv.ap.unsqueeze(2).to_broadcast([128, 2, 2, 16])), ALU.mult)
                            S.tt("pool", tmpa[:, :, 0, :], q4[:, :, 1, :], sinv, ALU.mult)
                            S.tt("pool", tmpa[:, :, 1, :], q4[:, :, 0, :], sinv, ALU.mult)
                            S.tt("dve", o4[:, :, 0, :], o4[:, :, 0, :], tmpa[:, :, 0, :], ALU.subtract)
                            S.tt("dve", o4[:, :, 1, :], o4[:, :, 1, :], tmpa[:, :, 1, :], ALU.add)
                        src = qr
                    S.tr(pb[2][0:64, 0:128], src[:, 0, :], ident.r)
                    S.copy("act", QT[:, t0:t0 + 128], pb[2][0:64, 0:128])
                    S.tr(pb[3][0:64, 0:128], src[:, 1, :], ident.r)
                    S.copy("act", KT[:, t0:t0 + 128], pb[3][0:64, 0:128])
            chk("attn_qk", QT[:, 0:512], KT[:, 0:512])
            nblk = len(tiles) * 2
            E = [S.tile(f"attE{i}", [128, 128]) for i in range(5)]
            esink = S.tile("esink", [128, 1])
            S.act(esink.r, scal[:, 0:1], AF.Exp)
            den = S.tile("attden", [128, 1])
            ao = S.tile("atto", [128, 64])
            for bi in range(nblk):
                t0 = bi * 128
                if bi < 2:
                    kbs = [(0, None), (1, None)]
                else:
                    kbs = [(0, None), (1, None)]
                    if bi - 1 >= 2:
                        kbs.append((bi - 1, trige))
                    kbs.append((bi, None))
                    if bi + 1 < nblk:
                        kbs.append((bi + 1, trile))
                for i, (kb, mask) in enumerate(kbs):
                    ps = pb[1 + (i % 2)]
                    S.mm(ps[:, 0:128], KT[:, kb * 128:(kb + 1) * 128], QT[:, t0:t0 + 128])
                    S.act(E[i].r, ps[:, 0:128], AF.Exp, scale=0.125)
                    if mask is not None:
                        S.tt("dve", E[i].r, E[i].r, mask.r, ALU.mult)
                chk("attn_E", E[0].r, E[1].r)
                for i, (kb, mask) in enumerate(kbs):
                    S.mm(pb[3][:, 0:65], E[i].r, V1[:, kb, :], start=(i == 0), stop=(i == len(kbs) - 1))
                chk("attn_pv", pb[3][:, 0:65])
                S.tt("dve", den.r, pb[3][:, 64:65], esink.r, ALU.add)
                S.recip(den.r, den.r)
                S.ts("dve", ao.r, pb[3][:, 0:64], den.r, None, op0=ALU.mult)
                S.act(junk.r, ao.r, AF.Square)
                S.reduce("dve", ssq[:, 0:1], junk.r, ALU.add)
                S.ts("dve", ssq[:, 0:1], ssq[:, 0:1], 1.0 / 64.0, EPS, op0=ALU.mult, op1=ALU.add)
                S.act(ssq[:, 0:1], ssq[:, 0:1], AF.Sqrt)
                S.recip(ssq[:, 0:1], ssq[:, 0:1])
                S.stt("dve", ao.r, ao.r, ssq[:, 0:1], rowbc[:, 2, :], ALU.mult, ALU.mult)
                chk("attn_ao", ao.r)
                S.tr(pb[4][0:64, 0:128], ao.r, ident.r)
                S.copy("act", qk[0:64, :, :].re("p a b -> p (a b)"), pb[4][0:64, 0:128])
                store_y(2, t0, 128, qk[0:64, :, :].re("p a b -> p (a b)"))
                if bi == int(os.environ.get("BLIM", "99")):
                    chk("attn_blk", ao.r)
            chk("attn", ao.r)
            S.pop_scope()

        if "mlstm" in mixers:
            S.push_scope()
            Wm = S.tile("Wm", [128, 8, 260])
            S.dma(Wm.r, Wm_d.r, "ldc")
            trile = S.tile("trile", [128, 128])
            trige = S.tile("trige", [128, 128])
            S.dma(trile.r, trile_d.r, "ldc")
            S.dma(trige.r, trige_d.r, "ldc")
            nblk = len(tiles) * 2
            Qm = S.tile("Qm", [128, 66, 64])
            Km = S.tile("Km", [128, 66, 64])
            Vm1 = S.tile("Vm1", [128, 66, 65])
            Om = S.tile("Om", [128, 66, 64])
            Hs = S.tile("Hs", [128, 66, 64])
            G = S.tile("G", [128, 66, 4])
            nfb = S.tile("nfb", [128, 2])
            S.memset("dve", Vm1.r, 1.0)
            S.ts("dve", nfb[:, 0:1], scal[:, 2:3], -1.0, None, op0=ALU.mult)
            S.ts("dve", nfb[:, 1:2], scal[:, 4:5], -1.0, None, op0=ALU.mult)
            for ti in tiles:
                load_h(ti)
                for sub in range(2):
                    bi = ti * 2 + sub
                    for kc in range(8):
                        S.mm(pb[1][:, 0:260], hT[:, kc, sub * 128:(sub + 1) * 128], Wm[:, kc, :], start=(kc == 0), stop=(kc == 7))
                    S.copy("act", Qm[:, bi, :], pb[1][:, 0:64])
                    chk("ml_a", Qm[:, 0, :])
                    S.ts("dve", Km[:, bi, :], pb[1][:, 64:128], 0.125, None, op0=ALU.mult)
                    S.copy("dve", Vm1[:, bi, 0:64], pb[1][:, 128:192])
                    chk("ml_b", Km[:, 0, :])
                    S.act(Om[:, bi, :], pb[1][:, 192:256], AF.Sigmoid)
                    chk("ml_c", Om[:, 0, :])
                    for d in range(2):
                        S.ts("dve", G[:, bi, 2 * d:2 * d + 1], pb[1][:, 256 + 2 * d:257 + 2 * d], scal[:, 1 + 2 * d:2 + 2 * d], None, op0=ALU.add)
                        chk("ml_d", G[:, 0, :])
                        S.act(G[:, bi, 2 * d + 1:2 * d + 2], pb[1][:, 257 + 2 * d:258 + 2 * d], AF.Exp, bias=nfb[:, d:d + 1], scale=-1.0)
                        chk("ml_e", G[:, 0, :])
                        S.act(G[:, bi, 2 * d + 1:2 * d + 2], G[:, bi, 2 * d + 1:2 * d + 2], AF.Ln, bias=1.0)
                        chk("ml_f", G[:, 0, :])
                        S.ts("dve", G[:, bi, 2 * d + 1:2 * d + 2], G[:, bi, 2 * d + 1:2 * d + 2], -1.0, None, op0=ALU.mult)
            chk("ml_p1", Qm[:, 0, :], Km[:, 0, :], G[:, 0, :])
            C1T = S.tile("C1T", [64, 65])
            eb = S.tile("mleb", [128, 1])
            ek = S.tile("mlek", [128, 1])
            eL = S.tile("mleL", [64, 1])
            qt = S.tile("mlqt", [128, 64])
            kt = S.tile("mlkt", [128, 64])
            qtT = S.tile("mlqtT", [64, 128])
            ktT = S.tile("mlktT", [64, 128])
            STs = S.tile("mlST", [128, 128])
            rden = S.tile("mlrden", [128, 1])
            for d in range(2):
                tri = trile if d == 0 else trige
                order = list(range(nblk)) if d == 0 else [1, 0] + list(range(nblk - 1, 1, -1))
                S.memset("dve", C1T.r, 0.0)
                for bi in order:
                    lf = G[:, bi, 2 * d + 1:2 * d + 2]
                    S.mm(pb[2][:, 0:1], tri.r, lf)
                    S.mm(pb[2][0:64, 1:2], ones[:, 0:64], lf)
                    S.act(eb.r, pb[2][:, 0:1], AF.Exp)
                    S.tt("dve", ek.r, G[:, bi, 2 * d:2 * d + 1], pb[2][:, 0:1], ALU.subtract)
                    S.act(ek.r, ek.r, AF.Exp)
                    S.act(eL.r, pb[2][0:64, 1:2], AF.Exp)
                    S.ts("dve", qt.r, Qm[:, bi, :], eb.r, None, op0=ALU.mult)
                    S.ts("pool", kt.r, Km[:, bi, :], ek.r, None, op0=ALU.mult)
                    S.tr(pb[3][0:64, 0:128], qt.r, ident.r)
                    S.copy("act", qtT.r, pb[3][0:64, 0:128])
                    S.tr(pb[4][0:64, 0:128], kt.r, ident.r)
                    S.copy("dve", ktT.r, pb[4][0:64, 0:128])
                    S.mm(pb[5][:, 0:128], ktT.r, qtT.r)
                    S.tt("dve", STs.r, pb[5][:, 0:128], tri.r, ALU.mult)
                    S.mm(pb[6][:, 0:65], STs.r, Vm1[:, bi, :], start=True, stop=False)
                    S.mm(pb[6][:, 0:65], qtT.r, C1T.r, start=False, stop=True)
                    S.ts("dve", rden.r, pb[6][:, 64:65], -1.0, None, op0=ALU.mult)
                    S.tt("dve", rden.r, rden.r, pb[6][:, 64:65], ALU.max)
                    S.ts("dve", rden.r, rden.r, 1.0, None, op0=ALU.max)
                    S.recip(rden.r, rden.r)
                    if d == 0:
                        S.ts("dve", Hs[:, bi, :], pb[6][:, 0:64], rden.r, None, op0=ALU.mult)
                    else:
                        S.stt("dve", Hs[:, bi, :], pb[6][:, 0:64], rden.r, Hs[:, bi, :], ALU.mult, ALU.add)
                    S.mm(pb[7][0:64, 0:65], ident[0:64, 0:64], C1T.r, start=True, stop=False)
                    S.mm(pb[7][0:64, 0:65], kt.r, Vm1[:, bi, :], start=False, stop=True)
                    S.ts("dve", C1T.r, pb[7][0:64, 0:65], eL.r, None, op0=ALU.mult)
                if d == 0:
                    chk("ml_fwd", Hs[:, 0, :], Hs[:, 1, :], Hs[:, 2, :])
            mlsq = S.tile("mlsq", [128, 64])
            mlss = S.tile("mlss", [128, 1])
            mly = S.tile("mly", [128, 64])
            mlyT = S.tile("mlyT", [64, 128])
            for bi in range(nblk):
                S.act(mlsq.r, Hs[:, bi, :], AF.Square)
                S.reduce("dve", mlss.r, mlsq.r, ALU.add)
                S.ts("dve", mlss.r, mlss.r, 1.0 / 64.0, EPS, op0=ALU.mult, op1=ALU.add)
                S.act(mlss.r, mlss.r, AF.Sqrt)
                S.recip(mlss.r, mlss.r)
                S.stt("dve", mly.r, Hs[:, bi, :], mlss.r, rowbc[:, 3, :], ALU.mult, ALU.mult)
                S.tt("dve", mly.r, mly.r, Om[:, bi, :], ALU.mult)
                S.tr(pb[3][0:64, 0:128], mly.r, ident.r)
                S.copy("act", mlyT.r, pb[3][0:64, 0:128])
                store_y(3, bi * 128, 128, mlyT.r)
            chk("mlstm", mly.r)
            S.pop_scope()
        if "rwkv" in mixers:
            S.push_scope()
            Wr = S.tile("Wr", [128, 8, 256])
            S.dma(Wr.r, Wr_d.r, "ldc")
            w2p = S.tile("w2p", [16, 2, 64])
            a2p = S.tile("a2p", [16, 2, 64])
            g2p = S.tile("g2p", [32, 64])
            rmask = [S.tile(f"rmask{d}", [64, 192]) for d in range(2)]
            cmask = S.tile("cmask", [64, 256])
            S.dma(w2p.r, w2_d.r, "ldc")
            S.dma(a2p.r, a2_d.r, "ldc")
            S.dma(g2p.r, g2_d.r, "ldc")
            for d in range(2):
                S.dma(rmask[d].r, rmask_d[d], "ldc")
            S.dma(cmask.r, cmask_d.r, "ldc")
            RKbc = S.tile("RKbc", [64, 64])
            S.ts("dve", RKbc.r, ones[0:64, 0:64], pp[:, 11:12], None, op0=ALU.mult)
            Yst = S.tile("Yst", [64, TT])
            rT = S.tile("rw_r", [64, N])
            kT = S.tile("rw_k", [64, N])
            vT = S.tile("rw_v", [64, N])
            gT = S.tile("rw_g", [64, N])
            kkT = S.tile("rw_kk", [64, N])
            tw = S.tile("rw_tw", [16, N])
            xa = S.tile("rw_xa", [16, N])
            sg = S.tile("rw_sg", [32, N])
            lw = S.tile("rw_lw", [64, N])
            aT = [S.tile(f"rw_a{d}", [64, N]) for d in range(2)]
            kd = [S.tile(f"rw_kd{d}", [64, N]) for d in range(2)]
            cum = S.tile("rw_cum", [64, N])
            tmp = S.tile("rw_tmp", [64, N])
            Pin = S.tile("rw_Pin", [64, N])
            Pinv = S.tile("rw_Pinv", [64, N])
            Pex = S.tile("rw_Pex", [64, N])
            AR = S.tile("rw_AR", [64, 4, 2, 64])
            BK = S.tile("rw_BK", [64, 4, 2, 64])
            Vtok = S.tile("rw_Vtok", [64, 4, 64])
            NM = [S.tile(f"rw_NM{i}", [64, 128]) for i in range(2)]
            Pw = [S.tile(f"rw_P{i}", [64, 64]) for i in range(2)]
            PwT = [S.tile(f"rw_PT{i}", [64, 64]) for i in range(2)]
            X = [S.tile(f"rw_X{i}", [64, 64]) for i in range(2)]
            Btok = S.tile("rw_Btok", [64, 64])
            Ktok = S.tile("rw_Ktok", [64, 64])
            ZT = S.tile("rw_ZT", [64, 64])
            UT = S.tile("rw_UT", [64, 64])
            S0T = S.tile("rw_S0T", [64, 64])
            sq = S.tile("rw_sq", [64, N])
            yo = S.tile("rw_yo", [64, N])
            i64 = ident[0:64, 0:64]
            o64 = ones[0:64, 0:64]

            def prep_tile(ti, d, both):
                load_h(ti)
                for g, dst in ((0, rT), (1, kT), (2, vT)):
                    for kc in range(8):
                        S.mm(pb[1][0:64, 0:N], Wr[:, kc, g * 64:(g + 1) * 64], hT[:, kc, :], start=(kc == 0), stop=(kc == 7))
                    S.copy("act", dst.r, pb[1][0:64, 0:N])
                for kc in range(8):
                    S.mm(pb[2][0:16, 0:N], Wr[:, kc, 192:208], hT[:, kc, :], start=(kc == 0), stop=(kc == 7))
                S.act(tw.r, pb[2][0:16, 0:N], AF.Tanh)
                for kc in range(8):
                    S.mm(pb[2][0:16, 0:N], Wr[:, kc, 208:224], hT[:, kc, :], start=(kc == 0), stop=(kc == 7))
                S.copy("act", xa.r, pb[2][0:16, 0:N])
                for kc in range(8):
                    S.mm(pb[2][0:32, 0:N], Wr[:, kc, 224:256], hT[:, kc, :], start=(kc == 0), stop=(kc == 7))
                S.act(sg.r, pb[2][0:32, 0:N], AF.Sigmoid)
                for c in range(4):
                    for kc in range(8):
                        S.mm(pb[3][0:64, c * 64:(c + 1) * 64], hT[:, kc, c * 64:(c + 1) * 64], Wr[:, kc, 128:192],
                             start=(kc == 0), stop=(kc == 7))
                S.copy("act", Vtok.r.re("p c v -> p (c v)"), pb[3][0:64, 0:256])
                dirs = (0, 1) if both else (d,)
                for dd in dirs:
                    S.mm(pb[1][0:64, 0:N], a2p[:, dd, :], xa.r)
                    S.act(aT[dd].r, pb[1][0:64, 0:N], AF.Sigmoid, bias=pp[:, 6 + dd:7 + dd])
                    S.ts("dve", kd[dd].r, aT[dd].r, pp[:, 9:10], omka.r, op0=ALU.mult, op1=ALU.add)
                    S.tt("dve", kd[dd].r, kd[dd].r, kT.r, ALU.mult)
                S.mm(pb[1][0:64, 0:N], w2p[:, d, :], tw.r)
                S.act(lw.r, pb[1][0:64, 0:N], AF.Sigmoid, bias=pp[:, 4 + d:5 + d])
                S.ts("dve", lw.r, lw.r, -RWKV_DECAY_SCALE, None, op0=ALU.mult)
                if both:
                    S.mm(pb[1][0:64, 0:N], g2p.r, sg.r)
                    S.copy("act", gT.r, pb[1][0:64, 0:N])
                S.ts("dve", kkT.r, kT.r, pp[:, 8:9], None, op0=ALU.mult)
                S.act(sq.r, kkT.r, AF.Square)
                S.mm(pb[1][0:64, 0:N], o64, sq.r)
                S.ts("dve", sq.r, pb[1][0:64, 0:N], EPS, None, op0=ALU.add)
                S.act(sq.r, sq.r, AF.Sqrt)
                S.recip(sq.r, sq.r)
                S.tt("dve", kkT.r, kkT.r, sq.r, ALU.mult)
                S.scan(cum.r, cmask.r, lw.r, 0.0, ALU.mult, ALU.add)
                if d == 1:
                    c3 = cum.ap.rearrange("p (c t) -> p c t", c=4)
                    S.tt("dve", tmp.r, lw.r, cum.r, ALU.subtract)
                    S.tt("dve", cum.r.re("p (c t) -> p c t", c=4), tmp.r.re("p (c t) -> p c t", c=4),
                         cum.r.with_ap(c3[:, :, 63:64].to_broadcast([64, 4, 64])), ALU.add)
                S.act(Pin.r, cum.r, AF.Exp)
                S.act(Pinv.r, cum.r, AF.Exp, scale=-1.0)
                S.tt("dve", tmp.r, cum.r, lw.r, ALU.subtract)
                S.act(Pex.r, tmp.r, AF.Exp)
                A_v = AR.r.re("p c two t -> p c (two t)")
                S.stt("dve", AR[:, :, 0, :], kkT.r.re("p (c t) -> p c t", c=4), -1.0, Pex.r.re("p (c t) -> p c t", c=4), ALU.mult, ALU.mult)
                S.tt("dve", AR[:, :, 1, :], rT.r.re("p (c t) -> p c t", c=4), Pin.r.re("p (c t) -> p c t", c=4), ALU.mult)
                S.tt("dve", tmp.r, kkT.r, aT[d].r, ALU.mult)
                S.tt("dve", BK[:, :, 0, :], tmp.r.re("p (c t) -> p c t", c=4), Pinv.r.re("p (c t) -> p c t", c=4), ALU.mult)
                S.tt("dve", BK[:, :, 1, :], kd[d].r.re("p (c t) -> p c t", c=4), Pinv.r.re("p (c t) -> p c t", c=4), ALU.mult)

            def chunk(ti, c, d):
                m = rmask[d]
                ARc = AR[:, c, :, :].re("p two t -> p (two t)")
                A_c, R_c = AR[:, c, 0, :], AR[:, c, 1, :]
                B_c, K_c = BK[:, c, 0, :], BK[:, c, 1, :]
                V_c = Vtok[:, c, :]
                S.mm(pb[1][0:64, 0:128], B_c, ARc)
                S.tt("dve", NM[0].r, pb[1][0:64, 0:128], m[:, 0:128], ALU.mult)
                S.mm(pb[2][0:64, 0:128], K_c, ARc)
                S.tt("dve", NM[1].r, pb[2][0:64, 0:128], m[:, 0:128], ALU.mult)
                S.mm(pb[3][0:64, 0:64], A_c, B_c)
                S.tt("dve", PwT[0].r, pb[3][0:64, 0:64], m[:, 128:192], ALU.mult)
                S.copy("act", Pw[0].r, NM[0][:, 0:64])
                S.tt("dve", X[0].r, NM[0][:, 0:64], i64, ALU.add)
                cur = 0
                for lev in range(5):
                    nxt = 1 - cur
                    S.mm(pb[1][0:64, 0:64], Pw[cur].r, PwT[cur].r)
                    S.copy("act", PwT[nxt].r, pb[1][0:64, 0:64])
                    if lev < 4:
                        S.mm(pb[2][0:64, 0:64], PwT[cur].r, Pw[cur].r)
                        S.copy("dve", Pw[nxt].r, pb[2][0:64, 0:64])
                    S.mm(pb[3][0:64, 0:64], PwT[nxt].r, X[cur].r)
                    S.tt("dve", X[nxt].r, pb[3][0:64, 0:64], X[cur].r, ALU.add)
                    cur = nxt
                Xf = X[cur]
                S.tr(pb[1][0:64, 0:64], B_c, i64)
                S.copy("act", Btok.r, pb[1][0:64, 0:64])
                S.tr(pb[2][0:64, 0:64], K_c, i64)
                S.copy("dve", Ktok.r, pb[2][0:64, 0:64])
                S.mm(pb[4][0:64, 0:64], A_c, S0T.r, start=True, stop=False)
                S.mm(pb[4][0:64, 0:64], NM[1][:, 0:64], V_c, start=False, stop=True)
                S.copy("act", ZT.r, pb[4][0:64, 0:64])
                S.mm(pb[5][0:64, 0:64], Xf.r, ZT.r)
                S.copy("act", UT.r, pb[5][0:64, 0:64])
                S.mm(pb[6][0:64, 0:64], S0T.r, R_c, start=True, stop=False)
                S.mm(pb[6][0:64, 0:64], UT.r, NM[0][:, 64:128], start=False, stop=False)
                S.mm(pb[6][0:64, 0:64], V_c, NM[1][:, 64:128], start=False, stop=True)
                t0 = ti * N + c * 64
                if d == 0:
                    S.copy("dve", Yst[:, t0:t0 + 64], pb[6][0:64, 0:64])
                else:
                    S.tt("dve", Yst[:, t0:t0 + 64], Yst[:, t0:t0 + 64], pb[6][0:64, 0:64], ALU.add)
                S.mm(pb[7][0:64, 0:64], i64, S0T.r, start=True, stop=False)
                S.mm(pb[7][0:64, 0:64], Btok.r, UT.r, start=False, stop=False)
                S.mm(pb[7][0:64, 0:64], Ktok.r, V_c, start=False, stop=True)
                pl = c * 64 + (63 if d == 0 else 0)
                S.ts("dve", S0T.r, pb[7][0:64, 0:64], Pin[:, pl:pl + 1], None, op0=ALU.mult)

            def finish_tile(ti):
                t0 = ti * N
                S.tt("dve", tmp.r, kd[0].r, kd[1].r, ALU.add)
                S.tt("dve", tmp.r, tmp.r, rT.r, ALU.mult)
                S.mm(pb[1][0:64, 0:N], RKbc.r, tmp.r)
                S.tt("dve", tmp.r, pb[1][0:64, 0:N], vT.r, ALU.mult)
                head_norm_fm(Yst[:, t0:t0 + N], sq.r, yo.r, pp[:, 12:13], pb[2])
                S.tt("dve", yo.r, yo.r, tmp.r, ALU.add)
                S.tt("dve", yo.r, yo.r, gT.r, ALU.mult)
                store_y(1, t0, N, yo.r)

            for d in range(2):
                order = tiles if d == 0 else [0] + tiles[:0:-1]
                S.memset("dve", S0T.r, 0.0)
                for ti in order:
                    prep_tile(ti, d, both=(d == 1))
                    chk("rw_prep", AR[:, 0, :, :], BK[:, 0, :, :], cum[:, 0:64], lw[:, 0:64])
                    for c in (range(4) if d == 0 else range(3, -1, -1)):
                        chunk(ti, c, d)
                        chk("rw_c0", Yst[:, 0:64], S0T.r, X[1].r, ZT.r, UT.r)
                    if d == 1:
                        finish_tile(ti)
                if d == 0:
                    chk("rw_fwd", Yst[:, 0:256])
            S.pop_scope()

    try:
        body()
    except Done:
        while len(S.scopes) > 1:
            S.scopes.pop().close()
        raise
    S.pop_scope()


class StageDone(Exception):
    pass


def build_A(stage=None, mixers=("conv", "attn", "mlstm", "rwkv"), tile_limit=None):
    nc = bass.Bass("TRN2", target_bir_lowering=False)
    S = Sched(nc)
    xT_d = Tile(nc.dram_tensor("xT", [128, 8, TT], F32, kind="ExternalInput").ap(), "xT")
    yT_d = Tile(nc.dram_tensor("yT", [4, 64, TT], F32, kind="ExternalOutput").ap(), "yT")
    dbg_d = Tile(nc.dram_tensor("dbg", [128, 4096], F32, kind="ExternalOutput").ap(), "dbg")
    pb = [S.ptile(f"pb{i}", [128, 512]) for i in range(8)]

    def load_x(ti, xs):
        S.dma(xs.r, xT_d[:, :, ti * N:(ti + 1) * N], "ldx")

    def store_y(m, t0, n, src):
        S.dma(yT_d[m, :, t0:t0 + n], src, "st")

    try:
        emit_A(S, nc, pb, "", load_x, store_y, stage, dbg_d, mixers, tile_limit)
    except Exception as e:
        if type(e).__name__ != "Done":
            raise
    S.wait_all("sp")
    while getattr(S, "scopes", None):
        S.scopes.pop().close()
    print("build_A instructions", S.n_ins, "sems", S.nsem, "sbuf left", nc.sbuf_bytes_remaining)
    S.close()
    return nc

EPS = 1e-6
POOLENG = os.environ.get("POOLENG", "pool")
NEG = -1.0e30


def emit_B(S, nc, pb, sfx, groups, x_src, y_src, out_sink, stage=None, dbg_d=None):
    def D(name, shape, kind="ExternalInput", dt=F32):
        t = nc.dram_tensor(name + sfx, list(shape), dt, kind=kind)
        return Tile(t.ap(), name + sfx)

    cv_d = S.shared_dram("cv", [128, 8, 2])
    adaw_d = S.shared_dram("adaw" + sfx, [6, 2, 128, 8, 512])
    adab_d = S.shared_dram("adab" + sfx, [128, 48])
    n2g_d = D("n2g", [128, 8])
    wout_d = D("wout", [8, 128, 8, 128])
    wq_d = D("wq", [16, 128, 8, 128])
    keys_d = D("keysT", [128, 16, 128])
    UT_d = D("UT", [128, 128, 8, 128])
    VJ_d = D("VJ", [128, 128, 1024])
    ident_d = S.shared_dram("ident", [128, 128])
    iota_d = S.shared_dram("iota", [128, 128])

    GM = max(g[1] for g in groups)
    S.push_scope()

    class Done(Exception):
        pass

    def chk(name, *refs):
        if stage != name:
            return
        o = 0
        for r in refs:
            n = 1
            for s_ in r.ap.shape[1:]:
                n *= s_
            t = S.tile(f"dbgt{o}", [128, n])
            S.copy("dve", t.r, r if len(r.ap.shape) == 2 else r)
            S.dma(dbg_d[0:r.ap.shape[0], o:o + n], t[0:r.ap.shape[0], :], "st")
            o += n
        raise Done()
    ident = S.tile("ident", [128, 128])
    iota = S.tile("iota", [128, 128])
    ones = S.tile("ones", [128, 128])
    cv = S.tile("cv", [128, 8, 2])
    adab = S.tile("adab", [128, 48])
    n2g = S.tile("n2g", [128, 8])
    keysT = S.tile("keysT", [128, 16, 128])
    mod = [S.tile(f"mod{i}", [128, 8, 2]) for i in range(6)]
    gm2 = S.tile("gm2", [128, 8, 2])
    scr = S.tile("scr", [128, 4096])
    xs = S.tile("xs", [128, 8, GM])
    ys = S.tile("ys", [128, 8, GM])
    x1 = S.tile("x1", [128, 8, GM])
    h2 = S.tile("h2", [128, 8, GM])
    rstd = S.tile("rstd", [128, GM])
    wbuf = [S.tile(f"wbuf{i}", [128, 8, 128]) for i in range(2)]
    ubuf = [S.tile(f"ubuf{i}", [128, 8, 128]) for i in range(2)]
    vbuf = [S.tile(f"vbuf{i}", [128, 1024]) for i in range(2)]
    sc = S.tile("sc", [128, 16, 128])
    sc2 = S.tile("sc2", [128, 16, 128])
    sv = S.tile("sv", [128, 16, 16])
    si = S.tile("si", [128, 16, 16], U32)
    sif = S.tile("sif", [128, 16, 16])
    cand = S.tile("cand", [128, 8, 256])
    tv = S.tile("tv", [128, 8, 16])
    ti = S.tile("ti", [128, 8, 16], U32)
    tiu = S.tile("tiu", [128, 8, 16], U32)
    aq = S.tile("aq", [128, 8, 16])
    bq = S.tile("bq", [128, 8, 16])
    If = S.tile("If", [128, 128])
    Jf = S.tile("Jf", [128, 128])
    Wf = S.tile("Wf", [128, 128])
    mx = S.tile("mx", [128, 8])
    zs = S.tile("zs", [128, 8])
    IT = S.tile("IT", [128, GM])
    JT = S.tile("JT", [128, GM])
    WT = S.tile("WT", [128, GM])
    oiw = [S.tile(f"oiw{i}", [128, 128], BF16) for i in range(2)]
    oj = [S.tile(f"oj{i}", [128, 128], BF16) for i in range(2)]
    iotab = S.tile("iotab", [128, 128], BF16)
    WW = S.tile("WW", [128, GM, 128], BF16)
    gj = [S.tile(f"gj{i}", [128, GM]) for i in range(2)]
    pjb = [S.tile(f"pjb{i}", [128, GM], BF16) for i in range(2)]
    ubf = [S.tile(f"ubf{i}", [128, 8, 128], BF16) for i in range(3)]
    vbf = [S.tile(f"vbf{i}", [128, 1024], BF16) for i in range(3)]
    acc = pb[0:4]
    pa = pb[4:6]
    pw = pb[6:8]

    def build_body():
        S.dma(ident.r, ident_d.r, "ldc")
        S.dma(iota.r, iota_d.r, "ldc")
        S.dma(cv.r, cv_d.r, "ldc")
        S.dma(adab.r, adab_d.r, "ldc")
        S.dma(n2g.r, n2g_d.r, "ldc")
        S.dma(keysT.r, keys_d.r, "ldc")
        S.memset("dve", ones.r, 1.0)
        S.copy("dve", iotab.r, iota.r)
        S.act(cv.r, cv.r, AF.Silu)
        adaw_t = scr[:, 0:4096].re("p (c f) -> p c f", c=8)
        for seg in (2, 3, 4, 5):
            for half in range(2):
                S.dma(adaw_t, adaw_d[seg, half], "ldc")
                for f4 in range(4):
                    fc = half * 4 + f4
                    for dc in range(8):
                        S.mm(pa[0][:, fc * 2:fc * 2 + 2], adaw_t[:, dc, f4 * 128:(f4 + 1) * 128], cv[:, dc, :],
                             start=(dc == 0), stop=(dc == 7))
            S.tt("dve", mod[seg].r, pa[0][:, 0:16].re("p (c t) -> p c t", t=2),
                 adab[:, seg * 8:(seg + 1) * 8].with_ap(adab.ap[:, seg * 8:(seg + 1) * 8].unsqueeze(2).to_broadcast([128, 8, 2])),
                 ALU.add)
        S.ts("dve", gm2.r, mod[4].r, 1.0, None, op0=ALU.add)
        S.tt("dve", gm2.r, gm2.r, n2g.r.with_ap(n2g.ap.unsqueeze(2).to_broadcast([128, 8, 2])), ALU.mult)
        gt1, sh2, gt2 = mod[2], mod[3], mod[5]
        chk("mod", mod[2].r, mod[3].r, mod[4].r, mod[5].r, gm2.r)

        wi = 0
        ui = 0
        for (g0, GN, col) in groups:
            NTL = GN // 128
            x_src(g0, GN, col, xs)
            y_src(g0, GN, col, ys, h2)
            for oc in range(8):
                wb = wbuf[wi % 2]
                S.dma(wb.r, wout_d[oc], f"ldw{wi % 2}")
                wi += 1
                p = pa[oc % 2]
                for kc in range(8):
                    S.mm(p[:, 0:GN], wb[:, kc, :], ys[:, kc, 0:GN], start=(kc == 0), stop=(kc == 7))
                S.stt("dve", x1[:, oc, 0:GN], p[:, 0:GN], gt1[:, oc, col:col + 1], xs[:, oc, 0:GN], ALU.mult, ALU.add)
            chk("x1", x1[:, :, 0:GN])
            S.act(ys[:, :, 0:GN], x1[:, :, 0:GN], AF.Square)
            for kc in range(8):
                S.mm(pa[0][:, 0:GN], ones.r, ys[:, kc, 0:GN], start=(kc == 0), stop=(kc == 7))
            S.ts("dve", rstd[:, 0:GN], pa[0][:, 0:GN], 1.0 / 1024.0, EPS, op0=ALU.mult, op1=ALU.add)
            S.act(rstd[:, 0:GN], rstd[:, 0:GN], AF.Sqrt)
            S.recip(rstd[:, 0:GN], rstd[:, 0:GN])
            for kc in range(8):
                S.tt("dve", h2[:, kc, 0:GN], x1[:, kc, 0:GN], rstd[:, 0:GN], ALU.mult)
                S.ts("dve", h2[:, kc, 0:GN], h2[:, kc, 0:GN], gm2[:, kc, col:col + 1], sh2[:, kc, col:col + 1],
                     op0=ALU.mult, op1=ALU.add)
            chk("h2", h2[:, :, 0:GN])
            qT = scr[:, 0:16 * GN].re("p (h t) -> p h t", h=16)
            for hp in range(16):
                wb = wbuf[wi % 2]
                S.dma(wb.r, wq_d[hp], f"ldw{wi % 2}")
                wi += 1
                p = pa[hp % 2]
                for kc in range(8):
                    S.mm(p[:, 0:GN], wb[:, kc, :], h2[:, kc, 0:GN], start=(kc == 0), stop=(kc == 7))
                S.copy("act", qT[:, hp, :], p[:, 0:GN])
            chk("qT", qT[:, :, 0:GN])
            for mt in range(NTL):
                ms = slice(mt * 128, (mt + 1) * 128)
                for hp in range(16):
                    S.mm(acc[hp // 4][:, (hp % 4) * 128:(hp % 4) * 128 + 128], qT[:, hp, ms], keysT[:, hp, :])
                for b4 in range(4):
                    S.copy("act" if b4 % 2 else "dve", sc[:, b4 * 4:(b4 + 1) * 4, :].re("p a k -> p (a k)"), acc[b4].r)
                chk("sc", sc.r)
                for hp in range(16):
                    S.vmax(sv[:, hp, 0:8], sc[:, hp, :])
                    S.vmax_index(si[:, hp, 0:8], sv[:, hp, 0:8], sc[:, hp, :])
                    S.vmatch_replace(sc2[:, hp, :], sv[:, hp, 0:8], sc[:, hp, :], NEG)
                    S.vmax(sv[:, hp, 8:16], sc2[:, hp, :])
                    S.vmax_index(si[:, hp, 8:16], sv[:, hp, 8:16], sc2[:, hp, :])
                S.copy("dve", sif.r, si.r)
                chk("top1", sv.r, sif.r)
                sv4 = sv.ap.rearrange("p (h two) a -> p h two a", two=2)
                sif4 = sif.ap.rearrange("p (h two) a -> p h two a", two=2)
                S.tt("dve", cand.r.re("p h (a b) -> p h a b", b=16),
                     sv.r.with_ap(sv4[:, :, 0, :].unsqueeze(3).to_broadcast([128, 8, 16, 16])),
                     sv.r.with_ap(sv4[:, :, 1, :].unsqueeze(2).to_broadcast([128, 8, 16, 16])), ALU.add)
                cand2 = sc2.r.re("p (h two) k -> p h (two k)", two=2)
                eq = sc.r.re("p (h two) (n a) -> p h (two n) a", two=2, a=16)
                for h in range(8):
                    S.vmax(tv[:, h, 0:8], cand[:, h, :])
                    S.vmax_index(ti[:, h, 0:8], tv[:, h, 0:8], cand[:, h, :])
                    S.vmatch_replace(cand2[:, h, :], tv[:, h, 0:8], cand[:, h, :], NEG)
                    S.vmax(tv[:, h, 8:16], cand2[:, h, :])
                    S.vmax_index(ti[:, h, 8:16], tv[:, h, 8:16], cand2[:, h, :])
                S.ts("dve", tiu.r, ti.r, 15, None, op0=ALU.bitwise_and)
                S.copy("dve", bq.r, tiu.r)
                S.ts("dve", tiu.r, ti.r, 4, None, op0=ALU.logical_shift_right)
                S.copy("dve", aq.r, tiu.r)
                iota16 = iota.r.with_ap(iota.ap[:, 0:16].unsqueeze(1).unsqueeze(1).to_broadcast([128, 8, 16, 16]))
                for (qv, half, dst) in ((aq, 0, If), (bq, 1, Jf)):
                    S.tt("dve", eq, qv.r.with_ap(qv.ap.unsqueeze(3).to_broadcast([128, 8, 16, 16])), iota16, ALU.is_equal)
                    S.tt("dve", eq, eq, sif.r.with_ap(sif4[:, :, half, :].unsqueeze(2).to_broadcast([128, 8, 16, 16])), ALU.mult)
                    S.reduce("dve", dst.r.re("p (h n) -> p h n", h=8), eq, ALU.add)
                chk("IJ", If.r, Jf.r, tv.r, aq.r, bq.r)
                S.reduce("dve", mx.r, tv.r, ALU.max)
                S.tt("dve", tv.r, tv.r, mx.r.with_ap(mx.ap.unsqueeze(2).to_broadcast([128, 8, 16])), ALU.subtract)
                S.act(tv.r, tv.r, AF.Exp)
                S.reduce("dve", zs.r, tv.r, ALU.add)
                S.recip(zs.r, zs.r)
                S.tt("dve", Wf.r.re("p (h n) -> p h n", h=8), tv.r,
                     zs.r.with_ap(zs.ap.unsqueeze(2).to_broadcast([128, 8, 16])), ALU.mult)
                for k, (src, dst) in enumerate(((If, IT), (Jf, JT), (Wf, WT))):
                    S.tr(pw[k % 2][:, 0:128], src.r, ident.r)
                    S.copy("act", dst[:, ms], pw[k % 2][:, 0:128])
            chk("ITW", IT[:, 0:GN], JT[:, 0:GN], WT[:, 0:GN])
            for m in range(int(os.environ.get('MLIM', GN))):
                a = oiw[m % 2]
                b = oj[m % 2]
                S.ts("dve", a.r, iotab.r, IT[:, m:m + 1], WT[:, m:m + 1], op0=ALU.is_equal, op1=ALU.mult)
                S.ts("dve", b.r, iotab.r, JT[:, m:m + 1], None, op0=ALU.is_equal)
                p = pw[m % 2]
                S.mm(p[:, 0:128], a.r, b.r)
                S.copy("act", WW[:, m, :], p[:, 0:128])
            chk("WW", WW[:, 0:16, :])
            h2b = ys.r.with_ap(ys.ap.bitcast(BF16))[:, :, 0:GN]
            S.copy("act", h2b, h2[:, :, 0:GN])
            NBF = 3

            def load_j(j):
                S.dma(ubuf[j % 2].r, UT_d[j], f"ldu{j % 2}")
                S.dma(vbuf[j % 2].r, VJ_d[j], f"ldv{j % 2}")

            def cast_j(j):
                S.copy("act", ubf[j % NBF].r, ubuf[j % 2].r)
                S.copy("dve", vbf[j % NBF].r, vbuf[j % 2].r)

            def a_mm(j):
                p = pb[4 + (j % 4)]
                for dc in range(8):
                    S.mm(p[:, 0:GN], ubf[j % NBF][:, dc, :], h2b[:, dc, :], start=(dc == 0), stop=(dc == 7))

            load_j(0)
            load_j(1)
            cast_j(0)
            load_j(2)
            a_mm(0)
            for j in range(128):
                if j + 1 < 128:
                    cast_j(j + 1)
                    if j + 3 < 128:
                        load_j(j + 3)
                    a_mm(j + 1)
                g = gj[j % 2]
                pp = pjb[j % 2]
                S.act(g[:, 0:GN], pb[4 + (j % 4)][:, 0:GN], AF.Gelu_apprx_tanh)
                S.tt("dve", pp[:, 0:GN], g[:, 0:GN], WW[:, 0:GN, j], ALU.mult)
                vb = vbf[j % NBF]
                for oc in range(8):
                    S.mm(acc[oc // 2][:, (oc % 2) * 256:(oc % 2) * 256 + GN], vb[:, oc * 128:(oc + 1) * 128], pp[:, 0:GN],
                         start=(j == 0 and oc % 2 == 0), stop=(j == 127), skip=True)
            for oc in range(8):
                S.stt("dve", xs[:, oc, 0:GN], acc[oc // 2][:, (oc % 2) * 256:(oc % 2) * 256 + GN], gt2[:, oc, col:col + 1],
                      x1[:, oc, 0:GN], ALU.mult, ALU.add)
            out_sink(g0, GN, col, xs)

    try:
        build_body()
    except Done:
        while len(S.scopes) > 1:
            S.scopes.pop().close()
        raise
    S.pop_scope()


def build_B(NT, groups, stage=None):
    nc = bass.Bass("TRN2", target_bir_lowering=False)
    S = Sched(nc)
    xT_d = Tile(nc.dram_tensor("xT", [128, 8, NT], F32, kind="ExternalInput").ap(), "xT")
    yT_d = Tile(nc.dram_tensor("yT", [128, 8, NT], F32, kind="ExternalInput").ap(), "yT")
    out_d = Tile(nc.dram_tensor("outT", [128, 8, NT], F32, kind="ExternalOutput").ap(), "outT")
    dbg_d = Tile(nc.dram_tensor("dbg", [128, 4096], F32, kind="ExternalOutput").ap(), "dbg")
    pb = [S.ptile(f"pb{i}", [128, 512]) for i in range(8)]

    def x_src(g0, GN, col, xs):
        S.dma(xs[:, :, 0:GN], xT_d[:, :, g0:g0 + GN], "ldx")

    def y_src(g0, GN, col, ys, tmp):
        S.dma(ys[:, :, 0:GN], yT_d[:, :, g0:g0 + GN], "ldx")

    def out_sink(g0, GN, col, xs):
        S.dma(out_d[:, :, g0:g0 + GN], xs[:, :, 0:GN], "st")

    try:
        emit_B(S, nc, pb, "", groups, x_src, y_src, out_sink, stage, dbg_d)
    except Exception as e:
        if type(e).__name__ != "Done":
            raise
    S.wait_all("sp")
    while getattr(S, "scopes", None):
        S.scopes.pop().close()
    print("build_B instructions", S.n_ins, "sems", S.nsem)
    S.close()
    return nc

def fm(X):
    NT = X.shape[0]
    return np.ascontiguousarray(X.T.reshape(8, 128, NT).transpose(1, 0, 2))

def unfm(XT):
    NT = XT.shape[2]
    return np.ascontiguousarray(XT.transpose(1, 0, 2).reshape(1024, NT).T)

def vec_fm(v):
    return np.ascontiguousarray(v.reshape(-1, 128).T)

def consts():
    ident = np.eye(128, dtype=np.float32)
    iota = np.tile(np.arange(128, dtype=np.float32)[None, :], (128, 1))
    return ident, iota

def prep_B_weights(inp, l, permute_wout=False):
    d = {}
    aw = inp["ada_w"][l]
    d["adaw"] = np.ascontiguousarray(aw.reshape(8, 128, 6, 2, 512).transpose(2, 3, 1, 0, 4))
    d["adab"] = vec_fm(inp["ada_b"][l])
    d["n2g"] = vec_fm(inp["norm2_g"][l])
    wo = inp["w_out"][l]
    if permute_wout:
        g = np.arange(1024)
        r_, m_, ch_ = g // 256, (g % 256) // 64, g % 64
        wo = wo[m_ * 256 + r_ * 64 + ch_, :]
    d["wout"] = np.ascontiguousarray(wo.reshape(8, 128, 8, 128).transpose(2, 1, 0, 3))
    wq = inp["peer_wq"][l]
    d["wq"] = np.ascontiguousarray(wq.reshape(8, 128, 16, 128).transpose(2, 1, 0, 3))
    ks = inp["peer_keys"][l]
    d["keysT"] = np.ascontiguousarray(ks.reshape(16, 128, 128).transpose(2, 0, 1))
    u = inp["peer_u"][l]
    d["UT"] = np.ascontiguousarray(u.reshape(128, 128, 8, 128).transpose(1, 3, 2, 0))
    v = inp["peer_v"][l]
    d["VJ"] = np.ascontiguousarray(v.reshape(128, 128, 1024).transpose(1, 0, 2))
    d["ident"], d["iota"] = consts()
    return d

def cvec(inp, b):
    return np.ascontiguousarray(np.stack([vec_fm(inp["c"][b]), vec_fm(inp["c_ctx"])], axis=-1))

OFF = {"hx": 0, "cB": 256, "cC": 512, "r": 768, "k": 1024, "v": 1280, "xw": 1536, "xa": 1552, "xg": 1568,
       "aq": 1600, "ak": 1856, "av": 1984, "mq": 2112, "mk": 2368, "mv": 2624, "mo": 2880, "mg": 3136}

def packW(W, cols):
    Wc = W[:, cols]
    return np.ascontiguousarray(Wc.reshape(8, 128, len(cols)).transpose(1, 0, 2))

def rope_table():
    quarter = 16
    inv = (10000.0 ** (-np.arange(quarter, dtype=np.float32) / quarter)).astype(np.float32)
    t = np.arange(8192)
    row = (t // 64).astype(np.float32); col = (t % 64).astype(np.float32)
    ar = row[:, None] * inv[None, :]; ac = col[:, None] * inv[None, :]
    tab = np.concatenate([np.cos(ar), np.sin(ar), np.cos(ac), np.sin(ac)], -1).astype(np.float32)
    return np.ascontiguousarray(tab.reshape(64, 128, 64))

def prep_A_weights(inp, l, j):
    d = {}
    aw = inp["ada_w"][l]
    d["adaw"] = np.ascontiguousarray(aw.reshape(8, 128, 6, 2, 512).transpose(2, 3, 1, 0, 4))
    d["adab"] = vec_fm(inp["ada_b"][l])
    d["n1g"] = vec_fm(inp["norm1_g"][l])
    W = inp["w_in"][l]
    h64 = np.arange(64) + j * 64
    kv64 = np.arange(64) + (j // 2) * 64
    d["Wc"] = packW(W, np.concatenate([OFF["hx"] + h64, OFF["cB"] + h64, OFF["cC"] + h64]))
    d["Wr"] = packW(W, np.concatenate([OFF["r"] + h64, OFF["k"] + h64, OFF["v"] + h64, OFF["xw"] + np.arange(16),
                                       OFF["xa"] + np.arange(16), OFF["xg"] + np.arange(32)]))
    d["Wa"] = packW(W, np.concatenate([OFF["aq"] + h64, OFF["ak"] + kv64, OFF["av"] + kv64]))
    gcols = np.array([OFF["mg"] + dd * 8 + g * 4 + j for dd in range(2) for g in range(2)])
    d["Wm"] = packW(W, np.concatenate([OFF["mq"] + h64, OFF["mk"] + h64, OFF["mv"] + h64, OFF["mo"] + h64, gcols]))
    pp = np.zeros((64, 16), np.float32)
    pp[:, 0:3] = inp["conv_w"][l][:, h64].T
    pp[:, 3] = inp["conv_g"][l][j]
    pp[:, 4:6] = inp["rwkv_w0"][l][:, h64].T
    pp[:, 6:8] = inp["rwkv_a0"][l][:, h64].T
    pp[:, 8] = inp["rwkv_kk"][l][h64]
    pp[:, 9] = inp["rwkv_ka"][l][h64]
    pp[:, 11] = inp["rwkv_rk"][l][j]
    pp[:, 12] = inp["rwkv_ln_g"][l][j]
    d["pp"] = pp
    d["w2p"] = np.ascontiguousarray(inp["rwkv_w2"][l][:, :, h64].transpose(1, 0, 2))
    d["a2p"] = np.ascontiguousarray(inp["rwkv_a2"][l][:, :, h64].transpose(1, 0, 2))
    d["g2p"] = np.ascontiguousarray(inp["rwkv_g2"][l][:, h64])
    rb = np.stack([inp["att_q_g"][l], inp["att_k_g"][l], inp["att_out_g"][l][j], inp["ml_out_g"][l][j]], 0)
    d["rowbc"] = np.ascontiguousarray(np.tile(rb[None], (128, 1, 1)))
    sc = np.zeros((8,), np.float32)
    sc[0] = inp["att_sink"][l][j]
    sc[1] = inp["ml_i_b"][l][0, j]; sc[2] = inp["ml_f_b"][l][0, j]
    sc[3] = inp["ml_i_b"][l][1, j]; sc[4] = inp["ml_f_b"][l][1, j]
    d["scal"] = np.ascontiguousarray(np.tile(sc[None], (128, 1)))
    ident, _ = consts()
    d["ident"] = ident
    i = np.arange(128)
    d["trile"] = (i[:, None] <= i[None, :]).astype(np.float32)
    d["trige"] = (i[:, None] >= i[None, :]).astype(np.float32)
    i = np.arange(64)
    su = (i[:, None] < i[None, :]).astype(np.float32); iu = (i[:, None] <= i[None, :]).astype(np.float32)
    sl = (i[:, None] > i[None, :]).astype(np.float32); il = (i[:, None] >= i[None, :]).astype(np.float32)
    d["rmask"] = np.ascontiguousarray(np.stack([np.concatenate([su, iu, sl], 1), np.concatenate([sl, il, su], 1)], 0))
    cm = np.ones((64, 256), np.float32); cm[:, ::64] = 0.0
    d["cmask"] = cm
    d["rope"] = rope_table()
    return d

_NC_CACHE = {}


def _get_nc(kind, *args):
    key = (kind,) + args
    if key not in _NC_CACHE:
        if kind == "A":
            _NC_CACHE[key] = build_A()
        else:
            _NC_CACHE[key] = build_B(*args)
    return _NC_CACHE[key]


def kernel_unfused(**inputs):
    inp = {k: np.ascontiguousarray(np.asarray(v, dtype=np.float32)) for k, v in inputs.items()}
    x = inp["x"].copy()
    ctx = inp["ctx"].copy()
    cores = list(range(8))
    for l in range(2):
        ncA = _get_nc("A")
        maps = []
        wA = [prep_A_weights(inp, l, j) for j in range(4)]
        xfull = [fm(np.concatenate([ctx[b], x[b]], 0)) for b in range(2)]
        cvs = [cvec(inp, b) for b in range(2)]
        for c in cores:
            b, j = c // 4, c % 4
            d = dict(wA[j])
            d["xT"] = xfull[b]
            d["cv"] = cvs[b]
            maps.append(d)
        res = run_bass_kernel_spmd(ncA, maps, core_ids=cores)
        ycat = np.zeros((2, TT, 1024), np.float32)
        for c in cores:
            b, j = c // 4, c % 4
            yT = res.results[c]["yT"]
            for m in range(4):
                ycat[b, :, m * 256 + j * 64:m * 256 + (j + 1) * 64] = yT[m].T
        del res, maps, xfull
        with_ctx = (l == 0)
        if with_ctx:
            NT = 2048 + 128
            groups = tuple((g * 256, 256, 0) for g in range(8)) + ((2048, 128, 1),)
        else:
            NT = 2048
            groups = tuple((g * 256, 256, 0) for g in range(8))
        ncB = _get_nc("B", NT, groups)
        wB = prep_B_weights(inp, l)
        maps = []
        for c in cores:
            b, q = c // 4, c % 4
            xs_ = x[b, q * 2048:(q + 1) * 2048]
            ys_ = ycat[b, 256 + q * 2048:256 + (q + 1) * 2048]
            if with_ctx:
                pad = np.zeros((64, 1024), np.float32)
                xs_ = np.concatenate([xs_, ctx[b, q * 64:(q + 1) * 64], pad], 0)
                ys_ = np.concatenate([ys_, ycat[b, q * 64:(q + 1) * 64], pad], 0)
            d = dict(wB)
            d["xT"] = fm(xs_)
            d["yT"] = fm(ys_)
            d["cv"] = cvs[b]
            maps.append(d)
        res = run_bass_kernel_spmd(ncB, maps, core_ids=cores)
        for c in cores:
            b, q = c // 4, c % 4
            o = unfm(res.results[c]["outT"])
            x[b, q * 2048:(q + 1) * 2048] = o[:2048]
            if with_ctx:
                ctx[b, q * 64:(q + 1) * 64] = o[2048:2048 + 64]
        del res, maps
    return x


NTB = 2176
GROUPS4 = [[0, 1, 2, 3], [4, 5, 6, 7]]


def build_fused():
    nc = bass.Bass("TRN2", target_bir_lowering=False)
    S = Sched(nc)
    pb = [S.ptile(f"pb{i}", [128, 512]) for i in range(8)]
    xT_d = Tile(nc.dram_tensor("xT", [128, 8, TT], F32, kind="ExternalInput").ap(), "xT")
    xB_d = Tile(nc.dram_tensor("xB", [128, 8, NTB], F32, kind="ExternalInput").ap(), "xB")
    sel_d = Tile(nc.dram_tensor("sel", [128, 4], F32, kind="ExternalInput").ap(), "sel")
    out_d = Tile(nc.dram_tensor("outT", [128, 8, 2048], F32, kind="ExternalOutput").ap(), "outT")
    YC = 768
    NYC = TT // YC
    ybuf = [[Tile(nc.dram_tensor(f"ybuf{l}_{k}", [256, YC], F32).ap(), f"ybuf{l}_{k}") for k in range(NYC)] for l in range(2)]
    ygath = [[Tile(nc.dram_tensor(f"ygath{l}_{k}", [1024, YC], F32).ap(), f"ygath{l}_{k}") for k in range(NYC)] for l in range(2)]
    xw = [256] * 8 + [128]
    xown = [Tile(nc.dram_tensor(f"xown{k}", [128, 8 * xw[k]], F32).ap(), f"xown{k}") for k in range(9)]
    xg = [Tile(nc.dram_tensor(f"xg{k}", [512, 8 * xw[k]], F32).ap(), f"xg{k}") for k in range(9)]
    sel = S.tile("sel", [128, 4])
    S.dma(sel.r, sel_d.r, "ldc")

    def xown_v(k):
        return xown[k].r.re("p (c t) -> p c t", c=8)

    def xg_v(k):
        return xg[k].r.re("(r p) (c t) -> r p c t", r=4, c=8)

    def yv(l, t0, n):
        k, o = t0 // YC, t0 % YC
        assert o + n <= YC
        return ygath[l][k].r.re("(kc p) t -> p kc t", p=128)[:, :, o:o + n]

    for l in range(2):
        sfx = f"_l{l}"
        def load_x(ti, xs, l=l):
            if l == 0:
                S.dma(xs.r, xT_d[:, :, ti * N:(ti + 1) * N], "ldx")
            elif ti == 0:
                for r in range(4):
                    S.dma(xs[:, :, r * 64:(r + 1) * 64], xg_v(8)[r, :, :, 0:64], "ldx")
            else:
                r, k = (ti - 1) // 8, (ti - 1) % 8
                S.dma(xs.r, xg_v(k)[r], "ldx")

        def store_y(m, t0, n, src, l=l):
            k, o = t0 // YC, t0 % YC
            assert o + n <= YC
            S.dma(ybuf[l][k][m * 64:(m + 1) * 64, o:o + n], src, "st")

        emit_A(S, nc, pb, sfx, load_x, store_y, tile_limit=(int(os.environ["FUSED_TILES"]) if "FUSED_TILES" in os.environ else None))
        for k in range(NYC):
            S.all_gather(ygath[l][k].r, ybuf[l][k].r, GROUPS4)
        groups = [(g * 256, 256, 0) for g in range(8)]
        if l == 0:
            groups.append((2048, 128, 1))

        def x_src(g0, GN, col, xs, l=l):
            if l == 0:
                S.dma(xs[:, :, 0:GN], xB_d[:, :, g0:g0 + GN], "ldx")
            else:
                S.dma(xs[:, :, 0:GN], xown_v(g0 // 256), "ldx")

        def y_src(g0, GN, col, ys, tmp, l=l):
            if col == 0:
                n = GN
                srcs = [yv(l, 256 + q * 2048 + g0, GN) for q in range(4)]
            else:
                n = 64
                S.memset("dve", ys[:, :, 0:GN], 0.0)
                srcs = [yv(l, q * 64, 64) for q in range(4)]
            for q in range(4):
                S.dma(tmp[:, :, 0:n], srcs[q], "ldx")
                if q == 0:
                    S.ts("dve", ys[:, :, 0:n], tmp[:, :, 0:n], sel[:, 0:1], None, op0=ALU.mult)
                else:
                    S.stt("dve", ys[:, :, 0:n], tmp[:, :, 0:n], sel[:, q:q + 1], ys[:, :, 0:n], ALU.mult, ALU.add)

        def out_sink(g0, GN, col, xs, l=l):
            if l == 0:
                k = g0 // 256
                S.dma(xown_v(k), xs[:, :, 0:GN], "st")
                S.all_gather(xg[k].r, xown[k].r, GROUPS4)
            else:
                S.dma(out_d[:, :, g0:g0 + GN], xs[:, :, 0:GN], "st")

        if "FUSED_GROUPS" in os.environ:
            groups = groups[:int(os.environ["FUSED_GROUPS"])]
        emit_B(S, nc, pb, sfx, groups, x_src, y_src, out_sink)
    S.barrier()
    print("fused instructions", S.n_ins, "sems", S.nsem, "sbuf left", nc.sbuf_bytes_remaining)
    S.close()
    return nc


_FUSED = {}


def kernel_fused(**inputs):
    inp = {k: np.ascontiguousarray(np.asarray(v, dtype=np.float32)) for k, v in inputs.items()}
    x, ctx = inp["x"], inp["ctx"]
    if "nc" not in _FUSED:
        _FUSED["nc"] = build_fused()
    nc = _FUSED["nc"]
    shared_keys = ("ident", "iota", "trile", "trige", "rmask", "cmask", "rope")
    base = {}
    wA = {}
    for l in range(2):
        wb = prep_B_weights(inp, l, permute_wout=True)
        for k, v in wb.items():
            if k in shared_keys:
                base[k] = v
            else:
                base[k + f"_l{l}"] = v
        for j in range(4):
            wa = prep_A_weights(inp, l, j)
            d = {}
            for k, v in wa.items():
                if k in shared_keys:
                    base[k] = v
                elif k in ("adaw", "adab"):
                    pass
                else:
                    d[k + f"_l{l}"] = v
            wA[(l, j)] = d
    pad = np.zeros((64, 1024), np.float32)
    maps = []
    for c in range(8):
        b, r = c // 4, c % 4
        d = dict(base)
        d.update(wA[(0, r)])
        d.update(wA[(1, r)])
        d["cv"] = cvec(inp, b)
        d["xT"] = fm(np.concatenate([ctx[b], x[b]], 0))
        d["xB"] = fm(np.concatenate([x[b, r * 2048:(r + 1) * 2048], ctx[b, r * 64:(r + 1) * 64], pad], 0))
        s = np.zeros((128, 4), np.float32)
        s[:, r] = 1.0
        d["sel"] = s
        maps.append(d)
    res = run_bass_kernel_spmd(nc, maps, core_ids=list(range(8)))
    out = np.zeros_like(x)
    for c in range(8):
        b, r = c // 4, c % 4
        out[b, r * 2048:(r + 1) * 2048] = unfm(res.results[c]["outT"])
    return out


def kernel(**inputs):
    return kernel_fused(**inputs)
```

```python
from concourse.bass_utils import run_bass_kernel_spmd
import contextlib
import numpy as np
import concourse.bass as bass
import concourse.mybir as mybir

F32 = mybir.dt.float32
BF16 = mybir.dt.bfloat16
U32 = mybir.dt.uint32
AF = mybir.ActivationFunctionType
ALU = mybir.AluOpType
AX = mybir.AxisListType


class Tile:
    def __init__(self, ap, name=""):
        self.ap = ap
        self.name = name
        self.writer = None
        self.readers = {}
        self.exclusive = False

    def __getitem__(self, key):
        return Ref(self, self.ap[key])

    @property
    def r(self):
        return Ref(self, self.ap)


class Ref:
    def __init__(self, tile, ap):
        self.tile = tile
        self.ap = ap

    def __getitem__(self, key):
        return Ref(self.tile, self.ap[key])

    def re(self, s, **kw):
        return Ref(self.tile, self.ap.rearrange(s, **kw))

    def bc(self, shape):
        return Ref(self.tile, self.ap.to_broadcast(shape))

    def with_ap(self, ap):
        return Ref(self.tile, ap)


def _ap(x):
    return x.ap if isinstance(x, Ref) else x


class Sched:
    COMPUTE_ROT = 30000
    DMA_ROT = 1900

    def __init__(self, nc):
        self.nc = nc
        self.es = contextlib.ExitStack()
        self.eng = {"pe": nc.tensor, "dve": nc.vector, "act": nc.scalar,
                    "pool": nc.gpsimd, "sp": nc.sync}
        self.sem = {}
        self.cnt = {}
        self.epoch = {}
        self.waited = {}
        self.nsem = 0
        self.n_ins = 0
        self.allsems = {}
        self.dram_cache = {}
        self.dma_keys = set()
        self.recording = None
        self.scopes = []

    def shared_dram(self, name, shape, dt=F32):
        if name not in self.dram_cache:
            t = self.nc.dram_tensor(name, list(shape), dt, kind="ExternalInput")
            self.dram_cache[name] = Tile(t.ap(), name)
        return self.dram_cache[name]

    def push_scope(self):
        self.scopes.append(contextlib.ExitStack())

    def pop_scope(self):
        print("scope end: sbuf left", self.nc.sbuf_bytes_remaining)
        self.barrier()
        self.scopes.pop().close()

    def barrier(self):
        for e in ("pe", "dve", "act", "pool", "sp"):
            self.wait_all(e)

    def sbuf(self, name, shape, dtype=F32):
        es = self.scopes[-1] if getattr(self, "scopes", None) else self.es
        self.nalloc = getattr(self, "nalloc", 0) + 1
        return es.enter_context(self.nc.sbuf_tensor(f"sb{self.nalloc}_" + name, list(shape), dtype))

    def psum(self, name, shape, dtype=F32):
        return self.es.enter_context(self.nc.psum_tensor("ps_" + name, list(shape), dtype))

    def tile(self, name, shape, dtype=F32):
        t = self.sbuf(name, shape, dtype)
        return Tile(t[tuple(slice(None) for _ in shape)], name)

    def ptile(self, name, shape, dtype=F32):
        t = self.psum(name, shape, dtype)
        tl = Tile(t[tuple(slice(None) for _ in shape)], name)
        tl.exclusive = True
        return tl

    def _stream(self, stream, is_dma):
        if stream not in self.cnt:
            self.epoch[stream] = 0
            self._newsem(stream)
        key, c = self.cnt[stream]
        lim = self.DMA_ROT if is_dma else self.COMPUTE_ROT
        if c >= lim:
            self.epoch[stream] += 1
            self._newsem(stream)
        return self.cnt[stream]

    def _newsem(self, stream):
        key = f"{stream}_{self.epoch[stream]}"
        h = self.es.enter_context(self.nc.semaphore(f"s_{key}"))
        self.sem[key] = h
        self.cnt[stream] = (key, 0)
        self.nsem += 1

    def emit(self, engine, fn, outs=(), ins=(), dma_group=None, inc_override=None):
        if self.recording is not None:
            self.recording.append((engine, fn, outs, ins, dma_group, inc_override))
            return None
        is_dma = dma_group is not None
        stream = dma_group if is_dma else engine
        key, c = self._stream(stream, is_dma)
        deps = {}

        def add(d):
            if d is None:
                return
            k, v = d
            if deps.get(k, 0) < v:
                deps[k] = v

        outs = list(outs) + [r for r in ins if isinstance(r, Ref) and r.tile.exclusive]
        for r in ins:
            if isinstance(r, Ref):
                add(r.tile.writer)
        for o in outs:
            if isinstance(o, Ref):
                add(o.tile.writer)
                for k, v in o.tile.readers.items():
                    add((k, v))
        e = self.eng[engine]
        for k, v in list(deps.items()):
            if k in self.dma_keys:
                v = max(v, self.allsems.get(k, v))
                deps[k] = v
        for k, v in deps.items():
            if engine == "pe" and not is_dma and k.rsplit("_", 1)[0] == "pe":
                continue
            if self.waited.get((engine, k), 0) >= v:
                continue
            e.wait_ge(self.sem[k], v)
            self.waited[(engine, k)] = v
        ins_obj = fn()
        inc = 16 if is_dma else 1
        if inc_override is not None:
            inc = inc_override
        c += inc
        self.cnt[stream] = (key, c)
        self.allsems[key] = c
        if is_dma:
            self.dma_keys.add(key)
        ins_obj.then_inc(self.sem[key], inc)
        self.n_ins += 1
        me = (key, c)
        for o in outs:
            if isinstance(o, Ref):
                o.tile.writer = me
                o.tile.readers = {}
        for r in ins:
            if isinstance(r, Ref):
                if not any(r.tile is o.tile for o in outs if isinstance(o, Ref)):
                    if r.tile.readers.get(key, 0) < c:
                        r.tile.readers[key] = c
        return ins_obj

    def wait_all(self, engine="sp"):
        e = self.eng[engine]
        for key, c in list(self.allsems.items()):
            if c > 0 and self.waited.get((engine, key), 0) < c:
                e.wait_ge(self.sem[key], c)
                self.waited[(engine, key)] = c

    def close(self):
        self.es.close()

    def record(self, fn):
        assert self.recording is None
        self.recording = []
        try:
            fn()
        finally:
            rec, self.recording = self.recording, None
        return rec

    def replay_interleaved(self, recs):
        idx = [0] * len(recs)
        live = True
        while live:
            live = False
            for i, r in enumerate(recs):
                if idx[i] < len(r):
                    self.emit(*r[idx[i]])
                    idx[i] += 1
                    live = True

    def all_gather(self, out, in_, groups):
        return self.emit("pool", lambda: self.nc.gpsimd.collective_compute(
            "AllGather", ALU.bypass, replica_groups=groups, ins=[_ap(in_).opt()], outs=[_ap(out).opt()]),
            outs=[out], ins=[in_], dma_group=f"cc{self._ncc()}", inc_override=1)

    def _ncc(self):
        self.ncc = getattr(self, "ncc", 0) + 1
        return self.ncc

    def dma(self, out, in_, group="ld0", q="sp"):
        return self.emit(q, lambda: self.eng[q].dma_start(out=_ap(out), in_=_ap(in_)),
                         outs=[out], ins=[in_], dma_group=group)

    def mm(self, out, lhsT, rhs, start=True, stop=True, skip=False):
        return self.emit("pe", lambda: self.nc.tensor.matmul(_ap(out), lhsT=_ap(lhsT), rhs=_ap(rhs),
                                                              start=start, stop=stop, skip_group_check=skip),
                         outs=[out], ins=[lhsT, rhs] + ([] if start else [out]))

    def tr(self, out, in_, ident):
        return self.emit("pe", lambda: self.nc.tensor.transpose(_ap(out), _ap(in_), _ap(ident)),
                         outs=[out], ins=[in_, ident])

    def act(self, out, in_, func, bias=None, scale=None, accum_out=None):
        kw = {}
        ins = [in_]
        outs = [out]
        if bias is not None:
            kw["bias"] = _ap(bias)
            ins.append(bias)
        if scale is not None:
            kw["scale"] = _ap(scale)
            ins.append(scale)
        if accum_out is not None:
            kw["accum_out"] = _ap(accum_out)
            outs.append(accum_out)
        return self.emit("act", lambda: self.nc.scalar.activation(out=_ap(out), in_=_ap(in_), func=func, **kw),
                         outs=outs, ins=ins)

    def tt(self, eng, out, in0, in1, op):
        return self.emit(eng, lambda: self.eng[eng].tensor_tensor(out=_ap(out), in0=_ap(in0), in1=_ap(in1), op=op),
                         outs=[out], ins=[in0, in1])

    def ts(self, eng, out, in0, s1, s2=None, op0=ALU.mult, op1=None, accum_out=None):
        kw = {}
        outs = [out]
        if op1 is not None:
            kw["op1"] = op1
        if accum_out is not None:
            kw["accum_out"] = _ap(accum_out)
            outs.append(accum_out)
        return self.emit(eng, lambda: self.eng[eng].tensor_scalar(out=_ap(out), in0=_ap(in0), scalar1=_ap(s1),
                                                                  scalar2=_ap(s2), op0=op0, **kw),
                         outs=outs, ins=[in0, s1, s2])

    def stt(self, eng, out, in0, scalar, in1, op0, op1):
        return self.emit(eng, lambda: self.eng[eng].scalar_tensor_tensor(out=_ap(out), in0=_ap(in0), scalar=_ap(scalar),
                                                                         in1=_ap(in1), op0=op0, op1=op1),
                         outs=[out], ins=[in0, scalar, in1])

    def copy(self, eng, out, in_):
        if eng == "act":
            return self.emit("act", lambda: self.nc.scalar.copy(out=_ap(out), in_=_ap(in_)), outs=[out], ins=[in_])
        return self.emit(eng, lambda: self.eng[eng].tensor_copy(out=_ap(out), in_=_ap(in_)), outs=[out], ins=[in_])

    def memset(self, eng, out, val):
        return self.emit(eng, lambda: self.eng[eng].memset(_ap(out), val), outs=[out], ins=[])

    def reduce(self, eng, out, in_, op, axis=AX.X):
        return self.emit(eng, lambda: self.eng[eng].tensor_reduce(out=_ap(out), in_=_ap(in_), axis=axis, op=op),
                         outs=[out], ins=[in_])

    def recip(self, out, in_):
        return self.emit("dve", lambda: self.nc.vector.reciprocal(out=_ap(out), in_=_ap(in_)), outs=[out], ins=[in_])

    def vmax(self, out, in_):
        return self.emit("dve", lambda: self.nc.vector.max(out=_ap(out), in_=_ap(in_)), outs=[out], ins=[in_])

    def vmax_index(self, out, in_max, in_values):
        return self.emit("dve", lambda: self.nc.vector.max_index(out=_ap(out), in_max=_ap(in_max), in_values=_ap(in_values)),
                         outs=[out], ins=[in_max, in_values])

    def vmatch_replace(self, out, in_to_replace, in_values, imm):
        return self.emit("dve", lambda: self.nc.vector.match_replace(out=_ap(out), in_to_replace=_ap(in_to_replace),
                                                                    in_values=_ap(in_values), imm_value=imm),
                         outs=[out], ins=[in_to_replace, in_values])

    def scan(self, out, data0, data1, initial, op0, op1):
        return self.emit("dve", lambda: self.nc.vector.tensor_tensor_scan(out=_ap(out), data0=_ap(data0), data1=_ap(data1),
                                                                          initial=_ap(initial), op0=op0, op1=op1),
                         outs=[out], ins=[data0, data1, initial])
import os

EPS = 1e-6
import math
RWKV_DECAY_SCALE = math.exp(-0.5)
TT = 8448
NTILE = 33
N = 256


def emit_A(S, nc, pb, sfx="", load_x=None, store_y=None, stage=None, dbg_d=None,
           mixers=("conv", "attn", "mlstm", "rwkv"), tile_limit=None):
    def D(name, shape, kind="ExternalInput", dt=F32):
        t = nc.dram_tensor(name + sfx, list(shape), dt, kind=kind)
        return Tile(t.ap(), name + sfx)

    cv_d = S.shared_dram("cv", [128, 8, 2])
    adaw_d = S.shared_dram("adaw" + sfx, [6, 2, 128, 8, 512])
    adab_d = S.shared_dram("adab" + sfx, [128, 48])
    n1g_d = D("n1g", [128, 8])
    Wc_d = D("Wc", [128, 8, 192])
    Wr_d = D("Wr", [128, 8, 256])
    Wa_d = D("Wa", [128, 8, 192])
    Wm_d = D("Wm", [128, 8, 260])
    pp_d = D("pp", [64, 16])
    w2_d = D("w2p", [16, 2, 64])
    a2_d = D("a2p", [16, 2, 64])
    g2_d = D("g2p", [32, 64])
    rowbc_d = D("rowbc", [128, 4, 64])
    scal_d = D("scal", [128, 8])
    ident_d = S.shared_dram("ident", [128, 128])
    trile_d = S.shared_dram("trile", [128, 128])
    trige_d = S.shared_dram("trige", [128, 128])
    rmask_d = S.shared_dram("rmask", [2, 64, 192])
    cmask_d = S.shared_dram("cmask", [64, 256])
    rope_d = S.shared_dram("rope", [64, 128, 64])

    class Done(Exception):
        pass

    dbg_n = [0]

    def chk(name, *refs):
        if stage != name:
            return
        o = 0
        for r in refs:
            n = 1
            for s_ in r.ap.shape[1:]:
                n *= s_
            P = r.ap.shape[0]
            t = S.tile(f"dbgt{dbg_n[0]}", [128, n])
            dbg_n[0] += 1
            tv = t[0:P, :]
            shp = r.ap.shape
            if len(shp) == 3:
                tv = tv.re("p (a b) -> p a b", a=shp[1])
            elif len(shp) == 4:
                tv = tv.re("p (a b c) -> p a b c", a=shp[1], b=shp[2])
            S.copy("dve", tv, r)
            S.dma(dbg_d[0:P, o:o + n], t[0:P, :], "st")
            o += n
        raise Done()

    S.push_scope()
    ident = S.tile("ident", [128, 128])
    ones = S.tile("ones", [128, 128])
    cv = S.tile("cv", [128, 8, 2])
    adab = S.tile("adab", [128, 48])
    n1g = S.tile("n1g", [128, 8])
    mod0 = S.tile("mod0", [128, 8, 2])
    mod1 = S.tile("mod1", [128, 8, 2])
    gm1 = S.tile("gm1", [128, 8, 2])
    pp = S.tile("pp", [64, 16])
    omka = S.tile("omka", [64, 1])
    rowbc = S.tile("rowbc", [128, 4, 64])
    scal = S.tile("scal", [128, 8])
    xs = S.tile("xs", [128, 8, N])
    hT = S.tile("hT", [128, 8, N])
    rstd = S.tile("rstd", [128, N])

    def body():
        S.dma(ident.r, ident_d.r, "ldc")
        S.dma(cv.r, cv_d.r, "ldc")
        S.dma(adab.r, adab_d.r, "ldc")
        S.dma(n1g.r, n1g_d.r, "ldc")
        S.dma(pp.r, pp_d.r, "ldc")
        S.dma(rowbc.r, rowbc_d.r, "ldc")
        S.dma(scal.r, scal_d.r, "ldc")
        S.memset("dve", ones.r, 1.0)
        S.act(cv.r, cv.r, AF.Silu)
        S.ts("dve", omka.r, pp[:, 9:10], -1.0, 1.0, op0=ALU.mult, op1=ALU.add)
        scr = S.tile("scr", [128, 4096])
        adaw_t = scr.r.re("p (c f) -> p c f", c=8)
        for seg, dst in ((0, mod0), (1, mod1)):
            for half in range(2):
                S.dma(adaw_t, adaw_d[seg, half], "ldc")
                for f4 in range(4):
                    fc = half * 4 + f4
                    for dc in range(8):
                        S.mm(pb[0][:, fc * 2:fc * 2 + 2], adaw_t[:, dc, f4 * 128:(f4 + 1) * 128], cv[:, dc, :],
                             start=(dc == 0), stop=(dc == 7))
            S.tt("dve", dst.r, pb[0][:, 0:16].re("p (c t) -> p c t", t=2),
                 adab.r.with_ap(adab.ap[:, seg * 8:(seg + 1) * 8].unsqueeze(2).to_broadcast([128, 8, 2])), ALU.add)
        S.ts("dve", gm1.r, mod1.r, 1.0, None, op0=ALU.add)
        S.tt("dve", gm1.r, gm1.r, n1g.r.with_ap(n1g.ap.unsqueeze(2).to_broadcast([128, 8, 2])), ALU.mult)
        sh1 = mod0
        chk("mod", mod0.r, gm1.r)

        hbuf = Tile(nc.dram_tensor("hbuf" + sfx, [128, 8, TT], F32).ap(), "hbuf" + sfx)
        h_done = set()

        def load_h(ti, hb=None):
            if hb is not None:
                return load_h_impl(ti, *hb)
            return load_h_impl(ti, hT, xs, rstd, pb[0])

        def load_h_impl(ti, hT, xs, rstd, pbank):
            t0 = ti * N
            col = 1 if ti == 0 else 0
            if ti in h_done:
                S.dma(hT.r, hbuf[:, :, t0:t0 + N], "ldx")
                return
            load_x(ti, xs)
            S.act(hT.r, xs.r, AF.Square)
            for kc in range(8):
                S.mm(pbank[:, 0:N], ones.r, hT[:, kc, :], start=(kc == 0), stop=(kc == 7))
            S.ts("dve", rstd.r, pbank[:, 0:N], 1.0 / 1024.0, EPS, op0=ALU.mult, op1=ALU.add)
            S.act(rstd.r, rstd.r, AF.Sqrt)
            S.recip(rstd.r, rstd.r)
            S.tt("dve", hT.r, xs.r, rstd.r.with_ap(rstd.ap.unsqueeze(1).to_broadcast([128, 8, N])), ALU.mult)
            for kc in range(8):
                S.ts("pool" if kc % 2 else "dve", hT[:, kc, :], hT[:, kc, :], gm1[:, kc, col:col + 1], sh1[:, kc, col:col + 1],
                     op0=ALU.mult, op1=ALU.add)
            S.dma(hbuf[:, :, t0:t0 + N], hT.r, "sth")
            if S.recording is None:
                h_done.add(ti)

        tiles = list(range(NTILE)) if tile_limit is None else list(range(tile_limit))

        def head_norm_fm(y, sq, out, g_col, psb, n=N):
            S.act(sq, y, AF.Square)
            S.mm(psb[0:64, 0:n], ones[0:64, 0:64], sq)
            S.ts("dve", sq, psb[0:64, 0:n], 1.0 / 64.0, EPS, op0=ALU.mult, op1=ALU.add)
            S.act(sq, sq, AF.Sqrt)
            S.recip(sq, sq)
            S.stt("dve", out, y, g_col, sq, ALU.mult, ALU.mult)

        if "conv" in mixers:
            S.push_scope()
            Wc = S.tile("Wc", [128, 8, 192])
            S.dma(Wc.r, Wc_d.r, "ldc")
            U = S.tile("convU", [64, TT + 4])
            Bg = S.tile("convB", [64, TT])
            S.memset("dve", U.r, 0.0)

            def ucol(t):
                return t + 1 if t < 256 else t + 3

            for ti in tiles:
                load_h(ti)
                t0 = ti * N
                for g in range(3):
                    for kc in range(8):
                        S.mm(pb[1 + g][0:64, 0:N], Wc[:, kc, g * 64:(g + 1) * 64], hT[:, kc, :], start=(kc == 0), stop=(kc == 7))
                S.copy("act", Bg[:, t0:t0 + N], pb[2][0:64, 0:N])
                S.copy("act", U[:, ucol(t0):ucol(t0) + N], pb[1][0:64, 0:N])
                S.tt("dve", U[:, ucol(t0):ucol(t0) + N], U[:, ucol(t0):ucol(t0) + N], pb[3][0:64, 0:N], ALU.mult)
            cy = S.tile("convy", [64, N])
            csq = S.tile("convsq", [64, N])
            for ti in tiles:
                t0 = ti * N
                u0 = ucol(t0)
                S.ts("dve", cy.r, U[:, u0 - 1:u0 - 1 + N], pp[:, 0:1], None, op0=ALU.mult)
                S.stt("dve", cy.r, U[:, u0:u0 + N], pp[:, 1:2], cy.r, ALU.mult, ALU.add)
                S.stt("dve", cy.r, U[:, u0 + 1:u0 + 1 + N], pp[:, 2:3], cy.r, ALU.mult, ALU.add)
                S.tt("dve", cy.r, cy.r, Bg[:, t0:t0 + N], ALU.mult)
                head_norm_fm(cy.r, csq.r, cy.r, pp[:, 3:4], pb[1])
                store_y(0, t0, N, cy.r)
            chk("conv", cy.r)
            S.pop_scope()

        if "attn" in mixers:
            S.push_scope()
            Wa = S.tile("Wa", [128, 8, 192])
            S.dma(Wa.r, Wa_d.r, "ldc")
            trile = S.tile("trile", [128, 128])
            trige = S.tile("trige", [128, 128])
            S.dma(trile.r, trile_d.r, "ldc")
            S.dma(trige.r, trige_d.r, "ldc")
            QT = S.tile("QT", [64, TT])
            KT = S.tile("KT", [64, TT])
            V1 = S.tile("V1", [128, 66, 65])
            S.memset("dve", V1.r, 1.0)
            rope = S.tile("ropet", [128, 64])
            qk = S.tile("qk", [128, 2, 64])
            qr = S.tile("qr", [128, 2, 64])
            tmpa = S.tile("tmpa", [128, 2, 2, 16])
            ssq = S.tile("ssq", [128, 2])
            junk = S.tile("junk", [128, 64])
            junk2 = S.tile("junk2", [128, 128])
            for ti in tiles:
                load_h(ti)
                for sub in range(2):
                    bi = ti * 2 + sub
                    t0 = bi * 128
                    for kc in range(8):
                        S.mm(pb[1][:, 0:192], hT[:, kc, sub * 128:(sub + 1) * 128], Wa[:, kc, :], start=(kc == 0), stop=(kc == 7))
                    S.copy("act", V1[:, bi, 0:64], pb[1][:, 128:192])
                    S.act(junk2.r, pb[1][:, 0:128], AF.Square)
                    S.reduce("dve", ssq.r, junk2.r.re("p (w f) -> p w f", w=2), ALU.add)
                    S.ts("dve", ssq.r, ssq.r, 1.0 / 64.0, EPS, op0=ALU.mult, op1=ALU.add)
                    S.act(ssq.r, ssq.r, AF.Sqrt)
                    S.recip(ssq.r, ssq.r)
                    for w in range(2):
                        S.stt("dve", qk[:, w, :], pb[1][:, w * 64:(w + 1) * 64], ssq[:, w:w + 1], rowbc[:, w, :], ALU.mult, ALU.mult)
                    src = qk
                    if bi >= 2:
                        S.dma(rope.r, rope_d[bi - 2], "ldr")
                        cosv = rope.r.with_ap(rope.ap.rearrange("p (h cs f) -> p h cs f", h=2, cs=2)[:, :, 0, :])
                        sinv = rope.r.with_ap(rope.ap.rearrange("p (h cs f) -> p h cs f", h=2, cs=2)[:, :, 1, :])
                        for w in range(2):
                            q4 = qk[:, w, :].re("p (h x f) -> p h x f", h=2, x=2)
                            o4 = qr[:, w, :].re("p (h x f) -> p h x f", h=2, x=2)
                            S.tt("dve", o4, q4, cosv.with_ap(cosv.ap.unsqueeze(2).to_broadcast([128, 2, 2, 16])), ALU.mult)
                            S.tt("pool", tmpa[:, :, 0, :], q4[:, :, 1, :], sinv, ALU.mult)
                            S.tt("pool", tmpa[:, :, 1, :], q4[:, :, 0, :], sinv, ALU.mult)
                            S.tt("dve", o4[:, :, 0, :], o4[:, :, 0, :], tmpa[:, :, 0, :], ALU.subtract)
                            S.tt("dve", o4[:, :, 1, :], o4[:, :, 1, :], tmpa[:, :, 1, :], ALU.add)
                        src = qr
                    S.tr(pb[2][0:64, 0:128], src[:, 0, :], ident.r)
                    S.copy("act", QT[:, t0:t0 + 128], pb[2][0:64, 0:128])
                    S.tr(pb[3][0:64, 0:128], src[:, 1, :], ident.r)
                    S.copy("act", KT[:, t0:t0 + 128], pb[3][0:64, 0:128])
            chk("attn_qk", QT[:, 0:512], KT[:, 0:512])
            nblk = len(tiles) * 2
            E = [S.tile(f"attE{i}", [128, 128]) for i in range(5)]
            esink = S.tile("esink", [128, 1])
            S.act(esink.r, scal[:, 0:1], AF.Exp)
            den = S.tile("attden", [128, 1])
            ao = S.tile("atto", [128, 64])
            for bi in range(nblk):
                t0 = bi * 128
                if bi < 2:
                    kbs = [(0, None), (1, None)]
                else:
                    kbs = [(0, None), (1, None)]
                    if bi - 1 >= 2:
                        kbs.append((bi - 1, trige))
                    kbs.append((bi, None))
                    if bi + 1 < nblk:
                        kbs.append((bi + 1, trile))
                for i, (kb, mask) in enumerate(kbs):
                    ps = pb[1 + (i % 2)]
                    S.mm(ps[:, 0:128], KT[:, kb * 128:(kb + 1) * 128], QT[:, t0:t0 + 128])
                    S.act(E[i].r, ps[:, 0:128], AF.Exp, scale=0.125)
                    if mask is not None:
                        S.tt("dve", E[i].r, E[i].r, mask.r, ALU.mult)
                chk("attn_E", E[0].r, E[1].r)
                for i, (kb, mask) in enumerate(kbs):
                    S.mm(pb[3][:, 0:65], E[i].r, V1[:, kb, :], start=(i == 0), stop=(i == len(kbs) - 1))
                chk("attn_pv", pb[3][:, 0:65])
                S.tt("dve", den.r, pb[3][:, 64:65], esink.r, ALU.add)
                S.recip(den.r, den.r)
                S.ts("dve", ao.r, pb[3][:, 0:64], den.r, None, op0=ALU.mult)
                S.act(junk.r, ao.r, AF.Square)
                S.reduce("dve", ssq[:, 0:1], junk.r, ALU.add)
                S.ts("dve", ssq[:, 0:1], ssq[:, 0:1], 1.0 / 64.0, EPS, op0=ALU.mult, op1=ALU.add)
                S.act(ssq[:, 0:1], ssq[:, 0:1], AF.Sqrt)
                S.recip(ssq[:, 0:1], ssq[:, 0:1])
                S.stt("dve", ao.r, ao.r, ssq[:, 0:1], rowbc[:, 2, :], ALU.mult, ALU.mult)
                chk("attn_ao", ao.r)
                S.tr(pb[4][0:64, 0:128], ao.r, ident.r)
                S.copy("act", qk[0:64, :, :].re("p a b -> p (a b)"), pb[4][0:64, 0:128])
                store_y(2, t0, 128, qk[0:64, :, :].re("p a b -> p (a b)"))
                if bi == int(os.environ.get("BLIM", "99")):
                    chk("attn_blk", ao.r)
            chk("attn", ao.r)
            S.pop_scope()

        if "mlstm" in mixers:
            S.push_scope()
            Wm = S.tile("Wm", [128, 8, 260])
            S.dma(Wm.r, Wm_d.r, "ldc")
            trile = S.tile("trile", [128, 128])
            trige = S.tile("trige", [128, 128])
            S.dma(trile.r, trile_d.r, "ldc")
            S.dma(trige.r, trige_d.r, "ldc")
            nblk = len(tiles) * 2
            Qm = S.tile("Qm", [128, 66, 64])
            Km = S.tile("Km", [128, 66, 64])
            Vm1 = S.tile("Vm1", [128, 66, 65])
            Om = S.tile("Om", [128, 66, 64])
            Hs = S.tile("Hs", [128, 66, 64])
            G = S.tile("G", [128, 66, 4])
            nfb = S.tile("nfb", [128, 2])
            S.memset("dve", Vm1.r, 1.0)
            S.ts("dve", nfb[:, 0:1], scal[:, 2:3], -1.0, None, op0=ALU.mult)
            S.ts("dve", nfb[:, 1:2], scal[:, 4:5], -1.0, None, op0=ALU.mult)
            for ti in tiles:
                load_h(ti)
                for sub in range(2):
                    bi = ti * 2 + sub
                    for kc in range(8):
                        S.mm(pb[1][:, 0:260], hT[:, kc, sub * 128:(sub + 1) * 128], Wm[:, kc, :], start=(kc == 0), stop=(kc == 7))
                    S.copy("act", Qm[:, bi, :], pb[1][:, 0:64])
                    chk("ml_a", Qm[:, 0, :])
                    S.ts("dve", Km[:, bi, :], pb[1][:, 64:128], 0.125, None, op0=ALU.mult)
                    S.copy("dve", Vm1[:, bi, 0:64], pb[1][:, 128:192])
                    chk("ml_b", Km[:, 0, :])
                    S.act(Om[:, bi, :], pb[1][:, 192:256], AF.Sigmoid)
                    chk("ml_c", Om[:, 0, :])
                    for d in range(2):
                        S.ts("dve", G[:, bi, 2 * d:2 * d + 1], pb[1][:, 256 + 2 * d:257 + 2 * d], scal[:, 1 + 2 * d:2 + 2 * d], None, op0=ALU.add)
                        chk("ml_d", G[:, 0, :])
                        S.act(G[:, bi, 2 * d + 1:2 * d + 2], pb[1][:, 257 + 2 * d:258 + 2 * d], AF.Exp, bias=nfb[:, d:d + 1], scale=-1.0)
                        chk("ml_e", G[:, 0, :])
                        S.act(G[:, bi, 2 * d + 1:2 * d + 2], G[:, bi, 2 * d + 1:2 * d + 2], AF.Ln, bias=1.0)
                        chk("ml_f", G[:, 0, :])
                        S.ts("dve", G[:, bi, 2 * d + 1:2 * d + 2], G[:, bi, 2 * d + 1:2 * d + 2], -1.0, None, op0=ALU.mult)
            chk("ml_p1", Qm[:, 0, :], Km[:, 0, :], G[:, 0, :])
            Hb = S.tile("Hb", [128, 66, 64])

            def ml_dir(d):
                q = [pb[4 * d + i] for i in range(4)]
                t = lambda nm, shp: S.tile(f"ml{d}_{nm}", shp)
                C1T = t("C1T", [64, 65])
                eb, ek, rden = t("eb", [128, 1]), t("ek", [128, 1]), t("rden", [128, 1])
                eL = t("eL", [64, 1])
                qt, kt = t("qt", [128, 64]), t("kt", [128, 64])
                qtT, ktT = t("qtT", [64, 128]), t("ktT", [64, 128])
                STs = t("ST", [128, 128])
                tri = trile if d == 0 else trige
                order = list(range(nblk)) if d == 0 else [1, 0] + list(range(nblk - 1, 1, -1))
                Hd = Hs if d == 0 else Hb

                def run():
                    S.memset("dve", C1T.r, 0.0)
                    for bi in order:
                        lf = G[:, bi, 2 * d + 1:2 * d + 2]
                        S.mm(q[0][:, 0:1], tri.r, lf)
                        S.mm(q[0][0:64, 1:2], ones[:, 0:64], lf)
                        S.act(eb.r, q[0][:, 0:1], AF.Exp)
                        S.tt("dve", ek.r, G[:, bi, 2 * d:2 * d + 1], q[0][:, 0:1], ALU.subtract)
                        S.act(ek.r, ek.r, AF.Exp)
                        S.act(eL.r, q[0][0:64, 1:2], AF.Exp)
                        S.ts("dve", qt.r, Qm[:, bi, :], eb.r, None, op0=ALU.mult)
                        S.ts("pool", kt.r, Km[:, bi, :], ek.r, None, op0=ALU.mult)
                        S.tr(q[1][0:64, 0:128], qt.r, ident.r)
                        S.copy("act", qtT.r, q[1][0:64, 0:128])
                        S.tr(q[2][0:64, 0:128], kt.r, ident.r)
                        S.copy("dve", ktT.r, q[2][0:64, 0:128])
                        S.mm(q[3][:, 0:128], ktT.r, qtT.r)
                        S.tt("dve", STs.r, q[3][:, 0:128], tri.r, ALU.mult)
                        S.mm(q[1][:, 0:65], STs.r, Vm1[:, bi, :], start=True, stop=False)
                        S.mm(q[1][:, 0:65], qtT.r, C1T.r, start=False, stop=True)
                        S.ts("dve", rden.r, q[1][:, 64:65], -1.0, None, op0=ALU.mult)
                        S.tt("dve", rden.r, rden.r, q[1][:, 64:65], ALU.max)
                        S.ts("dve", rden.r, rden.r, 1.0, None, op0=ALU.max)
                        S.recip(rden.r, rden.r)
                        S.ts("dve", Hd[:, bi, :], q[1][:, 0:64], rden.r, None, op0=ALU.mult)
                        S.mm(q[2][0:64, 0:65], ident[0:64, 0:64], C1T.r, start=True, stop=False)
                        S.mm(q[2][0:64, 0:65], kt.r, Vm1[:, bi, :], start=False, stop=True)
                        S.ts("dve", C1T.r, q[2][0:64, 0:65], eL.r, None, op0=ALU.mult)
                return run

            runs = [ml_dir(0), ml_dir(1)]
            S.replay_interleaved([S.record(runs[0]), S.record(runs[1])])
            S.tt("dve", Hs.r, Hs.r, Hb.r, ALU.add)
            mlsq = S.tile("mlsq", [128, 64])
            mlss = S.tile("mlss", [128, 1])
            mly = S.tile("mly", [128, 64])
            mlyT = S.tile("mlyT", [64, 128])
            for bi in range(nblk):
                S.act(mlsq.r, Hs[:, bi, :], AF.Square)
                S.reduce("dve", mlss.r, mlsq.r, ALU.add)
                S.ts("dve", mlss.r, mlss.r, 1.0 / 64.0, EPS, op0=ALU.mult, op1=ALU.add)
                S.act(mlss.r, mlss.r, AF.Sqrt)
                S.recip(mlss.r, mlss.r)
                S.stt("dve", mly.r, Hs[:, bi, :], mlss.r, rowbc[:, 3, :], ALU.mult, ALU.mult)
                S.tt("dve", mly.r, mly.r, Om[:, bi, :], ALU.mult)
                S.tr(pb[3][0:64, 0:128], mly.r, ident.r)
                S.copy("act", mlyT.r, pb[3][0:64, 0:128])
                store_y(3, bi * 128, 128, mlyT.r)
            chk("mlstm", mly.r)
            S.pop_scope()
        if "rwkv" in mixers:
            S.push_scope()
            Wr = S.tile("Wr", [128, 8, 256])
            S.dma(Wr.r, Wr_d.r, "ldc")
            w2p = S.tile("w2p", [16, 2, 64])
            a2p = S.tile("a2p", [16, 2, 64])
            g2p = S.tile("g2p", [32, 64])
            rmask = [S.tile(f"rmask{d}", [64, 192]) for d in range(2)]
            cmask = S.tile("cmask", [64, 256])
            S.dma(w2p.r, w2_d.r, "ldc")
            S.dma(a2p.r, a2_d.r, "ldc")
            S.dma(g2p.r, g2_d.r, "ldc")
            for d in range(2):
                S.dma(rmask[d].r, rmask_d[d], "ldc")
            S.dma(cmask.r, cmask_d.r, "ldc")
            RKbc = S.tile("RKbc", [64, 64])
            S.ts("dve", RKbc.r, ones[0:64, 0:64], pp[:, 11:12], None, op0=ALU.mult)
            Yst = S.tile("Yst", [64, TT])
            rwsc = Tile(nc.dram_tensor("rwsc" + sfx, [64, NTILE, 3, N], F32).ap(), "rwsc" + sfx)
            i64 = ident[0:64, 0:64]
            o64 = ones[0:64, 0:64]

            class B_:
                pass

            def mkbufs(d):
                b = B_()
                t = lambda nm, shp: S.tile(f"rw{d}_{nm}", shp)
                b.rT, b.kT, b.vT, b.gT, b.kkT = (t(n_, [64, N]) for n_ in ("r", "k", "v", "g", "kk"))
                b.tw, b.xa, b.sg = t("tw", [16, N]), t("xa", [16, N]), t("sg", [32, N])
                b.lw = t("lw", [64, N])
                b.aT = [t(f"a{i}", [64, N]) for i in range(2)]
                b.kd = [t(f"kd{i}", [64, N]) for i in range(2)]
                b.cum, b.tmp, b.Pin, b.Pinv, b.Pex = (t(n_, [64, N]) for n_ in ("cum", "tmp", "Pin", "Pinv", "Pex"))
                b.AR = t("AR", [64, 4, 2, 64])
                b.BK = t("BK", [64, 4, 2, 64])
                b.Vtok = t("Vtok", [64, 4, 64])
                b.NM = [t(f"NM{i}", [64, 128]) for i in range(2)]
                b.Pw = [t(f"P{i}", [64, 64]) for i in range(2)]
                b.PwT = [t(f"PT{i}", [64, 64]) for i in range(2)]
                b.X = [t(f"X{i}", [64, 64]) for i in range(2)]
                b.Btok, b.Ktok, b.ZT, b.UT, b.S0T = (t(n_, [64, 64]) for n_ in ("Btok", "Ktok", "ZT", "UT", "S0T"))
                b.sq = t("sq", [64, N])
                b.Yb = t("Yb", [64, 3, N])
                b.q = [pb[4 * d + i] for i in range(4)]
                if d == 0:
                    b.hb = (hT, xs, rstd, pb[0])
                else:
                    b.hb = (S.tile("rw1_hT", [128, 8, N]), S.tile("rw1_xs", [128, 8, N]), S.tile("rw1_rstd", [128, N]), pb[4])
                b.hT = b.hb[0]
                return b

            BF = [mkbufs(0), mkbufs(1)]

            def prep_tile(ti, d):
                b = BF[d]
                q = b.q
                both = (d == 1)
                load_h(ti, b.hb)
                hT = b.hT
                for g, dst in ((0, b.rT), (1, b.kT), (2, b.vT)):
                    for kc in range(8):
                        S.mm(q[1][0:64, 0:N], Wr[:, kc, g * 64:(g + 1) * 64], hT[:, kc, :], start=(kc == 0), stop=(kc == 7))
                    S.copy("act", dst.r, q[1][0:64, 0:N])
                for kc in range(8):
                    S.mm(q[2][0:16, 0:N], Wr[:, kc, 192:208], hT[:, kc, :], start=(kc == 0), stop=(kc == 7))
                S.act(b.tw.r, q[2][0:16, 0:N], AF.Tanh)
                for kc in range(8):
                    S.mm(q[2][0:16, 0:N], Wr[:, kc, 208:224], hT[:, kc, :], start=(kc == 0), stop=(kc == 7))
                S.copy("act", b.xa.r, q[2][0:16, 0:N])
                if both:
                    for kc in range(8):
                        S.mm(q[2][0:32, 0:N], Wr[:, kc, 224:256], hT[:, kc, :], start=(kc == 0), stop=(kc == 7))
                    S.act(b.sg.r, q[2][0:32, 0:N], AF.Sigmoid)
                for c in range(4):
                    for kc in range(8):
                        S.mm(q[3][0:64, c * 64:(c + 1) * 64], hT[:, kc, c * 64:(c + 1) * 64], Wr[:, kc, 128:192],
                             start=(kc == 0 and c == 0), stop=(kc == 7), skip=True)
                S.copy("act", b.Vtok.r.re("p c v -> p (c v)"), q[3][0:64, 0:256])
                dirs = (0, 1) if both else (d,)
                for dd in dirs:
                    S.mm(q[1][0:64, 0:N], a2p[:, dd, :], b.xa.r)
                    S.act(b.aT[dd].r, q[1][0:64, 0:N], AF.Sigmoid, bias=pp[:, 6 + dd:7 + dd])
                    S.ts("dve", b.kd[dd].r, b.aT[dd].r, pp[:, 9:10], omka.r, op0=ALU.mult, op1=ALU.add)
                    S.tt("dve", b.kd[dd].r, b.kd[dd].r, b.kT.r, ALU.mult)
                S.mm(q[1][0:64, 0:N], w2p[:, d, :], b.tw.r)
                S.act(b.lw.r, q[1][0:64, 0:N], AF.Sigmoid, bias=pp[:, 4 + d:5 + d])
                S.ts("dve", b.lw.r, b.lw.r, -RWKV_DECAY_SCALE, None, op0=ALU.mult)
                if both:
                    S.mm(q[1][0:64, 0:N], g2p.r, b.sg.r)
                    S.copy("act", b.Yb[:, 2, :], q[1][0:64, 0:N])
                S.ts("dve", b.kkT.r, b.kT.r, pp[:, 8:9], None, op0=ALU.mult)
                S.act(b.sq.r, b.kkT.r, AF.Square)
                S.mm(q[1][0:64, 0:N], o64, b.sq.r)
                S.ts("dve", b.sq.r, q[1][0:64, 0:N], EPS, None, op0=ALU.add)
                S.act(b.sq.r, b.sq.r, AF.Sqrt)
                S.recip(b.sq.r, b.sq.r)
                S.tt("dve", b.kkT.r, b.kkT.r, b.sq.r, ALU.mult)
                S.scan(b.cum.r, cmask.r, b.lw.r, 0.0, ALU.mult, ALU.add)
                if d == 1:
                    c3 = b.cum.ap.rearrange("p (c t) -> p c t", c=4)
                    S.tt("dve", b.tmp.r, b.lw.r, b.cum.r, ALU.subtract)
                    S.tt("dve", b.cum.r.re("p (c t) -> p c t", c=4), b.tmp.r.re("p (c t) -> p c t", c=4),
                         b.cum.r.with_ap(c3[:, :, 63:64].to_broadcast([64, 4, 64])), ALU.add)
                S.act(b.Pin.r, b.cum.r, AF.Exp)
                S.act(b.Pinv.r, b.cum.r, AF.Exp, scale=-1.0)
                S.tt("dve", b.tmp.r, b.cum.r, b.lw.r, ALU.subtract)
                S.act(b.Pex.r, b.tmp.r, AF.Exp)
                v4 = lambda tl: tl.r.re("p (c t) -> p c t", c=4)
                S.stt("dve", b.AR[:, :, 0, :], v4(b.kkT), -1.0, v4(b.Pex), ALU.mult, ALU.mult)
                S.tt("dve", b.AR[:, :, 1, :], v4(b.rT), v4(b.Pin), ALU.mult)
                S.tt("dve", b.tmp.r, b.kkT.r, b.aT[d].r, ALU.mult)
                S.tt("dve", b.BK[:, :, 0, :], v4(b.tmp), v4(b.Pinv), ALU.mult)
                S.tt("dve", b.BK[:, :, 1, :], v4(b.kd[d]), v4(b.Pinv), ALU.mult)
                if both:
                    S.tt("dve", b.tmp.r, b.kd[0].r, b.kd[1].r, ALU.add)
                    S.tt("dve", b.tmp.r, b.tmp.r, b.rT.r, ALU.mult)
                    S.mm(q[1][0:64, 0:N], RKbc.r, b.tmp.r)
                    S.tt("dve", b.Yb[:, 1, :], q[1][0:64, 0:N], b.vT.r, ALU.mult)

            def chunk(ti, c, d):
                b = BF[d]
                q = b.q
                m = rmask[d]
                NM, Pw, PwT, X = b.NM, b.Pw, b.PwT, b.X
                ARc = b.AR[:, c, :, :].re("p two t -> p (two t)")
                A_c, R_c = b.AR[:, c, 0, :], b.AR[:, c, 1, :]
                B_c, K_c = b.BK[:, c, 0, :], b.BK[:, c, 1, :]
                V_c = b.Vtok[:, c, :]
                S.mm(q[1][0:64, 0:128], B_c, ARc)
                S.tt("dve", NM[0].r, q[1][0:64, 0:128], m[:, 0:128], ALU.mult)
                S.mm(q[2][0:64, 0:128], K_c, ARc)
                S.tt("dve", NM[1].r, q[2][0:64, 0:128], m[:, 0:128], ALU.mult)
                S.mm(q[3][0:64, 0:64], A_c, B_c)
                S.tt("dve", PwT[0].r, q[3][0:64, 0:64], m[:, 128:192], ALU.mult)
                S.copy("act", Pw[0].r, NM[0][:, 0:64])
                S.tt("dve", X[0].r, NM[0][:, 0:64], i64, ALU.add)
                cur = 0
                for lev in range(5):
                    nxt = 1 - cur
                    S.mm(q[1][0:64, 0:64], Pw[cur].r, PwT[cur].r)
                    S.copy("act", PwT[nxt].r, q[1][0:64, 0:64])
                    if lev < 4:
                        S.mm(q[2][0:64, 0:64], PwT[cur].r, Pw[cur].r)
                        S.copy("dve", Pw[nxt].r, q[2][0:64, 0:64])
                    S.mm(q[3][0:64, 0:64], PwT[nxt].r, X[cur].r)
                    S.tt("dve", X[nxt].r, q[3][0:64, 0:64], X[cur].r, ALU.add)
                    cur = nxt
                Xf = X[cur]
                S.tr(q[1][0:64, 0:64], B_c, i64)
                S.copy("act", b.Btok.r, q[1][0:64, 0:64])
                S.tr(q[2][0:64, 0:64], K_c, i64)
                S.copy("dve", b.Ktok.r, q[2][0:64, 0:64])
                S.mm(q[3][0:64, 0:64], A_c, b.S0T.r, start=True, stop=False)
                S.mm(q[3][0:64, 0:64], NM[1][:, 0:64], V_c, start=False, stop=True)
                S.copy("act", b.ZT.r, q[3][0:64, 0:64])
                S.mm(q[1][0:64, 0:64], Xf.r, b.ZT.r)
                S.copy("act", b.UT.r, q[1][0:64, 0:64])
                S.mm(q[2][0:64, 0:64], b.S0T.r, R_c, start=True, stop=False)
                S.mm(q[2][0:64, 0:64], b.UT.r, NM[0][:, 64:128], start=False, stop=False)
                S.mm(q[2][0:64, 0:64], V_c, NM[1][:, 64:128], start=False, stop=True)
                if d == 0:
                    t0 = ti * N + c * 64
                    S.copy("dve", Yst[:, t0:t0 + 64], q[2][0:64, 0:64])
                else:
                    S.copy("dve", b.Yb[:, 0, c * 64:(c + 1) * 64], q[2][0:64, 0:64])
                S.mm(q[3][0:64, 0:64], i64, b.S0T.r, start=True, stop=False)
                S.mm(q[3][0:64, 0:64], b.Btok.r, b.UT.r, start=False, stop=False)
                S.mm(q[3][0:64, 0:64], b.Ktok.r, V_c, start=False, stop=True)
                pl = c * 64 + (63 if d == 0 else 0)
                S.ts("dve", b.S0T.r, q[3][0:64, 0:64], b.Pin[:, pl:pl + 1], None, op0=ALU.mult)

            orders = [tiles, [0] + tiles[:0:-1]]
            for d in range(2):
                S.memset("dve", BF[d].S0T.r, 0.0)
            for s_ in range(len(tiles)):
                tis = [orders[0][s_], orders[1][s_]]
                def stream0():
                    prep_tile(tis[0], 0)
                    for i in range(4):
                        chunk(tis[0], i, 0)

                def stream1():
                    prep_tile(tis[1], 1)
                    for i in range(4):
                        chunk(tis[1], 3 - i, 1)
                    S.dma(rwsc[:, tis[1], :, :], BF[1].Yb.r, "sth")

                S.replay_interleaved([S.record(stream0), S.record(stream1)])
            fin = BF[0].Yb
            yo = BF[0].tmp
            for ti in tiles:
                t0 = ti * N
                S.dma(fin.r, rwsc[:, ti, :, :], "ldx")
                S.tt("dve", yo.r, Yst[:, t0:t0 + N], fin[:, 0, :], ALU.add)
                head_norm_fm(yo.r, BF[0].sq.r, yo.r, pp[:, 12:13], pb[1])
                S.tt("dve", yo.r, yo.r, fin[:, 1, :], ALU.add)
                S.tt("dve", yo.r, yo.r, fin[:, 2, :], ALU.mult)
                store_y(1, t0, N, yo.r)
            S.pop_scope()

    try:
        body()
    except Done:
        while len(S.scopes) > 1:
            S.scopes.pop().close()
        raise
    S.pop_scope()


class StageDone(Exception):
    pass


def build_A(stage=None, mixers=("conv", "attn", "mlstm", "rwkv"), tile_limit=None):
    nc = bass.Bass("TRN2", target_bir_lowering=False)
    S = Sched(nc)
    xT_d = Tile(nc.dram_tensor("xT", [128, 8, TT], F32, kind="ExternalInput").ap(), "xT")
    yT_d = Tile(nc.dram_tensor("yT", [4, 64, TT], F32, kind="ExternalOutput").ap(), "yT")
    dbg_d = Tile(nc.dram_tensor("dbg", [128, 4096], F32, kind="ExternalOutput").ap(), "dbg")
    pb = [S.ptile(f"pb{i}", [128, 512]) for i in range(8)]

    def load_x(ti, xs):
        S.dma(xs.r, xT_d[:, :, ti * N:(ti + 1) * N], "ldx")

    def store_y(m, t0, n, src):
        S.dma(yT_d[m, :, t0:t0 + n], src, "st")

    try:
        emit_A(S, nc, pb, "", load_x, store_y, stage, dbg_d, mixers, tile_limit)
    except Exception as e:
        if type(e).__name__ != "Done":
            raise
    S.wait_all("sp")
    while getattr(S, "scopes", None):
        S.scopes.pop().close()
    print("build_A instructions", S.n_ins, "sems", S.nsem, "sbuf left", nc.sbuf_bytes_remaining)
    S.close()
    return nc

EPS = 1e-6
POOLENG = os.environ.get("POOLENG", "pool")
NEG = -1.0e30


def emit_B(S, nc, pb, sfx, groups, x_src, y_src, out_sink, stage=None, dbg_d=None):
    def D(name, shape, kind="ExternalInput", dt=F32):
        t = nc.dram_tensor(name + sfx, list(shape), dt, kind=kind)
        return Tile(t.ap(), name + sfx)

    cv_d = S.shared_dram("cv", [128, 8, 2])
    adaw_d = S.shared_dram("adaw" + sfx, [6, 2, 128, 8, 512])
    adab_d = S.shared_dram("adab" + sfx, [128, 48])
    n2g_d = D("n2g", [128, 8])
    wout_d = D("wout", [8, 128, 8, 128])
    wq_d = D("wq", [16, 128, 8, 128])
    keys_d = D("keysT", [128, 16, 128])
    UT_d = D("UT", [128, 128, 8, 128])
    VJ_d = D("VJ", [128, 128, 1024])
    ident_d = S.shared_dram("ident", [128, 128])
    iota_d = S.shared_dram("iota", [128, 128])

    GM = max(g[1] for g in groups)
    S.push_scope()

    class Done(Exception):
        pass

    def chk(name, *refs):
        if stage != name:
            return
        o = 0
        for r in refs:
            n = 1
            for s_ in r.ap.shape[1:]:
                n *= s_
            t = S.tile(f"dbgt{o}", [128, n])
            S.copy("dve", t.r, r if len(r.ap.shape) == 2 else r)
            S.dma(dbg_d[0:r.ap.shape[0], o:o + n], t[0:r.ap.shape[0], :], "st")
            o += n
        raise Done()
    ident = S.tile("ident", [128, 128])
    iota = S.tile("iota", [128, 128])
    ones = S.tile("ones", [128, 128])
    cv = S.tile("cv", [128, 8, 2])
    adab = S.tile("adab", [128, 48])
    n2g = S.tile("n2g", [128, 8])
    keysT = S.tile("keysT", [128, 16, 128])
    mod = [S.tile(f"mod{i}", [128, 8, 2]) for i in range(6)]
    gm2 = S.tile("gm2", [128, 8, 2])
    scr = S.tile("scr", [128, 4096])
    xs = S.tile("xs", [128, 8, GM])
    ys = S.tile("ys", [128, 8, GM])
    x1 = S.tile("x1", [128, 8, GM])
    h2 = S.tile("h2", [128, 8, GM])
    rstd = S.tile("rstd", [128, GM])
    wbuf = [S.tile(f"wbuf{i}", [128, 8, 128]) for i in range(2)]
    ubuf = [S.tile(f"ubuf{i}", [128, 8, 128]) for i in range(2)]
    vbuf = [S.tile(f"vbuf{i}", [128, 1024]) for i in range(2)]
    sc = S.tile("sc", [128, 16, 128])
    sc2 = S.tile("sc2", [128, 16, 128])
    sv = S.tile("sv", [128, 16, 16])
    si = S.tile("si", [128, 16, 16], U32)
    sif = S.tile("sif", [128, 16, 16])
    cand = S.tile("cand", [128, 8, 256])
    tv = S.tile("tv", [128, 8, 16])
    ti = S.tile("ti", [128, 8, 16], U32)
    tiu = S.tile("tiu", [128, 8, 16], U32)
    aq = S.tile("aq", [128, 8, 16])
    bq = S.tile("bq", [128, 8, 16])
    If = S.tile("If", [128, 128])
    Jf = S.tile("Jf", [128, 128])
    Wf = S.tile("Wf", [128, 128])
    mx = S.tile("mx", [128, 8])
    zs = S.tile("zs", [128, 8])
    IT = S.tile("IT", [128, GM])
    JT = S.tile("JT", [128, GM])
    WT = S.tile("WT", [128, GM])
    oiw = [S.tile(f"oiw{i}", [128, 128], BF16) for i in range(2)]
    oj = [S.tile(f"oj{i}", [128, 128], BF16) for i in range(2)]
    iotab = S.tile("iotab", [128, 128], BF16)
    WW = S.tile("WW", [128, GM, 128], BF16)
    gj = [S.tile(f"gj{i}", [128, GM]) for i in range(2)]
    pjb = [S.tile(f"pjb{i}", [128, GM], BF16) for i in range(2)]
    ubf = [S.tile(f"ubf{i}", [128, 8, 128], BF16) for i in range(3)]
    vbf = [S.tile(f"vbf{i}", [128, 1024], BF16) for i in range(3)]
    acc = pb[0:4]
    pa = pb[4:6]
    pw = pb[6:8]

    def build_body():
        S.dma(ident.r, ident_d.r, "ldc")
        S.dma(iota.r, iota_d.r, "ldc")
        S.dma(cv.r, cv_d.r, "ldc")
        S.dma(adab.r, adab_d.r, "ldc")
        S.dma(n2g.r, n2g_d.r, "ldc")
        S.dma(keysT.r, keys_d.r, "ldc")
        S.memset("dve", ones.r, 1.0)
        S.copy("dve", iotab.r, iota.r)
        S.act(cv.r, cv.r, AF.Silu)
        adaw_t = scr[:, 0:4096].re("p (c f) -> p c f", c=8)
        for seg in (2, 3, 4, 5):
            for half in range(2):
                S.dma(adaw_t, adaw_d[seg, half], "ldc")
                for f4 in range(4):
                    fc = half * 4 + f4
                    for dc in range(8):
                        S.mm(pa[0][:, fc * 2:fc * 2 + 2], adaw_t[:, dc, f4 * 128:(f4 + 1) * 128], cv[:, dc, :],
                             start=(dc == 0), stop=(dc == 7))
            S.tt("dve", mod[seg].r, pa[0][:, 0:16].re("p (c t) -> p c t", t=2),
                 adab[:, seg * 8:(seg + 1) * 8].with_ap(adab.ap[:, seg * 8:(seg + 1) * 8].unsqueeze(2).to_broadcast([128, 8, 2])),
                 ALU.add)
        S.ts("dve", gm2.r, mod[4].r, 1.0, None, op0=ALU.add)
        S.tt("dve", gm2.r, gm2.r, n2g.r.with_ap(n2g.ap.unsqueeze(2).to_broadcast([128, 8, 2])), ALU.mult)
        gt1, sh2, gt2 = mod[2], mod[3], mod[5]
        chk("mod", mod[2].r, mod[3].r, mod[4].r, mod[5].r, gm2.r)

        wi = 0
        ui = 0
        for (g0, GN, col) in groups:
            NTL = GN // 128
            x_src(g0, GN, col, xs)
            y_src(g0, GN, col, ys, h2)
            for oc in range(8):
                wb = wbuf[wi % 2]
                S.dma(wb.r, wout_d[oc], f"ldw{wi % 2}")
                wi += 1
                p = pa[oc % 2]
                for kc in range(8):
                    S.mm(p[:, 0:GN], wb[:, kc, :], ys[:, kc, 0:GN], start=(kc == 0), stop=(kc == 7))
                S.stt("dve", x1[:, oc, 0:GN], p[:, 0:GN], gt1[:, oc, col:col + 1], xs[:, oc, 0:GN], ALU.mult, ALU.add)
            chk("x1", x1[:, :, 0:GN])
            S.act(ys[:, :, 0:GN], x1[:, :, 0:GN], AF.Square)
            for kc in range(8):
                S.mm(pa[0][:, 0:GN], ones.r, ys[:, kc, 0:GN], start=(kc == 0), stop=(kc == 7))
            S.ts("dve", rstd[:, 0:GN], pa[0][:, 0:GN], 1.0 / 1024.0, EPS, op0=ALU.mult, op1=ALU.add)
            S.act(rstd[:, 0:GN], rstd[:, 0:GN], AF.Sqrt)
            S.recip(rstd[:, 0:GN], rstd[:, 0:GN])
            for kc in range(8):
                S.tt("dve", h2[:, kc, 0:GN], x1[:, kc, 0:GN], rstd[:, 0:GN], ALU.mult)
                S.ts("dve", h2[:, kc, 0:GN], h2[:, kc, 0:GN], gm2[:, kc, col:col + 1], sh2[:, kc, col:col + 1],
                     op0=ALU.mult, op1=ALU.add)
            chk("h2", h2[:, :, 0:GN])
            qT = scr[:, 0:16 * GN].re("p (h t) -> p h t", h=16)
            for hp in range(16):
                wb = wbuf[wi % 2]
                S.dma(wb.r, wq_d[hp], f"ldw{wi % 2}")
                wi += 1
                p = pa[hp % 2]
                for kc in range(8):
                    S.mm(p[:, 0:GN], wb[:, kc, :], h2[:, kc, 0:GN], start=(kc == 0), stop=(kc == 7))
                S.copy("act", qT[:, hp, :], p[:, 0:GN])
            chk("qT", qT[:, :, 0:GN])
            for mt in range(NTL):
                ms = slice(mt * 128, (mt + 1) * 128)
                for hp in range(16):
                    S.mm(acc[hp // 4][:, (hp % 4) * 128:(hp % 4) * 128 + 128], qT[:, hp, ms], keysT[:, hp, :])
                for b4 in range(4):
                    S.copy("act" if b4 % 2 else "dve", sc[:, b4 * 4:(b4 + 1) * 4, :].re("p a k -> p (a k)"), acc[b4].r)
                chk("sc", sc.r)
                for hp in range(16):
                    S.vmax(sv[:, hp, 0:8], sc[:, hp, :])
                    S.vmax_index(si[:, hp, 0:8], sv[:, hp, 0:8], sc[:, hp, :])
                    S.vmatch_replace(sc2[:, hp, :], sv[:, hp, 0:8], sc[:, hp, :], NEG)
                    S.vmax(sv[:, hp, 8:16], sc2[:, hp, :])
                    S.vmax_index(si[:, hp, 8:16], sv[:, hp, 8:16], sc2[:, hp, :])
                S.copy("dve", sif.r, si.r)
                chk("top1", sv.r, sif.r)
                sv4 = sv.ap.rearrange("p (h two) a -> p h two a", two=2)
                sif4 = sif.ap.rearrange("p (h two) a -> p h two a", two=2)
                S.tt("dve", cand.r.re("p h (a b) -> p h a b", b=16),
                     sv.r.with_ap(sv4[:, :, 0, :].unsqueeze(3).to_broadcast([128, 8, 16, 16])),
                     sv.r.with_ap(sv4[:, :, 1, :].unsqueeze(2).to_broadcast([128, 8, 16, 16])), ALU.add)
                cand2 = sc2.r.re("p (h two) k -> p h (two k)", two=2)
                eq = sc.r.re("p (h two) (n a) -> p h (two n) a", two=2, a=16)
                for h in range(8):
                    S.vmax(tv[:, h, 0:8], cand[:, h, :])
                    S.vmax_index(ti[:, h, 0:8], tv[:, h, 0:8], cand[:, h, :])
                    S.vmatch_replace(cand2[:, h, :], tv[:, h, 0:8], cand[:, h, :], NEG)
                    S.vmax(tv[:, h, 8:16], cand2[:, h, :])
                    S.vmax_index(ti[:, h, 8:16], tv[:, h, 8:16], cand2[:, h, :])
                S.ts("dve", tiu.r, ti.r, 15, None, op0=ALU.bitwise_and)
                S.copy("dve", bq.r, tiu.r)
                S.ts("dve", tiu.r, ti.r, 4, None, op0=ALU.logical_shift_right)
                S.copy("dve", aq.r, tiu.r)
                iota16 = iota.r.with_ap(iota.ap[:, 0:16].unsqueeze(1).unsqueeze(1).to_broadcast([128, 8, 16, 16]))
                for (qv, half, dst) in ((aq, 0, If), (bq, 1, Jf)):
                    S.tt("dve", eq, qv.r.with_ap(qv.ap.unsqueeze(3).to_broadcast([128, 8, 16, 16])), iota16, ALU.is_equal)
                    S.tt("dve", eq, eq, sif.r.with_ap(sif4[:, :, half, :].unsqueeze(2).to_broadcast([128, 8, 16, 16])), ALU.mult)
                    S.reduce("dve", dst.r.re("p (h n) -> p h n", h=8), eq, ALU.add)
                chk("IJ", If.r, Jf.r, tv.r, aq.r, bq.r)
                S.reduce("dve", mx.r, tv.r, ALU.max)
                S.tt("dve", tv.r, tv.r, mx.r.with_ap(mx.ap.unsqueeze(2).to_broadcast([128, 8, 16])), ALU.subtract)
                S.act(tv.r, tv.r, AF.Exp)
                S.reduce("dve", zs.r, tv.r, ALU.add)
                S.recip(zs.r, zs.r)
                S.tt("dve", Wf.r.re("p (h n) -> p h n", h=8), tv.r,
                     zs.r.with_ap(zs.ap.unsqueeze(2).to_broadcast([128, 8, 16])), ALU.mult)
                for k, (src, dst) in enumerate(((If, IT), (Jf, JT), (Wf, WT))):
                    S.tr(pw[k % 2][:, 0:128], src.r, ident.r)
                    S.copy("act", dst[:, ms], pw[k % 2][:, 0:128])
            chk("ITW", IT[:, 0:GN], JT[:, 0:GN], WT[:, 0:GN])
            for m in range(int(os.environ.get('MLIM', GN))):
                a = oiw[m % 2]
                b = oj[m % 2]
                S.ts("dve", a.r, iotab.r, IT[:, m:m + 1], WT[:, m:m + 1], op0=ALU.is_equal, op1=ALU.mult)
                S.ts("dve", b.r, iotab.r, JT[:, m:m + 1], None, op0=ALU.is_equal)
                p = pw[m % 2]
                S.mm(p[:, 0:128], a.r, b.r)
                S.copy("act", WW[:, m, :], p[:, 0:128])
            chk("WW", WW[:, 0:16, :])
            h2b = ys.r.with_ap(ys.ap.bitcast(BF16))[:, :, 0:GN]
            S.copy("act", h2b, h2[:, :, 0:GN])
            NBF = 3

            def load_j(j):
                S.dma(ubuf[j % 2].r, UT_d[j], f"ldu{j % 2}")
                S.dma(vbuf[j % 2].r, VJ_d[j], f"ldv{j % 2}")

            def cast_j(j):
                S.copy("act", ubf[j % NBF].r, ubuf[j % 2].r)
                S.copy("dve", vbf[j % NBF].r, vbuf[j % 2].r)

            def a_mm(j):
                p = pb[4 + (j % 4)]
                for dc in range(8):
                    S.mm(p[:, 0:GN], ubf[j % NBF][:, dc, :], h2b[:, dc, :], start=(dc == 0), stop=(dc == 7))

            load_j(0)
            load_j(1)
            cast_j(0)
            load_j(2)
            a_mm(0)
            for j in range(128):
                if j + 1 < 128:
                    cast_j(j + 1)
                    if j + 3 < 128:
                        load_j(j + 3)
                    a_mm(j + 1)
                g = gj[j % 2]
                pp = pjb[j % 2]
                S.act(g[:, 0:GN], pb[4 + (j % 4)][:, 0:GN], AF.Gelu_apprx_tanh)
                S.tt("dve", pp[:, 0:GN], g[:, 0:GN], WW[:, 0:GN, j], ALU.mult)
                vb = vbf[j % NBF]
                for oc in range(8):
                    S.mm(acc[oc // 2][:, (oc % 2) * 256:(oc % 2) * 256 + GN], vb[:, oc * 128:(oc + 1) * 128], pp[:, 0:GN],
                         start=(j == 0 and oc % 2 == 0), stop=(j == 127), skip=True)
            for oc in range(8):
                S.stt("dve", xs[:, oc, 0:GN], acc[oc // 2][:, (oc % 2) * 256:(oc % 2) * 256 + GN], gt2[:, oc, col:col + 1],
                      x1[:, oc, 0:GN], ALU.mult, ALU.add)
            out_sink(g0, GN, col, xs)

    try:
        build_body()
    except Done:
        while len(S.scopes) > 1:
            S.scopes.pop().close()
        raise
    S.pop_scope()


def build_B(NT, groups, stage=None):
    nc = bass.Bass("TRN2", target_bir_lowering=False)
    S = Sched(nc)
    xT_d = Tile(nc.dram_tensor("xT", [128, 8, NT], F32, kind="ExternalInput").ap(), "xT")
    yT_d = Tile(nc.dram_tensor("yT", [128, 8, NT], F32, kind="ExternalInput").ap(), "yT")
    out_d = Tile(nc.dram_tensor("outT", [128, 8, NT], F32, kind="ExternalOutput").ap(), "outT")
    dbg_d = Tile(nc.dram_tensor("dbg", [128, 4096], F32, kind="ExternalOutput").ap(), "dbg")
    pb = [S.ptile(f"pb{i}", [128, 512]) for i in range(8)]

    def x_src(g0, GN, col, xs):
        S.dma(xs[:, :, 0:GN], xT_d[:, :, g0:g0 + GN], "ldx")

    def y_src(g0, GN, col, ys, tmp):
        S.dma(ys[:, :, 0:GN], yT_d[:, :, g0:g0 + GN], "ldx")

    def out_sink(g0, GN, col, xs):
        S.dma(out_d[:, :, g0:g0 + GN], xs[:, :, 0:GN], "st")

    try:
        emit_B(S, nc, pb, "", groups, x_src, y_src, out_sink, stage, dbg_d)
    except Exception as e:
        if type(e).__name__ != "Done":
            raise
    S.wait_all("sp")
    while getattr(S, "scopes", None):
        S.scopes.pop().close()
    print("build_B instructions", S.n_ins, "sems", S.nsem)
    S.close()
    return nc

def fm(X):
    NT = X.shape[0]
    return np.ascontiguousarray(X.T.reshape(8, 128, NT).transpose(1, 0, 2))

def unfm(XT):
    NT = XT.shape[2]
    return np.ascontiguousarray(XT.transpose(1, 0, 2).reshape(1024, NT).T)

def vec_fm(v):
    return np.ascontiguousarray(v.reshape(-1, 128).T)

def consts():
    ident = np.eye(128, dtype=np.float32)
    iota = np.tile(np.arange(128, dtype=np.float32)[None, :], (128, 1))
    return ident, iota

def prep_B_weights(inp, l, permute_wout=False):
    d = {}
    aw = inp["ada_w"][l]
    d["adaw"] = np.ascontiguousarray(aw.reshape(8, 128, 6, 2, 512).transpose(2, 3, 1, 0, 4))
    d["adab"] = vec_fm(inp["ada_b"][l])
    d["n2g"] = vec_fm(inp["norm2_g"][l])
    wo = inp["w_out"][l]
    if permute_wout:
        g = np.arange(1024)
        r_, m_, ch_ = g // 256, (g % 256) // 64, g % 64
        wo = wo[m_ * 256 + r_ * 64 + ch_, :]
    d["wout"] = np.ascontiguousarray(wo.reshape(8, 128, 8, 128).transpose(2, 1, 0, 3))
    wq = inp["peer_wq"][l]
    d["wq"] = np.ascontiguousarray(wq.reshape(8, 128, 16, 128).transpose(2, 1, 0, 3))
    ks = inp["peer_keys"][l]
    d["keysT"] = np.ascontiguousarray(ks.reshape(16, 128, 128).transpose(2, 0, 1))
    u = inp["peer_u"][l]
    d["UT"] = np.ascontiguousarray(u.reshape(128, 128, 8, 128).transpose(1, 3, 2, 0))
    v = inp["peer_v"][l]
    d["VJ"] = np.ascontiguousarray(v.reshape(128, 128, 1024).transpose(1, 0, 2))
    d["ident"], d["iota"] = consts()
    return d

def cvec(inp, b):
    return np.ascontiguousarray(np.stack([vec_fm(inp["c"][b]), vec_fm(inp["c_ctx"])], axis=-1))

OFF = {"hx": 0, "cB": 256, "cC": 512, "r": 768, "k": 1024, "v": 1280, "xw": 1536, "xa": 1552, "xg": 1568,
       "aq": 1600, "ak": 1856, "av": 1984, "mq": 2112, "mk": 2368, "mv": 2624, "mo": 2880, "mg": 3136}

def packW(W, cols):
    Wc = W[:, cols]
    return np.ascontiguousarray(Wc.reshape(8, 128, len(cols)).transpose(1, 0, 2))

def rope_table():
    quarter = 16
    inv = (10000.0 ** (-np.arange(quarter, dtype=np.float32) / quarter)).astype(np.float32)
    t = np.arange(8192)
    row = (t // 64).astype(np.float32); col = (t % 64).astype(np.float32)
    ar = row[:, None] * inv[None, :]; ac = col[:, None] * inv[None, :]
    tab = np.concatenate([np.cos(ar), np.sin(ar), np.cos(ac), np.sin(ac)], -1).astype(np.float32)
    return np.ascontiguousarray(tab.reshape(64, 128, 64))

def prep_A_weights(inp, l, j):
    d = {}
    aw = inp["ada_w"][l]
    d["adaw"] = np.ascontiguousarray(aw.reshape(8, 128, 6, 2, 512).transpose(2, 3, 1, 0, 4))
    d["adab"] = vec_fm(inp["ada_b"][l])
    d["n1g"] = vec_fm(inp["norm1_g"][l])
    W = inp["w_in"][l]
    h64 = np.arange(64) + j * 64
    kv64 = np.arange(64) + (j // 2) * 64
    d["Wc"] = packW(W, np.concatenate([OFF["hx"] + h64, OFF["cB"] + h64, OFF["cC"] + h64]))
    d["Wr"] = packW(W, np.concatenate([OFF["r"] + h64, OFF["k"] + h64, OFF["v"] + h64, OFF["xw"] + np.arange(16),
                                       OFF["xa"] + np.arange(16), OFF["xg"] + np.arange(32)]))
    d["Wa"] = packW(W, np.concatenate([OFF["aq"] + h64, OFF["ak"] + kv64, OFF["av"] + kv64]))
    gcols = np.array([OFF["mg"] + dd * 8 + g * 4 + j for dd in range(2) for g in range(2)])
    d["Wm"] = packW(W, np.concatenate([OFF["mq"] + h64, OFF["mk"] + h64, OFF["mv"] + h64, OFF["mo"] + h64, gcols]))
    pp = np.zeros((64, 16), np.float32)
    pp[:, 0:3] = inp["conv_w"][l][:, h64].T
    pp[:, 3] = inp["conv_g"][l][j]
    pp[:, 4:6] = inp["rwkv_w0"][l][:, h64].T
    pp[:, 6:8] = inp["rwkv_a0"][l][:, h64].T
    pp[:, 8] = inp["rwkv_kk"][l][h64]
    pp[:, 9] = inp["rwkv_ka"][l][h64]
    pp[:, 11] = inp["rwkv_rk"][l][j]
    pp[:, 12] = inp["rwkv_ln_g"][l][j]
    d["pp"] = pp
    d["w2p"] = np.ascontiguousarray(inp["rwkv_w2"][l][:, :, h64].transpose(1, 0, 2))
    d["a2p"] = np.ascontiguousarray(inp["rwkv_a2"][l][:, :, h64].transpose(1, 0, 2))
    d["g2p"] = np.ascontiguousarray(inp["rwkv_g2"][l][:, h64])
    rb = np.stack([inp["att_q_g"][l], inp["att_k_g"][l], inp["att_out_g"][l][j], inp["ml_out_g"][l][j]], 0)
    d["rowbc"] = np.ascontiguousarray(np.tile(rb[None], (128, 1, 1)))
    sc = np.zeros((8,), np.float32)
    sc[0] = inp["att_sink"][l][j]
    sc[1] = inp["ml_i_b"][l][0, j]; sc[2] = inp["ml_f_b"][l][0, j]
    sc[3] = inp["ml_i_b"][l][1, j]; sc[4] = inp["ml_f_b"][l][1, j]
    d["scal"] = np.ascontiguousarray(np.tile(sc[None], (128, 1)))
    ident, _ = consts()
    d["ident"] = ident
    i = np.arange(128)
    d["trile"] = (i[:, None] <= i[None, :]).astype(np.float32)
    d["trige"] = (i[:, None] >= i[None, :]).astype(np.float32)
    i = np.arange(64)
    su = (i[:, None] < i[None, :]).astype(np.float32); iu = (i[:, None] <= i[None, :]).astype(np.float32)
    sl = (i[:, None] > i[None, :]).astype(np.float32); il = (i[:, None] >= i[None, :]).astype(np.float32)
    d["rmask"] = np.ascontiguousarray(np.stack([np.concatenate([su, iu, sl], 1), np.concatenate([sl, il, su], 1)], 0))
    cm = np.ones((64, 256), np.float32); cm[:, ::64] = 0.0
    d["cmask"] = cm
    d["rope"] = rope_table()
    return d

_NC_CACHE = {}


def _get_nc(kind, *args):
    key = (kind,) + args
    if key not in _NC_CACHE:
        if kind == "A":
            _NC_CACHE[key] = build_A()
        else:
            _NC_CACHE[key] = build_B(*args)
    return _NC_CACHE[key]


def kernel_unfused(**inputs):
    inp = {k: np.ascontiguousarray(np.asarray(v, dtype=np.float32)) for k, v in inputs.items()}
    x = inp["x"].copy()
    ctx = inp["ctx"].copy()
    cores = list(range(8))
    for l in range(2):
        ncA = _get_nc("A")
        maps = []
        wA = [prep_A_weights(inp, l, j) for j in range(4)]
        xfull = [fm(np.concatenate([ctx[b], x[b]], 0)) for b in range(2)]
        cvs = [cvec(inp, b) for b in range(2)]
        for c in cores:
            b, j = c // 4, c % 4
            d = dict(wA[j])
            d["xT"] = xfull[b]
            d["cv"] = cvs[b]
            maps.append(d)
        res = run_bass_kernel_spmd(ncA, maps, core_ids=cores)
        ycat = np.zeros((2, TT, 1024), np.float32)
        for c in cores:
            b, j = c // 4, c % 4
            yT = res.results[c]["yT"]
            for m in range(4):
                ycat[b, :, m * 256 + j * 64:m * 256 + (j + 1) * 64] = yT[m].T
        del res, maps, xfull
        with_ctx = (l == 0)
        if with_ctx:
            NT = 2048 + 128
            groups = tuple((g * 256, 256, 0) for g in range(8)) + ((2048, 128, 1),)
        else:
            NT = 2048
            groups = tuple((g * 256, 256, 0) for g in range(8))
        ncB = _get_nc("B", NT, groups)
        wB = prep_B_weights(inp, l)
        maps = []
        for c in cores:
            b, q = c // 4, c % 4
            xs_ = x[b, q * 2048:(q + 1) * 2048]
            ys_ = ycat[b, 256 + q * 2048:256 + (q + 1) * 2048]
            if with_ctx:
                pad = np.zeros((64, 1024), np.float32)
                xs_ = np.concatenate([xs_, ctx[b, q * 64:(q + 1) * 64], pad], 0)
                ys_ = np.concatenate([ys_, ycat[b, q * 64:(q + 1) * 64], pad], 0)
            d = dict(wB)
            d["xT"] = fm(xs_)
            d["yT"] = fm(ys_)
            d["cv"] = cvs[b]
            maps.append(d)
        res = run_bass_kernel_spmd(ncB, maps, core_ids=cores)
        for c in cores:
            b, q = c // 4, c % 4
            o = unfm(res.results[c]["outT"])
            x[b, q * 2048:(q + 1) * 2048] = o[:2048]
            if with_ctx:
                ctx[b, q * 64:(q + 1) * 64] = o[2048:2048 + 64]
        del res, maps
    return x


NTB = 2176
GROUPS4 = [[0, 1, 2, 3], [4, 5, 6, 7]]


def build_fused():
    nc = bass.Bass("TRN2", target_bir_lowering=False)
    S = Sched(nc)
    pb = [S.ptile(f"pb{i}", [128, 512]) for i in range(8)]
    xT_d = Tile(nc.dram_tensor("xT", [128, 8, TT], F32, kind="ExternalInput").ap(), "xT")
    xB_d = Tile(nc.dram_tensor("xB", [128, 8, NTB], F32, kind="ExternalInput").ap(), "xB")
    sel_d = Tile(nc.dram_tensor("sel", [128, 4], F32, kind="ExternalInput").ap(), "sel")
    out_d = Tile(nc.dram_tensor("outT", [128, 8, 2048], F32, kind="ExternalOutput").ap(), "outT")
    YC = 768
    NYC = TT // YC
    ybuf = [[Tile(nc.dram_tensor(f"ybuf{l}_{k}", [256, YC], F32).ap(), f"ybuf{l}_{k}") for k in range(NYC)] for l in range(2)]
    ygath = [[Tile(nc.dram_tensor(f"ygath{l}_{k}", [1024, YC], F32).ap(), f"ygath{l}_{k}") for k in range(NYC)] for l in range(2)]
    xw = [256] * 8 + [128]
    xown = [Tile(nc.dram_tensor(f"xown{k}", [128, 8 * xw[k]], F32).ap(), f"xown{k}") for k in range(9)]
    xg = [Tile(nc.dram_tensor(f"xg{k}", [512, 8 * xw[k]], F32).ap(), f"xg{k}") for k in range(9)]
    sel = S.tile("sel", [128, 4])
    S.dma(sel.r, sel_d.r, "ldc")

    def xown_v(k):
        return xown[k].r.re("p (c t) -> p c t", c=8)

    def xg_v(k):
        return xg[k].r.re("(r p) (c t) -> r p c t", r=4, c=8)

    def yv(l, t0, n):
        k, o = t0 // YC, t0 % YC
        assert o + n <= YC
        return ygath[l][k].r.re("(kc p) t -> p kc t", p=128)[:, :, o:o + n]

    for l in range(2):
        sfx = f"_l{l}"
        def load_x(ti, xs, l=l):
            if l == 0:
                S.dma(xs.r, xT_d[:, :, ti * N:(ti + 1) * N], "ldx")
            elif ti == 0:
                for r in range(4):
                    S.dma(xs[:, :, r * 64:(r + 1) * 64], xg_v(8)[r, :, :, 0:64], "ldx")
            else:
                r, k = (ti - 1) // 8, (ti - 1) % 8
                S.dma(xs.r, xg_v(k)[r], "ldx")

        def store_y(m, t0, n, src, l=l):
            k, o = t0 // YC, t0 % YC
            assert o + n <= YC
            S.dma(ybuf[l][k][m * 64:(m + 1) * 64, o:o + n], src, "st")

        emit_A(S, nc, pb, sfx, load_x, store_y, tile_limit=(int(os.environ["FUSED_TILES"]) if "FUSED_TILES" in os.environ else None))
        for k in range(NYC):
            S.all_gather(ygath[l][k].r, ybuf[l][k].r, GROUPS4)
        groups = [(g * 256, 256, 0) for g in range(8)]
        if l == 0:
            groups.append((2048, 128, 1))

        def x_src(g0, GN, col, xs, l=l):
            if l == 0:
                S.dma(xs[:, :, 0:GN], xB_d[:, :, g0:g0 + GN], "ldx")
            else:
                S.dma(xs[:, :, 0:GN], xown_v(g0 // 256), "ldx")

        def y_src(g0, GN, col, ys, tmp, l=l):
            if col == 0:
                n = GN
                srcs = [yv(l, 256 + q * 2048 + g0, GN) for q in range(4)]
            else:
                n = 64
                S.memset("dve", ys[:, :, 0:GN], 0.0)
                srcs = [yv(l, q * 64, 64) for q in range(4)]
            for q in range(4):
                S.dma(tmp[:, :, 0:n], srcs[q], "ldx")
                if q == 0:
                    S.ts("dve", ys[:, :, 0:n], tmp[:, :, 0:n], sel[:, 0:1], None, op0=ALU.mult)
                else:
                    S.stt("dve", ys[:, :, 0:n], tmp[:, :, 0:n], sel[:, q:q + 1], ys[:, :, 0:n], ALU.mult, ALU.add)

        def out_sink(g0, GN, col, xs, l=l):
            if l == 0:
                k = g0 // 256
                S.dma(xown_v(k), xs[:, :, 0:GN], "st")
                S.all_gather(xg[k].r, xown[k].r, GROUPS4)
            else:
                S.dma(out_d[:, :, g0:g0 + GN], xs[:, :, 0:GN], "st")

        if "FUSED_GROUPS" in os.environ:
            groups = groups[:int(os.environ["FUSED_GROUPS"])]
        emit_B(S, nc, pb, sfx, groups, x_src, y_src, out_sink)
    S.barrier()
    print("fused instructions", S.n_ins, "sems", S.nsem, "sbuf left", nc.sbuf_bytes_remaining)
    S.close()
    return nc


_FUSED = {}


def kernel_fused(**inputs):
    inp = {k: np.ascontiguousarray(np.asarray(v, dtype=np.float32)) for k, v in inputs.items()}
    x, ctx = inp["x"], inp["ctx"]
    if "nc" not in _FUSED:
        _FUSED["nc"] = build_fused()
    nc = _FUSED["nc"]
    shared_keys = ("ident", "iota", "trile", "trige", "rmask", "cmask", "rope")
    base = {}
    wA = {}
    for l in range(2):
        wb = prep_B_weights(inp, l, permute_wout=True)
        for k, v in wb.items():
            if k in shared_keys:
                base[k] = v
            else:
                base[k + f"_l{l}"] = v
        for j in range(4):
            wa = prep_A_weights(inp, l, j)
            d = {}
            for k, v in wa.items():
                if k in shared_keys:
                    base[k] = v
                elif k in ("adaw", "adab"):
                    pass
                else:
                    d[k + f"_l{l}"] = v
            wA[(l, j)] = d
    pad = np.zeros((64, 1024), np.float32)
    maps = []
    for c in range(8):
        b, r = c // 4, c % 4
        d = dict(base)
        d.update(wA[(0, r)])
        d.update(wA[(1, r)])
        d["cv"] = cvec(inp, b)
        d["xT"] = fm(np.concatenate([ctx[b], x[b]], 0))
        d["xB"] = fm(np.concatenate([x[b, r * 2048:(r + 1) * 2048], ctx[b, r * 64:(r + 1) * 64], pad], 0))
        s = np.zeros((128, 4), np.float32)
        s[:, r] = 1.0
        d["sel"] = s
        maps.append(d)
    res = run_bass_kernel_spmd(nc, maps, core_ids=list(range(8)))
    out = np.zeros_like(x)
    for c in range(8):
        b, r = c // 4, c % 4
        out[b, r * 2048:(r + 1) * 2048] = unfm(res.results[c]["outT"])
    return out


def kernel(**inputs):
    return kernel_fused(**inputs)
```

```python
from concourse.bass_utils import run_bass_kernel_spmd
import contextlib
import numpy as np
import concourse.bass as bass
import concourse.mybir as mybir

F32 = mybir.dt.float32
BF16 = mybir.dt.bfloat16
F32R = mybir.dt.float32r
U32 = mybir.dt.uint32
AF = mybir.ActivationFunctionType
ALU = mybir.AluOpType
AX = mybir.AxisListType


class Tile:
    def __init__(self, ap, name=""):
        self.ap = ap
        self.name = name
        self.writers = {}
        self.readers = {}
        self.exclusive = False

    def __getitem__(self, key):
        return Ref(self, self.ap[key])

    @property
    def r(self):
        return Ref(self, self.ap)


class Ref:
    def __init__(self, tile, ap):
        self.tile = tile
        self.ap = ap

    def __getitem__(self, key):
        return Ref(self.tile, self.ap[key])

    def re(self, s, **kw):
        return Ref(self.tile, self.ap.rearrange(s, **kw))

    def bc(self, shape):
        return Ref(self.tile, self.ap.to_broadcast(shape))

    def with_ap(self, ap):
        return Ref(self.tile, ap)


def _ap(x):
    return x.ap if isinstance(x, Ref) else x


class Sched:
    COMPUTE_ROT = 30000
    DMA_ROT = 1900

    def __init__(self, nc):
        self.nc = nc
        self.es = contextlib.ExitStack()
        self.eng = {"pe": nc.tensor, "dve": nc.vector, "act": nc.scalar,
                    "pool": nc.gpsimd, "sp": nc.sync}
        self.sem = {}
        self.cnt = {}
        self.epoch = {}
        self.waited = {}
        self.nsem = 0
        self.n_ins = 0
        self.allsems = {}
        self.dram_cache = {}
        self.dma_keys = set()
        self.recording = None
        self.scopes = []

    def shared_dram(self, name, shape, dt=F32):
        if name not in self.dram_cache:
            t = self.nc.dram_tensor(name, list(shape), dt, kind="ExternalInput")
            self.dram_cache[name] = Tile(t.ap(), name)
        return self.dram_cache[name]

    def push_scope(self):
        self.scopes.append(contextlib.ExitStack())

    def pop_scope(self):
        print("scope end: sbuf left", self.nc.sbuf_bytes_remaining)
        self.barrier()
        self.scopes.pop().close()

    def barrier(self):
        for e in ("pe", "dve", "act", "pool", "sp"):
            self.wait_all(e)

    def sbuf(self, name, shape, dtype=F32):
        es = self.scopes[-1] if getattr(self, "scopes", None) else self.es
        self.nalloc = getattr(self, "nalloc", 0) + 1
        return es.enter_context(self.nc.sbuf_tensor(f"sb{self.nalloc}_" + name, list(shape), dtype))

    def psum(self, name, shape, dtype=F32):
        return self.es.enter_context(self.nc.psum_tensor("ps_" + name, list(shape), dtype))

    def tile(self, name, shape, dtype=F32):
        t = self.sbuf(name, shape, dtype)
        return Tile(t[tuple(slice(None) for _ in shape)], name)

    def ptile(self, name, shape, dtype=F32):
        t = self.psum(name, shape, dtype)
        tl = Tile(t[tuple(slice(None) for _ in shape)], name)
        tl.exclusive = True
        return tl

    def _stream(self, stream, is_dma):
        if stream not in self.cnt:
            self.epoch[stream] = 0
            self._newsem(stream)
        key, c = self.cnt[stream]
        lim = self.DMA_ROT if is_dma else self.COMPUTE_ROT
        if c >= lim:
            self.epoch[stream] += 1
            self._newsem(stream)
        return self.cnt[stream]

    def _newsem(self, stream):
        key = f"{stream}_{self.epoch[stream]}"
        h = self.es.enter_context(self.nc.semaphore(f"s_{key}"))
        self.sem[key] = h
        self.cnt[stream] = (key, 0)
        self.nsem += 1

    def emit(self, engine, fn, outs=(), ins=(), dma_group=None, inc_override=None):
        if self.recording is not None:
            self.recording.append((engine, fn, outs, ins, dma_group, inc_override))
            return None
        is_dma = dma_group is not None
        stream = dma_group if is_dma else engine
        key, c = self._stream(stream, is_dma)
        deps = {}

        def add(d):
            if d is None:
                return
            k, v = d
            if deps.get(k, 0) < v:
                deps[k] = v

        outs = list(outs) + [r for r in ins if isinstance(r, Ref) and r.tile.exclusive]
        for r in ins:
            if isinstance(r, Ref):
                for k, v in r.tile.writers.items():
                    add((k, v))
        for o in outs:
            if isinstance(o, Ref):
                for k, v in o.tile.writers.items():
                    add((k, v))
                for k, v in o.tile.readers.items():
                    add((k, v))
        e = self.eng[engine]
        for k, v in list(deps.items()):
            if k in self.dma_keys:
                v = max(v, self.allsems.get(k, v))
                deps[k] = v
        for k, v in deps.items():
            if engine == "pe" and not is_dma and k.rsplit("_", 1)[0] == "pe":
                continue
            if self.waited.get((engine, k), 0) >= v:
                continue
            e.wait_ge(self.sem[k], v)
            self.waited[(engine, k)] = v
        ins_obj = fn()
        inc = 16 if is_dma else 1
        if inc_override is not None:
            inc = inc_override
        c += inc
        self.cnt[stream] = (key, c)
        self.allsems[key] = c
        if is_dma:
            self.dma_keys.add(key)
        ins_obj.then_inc(self.sem[key], inc)
        self.n_ins += 1
        me = (key, c)
        for o in outs:
            if isinstance(o, Ref):
                o.tile.writers[key] = c
                o.tile.readers = {}
        for r in ins:
            if isinstance(r, Ref):
                if not any(r.tile is o.tile for o in outs if isinstance(o, Ref)):
                    if r.tile.readers.get(key, 0) < c:
                        r.tile.readers[key] = c
        return ins_obj

    def wait_all(self, engine="sp"):
        e = self.eng[engine]
        for key, c in list(self.allsems.items()):
            if c > 0 and self.waited.get((engine, key), 0) < c:
                e.wait_ge(self.sem[key], c)
                self.waited[(engine, key)] = c

    def close(self):
        self.es.close()

    def record(self, fn):
        assert self.recording is None
        self.recording = []
        try:
            fn()
        finally:
            rec, self.recording = self.recording, None
        return rec

    def replay_interleaved(self, recs):
        idx = [0] * len(recs)
        live = True
        while live:
            live = False
            for i, r in enumerate(recs):
                if idx[i] < len(r):
                    self.emit(*r[idx[i]])
                    idx[i] += 1
                    live = True

    def all_gather(self, out, in_, groups):
        return self.emit("pool", lambda: self.nc.gpsimd.collective_compute(
            "AllGather", ALU.bypass, replica_groups=groups, ins=[_ap(in_).opt()], outs=[_ap(out).opt()]),
            outs=[out], ins=[in_], dma_group=f"cc{self._ncc()}", inc_override=1)

    def _ncc(self):
        self.ncc = getattr(self, "ncc", 0) + 1
        return self.ncc

    def dma(self, out, in_, group="ld0", q="sp"):
        return self.emit(q, lambda: self.eng[q].dma_start(out=_ap(out), in_=_ap(in_)),
                         outs=[out], ins=[in_], dma_group=group)

    def mm(self, out, lhsT, rhs, start=True, stop=True, skip=False):
        return self.emit("pe", lambda: self.nc.tensor.matmul(_ap(out), lhsT=_ap(lhsT), rhs=_ap(rhs),
                                                              start=start, stop=stop, skip_group_check=skip),
                         outs=[out], ins=[lhsT, rhs] + ([] if start else [out]))

    def tr(self, out, in_, ident):
        return self.emit("pe", lambda: self.nc.tensor.transpose(_ap(out), _ap(in_), _ap(ident)),
                         outs=[out], ins=[in_, ident])

    def act(self, out, in_, func, bias=None, scale=None, accum_out=None):
        kw = {}
        ins = [in_]
        outs = [out]
        if bias is not None:
            kw["bias"] = _ap(bias)
            ins.append(bias)
        if scale is not None:
            kw["scale"] = _ap(scale)
            ins.append(scale)
        if accum_out is not None:
            kw["accum_out"] = _ap(accum_out)
            outs.append(accum_out)
        return self.emit("act", lambda: self.nc.scalar.activation(out=_ap(out), in_=_ap(in_), func=func, **kw),
                         outs=outs, ins=ins)

    def tt(self, eng, out, in0, in1, op):
        return self.emit(eng, lambda: self.eng[eng].tensor_tensor(out=_ap(out), in0=_ap(in0), in1=_ap(in1), op=op),
                         outs=[out], ins=[in0, in1])

    def ts(self, eng, out, in0, s1, s2=None, op0=ALU.mult, op1=None, accum_out=None):
        kw = {}
        outs = [out]
        if op1 is not None:
            kw["op1"] = op1
        if accum_out is not None:
            kw["accum_out"] = _ap(accum_out)
            outs.append(accum_out)
        return self.emit(eng, lambda: self.eng[eng].tensor_scalar(out=_ap(out), in0=_ap(in0), scalar1=_ap(s1),
                                                                  scalar2=_ap(s2), op0=op0, **kw),
                         outs=outs, ins=[in0, s1, s2])

    def stt(self, eng, out, in0, scalar, in1, op0, op1):
        return self.emit(eng, lambda: self.eng[eng].scalar_tensor_tensor(out=_ap(out), in0=_ap(in0), scalar=_ap(scalar),
                                                                         in1=_ap(in1), op0=op0, op1=op1),
                         outs=[out], ins=[in0, scalar, in1])

    def copy(self, eng, out, in_):
        if eng == "act":
            return self.emit("act", lambda: self.nc.scalar.copy(out=_ap(out), in_=_ap(in_)), outs=[out], ins=[in_])
        return self.emit(eng, lambda: self.eng[eng].tensor_copy(out=_ap(out), in_=_ap(in_)), outs=[out], ins=[in_])

    def memset(self, eng, out, val):
        return self.emit(eng, lambda: self.eng[eng].memset(_ap(out), val), outs=[out], ins=[])

    def reduce(self, eng, out, in_, op, axis=AX.X):
        return self.emit(eng, lambda: self.eng[eng].tensor_reduce(out=_ap(out), in_=_ap(in_), axis=axis, op=op),
                         outs=[out], ins=[in_])

    def recip(self, out, in_):
        return self.emit("dve", lambda: self.nc.vector.reciprocal(out=_ap(out), in_=_ap(in_)), outs=[out], ins=[in_])

    def vmax(self, out, in_):
        return self.emit("dve", lambda: self.nc.vector.max(out=_ap(out), in_=_ap(in_)), outs=[out], ins=[in_])

    def vmax_index(self, out, in_max, in_values):
        return self.emit("dve", lambda: self.nc.vector.max_index(out=_ap(out), in_max=_ap(in_max), in_values=_ap(in_values)),
                         outs=[out], ins=[in_max, in_values])

    def vmatch_replace(self, out, in_to_replace, in_values, imm):
        return self.emit("dve", lambda: self.nc.vector.match_replace(out=_ap(out), in_to_replace=_ap(in_to_replace),
                                                                    in_values=_ap(in_values), imm_value=imm),
                         outs=[out], ins=[in_to_replace, in_values])

    def scan(self, out, data0, data1, initial, op0, op1):
        return self.emit("dve", lambda: self.nc.vector.tensor_tensor_scan(out=_ap(out), data0=_ap(data0), data1=_ap(data1),
                                                                          initial=_ap(initial), op0=op0, op1=op1),
                         outs=[out], ins=[data0, data1, initial])
import os

EPS = 1e-6
PROJ_DT = F32
import math
RWKV_DECAY_SCALE = math.exp(-0.5)
TT = 8448
NTILE = 33
N = 256


def emit_A(S, nc, pb, sfx="", load_x=None, store_y=None, stage=None, dbg_d=None,
           mixers=("conv", "attn", "mlstm", "rwkv"), tile_limit=None):
    def D(name, shape, kind="ExternalInput", dt=F32):
        t = nc.dram_tensor(name + sfx, list(shape), dt, kind=kind)
        return Tile(t.ap(), name + sfx)

    cv_d = S.shared_dram("cv", [128, 8, 2])
    adaw_d = S.shared_dram("adaw" + sfx, [6, 2, 128, 8, 512])
    adab_d = S.shared_dram("adab" + sfx, [128, 48])
    n1g_d = D("n1g", [128, 8])
    Wc_d = D("Wc", [128, 8, 192])
    Wr_d = D("Wr", [128, 8, 256])
    Wa_d = D("Wa", [128, 8, 192])
    Wm_d = D("Wm", [128, 8, 260])
    pp_d = D("pp", [64, 16])
    w2_d = D("w2p", [16, 2, 64])
    a2_d = D("a2p", [16, 2, 64])
    g2_d = D("g2p", [32, 64])
    rowbc_d = D("rowbc", [128, 4, 64])
    scal_d = D("scal", [128, 8])
    ident_d = S.shared_dram("ident", [128, 128])
    trile_d = S.shared_dram("trile", [128, 128])
    trige_d = S.shared_dram("trige", [128, 128])
    rmask_d = S.shared_dram("rmask", [2, 64, 192])
    cmask_d = S.shared_dram("cmask", [64, 256])
    rope_d = S.shared_dram("rope", [64, 128, 64])

    class Done(Exception):
        pass

    dbg_n = [0]

    def chk(name, *refs):
        if stage != name:
            return
        o = 0
        for r in refs:
            n = 1
            for s_ in r.ap.shape[1:]:
                n *= s_
            P = r.ap.shape[0]
            t = S.tile(f"dbgt{dbg_n[0]}", [128, n])
            dbg_n[0] += 1
            tv = t[0:P, :]
            shp = r.ap.shape
            if len(shp) == 3:
                tv = tv.re("p (a b) -> p a b", a=shp[1])
            elif len(shp) == 4:
                tv = tv.re("p (a b c) -> p a b c", a=shp[1], b=shp[2])
            S.copy("dve", tv, r)
            S.dma(dbg_d[0:P, o:o + n], t[0:P, :], "st")
            o += n
        raise Done()

    S.push_scope()
    ident = S.tile("ident", [128, 128])
    ones = S.tile("ones", [128, 128])
    cv = S.tile("cv", [128, 8, 2])
    adab = S.tile("adab", [128, 48])
    n1g = S.tile("n1g", [128, 8])
    mod0 = S.tile("mod0", [128, 8, 2])
    mod1 = S.tile("mod1", [128, 8, 2])
    gm1 = S.tile("gm1", [128, 8, 2])
    pp = S.tile("pp", [64, 16])
    omka = S.tile("omka", [64, 1])
    rowbc = S.tile("rowbc", [128, 4, 64])
    scal = S.tile("scal", [128, 8])
    xs = S.tile("xs", [128, 8, N])
    hT = S.tile("hT", [128, 8, N], PROJ_DT)
    sqs = S.tile("sqs", [128, 8, N])
    wstage = S.tile("wstage", [128, 8, 260])
    rstd = S.tile("rstd", [128, N])

    def body():
        S.dma(ident.r, ident_d.r, "ldc")
        S.dma(cv.r, cv_d.r, "ldc")
        S.dma(adab.r, adab_d.r, "ldc")
        S.dma(n1g.r, n1g_d.r, "ldc")
        S.dma(pp.r, pp_d.r, "ldc")
        S.dma(rowbc.r, rowbc_d.r, "ldc")
        S.dma(scal.r, scal_d.r, "ldc")
        S.memset("dve", ones.r, 1.0)
        S.act(cv.r, cv.r, AF.Silu)
        S.ts("dve", omka.r, pp[:, 9:10], -1.0, 1.0, op0=ALU.mult, op1=ALU.add)
        scr = S.tile("scr", [128, 4096])
        adaw_t = scr.r.re("p (c f) -> p c f", c=8)
        for seg, dst in ((0, mod0), (1, mod1)):
            for half in range(2):
                S.dma(adaw_t, adaw_d[seg, half], "ldc")
                for f4 in range(4):
                    fc = half * 4 + f4
                    for dc in range(8):
                        S.mm(pb[0][:, fc * 2:fc * 2 + 2], adaw_t[:, dc, f4 * 128:(f4 + 1) * 128], cv[:, dc, :],
                             start=(dc == 0), stop=(dc == 7))
            S.tt("dve", dst.r, pb[0][:, 0:16].re("p (c t) -> p c t", t=2),
                 adab.r.with_ap(adab.ap[:, seg * 8:(seg + 1) * 8].unsqueeze(2).to_broadcast([128, 8, 2])), ALU.add)
        S.ts("dve", gm1.r, mod1.r, 1.0, None, op0=ALU.add)
        S.tt("dve", gm1.r, gm1.r, n1g.r.with_ap(n1g.ap.unsqueeze(2).to_broadcast([128, 8, 2])), ALU.mult)
        sh1 = mod0
        chk("mod", mod0.r, gm1.r)

        hbuf = Tile(nc.dram_tensor("hbuf" + sfx, [128, 8, TT], F32).ap(), "hbuf" + sfx)
        h_done = set()

        def load_h(ti, hb=None):
            if hb is not None:
                return load_h_impl(ti, *hb)
            return load_h_impl(ti, hT, xs, rstd, pb[0], sqs)

        def load_w(dst, src_d, ncol):
            S.dma(wstage[:, :, 0:ncol], src_d.r, "ldc")
            S.copy("dve", dst[:, :, 0:ncol], wstage[:, :, 0:ncol])

        def load_h_impl(ti, hT, xs, rstd, pbank, sqs):
            t0 = ti * N
            col = 1 if ti == 0 else 0
            if ti in h_done:
                if PROJ_DT == F32:
                    S.dma(hT.r, hbuf[:, :, t0:t0 + N], "ldx")
                else:
                    S.dma(xs.r, hbuf[:, :, t0:t0 + N], "ldx")
                    S.copy("dve", hT.r, xs.r)
                return
            load_x(ti, xs)
            S.act(sqs.r, xs.r, AF.Square)
            for kc in range(8):
                S.mm(pbank[:, 0:N], ones.r, sqs[:, kc, :], start=(kc == 0), stop=(kc == 7))
            S.ts("dve", rstd.r, pbank[:, 0:N], 1.0 / 1024.0, EPS, op0=ALU.mult, op1=ALU.add)
            S.act(rstd.r, rstd.r, AF.Sqrt)
            S.recip(rstd.r, rstd.r)
            S.tt("dve", sqs.r, xs.r, rstd.r.with_ap(rstd.ap.unsqueeze(1).to_broadcast([128, 8, N])), ALU.mult)
            for kc in range(8):
                S.ts("pool" if kc % 2 else "dve", hT[:, kc, :], sqs[:, kc, :], gm1[:, kc, col:col + 1], sh1[:, kc, col:col + 1],
                     op0=ALU.mult, op1=ALU.add)
            S.dma(hbuf[:, :, t0:t0 + N], (hT.r if PROJ_DT == F32 else hT.r.with_ap(hT.ap.bitcast(F32))), "sth")
            if S.recording is None:
                h_done.add(ti)

        tiles = list(range(NTILE)) if tile_limit is None else list(range(tile_limit))

        def head_norm_fm(y, sq, out, g_col, psb, n=N):
            S.act(sq, y, AF.Square)
            S.mm(psb[0:64, 0:n], ones[0:64, 0:64], sq)
            S.ts("dve", sq, psb[0:64, 0:n], 1.0 / 64.0, EPS, op0=ALU.mult, op1=ALU.add)
            S.act(sq, sq, AF.Sqrt)
            S.recip(sq, sq)
            S.stt("dve", out, y, g_col, sq, ALU.mult, ALU.mult)

        if "conv" in mixers:
            S.push_scope()
            Wc = S.tile("Wc", [128, 8, 192], PROJ_DT)
            load_w(Wc, Wc_d, 192)
            U = S.tile("convU", [64, TT + 4])
            Bg = S.tile("convB", [64, TT])
            S.memset("dve", U.r, 0.0)

            def ucol(t):
                return t + 1 if t < 256 else t + 3

            for ti in tiles:
                load_h(ti)
                t0 = ti * N
                for g in range(3):
                    for kc in range(8):
                        S.mm(pb[1 + g][0:64, 0:N], Wc[:, kc, g * 64:(g + 1) * 64], hT[:, kc, :], start=(kc == 0), stop=(kc == 7))
                S.copy("act", Bg[:, t0:t0 + N], pb[2][0:64, 0:N])
                S.copy("act", U[:, ucol(t0):ucol(t0) + N], pb[1][0:64, 0:N])
                S.tt("dve", U[:, ucol(t0):ucol(t0) + N], U[:, ucol(t0):ucol(t0) + N], pb[3][0:64, 0:N], ALU.mult)
            cy = S.tile("convy", [64, N])
            csq = S.tile("convsq", [64, N])
            for ti in tiles:
                t0 = ti * N
                u0 = ucol(t0)
                S.ts("dve", cy.r, U[:, u0 - 1:u0 - 1 + N], pp[:, 0:1], None, op0=ALU.mult)
                S.stt("dve", cy.r, U[:, u0:u0 + N], pp[:, 1:2], cy.r, ALU.mult, ALU.add)
                S.stt("dve", cy.r, U[:, u0 + 1:u0 + 1 + N], pp[:, 2:3], cy.r, ALU.mult, ALU.add)
                S.tt("dve", cy.r, cy.r, Bg[:, t0:t0 + N], ALU.mult)
                head_norm_fm(cy.r, csq.r, cy.r, pp[:, 3:4], pb[1])
                store_y(0, t0, N, cy.r)
            chk("conv", cy.r)
            S.pop_scope()

        if "attn" in mixers:
            S.push_scope()
            NA = 192 if PROJ_DT == F32 else 256
            Wa = S.tile("Wa", [128, 8, 256], PROJ_DT)
            S.memset("dve", (Wa.r if PROJ_DT == F32 else Wa.r.with_ap(Wa.ap.bitcast(F32))), 0.0)
            load_w(Wa, Wa_d, 192)
            trile = S.tile("trile", [128, 128])
            trige = S.tile("trige", [128, 128])
            S.dma(trile.r, trile_d.r, "ldc")
            S.dma(trige.r, trige_d.r, "ldc")
            QT = S.tile("QT", [64, TT])
            KT = S.tile("KT", [64, TT])
            V1 = S.tile("V1", [128, 66, 65])
            S.memset("dve", V1.r, 1.0)
            rope = S.tile("ropet", [128, 64])
            qk = S.tile("qk", [128, 2, 64])
            qr = S.tile("qr", [128, 2, 64])
            tmpa = S.tile("tmpa", [128, 2, 2, 16])
            ssq = S.tile("ssq", [128, 2])
            junk = S.tile("junk", [128, 64])
            junk2 = S.tile("junk2", [128, 128])
            for ti in tiles:
                load_h(ti)
                for sub in range(2):
                    bi = ti * 2 + sub
                    t0 = bi * 128
                    for kc in range(8):
                        S.mm(pb[1][:, 0:NA], hT[:, kc, sub * 128:(sub + 1) * 128], Wa[:, kc, 0:NA], start=(kc == 0), stop=(kc == 7))
                    S.copy("act", V1[:, bi, 0:64], pb[1][:, 128:192])
                    S.act(junk2.r, pb[1][:, 0:128], AF.Square)
                    S.reduce("dve", ssq.r, junk2.r.re("p (w f) -> p w f", w=2), ALU.add)
                    S.ts("dve", ssq.r, ssq.r, 1.0 / 64.0, EPS, op0=ALU.mult, op1=ALU.add)
                    S.act(ssq.r, ssq.r, AF.Sqrt)
                    S.recip(ssq.r, ssq.r)
                    for w in range(2):
                        S.stt("dve", qk[:, w, :], pb[1][:, w * 64:(w + 1) * 64], ssq[:, w:w + 1], rowbc[:, w, :], ALU.mult, ALU.mult)
                    src = qk
                    if bi >= 2:
                        S.dma(rope.r, rope_d[bi - 2], "ldr")
                        cosv = rope.r.with_ap(rope.ap.rearrange("p (h cs f) -> p h cs f", h=2, cs=2)[:, :, 0, :])
                        sinv = rope.r.with_ap(rope.ap.rearrange("p (h cs f) -> p h cs f", h=2, cs=2)[:, :, 1, :])
                        for w in range(2):
                            q4 = qk[:, w, :].re("p (h x f) -> p h x f", h=2, x=2)
                            o4 = qr[:, w, :].re("p (h x f) -> p h x f", h=2, x=2)
                            S.tt("dve", o4, q4, cosv.with_ap(cosv.ap.unsqueeze(2).to_broadcast([128, 2, 2, 16])), ALU.mult)
                            S.tt("pool", tmpa[:, :, 0, :], q4[:, :, 1, :], sinv, ALU.mult)
                            S.tt("pool", tmpa[:, :, 1, :], q4[:, :, 0, :], sinv, ALU.mult)
                            S.tt("dve", o4[:, :, 0, :], o4[:, :, 0, :], tmpa[:, :, 0, :], ALU.subtract)
                            S.tt("dve", o4[:, :, 1, :], o4[:, :, 1, :], tmpa[:, :, 1, :], ALU.add)
                        src = qr
                    S.tr(pb[2][0:64, 0:128], src[:, 0, :], ident.r)
                    S.copy("act", QT[:, t0:t0 + 128], pb[2][0:64, 0:128])
                    S.tr(pb[3][0:64, 0:128], src[:, 1, :], ident.r)
                    S.copy("act", KT[:, t0:t0 + 128], pb[3][0:64, 0:128])
            chk("attn_qk", QT.r.re("p (t f) -> p t f", f=4)[:, :, 0])
            nblk = len(tiles) * 2
            E = [S.tile(f"attE{i}", [128, 128]) for i in range(5)]
            esink = S.tile("esink", [128, 1])
            S.act(esink.r, scal[:, 0:1], AF.Exp)
            den = S.tile("attden", [128, 1])
            ao = S.tile("atto", [128, 64])
            for bi in range(nblk):
                t0 = bi * 128
                if bi < 2:
                    kbs = [(0, None), (1, None)]
                else:
                    kbs = [(0, None), (1, None)]
                    if bi - 1 >= 2:
                        kbs.append((bi - 1, trige))
                    kbs.append((bi, None))
                    if bi + 1 < nblk:
                        kbs.append((bi + 1, trile))
                for i, (kb, mask) in enumerate(kbs):
                    ps = pb[1 + (i % 2)]
                    S.mm(ps[:, 0:128], KT[:, kb * 128:(kb + 1) * 128], QT[:, t0:t0 + 128])
                    S.act(E[i].r, ps[:, 0:128], AF.Exp, scale=0.125)
                    if mask is not None:
                        S.tt("dve", E[i].r, E[i].r, mask.r, ALU.mult)
                chk("attn_E", E[0].r, E[1].r)
                for i, (kb, mask) in enumerate(kbs):
                    S.mm(pb[3][:, 0:65], E[i].r, V1[:, kb, :], start=(i == 0), stop=(i == len(kbs) - 1))
                chk("attn_pv", pb[3][:, 0:65])
                S.tt("dve", den.r, pb[3][:, 64:65], esink.r, ALU.add)
                S.recip(den.r, den.r)
                S.ts("dve", ao.r, pb[3][:, 0:64], den.r, None, op0=ALU.mult)
                S.act(junk.r, ao.r, AF.Square)
                S.reduce("dve", ssq[:, 0:1], junk.r, ALU.add)
                S.ts("dve", ssq[:, 0:1], ssq[:, 0:1], 1.0 / 64.0, EPS, op0=ALU.mult, op1=ALU.add)
                S.act(ssq[:, 0:1], ssq[:, 0:1], AF.Sqrt)
                S.recip(ssq[:, 0:1], ssq[:, 0:1])
                S.stt("dve", ao.r, ao.r, ssq[:, 0:1], rowbc[:, 2, :], ALU.mult, ALU.mult)
                chk("attn_ao", ao.r)
                S.tr(pb[4][0:64, 0:128], ao.r, ident.r)
                S.copy("act", qk[0:64, :, :].re("p a b -> p (a b)"), pb[4][0:64, 0:128])
                store_y(2, t0, 128, qk[0:64, :, :].re("p a b -> p (a b)"))
                if bi == int(os.environ.get("BLIM", "99")):
                    chk("attn_blk", ao.r)
            chk("attn", ao.r)
            S.pop_scope()

        if "mlstm" in mixers:
            S.push_scope()
            Wm = S.tile("Wm", [128, 8, 260], PROJ_DT)
            load_w(Wm, Wm_d, 260)
            trile = S.tile("trile", [128, 128])
            trige = S.tile("trige", [128, 128])
            S.dma(trile.r, trile_d.r, "ldc")
            S.dma(trige.r, trige_d.r, "ldc")
            nblk = len(tiles) * 2
            Qm = S.tile("Qm", [128, 66, 64])
            Km = S.tile("Km", [128, 66, 64])
            Vm1 = S.tile("Vm1", [128, 66, 65])
            Om = S.tile("Om", [128, 66, 64])
            Hs = S.tile("Hs", [128, 66, 64])
            G = S.tile("G", [128, 66, 4])
            nfb = S.tile("nfb", [128, 2])
            S.memset("dve", Vm1.r, 1.0)
            S.ts("dve", nfb[:, 0:1], scal[:, 2:3], -1.0, None, op0=ALU.mult)
            S.ts("dve", nfb[:, 1:2], scal[:, 4:5], -1.0, None, op0=ALU.mult)
            for ti in tiles:
                load_h(ti)
                for sub in range(2):
                    bi = ti * 2 + sub
                    for kc in range(8):
                        S.mm(pb[1][:, 0:260], hT[:, kc, sub * 128:(sub + 1) * 128], Wm[:, kc, :], start=(kc == 0), stop=(kc == 7))
                    S.copy("act", Qm[:, bi, :], pb[1][:, 0:64])
                    chk("ml_a", Qm[:, 0, :])
                    S.ts("dve", Km[:, bi, :], pb[1][:, 64:128], 0.125, None, op0=ALU.mult)
                    S.copy("dve", Vm1[:, bi, 0:64], pb[1][:, 128:192])
                    chk("ml_b", Km[:, 0, :])
                    S.act(Om[:, bi, :], pb[1][:, 192:256], AF.Sigmoid)
                    chk("ml_c", Om[:, 0, :])
                    for d in range(2):
                        S.ts("dve", G[:, bi, 2 * d:2 * d + 1], pb[1][:, 256 + 2 * d:257 + 2 * d], scal[:, 1 + 2 * d:2 + 2 * d], None, op0=ALU.add)
                        chk("ml_d", G[:, 0, :])
                        S.act(G[:, bi, 2 * d + 1:2 * d + 2], pb[1][:, 257 + 2 * d:258 + 2 * d], AF.Exp, bias=nfb[:, d:d + 1], scale=-1.0)
                        chk("ml_e", G[:, 0, :])
                        S.act(G[:, bi, 2 * d + 1:2 * d + 2], G[:, bi, 2 * d + 1:2 * d + 2], AF.Ln, bias=1.0)
                        chk("ml_f", G[:, 0, :])
                        S.ts("dve", G[:, bi, 2 * d + 1:2 * d + 2], G[:, bi, 2 * d + 1:2 * d + 2], -1.0, None, op0=ALU.mult)
            chk("ml_p1", Qm[:, 0, :], Km[:, 0, :], G[:, 0, :])
            Hb = S.tile("Hb", [128, 66, 64])

            def ml_dir(d):
                q = [pb[4 * d + i] for i in range(4)]
                t = lambda nm, shp: S.tile(f"ml{d}_{nm}", shp)
                C1T = t("C1T", [64, 65])
                eb, ek, rden = t("eb", [128, 1]), t("ek", [128, 1]), t("rden", [128, 1])
                eL = t("eL", [64, 1])
                qt, kt = t("qt", [128, 64]), t("kt", [128, 64])
                qtT, ktT = t("qtT", [64, 128]), t("ktT", [64, 128])
                STs = t("ST", [128, 128])
                tri = trile if d == 0 else trige
                order = list(range(nblk)) if d == 0 else [1, 0] + list(range(nblk - 1, 1, -1))
                Hd = Hs if d == 0 else Hb

                def run():
                    S.memset("dve", C1T.r, 0.0)
                    for bi in order:
                        lf = G[:, bi, 2 * d + 1:2 * d + 2]
                        S.mm(q[0][:, 0:1], tri.r, lf)
                        S.mm(q[0][0:64, 1:2], ones[:, 0:64], lf)
                        S.act(eb.r, q[0][:, 0:1], AF.Exp)
                        S.tt("dve", ek.r, G[:, bi, 2 * d:2 * d + 1], q[0][:, 0:1], ALU.subtract)
                        S.act(ek.r, ek.r, AF.Exp)
                        S.act(eL.r, q[0][0:64, 1:2], AF.Exp)
                        S.ts("dve", qt.r, Qm[:, bi, :], eb.r, None, op0=ALU.mult)
                        S.ts("pool", kt.r, Km[:, bi, :], ek.r, None, op0=ALU.mult)
                        S.tr(q[1][0:64, 0:128], qt.r, ident.r)
                        S.copy("act", qtT.r, q[1][0:64, 0:128])
                        S.tr(q[2][0:64, 0:128], kt.r, ident.r)
                        S.copy("dve", ktT.r, q[2][0:64, 0:128])
                        S.mm(q[3][:, 0:128], ktT.r, qtT.r)
                        S.tt("dve", STs.r, q[3][:, 0:128], tri.r, ALU.mult)
                        S.mm(q[1][:, 0:65], STs.r, Vm1[:, bi, :], start=True, stop=False)
                        S.mm(q[1][:, 0:65], qtT.r, C1T.r, start=False, stop=True)
                        S.ts("dve", rden.r, q[1][:, 64:65], -1.0, None, op0=ALU.mult)
                        S.tt("dve", rden.r, rden.r, q[1][:, 64:65], ALU.max)
                        S.ts("dve", rden.r, rden.r, 1.0, None, op0=ALU.max)
                        S.recip(rden.r, rden.r)
                        S.ts("dve", Hd[:, bi, :], q[1][:, 0:64], rden.r, None, op0=ALU.mult)
                        S.mm(q[2][0:64, 0:65], ident[0:64, 0:64], C1T.r, start=True, stop=False)
                        S.mm(q[2][0:64, 0:65], kt.r, Vm1[:, bi, :], start=False, stop=True)
                        S.ts("dve", C1T.r, q[2][0:64, 0:65], eL.r, None, op0=ALU.mult)
                return run

            runs = [ml_dir(0), ml_dir(1)]
            S.replay_interleaved([S.record(runs[0]), S.record(runs[1])])
            S.tt("dve", Hs.r, Hs.r, Hb.r, ALU.add)
            mlsq = S.tile("mlsq", [128, 64])
            mlss = S.tile("mlss", [128, 1])
            mly = S.tile("mly", [128, 64])
            mlyT = S.tile("mlyT", [64, 128])
            for bi in range(nblk):
                S.act(mlsq.r, Hs[:, bi, :], AF.Square)
                S.reduce("dve", mlss.r, mlsq.r, ALU.add)
                S.ts("dve", mlss.r, mlss.r, 1.0 / 64.0, EPS, op0=ALU.mult, op1=ALU.add)
                S.act(mlss.r, mlss.r, AF.Sqrt)
                S.recip(mlss.r, mlss.r)
                S.stt("dve", mly.r, Hs[:, bi, :], mlss.r, rowbc[:, 3, :], ALU.mult, ALU.mult)
                S.tt("dve", mly.r, mly.r, Om[:, bi, :], ALU.mult)
                S.tr(pb[3][0:64, 0:128], mly.r, ident.r)
                S.copy("act", mlyT.r, pb[3][0:64, 0:128])
                store_y(3, bi * 128, 128, mlyT.r)
            chk("mlstm", mly.r)
            S.pop_scope()
        if "rwkv" in mixers:
            S.push_scope()
            Wr = S.tile("Wr", [128, 8, 256], PROJ_DT)
            load_w(Wr, Wr_d, 256)
            w2p = S.tile("w2p", [16, 2, 64])
            a2p = S.tile("a2p", [16, 2, 64])
            g2p = S.tile("g2p", [32, 64])
            rmask = [S.tile(f"rmask{d}", [64, 192]) for d in range(2)]
            cmask = S.tile("cmask", [64, 256])
            S.dma(w2p.r, w2_d.r, "ldc")
            S.dma(a2p.r, a2_d.r, "ldc")
            S.dma(g2p.r, g2_d.r, "ldc")
            for d in range(2):
                S.dma(rmask[d].r, rmask_d[d], "ldc")
            S.dma(cmask.r, cmask_d.r, "ldc")
            RKbc = S.tile("RKbc", [64, 64])
            S.ts("dve", RKbc.r, ones[0:64, 0:64], pp[:, 11:12], None, op0=ALU.mult)
            Yst = S.tile("Yst", [64, TT])
            rwsc = Tile(nc.dram_tensor("rwsc" + sfx, [64, NTILE, 3, N], F32).ap(), "rwsc" + sfx)
            i64 = ident[0:64, 0:64]
            o64 = ones[0:64, 0:64]

            class B_:
                pass

            def mkbufs(d):
                b = B_()
                t = lambda nm, shp: S.tile(f"rw{d}_{nm}", shp)
                b.rT, b.kT, b.vT, b.gT, b.kkT = (t(n_, [64, N]) for n_ in ("r", "k", "v", "g", "kk"))
                b.tw, b.xa, b.sg = t("tw", [16, N]), t("xa", [16, N]), t("sg", [32, N])
                b.lw = t("lw", [64, N])
                b.aT = [t(f"a{i}", [64, N]) for i in range(2)]
                b.kd = [t(f"kd{i}", [64, N]) for i in range(2)]
                b.cum, b.tmp, b.Pin, b.Pinv, b.Pex = (t(n_, [64, N]) for n_ in ("cum", "tmp", "Pin", "Pinv", "Pex"))
                b.AR = t("AR", [64, 4, 2, 64])
                b.BK = t("BK", [64, 4, 2, 64])
                b.Vtok = t("Vtok", [64, 4, 64])
                b.NM = [[t(f"NM{c}_{i}", [64, 128]) for i in range(2)] for c in range(4)]
                b.Pw = [[t(f"P{c}_{i}", [64, 64]) for i in range(2)] for c in range(4)]
                b.PwT = [[t(f"PT{c}_{i}", [64, 64]) for i in range(2)] for c in range(4)]
                b.X = [[t(f"X{c}_{i}", [64, 64]) for i in range(2)] for c in range(4)]
                b.Btok = [t(f"Btok{c}", [64, 64]) for c in range(4)]
                b.Ktok = [t(f"Ktok{c}", [64, 64]) for c in range(4)]
                b.Xf = [None] * 4
                b.ZT, b.UT, b.S0T = (t(n_, [64, 64]) for n_ in ("ZT", "UT", "S0T"))
                b.sq = t("sq", [64, N])
                b.Yb = t("Yb", [64, 3, N])
                b.q = [pb[4 * d + i] for i in range(4)]
                if d == 0:
                    b.hb = (hT, xs, rstd, pb[0], sqs)
                else:
                    b.hb = (S.tile("rw1_hT", [128, 8, N], PROJ_DT), S.tile("rw1_xs", [128, 8, N]), S.tile("rw1_rstd", [128, N]), pb[4],
                            S.tile("rw1_sqs", [128, 8, N]))
                b.hT = b.hb[0]
                return b

            BF = [mkbufs(0), mkbufs(1)]

            def prep_tile(ti, d):
                b = BF[d]
                q = b.q
                both = (d == 1)
                load_h(ti, b.hb)
                hT = b.hT
                for g, dst in ((0, b.rT), (1, b.kT), (2, b.vT)):
                    for kc in range(8):
                        S.mm(q[1][0:64, 0:N], Wr[:, kc, g * 64:(g + 1) * 64], hT[:, kc, :], start=(kc == 0), stop=(kc == 7))
                    S.copy("act", dst.r, q[1][0:64, 0:N])
                for kc in range(8):
                    S.mm(q[2][0:16, 0:N], Wr[:, kc, 192:208], hT[:, kc, :], start=(kc == 0), stop=(kc == 7))
                S.act(b.tw.r, q[2][0:16, 0:N], AF.Tanh)
                for kc in range(8):
                    S.mm(q[2][0:16, 0:N], Wr[:, kc, 208:224], hT[:, kc, :], start=(kc == 0), stop=(kc == 7))
                S.copy("act", b.xa.r, q[2][0:16, 0:N])
                if both:
                    for kc in range(8):
                        S.mm(q[2][0:32, 0:N], Wr[:, kc, 224:256], hT[:, kc, :], start=(kc == 0), stop=(kc == 7))
                    S.act(b.sg.r, q[2][0:32, 0:N], AF.Sigmoid)
                for c in range(4):
                    for kc in range(8):
                        S.mm(q[3][0:64, c * 64:(c + 1) * 64], hT[:, kc, c * 64:(c + 1) * 64], Wr[:, kc, 128:192],
                             start=(kc == 0 and c == 0), stop=(kc == 7), skip=True)
                S.copy("act", b.Vtok.r.re("p c v -> p (c v)"), q[3][0:64, 0:256])
                dirs = (0, 1) if both else (d,)
                for dd in dirs:
                    S.mm(q[1][0:64, 0:N], a2p[:, dd, :], b.xa.r)
                    S.act(b.aT[dd].r, q[1][0:64, 0:N], AF.Sigmoid, bias=pp[:, 6 + dd:7 + dd])
                    S.ts("dve", b.kd[dd].r, b.aT[dd].r, pp[:, 9:10], omka.r, op0=ALU.mult, op1=ALU.add)
                    S.tt("dve", b.kd[dd].r, b.kd[dd].r, b.kT.r, ALU.mult)
                S.mm(q[1][0:64, 0:N], w2p[:, d, :], b.tw.r)
                S.act(b.lw.r, q[1][0:64, 0:N], AF.Sigmoid, bias=pp[:, 4 + d:5 + d])
                S.ts("dve", b.lw.r, b.lw.r, -RWKV_DECAY_SCALE, None, op0=ALU.mult)
                if both:
                    S.mm(q[1][0:64, 0:N], g2p.r, b.sg.r)
                    S.copy("act", b.Yb[:, 2, :], q[1][0:64, 0:N])
                S.ts("dve", b.kkT.r, b.kT.r, pp[:, 8:9], None, op0=ALU.mult)
                S.act(b.sq.r, b.kkT.r, AF.Square)
                S.mm(q[1][0:64, 0:N], o64, b.sq.r)
                S.ts("dve", b.sq.r, q[1][0:64, 0:N], EPS, None, op0=ALU.add)
                S.act(b.sq.r, b.sq.r, AF.Sqrt)
                S.recip(b.sq.r, b.sq.r)
                S.tt("dve", b.kkT.r, b.kkT.r, b.sq.r, ALU.mult)
                S.scan(b.cum.r, cmask.r, b.lw.r, 0.0, ALU.mult, ALU.add)
                if d == 1:
                    c3 = b.cum.ap.rearrange("p (c t) -> p c t", c=4)
                    S.tt("dve", b.tmp.r, b.lw.r, b.cum.r, ALU.subtract)
                    S.tt("dve", b.cum.r.re("p (c t) -> p c t", c=4), b.tmp.r.re("p (c t) -> p c t", c=4),
                         b.cum.r.with_ap(c3[:, :, 63:64].to_broadcast([64, 4, 64])), ALU.add)
                S.act(b.Pin.r, b.cum.r, AF.Exp)
                S.act(b.Pinv.r, b.cum.r, AF.Exp, scale=-1.0)
                S.tt("dve", b.tmp.r, b.cum.r, b.lw.r, ALU.subtract)
                S.act(b.Pex.r, b.tmp.r, AF.Exp)
                v4 = lambda tl: tl.r.re("p (c t) -> p c t", c=4)
                S.stt("dve", b.AR[:, :, 0, :], v4(b.kkT), -1.0, v4(b.Pex), ALU.mult, ALU.mult)
                S.tt("dve", b.AR[:, :, 1, :], v4(b.rT), v4(b.Pin), ALU.mult)
                S.tt("dve", b.tmp.r, b.kkT.r, b.aT[d].r, ALU.mult)
                S.tt("dve", b.BK[:, :, 0, :], v4(b.tmp), v4(b.Pinv), ALU.mult)
                S.tt("dve", b.BK[:, :, 1, :], v4(b.kd[d]), v4(b.Pinv), ALU.mult)
                if both:
                    S.tt("dve", b.tmp.r, b.kd[0].r, b.kd[1].r, ALU.add)
                    S.tt("dve", b.tmp.r, b.tmp.r, b.rT.r, ALU.mult)
                    S.mm(q[1][0:64, 0:N], RKbc.r, b.tmp.r)
                    S.tt("dve", b.Yb[:, 1, :], q[1][0:64, 0:N], b.vT.r, ALU.mult)

            def chunk_pre(c, d):
                b = BF[d]
                pq = pb[4 * d + c]
                m = rmask[d]
                NM, Pw, PwT, X = b.NM[c], b.Pw[c], b.PwT[c], b.X[c]
                ARc = b.AR[:, c, :, :].re("p two t -> p (two t)")
                A_c = b.AR[:, c, 0, :]
                B_c, K_c = b.BK[:, c, 0, :], b.BK[:, c, 1, :]
                S.mm(pq[0:64, 0:128], B_c, ARc)
                S.mm(pq[0:64, 128:256], K_c, ARc, start=False, skip=True)
                S.mm(pq[0:64, 256:320], A_c, B_c, start=False, skip=True)
                S.tt("dve", NM[0].r, pq[0:64, 0:128], m[:, 0:128], ALU.mult)
                S.tt("dve", NM[1].r, pq[0:64, 128:256], m[:, 0:128], ALU.mult)
                S.tt("dve", PwT[0].r, pq[0:64, 256:320], m[:, 128:192], ALU.mult)
                S.copy("act", Pw[0].r, NM[0][:, 0:64])
                S.tt("dve", X[0].r, NM[0][:, 0:64], i64, ALU.add)
                cur = 0
                for lev in range(5):
                    nxt = 1 - cur
                    S.mm(pq[0:64, 0:64], Pw[cur].r, PwT[cur].r)
                    if lev < 4:
                        S.mm(pq[0:64, 64:128], PwT[cur].r, Pw[cur].r, start=False, skip=True)
                    S.copy("act", PwT[nxt].r, pq[0:64, 0:64])
                    if lev < 4:
                        S.copy("dve", Pw[nxt].r, pq[0:64, 64:128])
                    S.mm(pq[0:64, 128:192], PwT[nxt].r, X[cur].r, start=False, skip=True)
                    S.tt("dve", X[nxt].r, pq[0:64, 128:192], X[cur].r, ALU.add)
                    cur = nxt
                b.Xf[c] = X[cur]
                S.tr(pq[0:64, 256:320], B_c, i64)
                S.tr(pq[0:64, 320:384], K_c, i64)
                S.copy("act", b.Btok[c].r, pq[0:64, 256:320])
                S.copy("dve", b.Ktok[c].r, pq[0:64, 320:384])

            def chunk_seq(ti, c, d):
                b = BF[d]
                q = b.q
                NM = b.NM[c]
                A_c, R_c = b.AR[:, c, 0, :], b.AR[:, c, 1, :]
                V_c = b.Vtok[:, c, :]
                S.mm(q[3][0:64, 0:64], A_c, b.S0T.r, start=True, stop=False)
                S.mm(q[3][0:64, 0:64], NM[1][:, 0:64], V_c, start=False, stop=True)
                S.copy("act", b.ZT.r, q[3][0:64, 0:64])
                S.mm(q[1][0:64, 0:64], b.Xf[c].r, b.ZT.r)
                S.copy("act", b.UT.r, q[1][0:64, 0:64])
                S.mm(q[2][0:64, 0:64], b.S0T.r, R_c, start=True, stop=False)
                S.mm(q[2][0:64, 0:64], b.UT.r, NM[0][:, 64:128], start=False, stop=False)
                S.mm(q[2][0:64, 0:64], V_c, NM[1][:, 64:128], start=False, stop=True)
                if d == 0:
                    t0 = ti * N + c * 64
                    S.copy("dve", Yst[:, t0:t0 + 64], q[2][0:64, 0:64])
                else:
                    S.copy("dve", b.Yb[:, 0, c * 64:(c + 1) * 64], q[2][0:64, 0:64])
                S.mm(q[3][0:64, 0:64], i64, b.S0T.r, start=True, stop=False)
                S.mm(q[3][0:64, 0:64], b.Btok[c].r, b.UT.r, start=False, stop=False)
                S.mm(q[3][0:64, 0:64], b.Ktok[c].r, V_c, start=False, stop=True)
                pl = c * 64 + (63 if d == 0 else 0)
                S.ts("dve", b.S0T.r, q[3][0:64, 0:64], b.Pin[:, pl:pl + 1], None, op0=ALU.mult)

            orders = [tiles, [0] + tiles[:0:-1]]
            for d in range(2):
                S.memset("dve", BF[d].S0T.r, 0.0)
            for s_ in range(len(tiles)):
                tis = [orders[0][s_], orders[1][s_]]
                S.replay_interleaved([S.record(lambda: prep_tile(tis[0], 0)), S.record(lambda: prep_tile(tis[1], 1))])
                S.replay_interleaved([S.record(lambda d=d, c=c: chunk_pre(c, d)) for c in range(4) for d in range(2)])

                def seq0():
                    for i in range(4):
                        chunk_seq(tis[0], i, 0)

                def seq1():
                    for i in range(4):
                        chunk_seq(tis[1], 3 - i, 1)
                    S.dma(rwsc[:, tis[1], :, :], BF[1].Yb.r, "sth")

                S.replay_interleaved([S.record(seq0), S.record(seq1)])
            fin = BF[0].Yb
            yo = BF[0].tmp
            for ti in tiles:
                t0 = ti * N
                S.dma(fin.r, rwsc[:, ti, :, :], "ldx")
                S.tt("dve", yo.r, Yst[:, t0:t0 + N], fin[:, 0, :], ALU.add)
                head_norm_fm(yo.r, BF[0].sq.r, yo.r, pp[:, 12:13], pb[1])
                S.tt("dve", yo.r, yo.r, fin[:, 1, :], ALU.add)
                S.tt("dve", yo.r, yo.r, fin[:, 2, :], ALU.mult)
                store_y(1, t0, N, yo.r)
            S.pop_scope()

    try:
        body()
    except Done:
        while len(S.scopes) > 1:
            S.scopes.pop().close()
        raise
    S.pop_scope()


class StageDone(Exception):
    pass


def build_A(stage=None, mixers=("conv", "attn", "mlstm", "rwkv"), tile_limit=None):
    nc = bass.Bass("TRN2", target_bir_lowering=False)
    S = Sched(nc)
    xT_d = Tile(nc.dram_tensor("xT", [128, 8, TT], F32, kind="ExternalInput").ap(), "xT")
    yT_d = Tile(nc.dram_tensor("yT", [4, 64, TT], F32, kind="ExternalOutput").ap(), "yT")
    dbg_d = Tile(nc.dram_tensor("dbg", [128, 4096], F32, kind="ExternalOutput").ap(), "dbg")
    pb = [S.ptile(f"pb{i}", [128, 512]) for i in range(8)]

    def load_x(ti, xs):
        S.dma(xs.r, xT_d[:, :, ti * N:(ti + 1) * N], "ldx")

    def store_y(m, t0, n, src):
        S.dma(yT_d[m, :, t0:t0 + n], src, "st")

    try:
        emit_A(S, nc, pb, "", load_x, store_y, stage, dbg_d, mixers, tile_limit)
    except Exception as e:
        if type(e).__name__ != "Done":
            raise
    S.wait_all("sp")
    while getattr(S, "scopes", None):
        S.scopes.pop().close()
    print("build_A instructions", S.n_ins, "sems", S.nsem, "sbuf left", nc.sbuf_bytes_remaining)
    S.close()
    return nc

EPS = 1e-6
POOLENG = os.environ.get("POOLENG", "pool")
NEG = -1.0e30


def emit_B(S, nc, pb, sfx, groups, x_src, y_src, out_sink, stage=None, dbg_d=None):
    def D(name, shape, kind="ExternalInput", dt=F32):
        t = nc.dram_tensor(name + sfx, list(shape), dt, kind=kind)
        return Tile(t.ap(), name + sfx)

    cv_d = S.shared_dram("cv", [128, 8, 2])
    adaw_d = S.shared_dram("adaw" + sfx, [6, 2, 128, 8, 512])
    adab_d = S.shared_dram("adab" + sfx, [128, 48])
    n2g_d = D("n2g", [128, 8])
    wout_d = D("wout", [8, 128, 8, 128])
    wq_d = D("wq", [16, 128, 8, 128])
    keys_d = D("keysT", [128, 16, 128])
    UT_d = D("UT", [128, 128, 8, 128])
    VJ_d = D("VJ", [128, 128, 1024])
    ident_d = S.shared_dram("ident", [128, 128])
    iota_d = S.shared_dram("iota", [128, 128])

    GM = max(g[1] for g in groups)
    S.push_scope()

    class Done(Exception):
        pass

    def chk(name, *refs):
        if stage != name:
            return
        o = 0
        for r in refs:
            n = 1
            for s_ in r.ap.shape[1:]:
                n *= s_
            t = S.tile(f"dbgt{o}", [128, n])
            S.copy("dve", t.r, r if len(r.ap.shape) == 2 else r)
            S.dma(dbg_d[0:r.ap.shape[0], o:o + n], t[0:r.ap.shape[0], :], "st")
            o += n
        raise Done()
    ident = S.tile("ident", [128, 128])
    iota = S.tile("iota", [128, 128])
    ones = S.tile("ones", [128, 128])
    cv = S.tile("cv", [128, 8, 2])
    adab = S.tile("adab", [128, 48])
    n2g = S.tile("n2g", [128, 8])
    keysT = S.tile("keysT", [128, 16, 128])
    mod = [S.tile(f"mod{i}", [128, 8, 2]) for i in range(6)]
    gm2 = S.tile("gm2", [128, 8, 2])
    scr = S.tile("scr", [128, 4096])
    xs = S.tile("xs", [128, 8, GM])
    ys = S.tile("ys", [128, 8, GM])
    x1 = S.tile("x1", [128, 8, GM])
    h2 = S.tile("h2", [128, 8, GM])
    rstd = S.tile("rstd", [128, GM])
    wbuf = [S.tile(f"wbuf{i}", [128, 8, 128]) for i in range(2)]
    ubuf = [S.tile(f"ubuf{i}", [128, 8, 128]) for i in range(2)]
    vbuf = [S.tile(f"vbuf{i}", [128, 1024]) for i in range(2)]
    sc = S.tile("sc", [128, 16, 128])
    sc2 = S.tile("sc2", [128, 16, 128])
    sv = S.tile("sv", [128, 16, 16])
    si = S.tile("si", [128, 16, 16], U32)
    sif = S.tile("sif", [128, 16, 16])
    cand = S.tile("cand", [128, 8, 256])
    tv = S.tile("tv", [128, 8, 16])
    ti = S.tile("ti", [128, 8, 16], U32)
    tiu = S.tile("tiu", [128, 8, 16], U32)
    aq = S.tile("aq", [128, 8, 16])
    bq = S.tile("bq", [128, 8, 16])
    If = S.tile("If", [128, 128])
    Jf = S.tile("Jf", [128, 128])
    Wf = S.tile("Wf", [128, 128])
    mx = S.tile("mx", [128, 8])
    zs = S.tile("zs", [128, 8])
    IT = S.tile("IT", [128, GM])
    JT = S.tile("JT", [128, GM])
    WT = S.tile("WT", [128, GM])
    oiw = [S.tile(f"oiw{i}", [128, 128], BF16) for i in range(2)]
    oj = [S.tile(f"oj{i}", [128, 128], BF16) for i in range(2)]
    iotab = S.tile("iotab", [128, 128], BF16)
    WW = S.tile("WW", [128, GM, 128], BF16)
    gj = [S.tile(f"gj{i}", [128, GM]) for i in range(2)]
    pjb = [S.tile(f"pjb{i}", [128, GM], BF16) for i in range(2)]
    ubf = [S.tile(f"ubf{i}", [128, 8, 128], BF16) for i in range(3)]
    vbf = [S.tile(f"vbf{i}", [128, 1024], BF16) for i in range(3)]
    acc = pb[0:4]
    pa = pb[4:6]
    pw = pb[6:8]

    def build_body():
        S.dma(ident.r, ident_d.r, "ldc")
        S.dma(iota.r, iota_d.r, "ldc")
        S.dma(cv.r, cv_d.r, "ldc")
        S.dma(adab.r, adab_d.r, "ldc")
        S.dma(n2g.r, n2g_d.r, "ldc")
        S.dma(keysT.r, keys_d.r, "ldc")
        S.memset("dve", ones.r, 1.0)
        S.copy("dve", iotab.r, iota.r)
        S.act(cv.r, cv.r, AF.Silu)
        adaw_t = scr[:, 0:4096].re("p (c f) -> p c f", c=8)
        for seg in (2, 3, 4, 5):
            for half in range(2):
                S.dma(adaw_t, adaw_d[seg, half], "ldc")
                for f4 in range(4):
                    fc = half * 4 + f4
                    for dc in range(8):
                        S.mm(pa[0][:, fc * 2:fc * 2 + 2], adaw_t[:, dc, f4 * 128:(f4 + 1) * 128], cv[:, dc, :],
                             start=(dc == 0), stop=(dc == 7))
            S.tt("dve", mod[seg].r, pa[0][:, 0:16].re("p (c t) -> p c t", t=2),
                 adab[:, seg * 8:(seg + 1) * 8].with_ap(adab.ap[:, seg * 8:(seg + 1) * 8].unsqueeze(2).to_broadcast([128, 8, 2])),
                 ALU.add)
        S.ts("dve", gm2.r, mod[4].r, 1.0, None, op0=ALU.add)
        S.tt("dve", gm2.r, gm2.r, n2g.r.with_ap(n2g.ap.unsqueeze(2).to_broadcast([128, 8, 2])), ALU.mult)
        gt1, sh2, gt2 = mod[2], mod[3], mod[5]
        chk("mod", mod[2].r, mod[3].r, mod[4].r, mod[5].r, gm2.r)

        wi = 0
        ui = 0
        for (g0, GN, col) in groups:
            NTL = GN // 128
            x_src(g0, GN, col, xs)
            y_src(g0, GN, col, ys, h2)
            for oc in range(8):
                wb = wbuf[wi % 2]
                S.dma(wb.r, wout_d[oc], f"ldw{wi % 2}")
                wi += 1
                p = pa[oc % 2]
                for kc in range(8):
                    S.mm(p[:, 0:GN], wb[:, kc, :], ys[:, kc, 0:GN], start=(kc == 0), stop=(kc == 7))
                S.stt("dve", x1[:, oc, 0:GN], p[:, 0:GN], gt1[:, oc, col:col + 1], xs[:, oc, 0:GN], ALU.mult, ALU.add)
            chk("x1", x1[:, :, 0:GN])
            S.act(ys[:, :, 0:GN], x1[:, :, 0:GN], AF.Square)
            for kc in range(8):
                S.mm(pa[0][:, 0:GN], ones.r, ys[:, kc, 0:GN], start=(kc == 0), stop=(kc == 7))
            S.ts("dve", rstd[:, 0:GN], pa[0][:, 0:GN], 1.0 / 1024.0, EPS, op0=ALU.mult, op1=ALU.add)
            S.act(rstd[:, 0:GN], rstd[:, 0:GN], AF.Sqrt)
            S.recip(rstd[:, 0:GN], rstd[:, 0:GN])
            for kc in range(8):
                S.tt("dve", h2[:, kc, 0:GN], x1[:, kc, 0:GN], rstd[:, 0:GN], ALU.mult)
                S.ts("dve", h2[:, kc, 0:GN], h2[:, kc, 0:GN], gm2[:, kc, col:col + 1], sh2[:, kc, col:col + 1],
                     op0=ALU.mult, op1=ALU.add)
            chk("h2", h2[:, :, 0:GN])
            qT = scr[:, 0:16 * GN].re("p (h t) -> p h t", h=16)
            for hp in range(16):
                wb = wbuf[wi % 2]
                S.dma(wb.r, wq_d[hp], f"ldw{wi % 2}")
                wi += 1
                p = pa[hp % 2]
                for kc in range(8):
                    S.mm(p[:, 0:GN], wb[:, kc, :], h2[:, kc, 0:GN], start=(kc == 0), stop=(kc == 7))
                S.copy("act", qT[:, hp, :], p[:, 0:GN])
            chk("qT", qT[:, :, 0:GN])
            for mt in range(NTL):
                ms = slice(mt * 128, (mt + 1) * 128)
                for hp in range(16):
                    S.mm(acc[hp // 4][:, (hp % 4) * 128:(hp % 4) * 128 + 128], qT[:, hp, ms], keysT[:, hp, :])
                for b4 in range(4):
                    S.copy("act" if b4 % 2 else "dve", sc[:, b4 * 4:(b4 + 1) * 4, :].re("p a k -> p (a k)"), acc[b4].r)
                chk("sc", sc.r)
                for hp in range(16):
                    S.vmax(sv[:, hp, 0:8], sc[:, hp, :])
                    S.vmax_index(si[:, hp, 0:8], sv[:, hp, 0:8], sc[:, hp, :])
                    S.vmatch_replace(sc2[:, hp, :], sv[:, hp, 0:8], sc[:, hp, :], NEG)
                    S.vmax(sv[:, hp, 8:16], sc2[:, hp, :])
                    S.vmax_index(si[:, hp, 8:16], sv[:, hp, 8:16], sc2[:, hp, :])
                S.copy("dve", sif.r, si.r)
                chk("top1", sv.r, sif.r)
                sv4 = sv.ap.rearrange("p (h two) a -> p h two a", two=2)
                sif4 = sif.ap.rearrange("p (h two) a -> p h two a", two=2)
                S.tt("dve", cand.r.re("p h (a b) -> p h a b", b=16),
                     sv.r.with_ap(sv4[:, :, 0, :].unsqueeze(3).to_broadcast([128, 8, 16, 16])),
                     sv.r.with_ap(sv4[:, :, 1, :].unsqueeze(2).to_broadcast([128, 8, 16, 16])), ALU.add)
                cand2 = sc2.r.re("p (h two) k -> p h (two k)", two=2)
                eq = sc.r.re("p (h two) (n a) -> p h (two n) a", two=2, a=16)
                for h in range(8):
                    S.vmax(tv[:, h, 0:8], cand[:, h, :])
                    S.vmax_index(ti[:, h, 0:8], tv[:, h, 0:8], cand[:, h, :])
                    S.vmatch_replace(cand2[:, h, :], tv[:, h, 0:8], cand[:, h, :], NEG)
                    S.vmax(tv[:, h, 8:16], cand2[:, h, :])
                    S.vmax_index(ti[:, h, 8:16], tv[:, h, 8:16], cand2[:, h, :])
                S.ts("dve", tiu.r, ti.r, 15, None, op0=ALU.bitwise_and)
                S.copy("dve", bq.r, tiu.r)
                S.ts("dve", tiu.r, ti.r, 4, None, op0=ALU.logical_shift_right)
                S.copy("dve", aq.r, tiu.r)
                iota16 = iota.r.with_ap(iota.ap[:, 0:16].unsqueeze(1).unsqueeze(1).to_broadcast([128, 8, 16, 16]))
                for (qv, half, dst) in ((aq, 0, If), (bq, 1, Jf)):
                    S.tt("dve", eq, qv.r.with_ap(qv.ap.unsqueeze(3).to_broadcast([128, 8, 16, 16])), iota16, ALU.is_equal)
                    S.tt("dve", eq, eq, sif.r.with_ap(sif4[:, :, half, :].unsqueeze(2).to_broadcast([128, 8, 16, 16])), ALU.mult)
                    S.reduce("dve", dst.r.re("p (h n) -> p h n", h=8), eq, ALU.add)
                chk("IJ", If.r, Jf.r, tv.r, aq.r, bq.r)
                S.reduce("dve", mx.r, tv.r, ALU.max)
                S.tt("dve", tv.r, tv.r, mx.r.with_ap(mx.ap.unsqueeze(2).to_broadcast([128, 8, 16])), ALU.subtract)
                S.act(tv.r, tv.r, AF.Exp)
                S.reduce("dve", zs.r, tv.r, ALU.add)
                S.recip(zs.r, zs.r)
                S.tt("dve", Wf.r.re("p (h n) -> p h n", h=8), tv.r,
                     zs.r.with_ap(zs.ap.unsqueeze(2).to_broadcast([128, 8, 16])), ALU.mult)
                for k, (src, dst) in enumerate(((If, IT), (Jf, JT), (Wf, WT))):
                    S.tr(pw[k % 2][:, 0:128], src.r, ident.r)
                    S.copy("act", dst[:, ms], pw[k % 2][:, 0:128])
            chk("ITW", IT[:, 0:GN], JT[:, 0:GN], WT[:, 0:GN])
            for m in range(int(os.environ.get('MLIM', GN))):
                a = oiw[m % 2]
                b = oj[m % 2]
                S.ts("dve", a.r, iotab.r, IT[:, m:m + 1], WT[:, m:m + 1], op0=ALU.is_equal, op1=ALU.mult)
                S.ts("dve", b.r, iotab.r, JT[:, m:m + 1], None, op0=ALU.is_equal)
                p = pw[m % 2]
                S.mm(p[:, 0:128], a.r, b.r)
                S.copy("act", WW[:, m, :], p[:, 0:128])
            chk("WW", WW[:, 0:16, :])
            h2b = ys.r.with_ap(ys.ap.bitcast(BF16))[:, :, 0:GN]
            S.copy("act", h2b, h2[:, :, 0:GN])
            NBF = 3

            def load_j(j):
                S.dma(ubuf[j % 2].r, UT_d[j], f"ldu{j % 2}")
                S.dma(vbuf[j % 2].r, VJ_d[j], f"ldv{j % 2}")

            def cast_j(j):
                S.copy("act", ubf[j % NBF].r, ubuf[j % 2].r)
                S.copy("dve", vbf[j % NBF].r, vbuf[j % 2].r)

            def a_mm(j):
                p = pb[4 + (j % 4)]
                for dc in range(8):
                    S.mm(p[:, 0:GN], ubf[j % NBF][:, dc, :], h2b[:, dc, :], start=(dc == 0), stop=(dc == 7))

            load_j(0)
            load_j(1)
            cast_j(0)
            load_j(2)
            a_mm(0)
            for j in range(128):
                if j + 1 < 128:
                    cast_j(j + 1)
                    if j + 3 < 128:
                        load_j(j + 3)
                    a_mm(j + 1)
                g = gj[j % 2]
                pp = pjb[j % 2]
                S.act(g[:, 0:GN], pb[4 + (j % 4)][:, 0:GN], AF.Gelu_apprx_tanh)
                S.tt("dve", pp[:, 0:GN], g[:, 0:GN], WW[:, 0:GN, j], ALU.mult)
                vb = vbf[j % NBF]
                for oc in range(8):
                    S.mm(acc[oc // 2][:, (oc % 2) * 256:(oc % 2) * 256 + GN], vb[:, oc * 128:(oc + 1) * 128], pp[:, 0:GN],
                         start=(j == 0 and oc % 2 == 0), stop=(j == 127), skip=True)
            for oc in range(8):
                S.stt("dve", xs[:, oc, 0:GN], acc[oc // 2][:, (oc % 2) * 256:(oc % 2) * 256 + GN], gt2[:, oc, col:col + 1],
                      x1[:, oc, 0:GN], ALU.mult, ALU.add)
            out_sink(g0, GN, col, xs)

    try:
        build_body()
    except Done:
        while len(S.scopes) > 1:
            S.scopes.pop().close()
        raise
    S.pop_scope()


def build_B(NT, groups, stage=None):
    nc = bass.Bass("TRN2", target_bir_lowering=False)
    S = Sched(nc)
    xT_d = Tile(nc.dram_tensor("xT", [128, 8, NT], F32, kind="ExternalInput").ap(), "xT")
    yT_d = Tile(nc.dram_tensor("yT", [128, 8, NT], F32, kind="ExternalInput").ap(), "yT")
    out_d = Tile(nc.dram_tensor("outT", [128, 8, NT], F32, kind="ExternalOutput").ap(), "outT")
    dbg_d = Tile(nc.dram_tensor("dbg", [128, 4096], F32, kind="ExternalOutput").ap(), "dbg")
    pb = [S.ptile(f"pb{i}", [128, 512]) for i in range(8)]

    def x_src(g0, GN, col, xs):
        S.dma(xs[:, :, 0:GN], xT_d[:, :, g0:g0 + GN], "ldx")

    def y_src(g0, GN, col, ys, tmp):
        S.dma(ys[:, :, 0:GN], yT_d[:, :, g0:g0 + GN], "ldx")

    def out_sink(g0, GN, col, xs):
        S.dma(out_d[:, :, g0:g0 + GN], xs[:, :, 0:GN], "st")

    try:
        emit_B(S, nc, pb, "", groups, x_src, y_src, out_sink, stage, dbg_d)
    except Exception as e:
        if type(e).__name__ != "Done":
            raise
    S.wait_all("sp")
    while getattr(S, "scopes", None):
        S.scopes.pop().close()
    print("build_B instructions", S.n_ins, "sems", S.nsem)
    S.close()
    return nc

def fm(X):
    NT = X.shape[0]
    return np.ascontiguousarray(X.T.reshape(8, 128, NT).transpose(1, 0, 2))

def unfm(XT):
    NT = XT.shape[2]
    return np.ascontiguousarray(XT.transpose(1, 0, 2).reshape(1024, NT).T)

def vec_fm(v):
    return np.ascontiguousarray(v.reshape(-1, 128).T)

def consts():
    ident = np.eye(128, dtype=np.float32)
    iota = np.tile(np.arange(128, dtype=np.float32)[None, :], (128, 1))
    return ident, iota

def prep_B_weights(inp, l, permute_wout=False):
    d = {}
    aw = inp["ada_w"][l]
    d["adaw"] = np.ascontiguousarray(aw.reshape(8, 128, 6, 2, 512).transpose(2, 3, 1, 0, 4))
    d["adab"] = vec_fm(inp["ada_b"][l])
    d["n2g"] = vec_fm(inp["norm2_g"][l])
    wo = inp["w_out"][l]
    if permute_wout:
        g = np.arange(1024)
        r_, m_, ch_ = g // 256, (g % 256) // 64, g % 64
        wo = wo[m_ * 256 + r_ * 64 + ch_, :]
    d["wout"] = np.ascontiguousarray(wo.reshape(8, 128, 8, 128).transpose(2, 1, 0, 3))
    wq = inp["peer_wq"][l]
    d["wq"] = np.ascontiguousarray(wq.reshape(8, 128, 16, 128).transpose(2, 1, 0, 3))
    ks = inp["peer_keys"][l]
    d["keysT"] = np.ascontiguousarray(ks.reshape(16, 128, 128).transpose(2, 0, 1))
    u = inp["peer_u"][l]
    d["UT"] = np.ascontiguousarray(u.reshape(128, 128, 8, 128).transpose(1, 3, 2, 0))
    v = inp["peer_v"][l]
    d["VJ"] = np.ascontiguousarray(v.reshape(128, 128, 1024).transpose(1, 0, 2))
    d["ident"], d["iota"] = consts()
    return d

def cvec(inp, b):
    return np.ascontiguousarray(np.stack([vec_fm(inp["c"][b]), vec_fm(inp["c_ctx"])], axis=-1))

OFF = {"hx": 0, "cB": 256, "cC": 512, "r": 768, "k": 1024, "v": 1280, "xw": 1536, "xa": 1552, "xg": 1568,
       "aq": 1600, "ak": 1856, "av": 1984, "mq": 2112, "mk": 2368, "mv": 2624, "mo": 2880, "mg": 3136}

def packW(W, cols):
    Wc = W[:, cols]
    return np.ascontiguousarray(Wc.reshape(8, 128, len(cols)).transpose(1, 0, 2))

def rope_table():
    quarter = 16
    inv = (10000.0 ** (-np.arange(quarter, dtype=np.float32) / quarter)).astype(np.float32)
    t = np.arange(8192)
    row = (t // 64).astype(np.float32); col = (t % 64).astype(np.float32)
    ar = row[:, None] * inv[None, :]; ac = col[:, None] * inv[None, :]
    tab = np.concatenate([np.cos(ar), np.sin(ar), np.cos(ac), np.sin(ac)], -1).astype(np.float32)
    return np.ascontiguousarray(tab.reshape(64, 128, 64))

def prep_A_weights(inp, l, j):
    d = {}
    aw = inp["ada_w"][l]
    d["adaw"] = np.ascontiguousarray(aw.reshape(8, 128, 6, 2, 512).transpose(2, 3, 1, 0, 4))
    d["adab"] = vec_fm(inp["ada_b"][l])
    d["n1g"] = vec_fm(inp["norm1_g"][l])
    W = inp["w_in"][l]
    h64 = np.arange(64) + j * 64
    kv64 = np.arange(64) + (j // 2) * 64
    d["Wc"] = packW(W, np.concatenate([OFF["hx"] + h64, OFF["cB"] + h64, OFF["cC"] + h64]))
    d["Wr"] = packW(W, np.concatenate([OFF["r"] + h64, OFF["k"] + h64, OFF["v"] + h64, OFF["xw"] + np.arange(16),
                                       OFF["xa"] + np.arange(16), OFF["xg"] + np.arange(32)]))
    d["Wa"] = packW(W, np.concatenate([OFF["aq"] + h64, OFF["ak"] + kv64, OFF["av"] + kv64]))
    gcols = np.array([OFF["mg"] + dd * 8 + g * 4 + j for dd in range(2) for g in range(2)])
    d["Wm"] = packW(W, np.concatenate([OFF["mq"] + h64, OFF["mk"] + h64, OFF["mv"] + h64, OFF["mo"] + h64, gcols]))
    pp = np.zeros((64, 16), np.float32)
    pp[:, 0:3] = inp["conv_w"][l][:, h64].T
    pp[:, 3] = inp["conv_g"][l][j]
    pp[:, 4:6] = inp["rwkv_w0"][l][:, h64].T
    pp[:, 6:8] = inp["rwkv_a0"][l][:, h64].T
    pp[:, 8] = inp["rwkv_kk"][l][h64]
    pp[:, 9] = inp["rwkv_ka"][l][h64]
    pp[:, 11] = inp["rwkv_rk"][l][j]
    pp[:, 12] = inp["rwkv_ln_g"][l][j]
    d["pp"] = pp
    d["w2p"] = np.ascontiguousarray(inp["rwkv_w2"][l][:, :, h64].transpose(1, 0, 2))
    d["a2p"] = np.ascontiguousarray(inp["rwkv_a2"][l][:, :, h64].transpose(1, 0, 2))
    d["g2p"] = np.ascontiguousarray(inp["rwkv_g2"][l][:, h64])
    rb = np.stack([inp["att_q_g"][l], inp["att_k_g"][l], inp["att_out_g"][l][j], inp["ml_out_g"][l][j]], 0)
    d["rowbc"] = np.ascontiguousarray(np.tile(rb[None], (128, 1, 1)))
    sc = np.zeros((8,), np.float32)
    sc[0] = inp["att_sink"][l][j]
    sc[1] = inp["ml_i_b"][l][0, j]; sc[2] = inp["ml_f_b"][l][0, j]
    sc[3] = inp["ml_i_b"][l][1, j]; sc[4] = inp["ml_f_b"][l][1, j]
    d["scal"] = np.ascontiguousarray(np.tile(sc[None], (128, 1)))
    ident, _ = consts()
    d["ident"] = ident
    i = np.arange(128)
    d["trile"] = (i[:, None] <= i[None, :]).astype(np.float32)
    d["trige"] = (i[:, None] >= i[None, :]).astype(np.float32)
    i = np.arange(64)
    su = (i[:, None] < i[None, :]).astype(np.float32); iu = (i[:, None] <= i[None, :]).astype(np.float32)
    sl = (i[:, None] > i[None, :]).astype(np.float32); il = (i[:, None] >= i[None, :]).astype(np.float32)
    d["rmask"] = np.ascontiguousarray(np.stack([np.concatenate([su, iu, sl], 1), np.concatenate([sl, il, su], 1)], 0))
    cm = np.ones((64, 256), np.float32); cm[:, ::64] = 0.0
    d["cmask"] = cm
    d["rope"] = rope_table()
    return d

_NC_CACHE = {}


def _get_nc(kind, *args):
    key = (kind,) + args
    if key not in _NC_CACHE:
        if kind == "A":
            _NC_CACHE[key] = build_A()
        else:
            _NC_CACHE[key] = build_B(*args)
    return _NC_CACHE[key]


def kernel_unfused(**inputs):
    inp = {k: np.ascontiguousarray(np.asarray(v, dtype=np.float32)) for k, v in inputs.items()}
    x = inp["x"].copy()
    ctx = inp["ctx"].copy()
    cores = list(range(8))
    for l in range(2):
        ncA = _get_nc("A")
        maps = []
        wA = [prep_A_weights(inp, l, j) for j in range(4)]
        xfull = [fm(np.concatenate([ctx[b], x[b]], 0)) for b in range(2)]
        cvs = [cvec(inp, b) for b in range(2)]
        for c in cores:
            b, j = c // 4, c % 4
            d = dict(wA[j])
            d["xT"] = xfull[b]
            d["cv"] = cvs[b]
            maps.append(d)
        res = run_bass_kernel_spmd(ncA, maps, core_ids=cores)
        ycat = np.zeros((2, TT, 1024), np.float32)
        for c in cores:
            b, j = c // 4, c % 4
            yT = res.results[c]["yT"]
            for m in range(4):
                ycat[b, :, m * 256 + j * 64:m * 256 + (j + 1) * 64] = yT[m].T
        del res, maps, xfull
        with_ctx = (l == 0)
        if with_ctx:
            NT = 2048 + 128
            groups = tuple((g * 256, 256, 0) for g in range(8)) + ((2048, 128, 1),)
        else:
            NT = 2048
            groups = tuple((g * 256, 256, 0) for g in range(8))
        ncB = _get_nc("B", NT, groups)
        wB = prep_B_weights(inp, l)
        maps = []
        for c in cores:
            b, q = c // 4, c % 4
            xs_ = x[b, q * 2048:(q + 1) * 2048]
            ys_ = ycat[b, 256 + q * 2048:256 + (q + 1) * 2048]
            if with_ctx:
                pad = np.zeros((64, 1024), np.float32)
                xs_ = np.concatenate([xs_, ctx[b, q * 64:(q + 1) * 64], pad], 0)
                ys_ = np.concatenate([ys_, ycat[b, q * 64:(q + 1) * 64], pad], 0)
            d = dict(wB)
            d["xT"] = fm(xs_)
            d["yT"] = fm(ys_)
            d["cv"] = cvs[b]
            maps.append(d)
        res = run_bass_kernel_spmd(ncB, maps, core_ids=cores)
        for c in cores:
            b, q = c // 4, c % 4
            o = unfm(res.results[c]["outT"])
            x[b, q * 2048:(q + 1) * 2048] = o[:2048]
            if with_ctx:
                ctx[b, q * 64:(q + 1) * 64] = o[2048:2048 + 64]
        del res, maps
    return x


NTB = 2176
GROUPS4 = [[0, 1, 2, 3], [4, 5, 6, 7]]


def build_fused():
    nc = bass.Bass("TRN2", target_bir_lowering=False)
    S = Sched(nc)
    pb = [S.ptile(f"pb{i}", [128, 512]) for i in range(8)]
    xT_d = Tile(nc.dram_tensor("xT", [128, 8, TT], F32, kind="ExternalInput").ap(), "xT")
    xB_d = Tile(nc.dram_tensor("xB", [128, 8, NTB], F32, kind="ExternalInput").ap(), "xB")
    sel_d = Tile(nc.dram_tensor("sel", [128, 4], F32, kind="ExternalInput").ap(), "sel")
    out_d = Tile(nc.dram_tensor("outT", [128, 8, 2048], F32, kind="ExternalOutput").ap(), "outT")
    YC = 768
    NYC = TT // YC
    ybuf = [[Tile(nc.dram_tensor(f"ybuf{l}_{k}", [256, YC], F32).ap(), f"ybuf{l}_{k}") for k in range(NYC)] for l in range(2)]
    ygath = [[Tile(nc.dram_tensor(f"ygath{l}_{k}", [1024, YC], F32).ap(), f"ygath{l}_{k}") for k in range(NYC)] for l in range(2)]
    xw = [256] * 8 + [128]
    xown = [Tile(nc.dram_tensor(f"xown{k}", [128, 8 * xw[k]], F32).ap(), f"xown{k}") for k in range(9)]
    xg = [Tile(nc.dram_tensor(f"xg{k}", [512, 8 * xw[k]], F32).ap(), f"xg{k}") for k in range(9)]
    sel = S.tile("sel", [128, 4])
    S.dma(sel.r, sel_d.r, "ldc")

    def xown_v(k):
        return xown[k].r.re("p (c t) -> p c t", c=8)

    def xg_v(k):
        return xg[k].r.re("(r p) (c t) -> r p c t", r=4, c=8)

    def yv(l, t0, n):
        k, o = t0 // YC, t0 % YC
        assert o + n <= YC
        return ygath[l][k].r.re("(kc p) t -> p kc t", p=128)[:, :, o:o + n]

    for l in range(2):
        sfx = f"_l{l}"
        def load_x(ti, xs, l=l):
            if l == 0:
                S.dma(xs.r, xT_d[:, :, ti * N:(ti + 1) * N], "ldx")
            elif ti == 0:
                for r in range(4):
                    S.dma(xs[:, :, r * 64:(r + 1) * 64], xg_v(8)[r, :, :, 0:64], "ldx")
            else:
                r, k = (ti - 1) // 8, (ti - 1) % 8
                S.dma(xs.r, xg_v(k)[r], "ldx")

        def store_y(m, t0, n, src, l=l):
            k, o = t0 // YC, t0 % YC
            assert o + n <= YC
            S.dma(ybuf[l][k][m * 64:(m + 1) * 64, o:o + n], src, "st")

        emit_A(S, nc, pb, sfx, load_x, store_y, tile_limit=(int(os.environ["FUSED_TILES"]) if "FUSED_TILES" in os.environ else None))
        for k in range(NYC):
            S.all_gather(ygath[l][k].r, ybuf[l][k].r, GROUPS4)
        groups = [(g * 256, 256, 0) for g in range(8)]
        if l == 0:
            groups.append((2048, 128, 1))

        def x_src(g0, GN, col, xs, l=l):
            if l == 0:
                S.dma(xs[:, :, 0:GN], xB_d[:, :, g0:g0 + GN], "ldx")
            else:
                S.dma(xs[:, :, 0:GN], xown_v(g0 // 256), "ldx")

        def y_src(g0, GN, col, ys, tmp, l=l):
            if col == 0:
                n = GN
                srcs = [yv(l, 256 + q * 2048 + g0, GN) for q in range(4)]
            else:
                n = 64
                S.memset("dve", ys[:, :, 0:GN], 0.0)
                srcs = [yv(l, q * 64, 64) for q in range(4)]
            for q in range(4):
                S.dma(tmp[:, :, 0:n], srcs[q], "ldx")
                if q == 0:
                    S.ts("dve", ys[:, :, 0:n], tmp[:, :, 0:n], sel[:, 0:1], None, op0=ALU.mult)
                else:
                    S.stt("dve", ys[:, :, 0:n], tmp[:, :, 0:n], sel[:, q:q + 1], ys[:, :, 0:n], ALU.mult, ALU.add)

        def out_sink(g0, GN, col, xs, l=l):
            if l == 0:
                k = g0 // 256
                S.dma(xown_v(k), xs[:, :, 0:GN], "st")
                S.all_gather(xg[k].r, xown[k].r, GROUPS4)
            else:
                S.dma(out_d[:, :, g0:g0 + GN], xs[:, :, 0:GN], "st")

        if "FUSED_GROUPS" in os.environ:
            groups = groups[:int(os.environ["FUSED_GROUPS"])]
        emit_B(S, nc, pb, sfx, groups, x_src, y_src, out_sink)
    S.barrier()
    print("fused instructions", S.n_ins, "sems", S.nsem, "sbuf left", nc.sbuf_bytes_remaining)
    S.close()
    return nc


_FUSED = {}


def kernel_fused(**inputs):
    inp = {k: np.ascontiguousarray(np.asarray(v, dtype=np.float32)) for k, v in inputs.items()}
    x, ctx = inp["x"], inp["ctx"]
    if "nc" not in _FUSED:
        _FUSED["nc"] = build_fused()
    nc = _FUSED["nc"]
    shared_keys = ("ident", "iota", "trile", "trige", "rmask", "cmask", "rope")
    base = {}
    wA = {}
    for l in range(2):
        wb = prep_B_weights(inp, l, permute_wout=True)
        for k, v in wb.items():
            if k in shared_keys:
                base[k] = v
            else:
                base[k + f"_l{l}"] = v
        for j in range(4):
            wa = prep_A_weights(inp, l, j)
            d = {}
            for k, v in wa.items():
                if k in shared_keys:
                    base[k] = v
                elif k in ("adaw", "adab"):
                    pass
                else:
                    d[k + f"_l{l}"] = v
            wA[(l, j)] = d
    pad = np.zeros((64, 1024), np.float32)
    maps = []
    for c in range(8):
        b, r = c // 4, c % 4
        d = dict(base)
        d.update(wA[(0, r)])
        d.update(wA[(1, r)])
        d["cv"] = cvec(inp, b)
        d["xT"] = fm(np.concatenate([ctx[b], x[b]], 0))
        d["xB"] = fm(np.concatenate([x[b, r * 2048:(r + 1) * 2048], ctx[b, r * 64:(r + 1) * 64], pad], 0))
        s = np.zeros((128, 4), np.float32)
        s[:, r] = 1.0
        d["sel"] = s
        maps.append(d)
    res = run_bass_kernel_spmd(nc, maps, core_ids=list(range(8)))
    out = np.zeros_like(x)
    for c in range(8):
        b, r = c // 4, c % 4
        out[b, r * 2048:(r + 1) * 2048] = unfm(res.results[c]["outT"])
    return out


def kernel(**inputs):
    return kernel_fused(**inputs)
```

```python
from concourse.bass_utils import run_bass_kernel_spmd
import contextlib
import numpy as np
import concourse.bass as bass
import concourse.mybir as mybir

F32 = mybir.dt.float32
BF16 = mybir.dt.bfloat16
F32R = mybir.dt.float32r
U32 = mybir.dt.uint32
AF = mybir.ActivationFunctionType
ALU = mybir.AluOpType
AX = mybir.AxisListType


class Tile:
    def __init__(self, ap, name=""):
        self.ap = ap
        self.name = name
        self.writers = {}
        self.readers = {}
        self.exclusive = False

    def __getitem__(self, key):
        return Ref(self, self.ap[key])

    @property
    def r(self):
        return Ref(self, self.ap)


class Ref:
    def __init__(self, tile, ap):
        self.tile = tile
        self.ap = ap

    def __getitem__(self, key):
        return Ref(self.tile, self.ap[key])

    def re(self, s, **kw):
        return Ref(self.tile, self.ap.rearrange(s, **kw))

    def bc(self, shape):
        return Ref(self.tile, self.ap.to_broadcast(shape))

    def with_ap(self, ap):
        return Ref(self.tile, ap)


def _ap(x):
    return x.ap if isinstance(x, Ref) else x


class Sched:
    COMPUTE_ROT = 30000
    DMA_ROT = 1900

    def __init__(self, nc):
        self.nc = nc
        self.es = contextlib.ExitStack()
        self.eng = {"pe": nc.tensor, "dve": nc.vector, "act": nc.scalar,
                    "pool": nc.gpsimd, "sp": nc.sync}
        self.sem = {}
        self.cnt = {}
        self.epoch = {}
        self.waited = {}
        self.nsem = 0
        self.n_ins = 0
        self.allsems = {}
        self.dram_cache = {}
        self.dma_keys = set()
        self.recording = None
        self.scopes = []

    def shared_dram(self, name, shape, dt=F32):
        if name not in self.dram_cache:
            t = self.nc.dram_tensor(name, list(shape), dt, kind="ExternalInput")
            self.dram_cache[name] = Tile(t.ap(), name)
        return self.dram_cache[name]

    def push_scope(self):
        self.scopes.append(contextlib.ExitStack())

    def pop_scope(self):
        print("scope end: sbuf left", self.nc.sbuf_bytes_remaining)
        self.barrier()
        self.scopes.pop().close()

    def barrier(self):
        for e in ("pe", "dve", "act", "pool", "sp"):
            self.wait_all(e)

    def sbuf(self, name, shape, dtype=F32):
        es = self.scopes[-1] if getattr(self, "scopes", None) else self.es
        self.nalloc = getattr(self, "nalloc", 0) + 1
        return es.enter_context(self.nc.sbuf_tensor(f"sb{self.nalloc}_" + name, list(shape), dtype))

    def psum(self, name, shape, dtype=F32):
        return self.es.enter_context(self.nc.psum_tensor("ps_" + name, list(shape), dtype))

    def tile(self, name, shape, dtype=F32):
        t = self.sbuf(name, shape, dtype)
        return Tile(t[tuple(slice(None) for _ in shape)], name)

    def ptile(self, name, shape, dtype=F32):
        t = self.psum(name, shape, dtype)
        tl = Tile(t[tuple(slice(None) for _ in shape)], name)
        tl.exclusive = True
        return tl

    def _stream(self, stream, is_dma):
        if stream not in self.cnt:
            self.epoch[stream] = 0
            self._newsem(stream)
        key, c = self.cnt[stream]
        lim = self.DMA_ROT if is_dma else self.COMPUTE_ROT
        if c >= lim:
            self.epoch[stream] += 1
            self._newsem(stream)
        return self.cnt[stream]

    def _newsem(self, stream):
        key = f"{stream}_{self.epoch[stream]}"
        h = self.es.enter_context(self.nc.semaphore(f"s_{key}"))
        self.sem[key] = h
        self.cnt[stream] = (key, 0)
        self.nsem += 1

    def emit(self, engine, fn, outs=(), ins=(), dma_group=None, inc_override=None):
        if self.recording is not None:
            self.recording.append((engine, fn, outs, ins, dma_group, inc_override))
            return None
        is_dma = dma_group is not None
        stream = dma_group if is_dma else engine
        key, c = self._stream(stream, is_dma)
        deps = {}

        def add(d):
            if d is None:
                return
            k, v = d
            if deps.get(k, 0) < v:
                deps[k] = v

        outs = list(outs) + [r for r in ins if isinstance(r, Ref) and r.tile.exclusive]
        for r in ins:
            if isinstance(r, Ref):
                for k, v in r.tile.writers.items():
                    add((k, v))
        for o in outs:
            if isinstance(o, Ref):
                for k, v in o.tile.writers.items():
                    add((k, v))
                for k, v in o.tile.readers.items():
                    add((k, v))
        e = self.eng[engine]
        for k, v in list(deps.items()):
            if k in self.dma_keys:
                v = max(v, self.allsems.get(k, v))
                deps[k] = v
        for k, v in deps.items():
            if engine == "pe" and not is_dma and k.rsplit("_", 1)[0] == "pe":
                continue
            if self.waited.get((engine, k), 0) >= v:
                continue
            e.wait_ge(self.sem[k], v)
            self.waited[(engine, k)] = v
        ins_obj = fn()
        inc = 16 if is_dma else 1
        if inc_override is not None:
            inc = inc_override
        c += inc
        self.cnt[stream] = (key, c)
        self.allsems[key] = c
        if is_dma:
            self.dma_keys.add(key)
        ins_obj.then_inc(self.sem[key], inc)
        self.n_ins += 1
        me = (key, c)
        for o in outs:
            if isinstance(o, Ref):
                o.tile.writers[key] = c
                o.tile.readers = {}
        for r in ins:
            if isinstance(r, Ref):
                if not any(r.tile is o.tile for o in outs if isinstance(o, Ref)):
                    if r.tile.readers.get(key, 0) < c:
                        r.tile.readers[key] = c
        return ins_obj

    def wait_all(self, engine="sp"):
        e = self.eng[engine]
        for key, c in list(self.allsems.items()):
            if c > 0 and self.waited.get((engine, key), 0) < c:
                e.wait_ge(self.sem[key], c)
                self.waited[(engine, key)] = c

    def close(self):
        self.es.close()

    def record(self, fn):
        assert self.recording is None
        self.recording = []
        try:
            fn()
        finally:
            rec, self.recording = self.recording, None
        return rec

    def replay_interleaved(self, recs):
        idx = [0] * len(recs)
        live = True
        while live:
            live = False
            for i, r in enumerate(recs):
                if idx[i] < len(r):
                    self.emit(*r[idx[i]])
                    idx[i] += 1
                    live = True

    def all_gather(self, out, in_, groups):
        return self.emit("pool", lambda: self.nc.gpsimd.collective_compute(
            "AllGather", ALU.bypass, replica_groups=groups, ins=[_ap(in_).opt()], outs=[_ap(out).opt()]),
            outs=[out], ins=[in_], dma_group=f"cc{self._ncc()}", inc_override=1)

    def _ncc(self):
        self.ncc = getattr(self, "ncc", 0) + 1
        return self.ncc

    def dma(self, out, in_, group="ld0", q="sp"):
        return self.emit(q, lambda: self.eng[q].dma_start(out=_ap(out), in_=_ap(in_)),
                         outs=[out], ins=[in_], dma_group=group)

    def mm(self, out, lhsT, rhs, start=True, stop=True, skip=False):
        return self.emit("pe", lambda: self.nc.tensor.matmul(_ap(out), lhsT=_ap(lhsT), rhs=_ap(rhs),
                                                              start=start, stop=stop, skip_group_check=skip),
                         outs=[out], ins=[lhsT, rhs] + ([] if start else [out]))

    def tr(self, out, in_, ident):
        return self.emit("pe", lambda: self.nc.tensor.transpose(_ap(out), _ap(in_), _ap(ident)),
                         outs=[out], ins=[in_, ident])

    def act(self, out, in_, func, bias=None, scale=None, accum_out=None):
        kw = {}
        ins = [in_]
        outs = [out]
        if bias is not None:
            kw["bias"] = _ap(bias)
            ins.append(bias)
        if scale is not None:
            kw["scale"] = _ap(scale)
            ins.append(scale)
        if accum_out is not None:
            kw["accum_out"] = _ap(accum_out)
            outs.append(accum_out)
        return self.emit("act", lambda: self.nc.scalar.activation(out=_ap(out), in_=_ap(in_), func=func, **kw),
                         outs=outs, ins=ins)

    def tt(self, eng, out, in0, in1, op):
        return self.emit(eng, lambda: self.eng[eng].tensor_tensor(out=_ap(out), in0=_ap(in0), in1=_ap(in1), op=op),
                         outs=[out], ins=[in0, in1])

    def ts(self, eng, out, in0, s1, s2=None, op0=ALU.mult, op1=None, accum_out=None):
        kw = {}
        outs = [out]
        if op1 is not None:
            kw["op1"] = op1
        if accum_out is not None:
            kw["accum_out"] = _ap(accum_out)
            outs.append(accum_out)
        return self.emit(eng, lambda: self.eng[eng].tensor_scalar(out=_ap(out), in0=_ap(in0), scalar1=_ap(s1),
                                                                  scalar2=_ap(s2), op0=op0, **kw),
                         outs=outs, ins=[in0, s1, s2])

    def stt(self, eng, out, in0, scalar, in1, op0, op1):
        return self.emit(eng, lambda: self.eng[eng].scalar_tensor_tensor(out=_ap(out), in0=_ap(in0), scalar=_ap(scalar),
                                                                         in1=_ap(in1), op0=op0, op1=op1),
                         outs=[out], ins=[in0, scalar, in1])

    def copy(self, eng, out, in_):
        if eng == "act":
            return self.emit("act", lambda: self.nc.scalar.copy(out=_ap(out), in_=_ap(in_)), outs=[out], ins=[in_])
        return self.emit(eng, lambda: self.eng[eng].tensor_copy(out=_ap(out), in_=_ap(in_)), outs=[out], ins=[in_])

    def memset(self, eng, out, val):
        return self.emit(eng, lambda: self.eng[eng].memset(_ap(out), val), outs=[out], ins=[])

    def reduce(self, eng, out, in_, op, axis=AX.X):
        return self.emit(eng, lambda: self.eng[eng].tensor_reduce(out=_ap(out), in_=_ap(in_), axis=axis, op=op),
                         outs=[out], ins=[in_])

    def recip(self, out, in_):
        return self.emit("dve", lambda: self.nc.vector.reciprocal(out=_ap(out), in_=_ap(in_)), outs=[out], ins=[in_])

    def vmax(self, out, in_):
        return self.emit("dve", lambda: self.nc.vector.max(out=_ap(out), in_=_ap(in_)), outs=[out], ins=[in_])

    def vmax_index(self, out, in_max, in_values):
        return self.emit("dve", lambda: self.nc.vector.max_index(out=_ap(out), in_max=_ap(in_max), in_values=_ap(in_values)),
                         outs=[out], ins=[in_max, in_values])

    def vmatch_replace(self, out, in_to_replace, in_values, imm):
        return self.emit("dve", lambda: self.nc.vector.match_replace(out=_ap(out), in_to_replace=_ap(in_to_replace),
                                                                    in_values=_ap(in_values), imm_value=imm),
                         outs=[out], ins=[in_to_replace, in_values])

    def scan(self, out, data0, data1, initial, op0, op1):
        return self.emit("dve", lambda: self.nc.vector.tensor_tensor_scan(out=_ap(out), data0=_ap(data0), data1=_ap(data1),
                                                                          initial=_ap(initial), op0=op0, op1=op1),
                         outs=[out], ins=[data0, data1, initial])
import os

EPS = 1e-6
PROJ_DT = F32
import math
RWKV_DECAY_SCALE = math.exp(-0.5)
TT = 8448
NTILE = 33
N = 256


def emit_A(S, nc, pb, sfx="", load_x=None, store_y=None, stage=None, dbg_d=None,
           mixers=("conv", "attn", "mlstm", "rwkv"), tile_limit=None):
    def D(name, shape, kind="ExternalInput", dt=F32):
        t = nc.dram_tensor(name + sfx, list(shape), dt, kind=kind)
        return Tile(t.ap(), name + sfx)

    cv_d = S.shared_dram("cv", [128, 8, 2])
    adaw_d = S.shared_dram("adaw" + sfx, [6, 2, 128, 8, 512])
    adab_d = S.shared_dram("adab" + sfx, [128, 48])
    n1g_d = D("n1g", [128, 8])
    Wc_d = D("Wc", [128, 8, 192])
    Wr_d = D("Wr", [128, 8, 256])
    Wa_d = D("Wa", [128, 8, 192])
    Wm_d = D("Wm", [128, 8, 260])
    pp_d = D("pp", [64, 16])
    w2_d = D("w2p", [16, 2, 64])
    a2_d = D("a2p", [16, 2, 64])
    g2_d = D("g2p", [32, 64])
    rowbc_d = D("rowbc", [128, 4, 64])
    scal_d = D("scal", [128, 8])
    ident_d = S.shared_dram("ident", [128, 128])
    trile_d = S.shared_dram("trile", [128, 128])
    trige_d = S.shared_dram("trige", [128, 128])
    rmask_d = S.shared_dram("rmask", [2, 64, 192])
    cmask_d = S.shared_dram("cmask", [64, 256])
    rope_d = S.shared_dram("rope", [64, 128, 64])

    class Done(Exception):
        pass

    dbg_n = [0]

    def chk(name, *refs):
        if stage != name:
            return
        o = 0
        for r in refs:
            n = 1
            for s_ in r.ap.shape[1:]:
                n *= s_
            P = r.ap.shape[0]
            t = S.tile(f"dbgt{dbg_n[0]}", [128, n])
            dbg_n[0] += 1
            tv = t[0:P, :]
            shp = r.ap.shape
            if len(shp) == 3:
                tv = tv.re("p (a b) -> p a b", a=shp[1])
            elif len(shp) == 4:
                tv = tv.re("p (a b c) -> p a b c", a=shp[1], b=shp[2])
            S.copy("dve", tv, r)
            S.dma(dbg_d[0:P, o:o + n], t[0:P, :], "st")
            o += n
        raise Done()

    S.push_scope()
    ident = S.tile("ident", [128, 128])
    ones = S.tile("ones", [128, 128])
    cv = S.tile("cv", [128, 8, 2])
    adab = S.tile("adab", [128, 48])
    n1g = S.tile("n1g", [128, 8])
    mod0 = S.tile("mod0", [128, 8, 2])
    mod1 = S.tile("mod1", [128, 8, 2])
    gm1 = S.tile("gm1", [128, 8, 2])
    pp = S.tile("pp", [64, 16])
    omka = S.tile("omka", [64, 1])
    rowbc = S.tile("rowbc", [128, 4, 64])
    scal = S.tile("scal", [128, 8])
    xs = S.tile("xs", [128, 8, N])
    hT = S.tile("hT", [128, 8, N], PROJ_DT)
    sqs = S.tile("sqs", [128, 8, N])
    wstage = S.tile("wstage", [128, 8, 260])
    rstd = S.tile("rstd", [128, N])

    def body():
        S.dma(ident.r, ident_d.r, "ldc")
        S.dma(cv.r, cv_d.r, "ldc")
        S.dma(adab.r, adab_d.r, "ldc")
        S.dma(n1g.r, n1g_d.r, "ldc")
        S.dma(pp.r, pp_d.r, "ldc")
        S.dma(rowbc.r, rowbc_d.r, "ldc")
        S.dma(scal.r, scal_d.r, "ldc")
        S.memset("dve", ones.r, 1.0)
        S.act(cv.r, cv.r, AF.Silu)
        S.ts("dve", omka.r, pp[:, 9:10], -1.0, 1.0, op0=ALU.mult, op1=ALU.add)
        scr = S.tile("scr", [128, 4096])
        adaw_t = scr.r.re("p (c f) -> p c f", c=8)
        for seg, dst in ((0, mod0), (1, mod1)):
            for half in range(2):
                S.dma(adaw_t, adaw_d[seg, half], "ldc")
                for f4 in range(4):
                    fc = half * 4 + f4
                    for dc in range(8):
                        S.mm(pb[0][:, fc * 2:fc * 2 + 2], adaw_t[:, dc, f4 * 128:(f4 + 1) * 128], cv[:, dc, :],
                             start=(dc == 0), stop=(dc == 7))
            S.tt("dve", dst.r, pb[0][:, 0:16].re("p (c t) -> p c t", t=2),
                 adab.r.with_ap(adab.ap[:, seg * 8:(seg + 1) * 8].unsqueeze(2).to_broadcast([128, 8, 2])), ALU.add)
        S.ts("dve", gm1.r, mod1.r, 1.0, None, op0=ALU.add)
        S.tt("dve", gm1.r, gm1.r, n1g.r.with_ap(n1g.ap.unsqueeze(2).to_broadcast([128, 8, 2])), ALU.mult)
        sh1 = mod0
        chk("mod", mod0.r, gm1.r)

        hbuf = Tile(nc.dram_tensor("hbuf" + sfx, [128, 8, TT], F32).ap(), "hbuf" + sfx)
        h_done = set()

        def load_h(ti, hb=None):
            if hb is not None:
                return load_h_impl(ti, *hb)
            return load_h_impl(ti, hT, xs, rstd, pb[0], sqs)

        def load_w(dst, src_d, ncol):
            S.dma(wstage[:, :, 0:ncol], src_d.r, "ldc")
            S.copy("dve", dst[:, :, 0:ncol], wstage[:, :, 0:ncol])

        def load_h_impl(ti, hT, xs, rstd, pbank, sqs):
            t0 = ti * N
            col = 1 if ti == 0 else 0
            if ti in h_done:
                if PROJ_DT == F32:
                    S.dma(hT.r, hbuf[:, :, t0:t0 + N], "ldx")
                else:
                    S.dma(xs.r, hbuf[:, :, t0:t0 + N], "ldx")
                    S.copy("dve", hT.r, xs.r)
                return
            load_x(ti, xs)
            S.act(sqs.r, xs.r, AF.Square)
            for kc in range(8):
                S.mm(pbank[:, 0:N], ones.r, sqs[:, kc, :], start=(kc == 0), stop=(kc == 7))
            S.ts("dve", rstd.r, pbank[:, 0:N], 1.0 / 1024.0, EPS, op0=ALU.mult, op1=ALU.add)
            S.act(rstd.r, rstd.r, AF.Sqrt)
            S.recip(rstd.r, rstd.r)
            S.tt("dve", sqs.r, xs.r, rstd.r.with_ap(rstd.ap.unsqueeze(1).to_broadcast([128, 8, N])), ALU.mult)
            for kc in range(8):
                S.ts("dve", hT[:, kc, :], sqs[:, kc, :], gm1[:, kc, col:col + 1], sh1[:, kc, col:col + 1],
                     op0=ALU.mult, op1=ALU.add)
            S.dma(hbuf[:, :, t0:t0 + N], (hT.r if PROJ_DT == F32 else hT.r.with_ap(hT.ap.bitcast(F32))), "sth")
            if S.recording is None:
                h_done.add(ti)

        tiles = list(range(NTILE)) if tile_limit is None else list(range(tile_limit))

        def head_norm_fm(y, sq, out, g_col, psb, n=N):
            S.act(sq, y, AF.Square)
            S.mm(psb[0:64, 0:n], ones[0:64, 0:64], sq)
            S.ts("dve", sq, psb[0:64, 0:n], 1.0 / 64.0, EPS, op0=ALU.mult, op1=ALU.add)
            S.act(sq, sq, AF.Sqrt)
            S.recip(sq, sq)
            S.stt("dve", out, y, g_col, sq, ALU.mult, ALU.mult)

        if "conv" in mixers:
            S.push_scope()
            Wc = S.tile("Wc", [128, 8, 192], PROJ_DT)
            load_w(Wc, Wc_d, 192)
            U = S.tile("convU", [64, TT + 4])
            Bg = S.tile("convB", [64, TT])
            S.memset("dve", U.r, 0.0)

            def ucol(t):
                return t + 1 if t < 256 else t + 3

            for ti in tiles:
                load_h(ti)
                t0 = ti * N
                for g in range(3):
                    for kc in range(8):
                        S.mm(pb[1 + g][0:64, 0:N], Wc[:, kc, g * 64:(g + 1) * 64], hT[:, kc, :], start=(kc == 0), stop=(kc == 7))
                S.copy("act", Bg[:, t0:t0 + N], pb[2][0:64, 0:N])
                S.copy("act", U[:, ucol(t0):ucol(t0) + N], pb[1][0:64, 0:N])
                S.tt("dve", U[:, ucol(t0):ucol(t0) + N], U[:, ucol(t0):ucol(t0) + N], pb[3][0:64, 0:N], ALU.mult)
            cy = S.tile("convy", [64, N])
            csq = S.tile("convsq", [64, N])
            for ti in tiles:
                t0 = ti * N
                u0 = ucol(t0)
                S.ts("dve", cy.r, U[:, u0 - 1:u0 - 1 + N], pp[:, 0:1], None, op0=ALU.mult)
                S.stt("dve", cy.r, U[:, u0:u0 + N], pp[:, 1:2], cy.r, ALU.mult, ALU.add)
                S.stt("dve", cy.r, U[:, u0 + 1:u0 + 1 + N], pp[:, 2:3], cy.r, ALU.mult, ALU.add)
                S.tt("dve", cy.r, cy.r, Bg[:, t0:t0 + N], ALU.mult)
                head_norm_fm(cy.r, csq.r, cy.r, pp[:, 3:4], pb[1])
                store_y(0, t0, N, cy.r)
            chk("conv", cy.r)
            S.pop_scope()

        if "attn" in mixers:
            S.push_scope()
            NA = 192 if PROJ_DT == F32 else 256
            Wa = S.tile("Wa", [128, 8, 256], PROJ_DT)
            S.memset("dve", (Wa.r if PROJ_DT == F32 else Wa.r.with_ap(Wa.ap.bitcast(F32))), 0.0)
            load_w(Wa, Wa_d, 192)
            trile = S.tile("trile", [128, 128])
            trige = S.tile("trige", [128, 128])
            S.dma(trile.r, trile_d.r, "ldc")
            S.dma(trige.r, trige_d.r, "ldc")
            QT = S.tile("QT", [64, TT])
            KT = S.tile("KT", [64, TT])
            V1 = S.tile("V1", [128, 66, 65])
            S.memset("dve", V1.r, 1.0)
            rope = S.tile("ropet", [128, 64])
            qk = S.tile("qk", [128, 2, 64])
            qr = S.tile("qr", [128, 2, 64])
            tmpa = S.tile("tmpa", [128, 2, 2, 16])
            ssq = S.tile("ssq", [128, 2])
            junk = S.tile("junk", [128, 64])
            junk2 = S.tile("junk2", [128, 128])
            for ti in tiles:
                load_h(ti)
                for sub in range(2):
                    bi = ti * 2 + sub
                    t0 = bi * 128
                    for kc in range(8):
                        S.mm(pb[1][:, 0:NA], hT[:, kc, sub * 128:(sub + 1) * 128], Wa[:, kc, 0:NA], start=(kc == 0), stop=(kc == 7))
                    S.copy("act", V1[:, bi, 0:64], pb[1][:, 128:192])
                    S.act(junk2.r, pb[1][:, 0:128], AF.Square)
                    S.reduce("dve", ssq.r, junk2.r.re("p (w f) -> p w f", w=2), ALU.add)
                    S.ts("dve", ssq.r, ssq.r, 1.0 / 64.0, EPS, op0=ALU.mult, op1=ALU.add)
                    S.act(ssq.r, ssq.r, AF.Sqrt)
                    S.recip(ssq.r, ssq.r)
                    for w in range(2):
                        S.stt("dve", qk[:, w, :], pb[1][:, w * 64:(w + 1) * 64], ssq[:, w:w + 1], rowbc[:, w, :], ALU.mult, ALU.mult)
                    src = qk
                    if bi >= 2:
                        S.dma(rope.r, rope_d[bi - 2], "ldr")
                        cosv = rope.r.with_ap(rope.ap.rearrange("p (h cs f) -> p h cs f", h=2, cs=2)[:, :, 0, :])
                        sinv = rope.r.with_ap(rope.ap.rearrange("p (h cs f) -> p h cs f", h=2, cs=2)[:, :, 1, :])
                        for w in range(2):
                            q4 = qk[:, w, :].re("p (h x f) -> p h x f", h=2, x=2)
                            o4 = qr[:, w, :].re("p (h x f) -> p h x f", h=2, x=2)
                            S.tt("dve", o4, q4, cosv.with_ap(cosv.ap.unsqueeze(2).to_broadcast([128, 2, 2, 16])), ALU.mult)
                            S.tt("dve", tmpa[:, :, 0, :], q4[:, :, 1, :], sinv, ALU.mult)
                            S.tt("dve", tmpa[:, :, 1, :], q4[:, :, 0, :], sinv, ALU.mult)
                            S.tt("dve", o4[:, :, 0, :], o4[:, :, 0, :], tmpa[:, :, 0, :], ALU.subtract)
                            S.tt("dve", o4[:, :, 1, :], o4[:, :, 1, :], tmpa[:, :, 1, :], ALU.add)
                        src = qr
                    S.tr(pb[2][0:64, 0:128], src[:, 0, :], ident.r)
                    S.copy("act", QT[:, t0:t0 + 128], pb[2][0:64, 0:128])
                    S.tr(pb[3][0:64, 0:128], src[:, 1, :], ident.r)
                    S.copy("act", KT[:, t0:t0 + 128], pb[3][0:64, 0:128])
            chk("attn_qk", QT.r.re("p (t f) -> p t f", f=4)[:, :, 0])
            nblk = len(tiles) * 2
            E = [S.tile(f"attE{i}", [128, 128]) for i in range(5)]
            esink = S.tile("esink", [128, 1])
            S.act(esink.r, scal[:, 0:1], AF.Exp)
            den = S.tile("attden", [128, 1])
            ao = S.tile("atto", [128, 64])
            for bi in range(nblk):
                t0 = bi * 128
                if bi < 2:
                    kbs = [(0, None), (1, None)]
                else:
                    kbs = [(0, None), (1, None)]
                    if bi - 1 >= 2:
                        kbs.append((bi - 1, trige))
                    kbs.append((bi, None))
                    if bi + 1 < nblk:
                        kbs.append((bi + 1, trile))
                for i, (kb, mask) in enumerate(kbs):
                    ps = pb[1 + (i % 2)]
                    S.mm(ps[:, 0:128], KT[:, kb * 128:(kb + 1) * 128], QT[:, t0:t0 + 128])
                    S.act(E[i].r, ps[:, 0:128], AF.Exp, scale=0.125)
                    if mask is not None:
                        S.tt("dve", E[i].r, E[i].r, mask.r, ALU.mult)
                chk("attn_E", E[0].r, E[1].r)
                for i, (kb, mask) in enumerate(kbs):
                    S.mm(pb[3][:, 0:65], E[i].r, V1[:, kb, :], start=(i == 0), stop=(i == len(kbs) - 1))
                chk("attn_pv", pb[3][:, 0:65])
                S.tt("dve", den.r, pb[3][:, 64:65], esink.r, ALU.add)
                S.recip(den.r, den.r)
                S.ts("dve", ao.r, pb[3][:, 0:64], den.r, None, op0=ALU.mult)
                S.act(junk.r, ao.r, AF.Square)
                S.reduce("dve", ssq[:, 0:1], junk.r, ALU.add)
                S.ts("dve", ssq[:, 0:1], ssq[:, 0:1], 1.0 / 64.0, EPS, op0=ALU.mult, op1=ALU.add)
                S.act(ssq[:, 0:1], ssq[:, 0:1], AF.Sqrt)
                S.recip(ssq[:, 0:1], ssq[:, 0:1])
                S.stt("dve", ao.r, ao.r, ssq[:, 0:1], rowbc[:, 2, :], ALU.mult, ALU.mult)
                chk("attn_ao", ao.r)
                S.tr(pb[4][0:64, 0:128], ao.r, ident.r)
                S.copy("act", qk[0:64, :, :].re("p a b -> p (a b)"), pb[4][0:64, 0:128])
                store_y(2, t0, 128, qk[0:64, :, :].re("p a b -> p (a b)"))
                if bi == int(os.environ.get("BLIM", "99")):
                    chk("attn_blk", ao.r)
            chk("attn", ao.r)
            S.pop_scope()

        if "mlstm" in mixers:
            S.push_scope()
            Wm = S.tile("Wm", [128, 8, 260], PROJ_DT)
            load_w(Wm, Wm_d, 260)
            trile = S.tile("trile", [128, 128])
            trige = S.tile("trige", [128, 128])
            S.dma(trile.r, trile_d.r, "ldc")
            S.dma(trige.r, trige_d.r, "ldc")
            nblk = len(tiles) * 2
            Qm = S.tile("Qm", [128, 66, 64])
            Km = S.tile("Km", [128, 66, 64])
            Vm1 = S.tile("Vm1", [128, 66, 65])
            Om = S.tile("Om", [128, 66, 64])
            Hs = S.tile("Hs", [128, 66, 64])
            G = S.tile("G", [128, 66, 4])
            nfb = S.tile("nfb", [128, 2])
            S.memset("dve", Vm1.r, 1.0)
            S.ts("dve", nfb[:, 0:1], scal[:, 2:3], -1.0, None, op0=ALU.mult)
            S.ts("dve", nfb[:, 1:2], scal[:, 4:5], -1.0, None, op0=ALU.mult)
            for ti in tiles:
                load_h(ti)
                for sub in range(2):
                    bi = ti * 2 + sub
                    for kc in range(8):
                        S.mm(pb[1][:, 0:260], hT[:, kc, sub * 128:(sub + 1) * 128], Wm[:, kc, :], start=(kc == 0), stop=(kc == 7))
                    S.copy("act", Qm[:, bi, :], pb[1][:, 0:64])
                    chk("ml_a", Qm[:, 0, :])
                    S.ts("dve", Km[:, bi, :], pb[1][:, 64:128], 0.125, None, op0=ALU.mult)
                    S.copy("dve", Vm1[:, bi, 0:64], pb[1][:, 128:192])
                    chk("ml_b", Km[:, 0, :])
                    S.act(Om[:, bi, :], pb[1][:, 192:256], AF.Sigmoid)
                    chk("ml_c", Om[:, 0, :])
                    for d in range(2):
                        S.ts("dve", G[:, bi, 2 * d:2 * d + 1], pb[1][:, 256 + 2 * d:257 + 2 * d], scal[:, 1 + 2 * d:2 + 2 * d], None, op0=ALU.add)
                        chk("ml_d", G[:, 0, :])
                        S.act(G[:, bi, 2 * d + 1:2 * d + 2], pb[1][:, 257 + 2 * d:258 + 2 * d], AF.Exp, bias=nfb[:, d:d + 1], scale=-1.0)
                        chk("ml_e", G[:, 0, :])
                        S.act(G[:, bi, 2 * d + 1:2 * d + 2], G[:, bi, 2 * d + 1:2 * d + 2], AF.Ln, bias=1.0)
                        chk("ml_f", G[:, 0, :])
                        S.ts("dve", G[:, bi, 2 * d + 1:2 * d + 2], G[:, bi, 2 * d + 1:2 * d + 2], -1.0, None, op0=ALU.mult)
            chk("ml_p1", Qm[:, 0, :], Km[:, 0, :], G[:, 0, :])
            Hb = S.tile("Hb", [128, 66, 64])

            def ml_dir(d):
                q = [pb[4 * d + i] for i in range(4)]
                t = lambda nm, shp: S.tile(f"ml{d}_{nm}", shp)
                C1T = t("C1T", [64, 65])
                eb, ek, rden = t("eb", [128, 1]), t("ek", [128, 1]), t("rden", [128, 1])
                eL = t("eL", [64, 1])
                qt, kt = t("qt", [128, 64]), t("kt", [128, 64])
                qtT, ktT = t("qtT", [64, 128]), t("ktT", [64, 128])
                STs = t("ST", [128, 128])
                tri = trile if d == 0 else trige
                order = list(range(nblk)) if d == 0 else [1, 0] + list(range(nblk - 1, 1, -1))
                Hd = Hs if d == 0 else Hb

                def run():
                    S.memset("dve", C1T.r, 0.0)
                    for bi in order:
                        lf = G[:, bi, 2 * d + 1:2 * d + 2]
                        S.mm(q[0][:, 0:1], tri.r, lf)
                        S.mm(q[0][0:64, 1:2], ones[:, 0:64], lf)
                        S.act(eb.r, q[0][:, 0:1], AF.Exp)
                        S.tt("dve", ek.r, G[:, bi, 2 * d:2 * d + 1], q[0][:, 0:1], ALU.subtract)
                        S.act(ek.r, ek.r, AF.Exp)
                        S.act(eL.r, q[0][0:64, 1:2], AF.Exp)
                        S.ts("dve", qt.r, Qm[:, bi, :], eb.r, None, op0=ALU.mult)
                        S.ts("dve", kt.r, Km[:, bi, :], ek.r, None, op0=ALU.mult)
                        S.tr(q[1][0:64, 0:128], qt.r, ident.r)
                        S.copy("act", qtT.r, q[1][0:64, 0:128])
                        S.tr(q[2][0:64, 0:128], kt.r, ident.r)
                        S.copy("dve", ktT.r, q[2][0:64, 0:128])
                        S.mm(q[3][:, 0:128], ktT.r, qtT.r)
                        S.tt("dve", STs.r, q[3][:, 0:128], tri.r, ALU.mult)
                        S.mm(q[1][:, 0:65], STs.r, Vm1[:, bi, :], start=True, stop=False)
                        S.mm(q[1][:, 0:65], qtT.r, C1T.r, start=False, stop=True)
                        S.ts("dve", rden.r, q[1][:, 64:65], -1.0, None, op0=ALU.mult)
                        S.tt("dve", rden.r, rden.r, q[1][:, 64:65], ALU.max)
                        S.ts("dve", rden.r, rden.r, 1.0, None, op0=ALU.max)
                        S.recip(rden.r, rden.r)
                        S.ts("dve", Hd[:, bi, :], q[1][:, 0:64], rden.r, None, op0=ALU.mult)
                        S.mm(q[2][0:64, 0:65], ident[0:64, 0:64], C1T.r, start=True, stop=False)
                        S.mm(q[2][0:64, 0:65], kt.r, Vm1[:, bi, :], start=False, stop=True)
                        S.ts("dve", C1T.r, q[2][0:64, 0:65], eL.r, None, op0=ALU.mult)
                return run

            runs = [ml_dir(0), ml_dir(1)]
            S.replay_interleaved([S.record(runs[0]), S.record(runs[1])])
            S.tt("dve", Hs.r, Hs.r, Hb.r, ALU.add)
            mlsq = S.tile("mlsq", [128, 64])
            mlss = S.tile("mlss", [128, 1])
            mly = S.tile("mly", [128, 64])
            mlyT = S.tile("mlyT", [64, 128])
            for bi in range(nblk):
                S.act(mlsq.r, Hs[:, bi, :], AF.Square)
                S.reduce("dve", mlss.r, mlsq.r, ALU.add)
                S.ts("dve", mlss.r, mlss.r, 1.0 / 64.0, EPS, op0=ALU.mult, op1=ALU.add)
                S.act(mlss.r, mlss.r, AF.Sqrt)
                S.recip(mlss.r, mlss.r)
                S.stt("dve", mly.r, Hs[:, bi, :], mlss.r, rowbc[:, 3, :], ALU.mult, ALU.mult)
                S.tt("dve", mly.r, mly.r, Om[:, bi, :], ALU.mult)
                S.tr(pb[3][0:64, 0:128], mly.r, ident.r)
                S.copy("act", mlyT.r, pb[3][0:64, 0:128])
                store_y(3, bi * 128, 128, mlyT.r)
            chk("mlstm", mly.r)
            S.pop_scope()
        if "rwkv" in mixers:
            S.push_scope()
            Wr = S.tile("Wr", [128, 8, 256], PROJ_DT)
            load_w(Wr, Wr_d, 256)
            w2p = S.tile("w2p", [16, 2, 64])
            a2p = S.tile("a2p", [16, 2, 64])
            g2p = S.tile("g2p", [32, 64])
            rmask = [S.tile(f"rmask{d}", [64, 192]) for d in range(2)]
            cmask = S.tile("cmask", [64, 256])
            S.dma(w2p.r, w2_d.r, "ldc")
            S.dma(a2p.r, a2_d.r, "ldc")
            S.dma(g2p.r, g2_d.r, "ldc")
            for d in range(2):
                S.dma(rmask[d].r, rmask_d[d], "ldc")
            S.dma(cmask.r, cmask_d.r, "ldc")
            RKbc = S.tile("RKbc", [64, 64])
            S.ts("dve", RKbc.r, ones[0:64, 0:64], pp[:, 11:12], None, op0=ALU.mult)
            Yst = S.tile("Yst", [64, TT])
            rwsc = Tile(nc.dram_tensor("rwsc" + sfx, [64, NTILE, 3, N], F32).ap(), "rwsc" + sfx)
            i64 = ident[0:64, 0:64]
            o64 = ones[0:64, 0:64]

            class B_:
                pass

            def mkbufs(d):
                b = B_()
                t = lambda nm, shp: S.tile(f"rw{d}_{nm}", shp)
                b.rT, b.kT, b.vT, b.gT, b.kkT = (t(n_, [64, N]) for n_ in ("r", "k", "v", "g", "kk"))
                b.tw, b.xa, b.sg = t("tw", [16, N]), t("xa", [16, N]), t("sg", [32, N])
                b.lw = t("lw", [64, N])
                b.aT = [t(f"a{i}", [64, N]) for i in range(2)]
                b.kd = [t(f"kd{i}", [64, N]) for i in range(2)]
                b.cum, b.tmp, b.Pin, b.Pinv, b.Pex = (t(n_, [64, N]) for n_ in ("cum", "tmp", "Pin", "Pinv", "Pex"))
                b.AR = t("AR", [64, 4, 2, 64])
                b.BK = t("BK", [64, 4, 2, 64])
                b.Vtok = t("Vtok", [64, 4, 64])
                b.NM = [[t(f"NM{c}_{i}", [64, 128]) for i in range(2)] for c in range(4)]
                b.Pw = [[t(f"P{c}_{i}", [64, 64]) for i in range(2)] for c in range(4)]
                b.PwT = [[t(f"PT{c}_{i}", [64, 64]) for i in range(2)] for c in range(4)]
                b.X = [[t(f"X{c}_{i}", [64, 64]) for i in range(2)] for c in range(4)]
                b.Btok = [t(f"Btok{c}", [64, 64]) for c in range(4)]
                b.Ktok = [t(f"Ktok{c}", [64, 64]) for c in range(4)]
                b.Xf = [None] * 4
                b.ZT, b.UT, b.S0T = (t(n_, [64, 64]) for n_ in ("ZT", "UT", "S0T"))
                b.sq = t("sq", [64, N])
                b.Yb = t("Yb", [64, 3, N])
                b.q = [pb[4 * d + i] for i in range(4)]
                if d == 0:
                    b.hb = (hT, xs, rstd, pb[0], sqs)
                else:
                    b.hb = (S.tile("rw1_hT", [128, 8, N], PROJ_DT), S.tile("rw1_xs", [128, 8, N]), S.tile("rw1_rstd", [128, N]), pb[4],
                            S.tile("rw1_sqs", [128, 8, N]))
                b.hT = b.hb[0]
                return b

            BF = [mkbufs(0), mkbufs(1)]

            def prep_tile(ti, d):
                b = BF[d]
                q = b.q
                both = (d == 1)
                load_h(ti, b.hb)
                hT = b.hT
                for g, dst in ((0, b.rT), (1, b.kT), (2, b.vT)):
                    for kc in range(8):
                        S.mm(q[1][0:64, 0:N], Wr[:, kc, g * 64:(g + 1) * 64], hT[:, kc, :], start=(kc == 0), stop=(kc == 7))
                    S.copy("act", dst.r, q[1][0:64, 0:N])
                for kc in range(8):
                    S.mm(q[2][0:16, 0:N], Wr[:, kc, 192:208], hT[:, kc, :], start=(kc == 0), stop=(kc == 7))
                S.act(b.tw.r, q[2][0:16, 0:N], AF.Tanh)
                for kc in range(8):
                    S.mm(q[2][0:16, 0:N], Wr[:, kc, 208:224], hT[:, kc, :], start=(kc == 0), stop=(kc == 7))
                S.copy("act", b.xa.r, q[2][0:16, 0:N])
                if both:
                    for kc in range(8):
                        S.mm(q[2][0:32, 0:N], Wr[:, kc, 224:256], hT[:, kc, :], start=(kc == 0), stop=(kc == 7))
                    S.act(b.sg.r, q[2][0:32, 0:N], AF.Sigmoid)
                for c in range(4):
                    for kc in range(8):
                        S.mm(q[3][0:64, c * 64:(c + 1) * 64], hT[:, kc, c * 64:(c + 1) * 64], Wr[:, kc, 128:192],
                             start=(kc == 0 and c == 0), stop=(kc == 7), skip=True)
                S.copy("act", b.Vtok.r.re("p c v -> p (c v)"), q[3][0:64, 0:256])
                dirs = (0, 1) if both else (d,)
                for dd in dirs:
                    S.mm(q[1][0:64, 0:N], a2p[:, dd, :], b.xa.r)
                    S.act(b.aT[dd].r, q[1][0:64, 0:N], AF.Sigmoid, bias=pp[:, 6 + dd:7 + dd])
                    S.ts("dve", b.kd[dd].r, b.aT[dd].r, pp[:, 9:10], omka.r, op0=ALU.mult, op1=ALU.add)
                    S.tt("dve", b.kd[dd].r, b.kd[dd].r, b.kT.r, ALU.mult)
                S.mm(q[1][0:64, 0:N], w2p[:, d, :], b.tw.r)
                S.act(b.lw.r, q[1][0:64, 0:N], AF.Sigmoid, bias=pp[:, 4 + d:5 + d])
                S.ts("dve", b.lw.r, b.lw.r, -RWKV_DECAY_SCALE, None, op0=ALU.mult)
                if both:
                    S.mm(q[1][0:64, 0:N], g2p.r, b.sg.r)
                    S.copy("act", b.Yb[:, 2, :], q[1][0:64, 0:N])
                S.ts("dve", b.kkT.r, b.kT.r, pp[:, 8:9], None, op0=ALU.mult)
                S.act(b.sq.r, b.kkT.r, AF.Square)
                S.mm(q[1][0:64, 0:N], o64, b.sq.r)
                S.ts("dve", b.sq.r, q[1][0:64, 0:N], EPS, None, op0=ALU.add)
                S.act(b.sq.r, b.sq.r, AF.Sqrt)
                S.recip(b.sq.r, b.sq.r)
                S.tt("dve", b.kkT.r, b.kkT.r, b.sq.r, ALU.mult)
                S.scan(b.cum.r, cmask.r, b.lw.r, 0.0, ALU.mult, ALU.add)
                if d == 1:
                    c3 = b.cum.ap.rearrange("p (c t) -> p c t", c=4)
                    S.tt("dve", b.tmp.r, b.lw.r, b.cum.r, ALU.subtract)
                    S.tt("dve", b.cum.r.re("p (c t) -> p c t", c=4), b.tmp.r.re("p (c t) -> p c t", c=4),
                         b.cum.r.with_ap(c3[:, :, 63:64].to_broadcast([64, 4, 64])), ALU.add)
                S.act(b.Pin.r, b.cum.r, AF.Exp)
                S.act(b.Pinv.r, b.cum.r, AF.Exp, scale=-1.0)
                S.tt("dve", b.tmp.r, b.cum.r, b.lw.r, ALU.subtract)
                S.act(b.Pex.r, b.tmp.r, AF.Exp)
                v4 = lambda tl: tl.r.re("p (c t) -> p c t", c=4)
                S.stt("dve", b.AR[:, :, 0, :], v4(b.kkT), -1.0, v4(b.Pex), ALU.mult, ALU.mult)
                S.tt("dve", b.AR[:, :, 1, :], v4(b.rT), v4(b.Pin), ALU.mult)
                S.tt("dve", b.tmp.r, b.kkT.r, b.aT[d].r, ALU.mult)
                S.tt("dve", b.BK[:, :, 0, :], v4(b.tmp), v4(b.Pinv), ALU.mult)
                S.tt("dve", b.BK[:, :, 1, :], v4(b.kd[d]), v4(b.Pinv), ALU.mult)
                if both:
                    S.tt("dve", b.tmp.r, b.kd[0].r, b.kd[1].r, ALU.add)
                    S.tt("dve", b.tmp.r, b.tmp.r, b.rT.r, ALU.mult)
                    S.mm(q[1][0:64, 0:N], RKbc.r, b.tmp.r)
                    S.tt("dve", b.Yb[:, 1, :], q[1][0:64, 0:N], b.vT.r, ALU.mult)

            def chunk_pre(c, d):
                b = BF[d]
                pq = pb[4 * d + c]
                m = rmask[d]
                NM, Pw, PwT, X = b.NM[c], b.Pw[c], b.PwT[c], b.X[c]
                ARc = b.AR[:, c, :, :].re("p two t -> p (two t)")
                A_c = b.AR[:, c, 0, :]
                B_c, K_c = b.BK[:, c, 0, :], b.BK[:, c, 1, :]
                S.mm(pq[0:64, 0:128], B_c, ARc)
                S.mm(pq[0:64, 128:256], K_c, ARc, start=False, skip=True)
                S.mm(pq[0:64, 256:320], A_c, B_c, start=False, skip=True)
                S.tt("dve", NM[0].r, pq[0:64, 0:128], m[:, 0:128], ALU.mult)
                S.tt("dve", NM[1].r, pq[0:64, 128:256], m[:, 0:128], ALU.mult)
                S.tt("dve", PwT[0].r, pq[0:64, 256:320], m[:, 128:192], ALU.mult)
                S.copy("act", Pw[0].r, NM[0][:, 0:64])
                S.tt("dve", X[0].r, NM[0][:, 0:64], i64, ALU.add)
                cur = 0
                for lev in range(5):
                    nxt = 1 - cur
                    S.mm(pq[0:64, 0:64], Pw[cur].r, PwT[cur].r)
                    if lev < 4:
                        S.mm(pq[0:64, 64:128], PwT[cur].r, Pw[cur].r, start=False, skip=True)
                    S.copy("act", PwT[nxt].r, pq[0:64, 0:64])
                    if lev < 4:
                        S.copy("dve", Pw[nxt].r, pq[0:64, 64:128])
                    S.mm(pq[0:64, 128:192], PwT[nxt].r, X[cur].r, start=False, skip=True)
                    S.tt("dve", X[nxt].r, pq[0:64, 128:192], X[cur].r, ALU.add)
                    cur = nxt
                b.Xf[c] = X[cur]
                S.tr(pq[0:64, 256:320], B_c, i64)
                S.tr(pq[0:64, 320:384], K_c, i64)
                S.copy("act", b.Btok[c].r, pq[0:64, 256:320])
                S.copy("dve", b.Ktok[c].r, pq[0:64, 320:384])

            def chunk_seq(ti, c, d):
                b = BF[d]
                q = b.q
                NM = b.NM[c]
                A_c, R_c = b.AR[:, c, 0, :], b.AR[:, c, 1, :]
                V_c = b.Vtok[:, c, :]
                S.mm(q[3][0:64, 0:64], A_c, b.S0T.r, start=True, stop=False)
                S.mm(q[3][0:64, 0:64], NM[1][:, 0:64], V_c, start=False, stop=True)
                S.copy("act", b.ZT.r, q[3][0:64, 0:64])
                S.mm(q[1][0:64, 0:64], b.Xf[c].r, b.ZT.r)
                S.copy("act", b.UT.r, q[1][0:64, 0:64])
                S.mm(q[2][0:64, 0:64], b.S0T.r, R_c, start=True, stop=False)
                S.mm(q[2][0:64, 0:64], b.UT.r, NM[0][:, 64:128], start=False, stop=False)
                S.mm(q[2][0:64, 0:64], V_c, NM[1][:, 64:128], start=False, stop=True)
                if d == 0:
                    t0 = ti * N + c * 64
                    S.copy("dve", Yst[:, t0:t0 + 64], q[2][0:64, 0:64])
                else:
                    S.copy("dve", b.Yb[:, 0, c * 64:(c + 1) * 64], q[2][0:64, 0:64])
                S.mm(q[3][0:64, 0:64], i64, b.S0T.r, start=True, stop=False)
                S.mm(q[3][0:64, 0:64], b.Btok[c].r, b.UT.r, start=False, stop=False)
                S.mm(q[3][0:64, 0:64], b.Ktok[c].r, V_c, start=False, stop=True)
                pl = c * 64 + (63 if d == 0 else 0)
                S.ts("dve", b.S0T.r, q[3][0:64, 0:64], b.Pin[:, pl:pl + 1], None, op0=ALU.mult)

            orders = [tiles, [0] + tiles[:0:-1]]
            for d in range(2):
                S.memset("dve", BF[d].S0T.r, 0.0)
            for s_ in range(len(tiles)):
                tis = [orders[0][s_], orders[1][s_]]
                S.replay_interleaved([S.record(lambda: prep_tile(tis[0], 0)), S.record(lambda: prep_tile(tis[1], 1))])
                S.replay_interleaved([S.record(lambda d=d, c=c: chunk_pre(c, d)) for c in range(4) for d in range(2)])

                def seq0():
                    for i in range(4):
                        chunk_seq(tis[0], i, 0)

                def seq1():
                    for i in range(4):
                        chunk_seq(tis[1], 3 - i, 1)
                    S.dma(rwsc[:, tis[1], :, :], BF[1].Yb.r, "sth")

                S.replay_interleaved([S.record(seq0), S.record(seq1)])
            fin = BF[0].Yb
            yo = BF[0].tmp
            for ti in tiles:
                t0 = ti * N
                S.dma(fin.r, rwsc[:, ti, :, :], "ldx")
                S.tt("dve", yo.r, Yst[:, t0:t0 + N], fin[:, 0, :], ALU.add)
                head_norm_fm(yo.r, BF[0].sq.r, yo.r, pp[:, 12:13], pb[1])
                S.tt("dve", yo.r, yo.r, fin[:, 1, :], ALU.add)
                S.tt("dve", yo.r, yo.r, fin[:, 2, :], ALU.mult)
                store_y(1, t0, N, yo.r)
            S.pop_scope()

    try:
        body()
    except Done:
        while len(S.scopes) > 1:
            S.scopes.pop().close()
        raise
    S.pop_scope()


class StageDone(Exception):
    pass


def build_A(stage=None, mixers=("conv", "attn", "mlstm", "rwkv"), tile_limit=None):
    nc = bass.Bass("TRN2", target_bir_lowering=False)
    S = Sched(nc)
    xT_d = Tile(nc.dram_tensor("xT", [128, 8, TT], F32, kind="ExternalInput").ap(), "xT")
    yT_d = Tile(nc.dram_tensor("yT", [4, 64, TT], F32, kind="ExternalOutput").ap(), "yT")
    dbg_d = Tile(nc.dram_tensor("dbg", [128, 4096], F32, kind="ExternalOutput").ap(), "dbg")
    pb = [S.ptile(f"pb{i}", [128, 512]) for i in range(8)]

    def load_x(ti, xs):
        S.dma(xs.r, xT_d[:, :, ti * N:(ti + 1) * N], "ldx")

    def store_y(m, t0, n, src):
        S.dma(yT_d[m, :, t0:t0 + n], src, "st")

    try:
        emit_A(S, nc, pb, "", load_x, store_y, stage, dbg_d, mixers, tile_limit)
    except Exception as e:
        if type(e).__name__ != "Done":
            raise
    S.wait_all("sp")
    while getattr(S, "scopes", None):
        S.scopes.pop().close()
    print("build_A instructions", S.n_ins, "sems", S.nsem, "sbuf left", nc.sbuf_bytes_remaining)
    S.close()
    return nc

EPS = 1e-6
POOLENG = os.environ.get("POOLENG", "pool")
NEG = -1.0e30


def emit_B(S, nc, pb, sfx, groups, x_src, y_src, out_sink, stage=None, dbg_d=None):
    def D(name, shape, kind="ExternalInput", dt=F32):
        t = nc.dram_tensor(name + sfx, list(shape), dt, kind=kind)
        return Tile(t.ap(), name + sfx)

    cv_d = S.shared_dram("cv", [128, 8, 2])
    adaw_d = S.shared_dram("adaw" + sfx, [6, 2, 128, 8, 512])
    adab_d = S.shared_dram("adab" + sfx, [128, 48])
    n2g_d = D("n2g", [128, 8])
    wout_d = D("wout", [8, 128, 8, 128])
    wq_d = D("wq", [16, 128, 8, 128])
    keys_d = D("keysT", [128, 16, 128])
    UT_d = D("UT", [128, 128, 8, 128])
    VJ_d = D("VJ", [128, 128, 1024])
    ident_d = S.shared_dram("ident", [128, 128])
    iota_d = S.shared_dram("iota", [128, 128])

    GM = max(g[1] for g in groups)
    S.push_scope()

    class Done(Exception):
        pass

    def chk(name, *refs):
        if stage != name:
            return
        o = 0
        for r in refs:
            n = 1
            for s_ in r.ap.shape[1:]:
                n *= s_
            t = S.tile(f"dbgt{o}", [128, n])
            S.copy("dve", t.r, r if len(r.ap.shape) == 2 else r)
            S.dma(dbg_d[0:r.ap.shape[0], o:o + n], t[0:r.ap.shape[0], :], "st")
            o += n
        raise Done()
    ident = S.tile("ident", [128, 128])
    iota = S.tile("iota", [128, 128])
    ones = S.tile("ones", [128, 128])
    cv = S.tile("cv", [128, 8, 2])
    adab = S.tile("adab", [128, 48])
    n2g = S.tile("n2g", [128, 8])
    keysT = S.tile("keysT", [128, 16, 128])
    mod = [S.tile(f"mod{i}", [128, 8, 2]) for i in range(6)]
    gm2 = S.tile("gm2", [128, 8, 2])
    scr = S.tile("scr", [128, 4096])
    xs = S.tile("xs", [128, 8, GM])
    ys = S.tile("ys", [128, 8, GM])
    x1 = S.tile("x1", [128, 8, GM])
    h2 = S.tile("h2", [128, 8, GM])
    rstd = S.tile("rstd", [128, GM])
    wbuf = [S.tile(f"wbuf{i}", [128, 8, 128]) for i in range(2)]
    ubuf = [S.tile(f"ubuf{i}", [128, 8, 128]) for i in range(2)]
    vbuf = [S.tile(f"vbuf{i}", [128, 1024]) for i in range(2)]
    sc = S.tile("sc", [128, 16, 128])
    sc2 = S.tile("sc2", [128, 16, 128])
    sv = S.tile("sv", [128, 16, 16])
    si = S.tile("si", [128, 16, 16], U32)
    sif = S.tile("sif", [128, 16, 16])
    cand = S.tile("cand", [128, 8, 256])
    tv = S.tile("tv", [128, 8, 16])
    ti = S.tile("ti", [128, 8, 16], U32)
    tiu = S.tile("tiu", [128, 8, 16], U32)
    aq = S.tile("aq", [128, 8, 16])
    bq = S.tile("bq", [128, 8, 16])
    If = S.tile("If", [128, 128])
    Jf = S.tile("Jf", [128, 128])
    Wf = S.tile("Wf", [128, 128])
    mx = S.tile("mx", [128, 8])
    zs = S.tile("zs", [128, 8])
    IT = S.tile("IT", [128, GM])
    JT = S.tile("JT", [128, GM])
    WT = S.tile("WT", [128, GM])
    oiw = [S.tile(f"oiw{i}", [128, 128], BF16) for i in range(2)]
    oj = [S.tile(f"oj{i}", [128, 128], BF16) for i in range(2)]
    iotab = S.tile("iotab", [128, 128], BF16)
    WW = S.tile("WW", [128, GM, 128], BF16)
    gj = [S.tile(f"gj{i}", [128, GM]) for i in range(2)]
    pjb = [S.tile(f"pjb{i}", [128, GM], BF16) for i in range(2)]
    ubf = [S.tile(f"ubf{i}", [128, 8, 128], BF16) for i in range(3)]
    vbf = [S.tile(f"vbf{i}", [128, 1024], BF16) for i in range(3)]
    acc = pb[0:4]
    pa = pb[4:6]
    pw = pb[6:8]

    def build_body():
        S.dma(ident.r, ident_d.r, "ldc")
        S.dma(iota.r, iota_d.r, "ldc")
        S.dma(cv.r, cv_d.r, "ldc")
        S.dma(adab.r, adab_d.r, "ldc")
        S.dma(n2g.r, n2g_d.r, "ldc")
        S.dma(keysT.r, keys_d.r, "ldc")
        S.memset("dve", ones.r, 1.0)
        S.copy("dve", iotab.r, iota.r)
        S.act(cv.r, cv.r, AF.Silu)
        adaw_t = scr[:, 0:4096].re("p (c f) -> p c f", c=8)
        for seg in (2, 3, 4, 5):
            for half in range(2):
                S.dma(adaw_t, adaw_d[seg, half], "ldc")
                for f4 in range(4):
                    fc = half * 4 + f4
                    for dc in range(8):
                        S.mm(pa[0][:, fc * 2:fc * 2 + 2], adaw_t[:, dc, f4 * 128:(f4 + 1) * 128], cv[:, dc, :],
                             start=(dc == 0), stop=(dc == 7))
            S.tt("dve", mod[seg].r, pa[0][:, 0:16].re("p (c t) -> p c t", t=2),
                 adab[:, seg * 8:(seg + 1) * 8].with_ap(adab.ap[:, seg * 8:(seg + 1) * 8].unsqueeze(2).to_broadcast([128, 8, 2])),
                 ALU.add)
        S.ts("dve", gm2.r, mod[4].r, 1.0, None, op0=ALU.add)
        S.tt("dve", gm2.r, gm2.r, n2g.r.with_ap(n2g.ap.unsqueeze(2).to_broadcast([128, 8, 2])), ALU.mult)
        gt1, sh2, gt2 = mod[2], mod[3], mod[5]
        chk("mod", mod[2].r, mod[3].r, mod[4].r, mod[5].r, gm2.r)

        wi = 0
        ui = 0
        for (g0, GN, col) in groups:
            NTL = GN // 128
            x_src(g0, GN, col, xs)
            y_src(g0, GN, col, ys, h2)
            for oc in range(8):
                wb = wbuf[wi % 2]
                S.dma(wb.r, wout_d[oc], f"ldw{wi % 2}")
                wi += 1
                p = pa[oc % 2]
                for kc in range(8):
                    S.mm(p[:, 0:GN], wb[:, kc, :], ys[:, kc, 0:GN], start=(kc == 0), stop=(kc == 7))
                S.stt("dve", x1[:, oc, 0:GN], p[:, 0:GN], gt1[:, oc, col:col + 1], xs[:, oc, 0:GN], ALU.mult, ALU.add)
            chk("x1", x1[:, :, 0:GN])
            S.act(ys[:, :, 0:GN], x1[:, :, 0:GN], AF.Square)
            for kc in range(8):
                S.mm(pa[0][:, 0:GN], ones.r, ys[:, kc, 0:GN], start=(kc == 0), stop=(kc == 7))
            S.ts("dve", rstd[:, 0:GN], pa[0][:, 0:GN], 1.0 / 1024.0, EPS, op0=ALU.mult, op1=ALU.add)
            S.act(rstd[:, 0:GN], rstd[:, 0:GN], AF.Sqrt)
            S.recip(rstd[:, 0:GN], rstd[:, 0:GN])
            for kc in range(8):
                S.tt("dve", h2[:, kc, 0:GN], x1[:, kc, 0:GN], rstd[:, 0:GN], ALU.mult)
                S.ts("dve", h2[:, kc, 0:GN], h2[:, kc, 0:GN], gm2[:, kc, col:col + 1], sh2[:, kc, col:col + 1],
                     op0=ALU.mult, op1=ALU.add)
            chk("h2", h2[:, :, 0:GN])
            qT = scr[:, 0:16 * GN].re("p (h t) -> p h t", h=16)
            for hp in range(16):
                wb = wbuf[wi % 2]
                S.dma(wb.r, wq_d[hp], f"ldw{wi % 2}")
                wi += 1
                p = pa[hp % 2]
                for kc in range(8):
                    S.mm(p[:, 0:GN], wb[:, kc, :], h2[:, kc, 0:GN], start=(kc == 0), stop=(kc == 7))
                S.copy("act", qT[:, hp, :], p[:, 0:GN])
            chk("qT", qT[:, :, 0:GN])
            for mt in range(NTL):
                ms = slice(mt * 128, (mt + 1) * 128)
                for hp in range(16):
                    S.mm(acc[hp // 4][:, (hp % 4) * 128:(hp % 4) * 128 + 128], qT[:, hp, ms], keysT[:, hp, :])
                for b4 in range(4):
                    S.copy("act" if b4 % 2 else "dve", sc[:, b4 * 4:(b4 + 1) * 4, :].re("p a k -> p (a k)"), acc[b4].r)
                chk("sc", sc.r)
                for hp in range(16):
                    S.vmax(sv[:, hp, 0:8], sc[:, hp, :])
                    S.vmax_index(si[:, hp, 0:8], sv[:, hp, 0:8], sc[:, hp, :])
                    S.vmatch_replace(sc2[:, hp, :], sv[:, hp, 0:8], sc[:, hp, :], NEG)
                    S.vmax(sv[:, hp, 8:16], sc2[:, hp, :])
                    S.vmax_index(si[:, hp, 8:16], sv[:, hp, 8:16], sc2[:, hp, :])
                S.copy("dve", sif.r, si.r)
                chk("top1", sv.r, sif.r)
                sv4 = sv.ap.rearrange("p (h two) a -> p h two a", two=2)
                sif4 = sif.ap.rearrange("p (h two) a -> p h two a", two=2)
                S.tt("dve", cand.r.re("p h (a b) -> p h a b", b=16),
                     sv.r.with_ap(sv4[:, :, 0, :].unsqueeze(3).to_broadcast([128, 8, 16, 16])),
                     sv.r.with_ap(sv4[:, :, 1, :].unsqueeze(2).to_broadcast([128, 8, 16, 16])), ALU.add)
                cand2 = sc2.r.re("p (h two) k -> p h (two k)", two=2)
                eq = sc.r.re("p (h two) (n a) -> p h (two n) a", two=2, a=16)
                for h in range(8):
                    S.vmax(tv[:, h, 0:8], cand[:, h, :])
                    S.vmax_index(ti[:, h, 0:8], tv[:, h, 0:8], cand[:, h, :])
                    S.vmatch_replace(cand2[:, h, :], tv[:, h, 0:8], cand[:, h, :], NEG)
                    S.vmax(tv[:, h, 8:16], cand2[:, h, :])
                    S.vmax_index(ti[:, h, 8:16], tv[:, h, 8:16], cand2[:, h, :])
                S.ts("dve", tiu.r, ti.r, 15, None, op0=ALU.bitwise_and)
                S.copy("dve", bq.r, tiu.r)
                S.ts("dve", tiu.r, ti.r, 4, None, op0=ALU.logical_shift_right)
                S.copy("dve", aq.r, tiu.r)
                iota16 = iota.r.with_ap(iota.ap[:, 0:16].unsqueeze(1).unsqueeze(1).to_broadcast([128, 8, 16, 16]))
                for (qv, half, dst) in ((aq, 0, If), (bq, 1, Jf)):
                    S.tt("dve", eq, qv.r.with_ap(qv.ap.unsqueeze(3).to_broadcast([128, 8, 16, 16])), iota16, ALU.is_equal)
                    S.tt("dve", eq, eq, sif.r.with_ap(sif4[:, :, half, :].unsqueeze(2).to_broadcast([128, 8, 16, 16])), ALU.mult)
                    S.reduce("dve", dst.r.re("p (h n) -> p h n", h=8), eq, ALU.add)
                chk("IJ", If.r, Jf.r, tv.r, aq.r, bq.r)
                S.reduce("dve", mx.r, tv.r, ALU.max)
                S.tt("dve", tv.r, tv.r, mx.r.with_ap(mx.ap.unsqueeze(2).to_broadcast([128, 8, 16])), ALU.subtract)
                S.act(tv.r, tv.r, AF.Exp)
                S.reduce("dve", zs.r, tv.r, ALU.add)
                S.recip(zs.r, zs.r)
                S.tt("dve", Wf.r.re("p (h n) -> p h n", h=8), tv.r,
                     zs.r.with_ap(zs.ap.unsqueeze(2).to_broadcast([128, 8, 16])), ALU.mult)
                for k, (src, dst) in enumerate(((If, IT), (Jf, JT), (Wf, WT))):
                    S.tr(pw[k % 2][:, 0:128], src.r, ident.r)
                    S.copy("act", dst[:, ms], pw[k % 2][:, 0:128])
            chk("ITW", IT[:, 0:GN], JT[:, 0:GN], WT[:, 0:GN])
            for m in range(int(os.environ.get('MLIM', GN))):
                a = oiw[m % 2]
                b = oj[m % 2]
                S.ts("dve", a.r, iotab.r, IT[:, m:m + 1], WT[:, m:m + 1], op0=ALU.is_equal, op1=ALU.mult)
                S.ts("dve", b.r, iotab.r, JT[:, m:m + 1], None, op0=ALU.is_equal)
                p = pw[m % 2]
                S.mm(p[:, 0:128], a.r, b.r)
                S.copy("act", WW[:, m, :], p[:, 0:128])
            chk("WW", WW[:, 0:16, :])
            h2b = ys.r.with_ap(ys.ap.bitcast(BF16))[:, :, 0:GN]
            S.copy("act", h2b, h2[:, :, 0:GN])
            NBF = 3

            def load_j(j):
                S.dma(ubuf[j % 2].r, UT_d[j], f"ldu{j % 2}")
                S.dma(vbuf[j % 2].r, VJ_d[j], f"ldv{j % 2}")

            def cast_j(j):
                S.copy("act", ubf[j % NBF].r, ubuf[j % 2].r)
                S.copy("dve", vbf[j % NBF].r, vbuf[j % 2].r)

            def a_mm(j):
                p = pb[4 + (j % 4)]
                for dc in range(8):
                    S.mm(p[:, 0:GN], ubf[j % NBF][:, dc, :], h2b[:, dc, :], start=(dc == 0), stop=(dc == 7))

            load_j(0)
            load_j(1)
            cast_j(0)
            load_j(2)
            a_mm(0)
            for j in range(128):
                if j + 1 < 128:
                    cast_j(j + 1)
                    if j + 3 < 128:
                        load_j(j + 3)
                    a_mm(j + 1)
                g = gj[j % 2]
                pp = pjb[j % 2]
                S.act(g[:, 0:GN], pb[4 + (j % 4)][:, 0:GN], AF.Gelu_apprx_tanh)
                S.tt("dve", pp[:, 0:GN], g[:, 0:GN], WW[:, 0:GN, j], ALU.mult)
                vb = vbf[j % NBF]
                for oc in range(8):
                    S.mm(acc[oc // 2][:, (oc % 2) * 256:(oc % 2) * 256 + GN], vb[:, oc * 128:(oc + 1) * 128], pp[:, 0:GN],
                         start=(j == 0 and oc % 2 == 0), stop=(j == 127), skip=True)
            for oc in range(8):
                S.stt("dve", xs[:, oc, 0:GN], acc[oc // 2][:, (oc % 2) * 256:(oc % 2) * 256 + GN], gt2[:, oc, col:col + 1],
                      x1[:, oc, 0:GN], ALU.mult, ALU.add)
            out_sink(g0, GN, col, xs)

    try:
        build_body()
    except Done:
        while len(S.scopes) > 1:
            S.scopes.pop().close()
        raise
    S.pop_scope()


def build_B(NT, groups, stage=None):
    nc = bass.Bass("TRN2", target_bir_lowering=False)
    S = Sched(nc)
    xT_d = Tile(nc.dram_tensor("xT", [128, 8, NT], F32, kind="ExternalInput").ap(), "xT")
    yT_d = Tile(nc.dram_tensor("yT", [128, 8, NT], F32, kind="ExternalInput").ap(), "yT")
    out_d = Tile(nc.dram_tensor("outT", [128, 8, NT], F32, kind="ExternalOutput").ap(), "outT")
    dbg_d = Tile(nc.dram_tensor("dbg", [128, 4096], F32, kind="ExternalOutput").ap(), "dbg")
    pb = [S.ptile(f"pb{i}", [128, 512]) for i in range(8)]

    def x_src(g0, GN, col, xs):
        S.dma(xs[:, :, 0:GN], xT_d[:, :, g0:g0 + GN], "ldx")

    def y_src(g0, GN, col, ys, tmp):
        S.dma(ys[:, :, 0:GN], yT_d[:, :, g0:g0 + GN], "ldx")

    def out_sink(g0, GN, col, xs):
        S.dma(out_d[:, :, g0:g0 + GN], xs[:, :, 0:GN], "st")

    try:
        emit_B(S, nc, pb, "", groups, x_src, y_src, out_sink, stage, dbg_d)
    except Exception as e:
        if type(e).__name__ != "Done":
            raise
    S.wait_all("sp")
    while getattr(S, "scopes", None):
        S.scopes.pop().close()
    print("build_B instructions", S.n_ins, "sems", S.nsem)
    S.close()
    return nc

def fm(X):
    NT = X.shape[0]
    return np.ascontiguousarray(X.T.reshape(8, 128, NT).transpose(1, 0, 2))

def unfm(XT):
    NT = XT.shape[2]
    return np.ascontiguousarray(XT.transpose(1, 0, 2).reshape(1024, NT).T)

def vec_fm(v):
    return np.ascontiguousarray(v.reshape(-1, 128).T)

def consts():
    ident = np.eye(128, dtype=np.float32)
    iota = np.tile(np.arange(128, dtype=np.float32)[None, :], (128, 1))
    return ident, iota

def prep_B_weights(inp, l, permute_wout=False):
    d = {}
    aw = inp["ada_w"][l]
    d["adaw"] = np.ascontiguousarray(aw.reshape(8, 128, 6, 2, 512).transpose(2, 3, 1, 0, 4))
    d["adab"] = vec_fm(inp["ada_b"][l])
    d["n2g"] = vec_fm(inp["norm2_g"][l])
    wo = inp["w_out"][l]
    if permute_wout:
        g = np.arange(1024)
        r_, m_, ch_ = g // 256, (g % 256) // 64, g % 64
        wo = wo[m_ * 256 + r_ * 64 + ch_, :]
    d["wout"] = np.ascontiguousarray(wo.reshape(8, 128, 8, 128).transpose(2, 1, 0, 3))
    wq = inp["peer_wq"][l]
    d["wq"] = np.ascontiguousarray(wq.reshape(8, 128, 16, 128).transpose(2, 1, 0, 3))
    ks = inp["peer_keys"][l]
    d["keysT"] = np.ascontiguousarray(ks.reshape(16, 128, 128).transpose(2, 0, 1))
    u = inp["peer_u"][l]
    d["UT"] = np.ascontiguousarray(u.reshape(128, 128, 8, 128).transpose(1, 3, 2, 0))
    v = inp["peer_v"][l]
    d["VJ"] = np.ascontiguousarray(v.reshape(128, 128, 1024).transpose(1, 0, 2))
    d["ident"], d["iota"] = consts()
    return d

def cvec(inp, b):
    return np.ascontiguousarray(np.stack([vec_fm(inp["c"][b]), vec_fm(inp["c_ctx"])], axis=-1))

OFF = {"hx": 0, "cB": 256, "cC": 512, "r": 768, "k": 1024, "v": 1280, "xw": 1536, "xa": 1552, "xg": 1568,
       "aq": 1600, "ak": 1856, "av": 1984, "mq": 2112, "mk": 2368, "mv": 2624, "mo": 2880, "mg": 3136}

def packW(W, cols):
    Wc = W[:, cols]
    return np.ascontiguousarray(Wc.reshape(8, 128, len(cols)).transpose(1, 0, 2))

def rope_table():
    quarter = 16
    inv = (10000.0 ** (-np.arange(quarter, dtype=np.float32) / quarter)).astype(np.float32)
    t = np.arange(8192)
    row = (t // 64).astype(np.float32); col = (t % 64).astype(np.float32)
    ar = row[:, None] * inv[None, :]; ac = col[:, None] * inv[None, :]
    tab = np.concatenate([np.cos(ar), np.sin(ar), np.cos(ac), np.sin(ac)], -1).astype(np.float32)
    return np.ascontiguousarray(tab.reshape(64, 128, 64))

def prep_A_weights(inp, l, j):
    d = {}
    aw = inp["ada_w"][l]
    d["adaw"] = np.ascontiguousarray(aw.reshape(8, 128, 6, 2, 512).transpose(2, 3, 1, 0, 4))
    d["adab"] = vec_fm(inp["ada_b"][l])
    d["n1g"] = vec_fm(inp["norm1_g"][l])
    W = inp["w_in"][l]
    h64 = np.arange(64) + j * 64
    kv64 = np.arange(64) + (j // 2) * 64
    d["Wc"] = packW(W, np.concatenate([OFF["hx"] + h64, OFF["cB"] + h64, OFF["cC"] + h64]))
    d["Wr"] = packW(W, np.concatenate([OFF["r"] + h64, OFF["k"] + h64, OFF["v"] + h64, OFF["xw"] + np.arange(16),
                                       OFF["xa"] + np.arange(16), OFF["xg"] + np.arange(32)]))
    d["Wa"] = packW(W, np.concatenate([OFF["aq"] + h64, OFF["ak"] + kv64, OFF["av"] + kv64]))
    gcols = np.array([OFF["mg"] + dd * 8 + g * 4 + j for dd in range(2) for g in range(2)])
    d["Wm"] = packW(W, np.concatenate([OFF["mq"] + h64, OFF["mk"] + h64, OFF["mv"] + h64, OFF["mo"] + h64, gcols]))
    pp = np.zeros((64, 16), np.float32)
    pp[:, 0:3] = inp["conv_w"][l][:, h64].T
    pp[:, 3] = inp["conv_g"][l][j]
    pp[:, 4:6] = inp["rwkv_w0"][l][:, h64].T
    pp[:, 6:8] = inp["rwkv_a0"][l][:, h64].T
    pp[:, 8] = inp["rwkv_kk"][l][h64]
    pp[:, 9] = inp["rwkv_ka"][l][h64]
    pp[:, 11] = inp["rwkv_rk"][l][j]
    pp[:, 12] = inp["rwkv_ln_g"][l][j]
    d["pp"] = pp
    d["w2p"] = np.ascontiguousarray(inp["rwkv_w2"][l][:, :, h64].transpose(1, 0, 2))
    d["a2p"] = np.ascontiguousarray(inp["rwkv_a2"][l][:, :, h64].transpose(1, 0, 2))
    d["g2p"] = np.ascontiguousarray(inp["rwkv_g2"][l][:, h64])
    rb = np.stack([inp["att_q_g"][l], inp["att_k_g"][l], inp["att_out_g"][l][j], inp["ml_out_g"][l][j]], 0)
    d["rowbc"] = np.ascontiguousarray(np.tile(rb[None], (128, 1, 1)))
    sc = np.zeros((8,), np.float32)
    sc[0] = inp["att_sink"][l][j]
    sc[1] = inp["ml_i_b"][l][0, j]; sc[2] = inp["ml_f_b"][l][0, j]
    sc[3] = inp["ml_i_b"][l][1, j]; sc[4] = inp["ml_f_b"][l][1, j]
    d["scal"] = np.ascontiguousarray(np.tile(sc[None], (128, 1)))
    ident, _ = consts()
    d["ident"] = ident
    i = np.arange(128)
    d["trile"] = (i[:, None] <= i[None, :]).astype(np.float32)
    d["trige"] = (i[:, None] >= i[None, :]).astype(np.float32)
    i = np.arange(64)
    su = (i[:, None] < i[None, :]).astype(np.float32); iu = (i[:, None] <= i[None, :]).astype(np.float32)
    sl = (i[:, None] > i[None, :]).astype(np.float32); il = (i[:, None] >= i[None, :]).astype(np.float32)
    d["rmask"] = np.ascontiguousarray(np.stack([np.concatenate([su, iu, sl], 1), np.concatenate([sl, il, su], 1)], 0))
    cm = np.ones((64, 256), np.float32); cm[:, ::64] = 0.0
    d["cmask"] = cm
    d["rope"] = rope_table()
    return d

_NC_CACHE = {}


def _get_nc(kind, *args):
    key = (kind,) + args
    if key not in _NC_CACHE:
        if kind == "A":
            _NC_CACHE[key] = build_A()
        else:
            _NC_CACHE[key] = build_B(*args)
    return _NC_CACHE[key]


def kernel_unfused(**inputs):
    inp = {k: np.ascontiguousarray(np.asarray(v, dtype=np.float32)) for k, v in inputs.items()}
    x = inp["x"].copy()
    ctx = inp["ctx"].copy()
    cores = list(range(8))
    for l in range(2):
        ncA = _get_nc("A")
        maps = []
        wA = [prep_A_weights(inp, l, j) for j in range(4)]
        xfull = [fm(np.concatenate([ctx[b], x[b]], 0)) for b in range(2)]
        cvs = [cvec(inp, b) for b in range(2)]
        for c in cores:
            b, j = c // 4, c % 4
            d = dict(wA[j])
            d["xT"] = xfull[b]
            d["cv"] = cvs[b]
            maps.append(d)
        res = run_bass_kernel_spmd(ncA, maps, core_ids=cores)
        ycat = np.zeros((2, TT, 1024), np.float32)
        for c in cores:
            b, j = c // 4, c % 4
            yT = res.results[c]["yT"]
            for m in range(4):
                ycat[b, :, m * 256 + j * 64:m * 256 + (j + 1) * 64] = yT[m].T
        del res, maps, xfull
        with_ctx = (l == 0)
        if with_ctx:
            NT = 2048 + 128
            groups = tuple((g * 256, 256, 0) for g in range(8)) + ((2048, 128, 1),)
        else:
            NT = 2048
            groups = tuple((g * 256, 256, 0) for g in range(8))
        ncB = _get_nc("B", NT, groups)
        wB = prep_B_weights(inp, l)
        maps = []
        for c in cores:
            b, q = c // 4, c % 4
            xs_ = x[b, q * 2048:(q + 1) * 2048]
            ys_ = ycat[b, 256 + q * 2048:256 + (q + 1) * 2048]
            if with_ctx:
                pad = np.zeros((64, 1024), np.float32)
                xs_ = np.concatenate([xs_, ctx[b, q * 64:(q + 1) * 64], pad], 0)
                ys_ = np.concatenate([ys_, ycat[b, q * 64:(q + 1) * 64], pad], 0)
            d = dict(wB)
            d["xT"] = fm(xs_)
            d["yT"] = fm(ys_)
            d["cv"] = cvs[b]
            maps.append(d)
        res = run_bass_kernel_spmd(ncB, maps, core_ids=cores)
        for c in cores:
            b, q = c // 4, c % 4
            o = unfm(res.results[c]["outT"])
            x[b, q * 2048:(q + 1) * 2048] = o[:2048]
            if with_ctx:
                ctx[b, q * 64:(q + 1) * 64] = o[2048:2048 + 64]
        del res, maps
    return x


NTB = 2176
GROUPS4 = [[0, 1, 2, 3], [4, 5, 6, 7]]


def build_fused():
    nc = bass.Bass("TRN2", target_bir_lowering=False)
    S = Sched(nc)
    pb = [S.ptile(f"pb{i}", [128, 512]) for i in range(8)]
    xT_d = Tile(nc.dram_tensor("xT", [128, 8, TT], F32, kind="ExternalInput").ap(), "xT")
    xB_d = Tile(nc.dram_tensor("xB", [128, 8, NTB], F32, kind="ExternalInput").ap(), "xB")
    sel_d = Tile(nc.dram_tensor("sel", [128, 4], F32, kind="ExternalInput").ap(), "sel")
    out_d = Tile(nc.dram_tensor("outT", [128, 8, 2048], F32, kind="ExternalOutput").ap(), "outT")
    YC = 768
    NYC = TT // YC
    ybuf = [[Tile(nc.dram_tensor(f"ybuf{l}_{k}", [256, YC], F32).ap(), f"ybuf{l}_{k}") for k in range(NYC)] for l in range(2)]
    ygath = [[Tile(nc.dram_tensor(f"ygath{l}_{k}", [1024, YC], F32).ap(), f"ygath{l}_{k}") for k in range(NYC)] for l in range(2)]
    xw = [256] * 8 + [128]
    xown = [Tile(nc.dram_tensor(f"xown{k}", [128, 8 * xw[k]], F32).ap(), f"xown{k}") for k in range(9)]
    xg = [Tile(nc.dram_tensor(f"xg{k}", [512, 8 * xw[k]], F32).ap(), f"xg{k}") for k in range(9)]
    sel = S.tile("sel", [128, 4])
    S.dma(sel.r, sel_d.r, "ldc")

    def xown_v(k):
        return xown[k].r.re("p (c t) -> p c t", c=8)

    def xg_v(k):
        return xg[k].r.re("(r p) (c t) -> r p c t", r=4, c=8)

    def yv(l, t0, n):
        k, o = t0 // YC, t0 % YC
        assert o + n <= YC
        return ygath[l][k].r.re("(kc p) t -> p kc t", p=128)[:, :, o:o + n]

    for l in range(2):
        sfx = f"_l{l}"
        def load_x(ti, xs, l=l):
            if l == 0:
                S.dma(xs.r, xT_d[:, :, ti * N:(ti + 1) * N], "ldx")
            elif ti == 0:
                for r in range(4):
                    S.dma(xs[:, :, r * 64:(r + 1) * 64], xg_v(8)[r, :, :, 0:64], "ldx")
            else:
                r, k = (ti - 1) // 8, (ti - 1) % 8
                S.dma(xs.r, xg_v(k)[r], "ldx")

        def store_y(m, t0, n, src, l=l):
            k, o = t0 // YC, t0 % YC
            assert o + n <= YC
            S.dma(ybuf[l][k][m * 64:(m + 1) * 64, o:o + n], src, "st")

        emit_A(S, nc, pb, sfx, load_x, store_y, tile_limit=(int(os.environ["FUSED_TILES"]) if "FUSED_TILES" in os.environ else None))
        for k in range(NYC):
            S.all_gather(ygath[l][k].r, ybuf[l][k].r, GROUPS4)
        groups = [(g * 256, 256, 0) for g in range(8)]
        if l == 0:
            groups.append((2048, 128, 1))

        def x_src(g0, GN, col, xs, l=l):
            if l == 0:
                S.dma(xs[:, :, 0:GN], xB_d[:, :, g0:g0 + GN], "ldx")
            else:
                S.dma(xs[:, :, 0:GN], xown_v(g0 // 256), "ldx")

        def y_src(g0, GN, col, ys, tmp, l=l):
            if col == 0:
                n = GN
                srcs = [yv(l, 256 + q * 2048 + g0, GN) for q in range(4)]
            else:
                n = 64
                S.memset("dve", ys[:, :, 0:GN], 0.0)
                srcs = [yv(l, q * 64, 64) for q in range(4)]
            for q in range(4):
                S.dma(tmp[:, :, 0:n], srcs[q], "ldx")
                if q == 0:
                    S.ts("dve", ys[:, :, 0:n], tmp[:, :, 0:n], sel[:, 0:1], None, op0=ALU.mult)
                else:
                    S.stt("dve", ys[:, :, 0:n], tmp[:, :, 0:n], sel[:, q:q + 1], ys[:, :, 0:n], ALU.mult, ALU.add)

        def out_sink(g0, GN, col, xs, l=l):
            if l == 0:
                k = g0 // 256
                S.dma(xown_v(k), xs[:, :, 0:GN], "st")
                S.all_gather(xg[k].r, xown[k].r, GROUPS4)
            else:
                S.dma(out_d[:, :, g0:g0 + GN], xs[:, :, 0:GN], "st")

        if "FUSED_GROUPS" in os.environ:
            groups = groups[:int(os.environ["FUSED_GROUPS"])]
        emit_B(S, nc, pb, sfx, groups, x_src, y_src, out_sink)
    S.barrier()
    print("fused instructions", S.n_ins, "sems", S.nsem, "sbuf left", nc.sbuf_bytes_remaining)
    S.close()
    return nc


_FUSED = {}


def kernel_fused(**inputs):
    inp = {k: np.ascontiguousarray(np.asarray(v, dtype=np.float32)) for k, v in inputs.items()}
    x, ctx = inp["x"], inp["ctx"]
    if "nc" not in _FUSED:
        _FUSED["nc"] = build_fused()
    nc = _FUSED["nc"]
    shared_keys = ("ident", "iota", "trile", "trige", "rmask", "cmask", "rope")
    base = {}
    wA = {}
    for l in range(2):
        wb = prep_B_weights(inp, l, permute_wout=True)
        for k, v in wb.items():
            if k in shared_keys:
                base[k] = v
            else:
                base[k + f"_l{l}"] = v
        for j in range(4):
            wa = prep_A_weights(inp, l, j)
            d = {}
            for k, v in wa.items():
                if k in shared_keys:
                    base[k] = v
                elif k in ("adaw", "adab"):
                    pass
                else:
                    d[k + f"_l{l}"] = v
            wA[(l, j)] = d
    pad = np.zeros((64, 1024), np.float32)
    maps = []
    for c in range(8):
        b, r = c // 4, c % 4
        d = dict(base)
        d.update(wA[(0, r)])
        d.update(wA[(1, r)])
        d["cv"] = cvec(inp, b)
        d["xT"] = fm(np.concatenate([ctx[b], x[b]], 0))
        d["xB"] = fm(np.concatenate([x[b, r * 2048:(r + 1) * 2048], ctx[b, r * 64:(r + 1) * 64], pad], 0))
        s = np.zeros((128, 4), np.float32)
        s[:, r] = 1.0
        d["sel"] = s
        maps.append(d)
    res = run_bass_kernel_spmd(nc, maps, core_ids=list(range(8)))
    out = np.zeros_like(x)
    for c in range(8):
        b, r = c // 4, c % 4
        out[b, r * 2048:(r + 1) * 2048] = unfm(res.results[c]["outT"])
    return out


def kernel(**inputs):
    return kernel_fused(**inputs)
```
